# Optimizing a Trainium2 kernel written in Bass

```python
import math
import jax, jax.numpy as jnp
from jax import lax
import numpy as np

D_MODEL = 1024
BATCH = 8
SEQ = 4096
DEPTH = 1

DA_HEADS = 4
DA_HEAD_DIM = 64
DA_V_DIM = 2 * DA_HEAD_DIM
DA_WIDTH = DA_HEADS * DA_V_DIM
Q_BLOCK = 128
ML_HEADS = 4
ML_HEAD_DIM = 128
ML_WIDTH = ML_HEADS * ML_HEAD_DIM
ML_CHUNK = 64
CONV_WIDTH = 4
N_GROUPS = 4
EXPERTS_PER_GROUP = 8
N_EXPERTS = N_GROUPS * EXPERTS_PER_GROUP
TOP_K_IN_GROUP = 2
EXPERT_FF = 512
MOE_BLOCK = 128
NORM_EPS = 1e-6

OFF_DA_Q = 0
OFF_DA_K = OFF_DA_Q + DA_HEADS * 2 * DA_HEAD_DIM
OFF_DA_V = OFF_DA_K + DA_HEADS * 2 * DA_HEAD_DIM
OFF_ML_QK = OFF_DA_V + DA_WIDTH
OFF_ML_V = OFF_ML_QK + 2 * ML_WIDTH
OFF_ML_O = OFF_ML_V + ML_WIDTH
OFF_ML_I = OFF_ML_O + ML_WIDTH
OFF_ML_F = OFF_ML_I + ML_HEADS
IN_WIDTH = OFF_ML_F + ML_HEADS

kernel_name = 'hybrid_diffattn_mlstm_hmoe'


def rms_norm(x, g):
    x32 = x.astype(jnp.float32)
    y = x32 * lax.rsqrt(jnp.mean(x32 * x32, axis=-1, keepdims=True) + NORM_EPS)
    return (y * g.astype(jnp.float32)).astype(x.dtype)


def alibi_slopes(n_heads):
    return jnp.asarray([2.0 ** (-8.0 * (i + 1) / n_heads) for i in range(n_heads)], jnp.float32)


def causal_conv(u, w, b):
    width = w.shape[0]
    s = u.shape[1]
    up = jnp.pad(u, ((0, 0), (width - 1, 0), (0, 0)))
    out = b
    for j in range(width):
        out = out + up[:, j:j + s] * w[j]
    return out


def diff_attention(q, k, v, lam, slopes):
    s_len = q.shape[3]
    scale = DA_HEAD_DIM ** -0.5
    outs = []
    for blk in range(s_len // Q_BLOCK):
        lo, hi = blk * Q_BLOCK, (blk + 1) * Q_BLOCK
        qb = q[:, :, :, lo:hi]
        kb = k[:, :, :, :hi]
        vb = v[:, :, :hi]
        sc = jnp.einsum('bhmqd,bhmkd->bhmqk', qb, kb).astype(jnp.float32) * scale
        dist = (jnp.arange(lo, hi)[:, None] - jnp.arange(hi)[None, :]).astype(jnp.float32)
        bias = -slopes[:, None, None, None] * dist
        sc = jnp.where(dist >= 0, sc + bias, -jnp.inf)
        p = jax.nn.softmax(sc, axis=-1)
        a = p[:, :, 0] - lam * p[:, :, 1]
        outs.append(jnp.einsum('bhqk,bhkd->bhqd', a.astype(v.dtype), vb))
    return jnp.concatenate(outs, axis=2)


def mlstm_chunkwise(q, k, v, i_pre, log_f):
    b_sz, n_h, s_len, dh = q.shape
    n_chunks = s_len // ML_CHUNK
    causal = jnp.tril(jnp.ones((ML_CHUNK, ML_CHUNK), dtype=bool))

    def to_chunks(t):
        t = t.reshape(b_sz, n_h, n_chunks, ML_CHUNK, *t.shape[3:])
        return jnp.moveaxis(t, 2, 0)

    def step(carry, xs):
        c_state, n_state, m_state = carry
        qc, kc, vc, ic, fc = xs
        b = jnp.cumsum(fc, axis=-1)
        d = jnp.where(causal, b[..., :, None] - b[..., None, :] + ic[..., None, :], -jnp.inf)
        inter = b + m_state[..., None]
        m_row = jnp.maximum(jnp.max(d, axis=-1), inter)
        sc = jnp.einsum('bhld,bhsd->bhls', qc, kc) * jnp.exp(d - m_row[..., None])
        w_inter = jnp.exp(inter - m_row)
        num = jnp.einsum('bhls,bhsd->bhld', sc, vc) + w_inter[..., None] * jnp.einsum('bhld,bhde->bhle', qc, c_state)
        den = jnp.sum(sc, axis=-1) + w_inter * jnp.einsum('bhld,bhd->bhl', qc, n_state)
        h = num / jnp.maximum(jnp.abs(den), jnp.exp(-m_row))[..., None]
        b_last = b[..., -1]
        w_log = b_last[..., None] - b + ic
        m_new = jnp.maximum(b_last + m_state, jnp.max(w_log, axis=-1))
        decay = jnp.exp(b_last + m_state - m_new)
        w_in = jnp.exp(w_log - m_new[..., None])
        c_new = decay[..., None, None] * c_state + jnp.einsum('bhs,bhsd,bhse->bhde', w_in, kc, vc)
        n_new = decay[..., None] * n_state + jnp.einsum('bhs,bhsd->bhd', w_in, kc)
        return (c_new, n_new, m_new), h

    init = (jnp.zeros((b_sz, n_h, dh, dh), jnp.float32),
            jnp.zeros((b_sz, n_h, dh), jnp.float32),
            jnp.zeros((b_sz, n_h), jnp.float32))
    xs = (to_chunks(q), to_chunks(k), to_chunks(v), to_chunks(i_pre), to_chunks(log_f))
    _, h = lax.scan(step, init, xs)
    return jnp.moveaxis(h, 0, 2).reshape(b_sz, n_h, s_len, dh)


def hierarchical_moe(xn, w_group, b_group, w_router, b_router, w1, w3, w2):
    n_tok, d = xn.shape
    g_prob = jax.nn.softmax(jnp.einsum('td,dg->tg', xn, w_group).astype(jnp.float32) + b_group.astype(jnp.float32), axis=-1)
    g_top, g_idx = lax.top_k(g_prob, 1)
    e_logits = (jnp.einsum('td,de->te', xn, w_router).astype(jnp.float32) + b_router.astype(jnp.float32))
    e_logits = e_logits.reshape(n_tok, N_GROUPS, EXPERTS_PER_GROUP)[jnp.arange(n_tok), g_idx[:, 0]]
    e_top, e_local = lax.top_k(jax.nn.softmax(e_logits, axis=-1), TOP_K_IN_GROUP)
    e_top = e_top / jnp.sum(e_top, axis=-1, keepdims=True)
    gate = g_top * e_top
    expert_id = g_idx * EXPERTS_PER_GROUP + e_local

    n_assign = n_tok * TOP_K_IN_GROUP
    flat_e = expert_id.reshape(n_assign)
    flat_tok = jnp.repeat(jnp.arange(n_tok, dtype=jnp.int32), TOP_K_IN_GROUP)
    flat_w = gate.reshape(n_assign)
    order = jnp.argsort(flat_e)
    sorted_e = flat_e[order]
    counts = jax.ops.segment_sum(jnp.ones(n_assign, jnp.int32), flat_e, num_segments=N_EXPERTS)
    padded = (counts + MOE_BLOCK - 1) // MOE_BLOCK * MOE_BLOCK
    start = jnp.cumsum(counts) - counts
    pend = jnp.cumsum(padded)
    pstart = pend - padded
    dest = pstart[sorted_e] + (jnp.arange(n_assign, dtype=jnp.int32) - start[sorted_e])
    cap = n_assign + N_EXPERTS * MOE_BLOCK
    n_blocks = cap // MOE_BLOCK
    buf_tok = jnp.zeros(cap, jnp.int32).at[dest].set(flat_tok[order])
    buf_w = jnp.zeros(cap, jnp.float32).at[dest].set(flat_w[order])
    blk_expert = jnp.minimum(jnp.searchsorted(pend, jnp.arange(n_blocks, dtype=jnp.int32) * MOE_BLOCK, side='right'),
                             N_EXPERTS - 1).astype(jnp.int32)
    x_blocks = xn[buf_tok].reshape(n_blocks, MOE_BLOCK, d)

    def run_block(args):
        xb, e = args
        hid = jax.nn.silu(xb @ w1[e]) * (xb @ w3[e])
        return hid @ w2[e]

    y_blocks = lax.map(run_block, (x_blocks, blk_expert))
    y = y_blocks.reshape(cap, d) * buf_w[:, None].astype(xn.dtype)
    return jnp.zeros_like(xn).at[buf_tok].add(y)


def setup_inputs(seed: int = 0) -> dict:
    key = jax.random.key(seed)
    ks = jax.random.split(key, 32)
    f32 = jnp.float32
    L = DEPTH
    D = D_MODEL

    def nrm(k, shape, scale):
        return jax.random.normal(k, shape, f32) * scale

    def gain(k, shape):
        return 1.0 + 0.02 * jax.random.normal(k, shape, f32)

    return {
        'x': nrm(ks[0], (BATCH, SEQ, D), 1.0),
        'attn_norm_g': gain(ks[1], (L, D)),
        'w_in': nrm(ks[2], (L, D, IN_WIDTH), D ** -0.5),
        'da_q_norm_g': gain(ks[3], (L, DA_HEAD_DIM)),
        'da_k_norm_g': gain(ks[4], (L, DA_HEAD_DIM)),
        'da_lambda_q1': nrm(ks[5], (L, DA_HEAD_DIM), 0.1),
        'da_lambda_k1': nrm(ks[6], (L, DA_HEAD_DIM), 0.1),
        'da_lambda_q2': nrm(ks[7], (L, DA_HEAD_DIM), 0.1),
        'da_lambda_k2': nrm(ks[8], (L, DA_HEAD_DIM), 0.1),
        'da_out_norm_g': gain(ks[9], (L, DA_V_DIM)),
        'ml_conv_w': nrm(ks[10], (L, CONV_WIDTH, 2 * ML_WIDTH), CONV_WIDTH ** -0.5),
        'ml_conv_b': nrm(ks[11], (L, 2 * ML_WIDTH), 0.02),
        'ml_i_bias': nrm(ks[12], (L, ML_HEADS), 0.1),
        'ml_f_bias': jnp.linspace(3.0, 6.0, ML_HEADS, dtype=f32)[None, :] + nrm(ks[13], (L, ML_HEADS), 0.1),
        'ml_out_norm_g': gain(ks[14], (L, ML_WIDTH)),
        'w_branch_da': nrm(ks[15], (L, DA_WIDTH, D), DA_WIDTH ** -0.5),
        'w_branch_ml': nrm(ks[16], (L, ML_WIDTH, D), ML_WIDTH ** -0.5),
        'w_gate': nrm(ks[17], (L, D, 2 * D), D ** -0.5),
        'b_gate': nrm(ks[18], (L, 2 * D), 0.02),
        'w_out': nrm(ks[19], (L, D, D), D ** -0.5),
        'ffn_norm_g': gain(ks[20], (L, D)),
        'w_group': nrm(ks[21], (L, D, N_GROUPS), D ** -0.5),
        'b_group': nrm(ks[22], (L, N_GROUPS), 0.01),
        'w_router': nrm(ks[23], (L, D, N_EXPERTS), D ** -0.5),
        'b_router': nrm(ks[24], (L, N_EXPERTS), 0.01),
        'w1': nrm(ks[25], (L, N_EXPERTS, D, EXPERT_FF), D ** -0.5),
        'w3': nrm(ks[26], (L, N_EXPERTS, D, EXPERT_FF), D ** -0.5),
        'w2': nrm(ks[27], (L, N_EXPERTS, EXPERT_FF, D), EXPERT_FF ** -0.5),
    }


def reference(x, attn_norm_g, w_in, da_q_norm_g, da_k_norm_g, da_lambda_q1, da_lambda_k1,
              da_lambda_q2, da_lambda_k2, da_out_norm_g, ml_conv_w, ml_conv_b, ml_i_bias,
              ml_f_bias, ml_out_norm_g, w_branch_da, w_branch_ml, w_gate, b_gate, w_out,
              ffn_norm_g, w_group, b_group, w_router, b_router, w1, w3, w2):
    f32 = jnp.float32
    b_sz, s_len, d = x.shape
    slopes = alibi_slopes(DA_HEADS)
    for layer in range(DEPTH):
        h = rms_norm(x, attn_norm_g[layer])
        proj = jnp.einsum('bsd,de->bse', h, w_in[layer])

        q_da = proj[..., OFF_DA_Q:OFF_DA_K].reshape(b_sz, s_len, DA_HEADS, 2, DA_HEAD_DIM).transpose(0, 2, 3, 1, 4)
        k_da = proj[..., OFF_DA_K:OFF_DA_V].reshape(b_sz, s_len, DA_HEADS, 2, DA_HEAD_DIM).transpose(0, 2, 3, 1, 4)
        v_da = proj[..., OFF_DA_V:OFF_ML_QK].reshape(b_sz, s_len, DA_HEADS, DA_V_DIM).transpose(0, 2, 1, 3)
        q_da = rms_norm(q_da, da_q_norm_g[layer])
        k_da = rms_norm(k_da, da_k_norm_g[layer])
        lam_init = 0.8 - 0.6 * math.exp(-0.3 * layer)
        lam = (jnp.exp(jnp.sum((da_lambda_q1[layer] * da_lambda_k1[layer]).astype(f32)))
               - jnp.exp(jnp.sum((da_lambda_q2[layer] * da_lambda_k2[layer]).astype(f32))) + lam_init)
        o_da = diff_attention(q_da, k_da, v_da, lam, slopes)
        o_da = rms_norm(o_da, da_out_norm_g[layer]) * (1.0 - lam_init)
        y_da = o_da.transpose(0, 2, 1, 3).reshape(b_sz, s_len, DA_WIDTH)

        qk = jax.nn.silu(causal_conv(proj[..., OFF_ML_QK:OFF_ML_V], ml_conv_w[layer], ml_conv_b[layer]))

        def to_heads(t):
            return t.reshape(b_sz, s_len, ML_HEADS, ML_HEAD_DIM).transpose(0, 2, 1, 3).astype(f32)

        q_ml = to_heads(qk[..., :ML_WIDTH]) * (ML_HEAD_DIM ** -0.5)
        k_ml = to_heads(qk[..., ML_WIDTH:])
        v_ml = to_heads(proj[..., OFF_ML_V:OFF_ML_O])
        i_pre = (proj[..., OFF_ML_I:OFF_ML_F] + ml_i_bias[layer]).astype(f32).transpose(0, 2, 1)
        log_f = jax.nn.log_sigmoid((proj[..., OFF_ML_F:IN_WIDTH] + ml_f_bias[layer]).astype(f32)).transpose(0, 2, 1)
        h_ml = mlstm_chunkwise(q_ml, k_ml, v_ml, i_pre, log_f)
        h_ml = rms_norm(h_ml, ml_out_norm_g[layer].reshape(ML_HEADS, 1, ML_HEAD_DIM))
        o_gate = jax.nn.sigmoid(proj[..., OFF_ML_O:OFF_ML_I].astype(f32))
        y_ml = (h_ml.transpose(0, 2, 1, 3).reshape(b_sz, s_len, ML_WIDTH) * o_gate).astype(x.dtype)

        gates = jax.nn.sigmoid(jnp.einsum('bsd,de->bse', h, w_gate[layer]) + b_gate[layer])
        mixed = (gates[..., :d] * jnp.einsum('bsc,cd->bsd', y_da, w_branch_da[layer])
                 + gates[..., d:] * jnp.einsum('bsc,cd->bsd', y_ml, w_branch_ml[layer]))
        x = x + jnp.einsum('bsd,de->bse', mixed, w_out[layer])

        hn = rms_norm(x, ffn_norm_g[layer]).reshape(b_sz * s_len, d)
        moe_out = hierarchical_moe(hn, w_group[layer], b_group[layer], w_router[layer], b_router[layer],
                                   w1[layer], w3[layer], w2[layer])
        x = x + moe_out.reshape(b_sz, s_len, d)
    return x
```

```python
import math
from contextlib import ExitStack
import numpy as np
import ml_dtypes
import concourse.bass as bass
import concourse.mybir as mybir
from concourse.bass_utils import run_bass_kernel_spmd

F32 = mybir.dt.float32
BF16 = mybir.dt.bfloat16
I32 = mybir.dt.int32
AF = mybir.ActivationFunctionType
ALU = mybir.AluOpType
AX = mybir.AxisListType

D = 1024
NH = 4
IN_W = 3592
OFF_DA_Q, OFF_DA_K, OFF_DA_V = 0, 512, 1024
OFF_ML_QK, OFF_ML_V, OFF_ML_O, OFF_ML_I = 1536, 2560, 3072, 3584
NE = 32
EPS = 1e-6
LAM_INIT = 0.8 - 0.6 * math.exp(-0.3 * 0)
SLOPES = [2.0 ** (-8.0 * (i + 1) / NH) for i in range(NH)]
SKIP_T = 60.0
NOFF = 36
OFF0 = 31
ML_LOOKAHEAD = 3
BS = 256


class Buf:
    __slots__ = ("w", "r", "name")

    def __init__(self, name=""):
        self.w = None
        self.r = {}
        self.name = name


class KB:
    def __init__(self, nc, es):
        self.nc = nc
        self.es = es
        self.eng = {"pe": nc.tensor, "act": nc.scalar, "dve": nc.vector, "pool": nc.gpsimd, "sp": nc.sync}
        self.sem = {k: es.enter_context(nc.semaphore("sem_" + k)) for k in self.eng}
        self.cnt = {k: 0 for k in self.eng}
        self.seen = {k: {} for k in self.eng}
        self.dsem = {}
        self.dcnt = {}
        self.nbuf = 0

    def buf(self, name=""):
        self.nbuf += 1
        return Buf(name)

    def bufs(self, n, name=""):
        return [self.buf(name + str(i)) for i in range(n)]

    def dma_sem(self, name):
        if name not in self.dsem:
            self.dsem[name] = self.es.enter_context(self.nc.semaphore("dsem_" + name))
            self.dcnt[name] = 0
        return name

    def _semh(self, key):
        return self.sem[key] if key in self.sem else self.dsem[key]

    def _wait(self, e, reads, writes, same=True, skip=None):
        deps = {}
        for b in reads:
            if b.w is not None:
                k, v = b.w
                deps[k] = max(deps.get(k, 0), v)
        for b in writes:
            if b.w is not None:
                k, v = b.w
                deps[k] = max(deps.get(k, 0), v)
            for k, v in b.r.items():
                deps[k] = max(deps.get(k, 0), v)
        for k, v in deps.items():
            if k == e and not same:
                continue
            if k == skip:
                continue
            if k in self.dcnt:
                v = self.dcnt[k]
            if self.seen[e].get(k, 0) >= v:
                continue
            self.eng[e].wait_ge(self._semh(k), v)
            self.seen[e][k] = v

    def op(self, e, fn, r=(), w=(), same=None):
        if same is None:
            same = (e != "pe")
        self._wait(e, r, w, same)
        inst = fn(self.eng[e])
        self.cnt[e] += 1
        inst.then_inc(self.sem[e], 1)
        ev = (e, self.cnt[e])
        for b in w:
            b.w = ev
            b.r = {}
        for b in r:
            if b not in w:
                b.r[e] = self.cnt[e]
        return inst

    def dma(self, q, sname, fn, r=(), w=()):
        self.dma_sem(sname)
        self._wait(q, r, w, True, skip=sname)
        inst = fn(self.eng[q])
        self.dcnt[sname] += 16
        inst.then_inc(self.dsem[sname], 16)
        ev = (sname, self.dcnt[sname])
        for b in w:
            b.w = ev
            b.r = {}
        for b in r:
            if b not in w:
                b.r[sname] = self.dcnt[sname]
        return inst

    def barrier(self):
        evs = [(k, self.cnt[k]) for k in self.sem if self.cnt[k] > 0] + [(k, v) for k, v in self.dcnt.items() if v > 0]
        for e in self.eng:
            for k, v in evs:
                if k == e:
                    continue
                if self.seen[e].get(k, 0) >= v:
                    continue
                self.eng[e].wait_ge(self._semh(k), v)
                self.seen[e][k] = v

    def wait_all(self, e, bufs):
        self._wait(e, bufs, bufs, True)


def _mk_consts(S):
    c = {}
    bf = ml_dtypes.bfloat16
    c["ident_bf"] = np.eye(128, dtype=np.float32).astype(bf)
    c["ident_f"] = np.eye(128, dtype=np.float32)
    blk = np.zeros((128, 128), np.float32)
    blk[:64, :64] = 1.0 / 64
    blk[64:, 64:] = 1.0 / 64
    c["blk64"] = blk.astype(bf)
    k = np.arange(128)[:, None]
    q = np.arange(128)[None, :]
    c["cmask"] = (k <= q).astype(np.float32).astype(bf)
    c["negm"] = np.where(k <= q, 0.0, -30000.0).astype(np.float32)
    c["ones_f"] = np.ones((128, 128), np.float32)
    c["ustrict"] = (k < q).astype(np.float32)
    tab = np.zeros((128, NH * NOFF), np.float32)
    for h in range(NH):
        for j in range(NOFF):
            tab[:, h * NOFF + j] = SLOPES[h] * (np.arange(128) + 128.0 * (j - OFF0))
    c["atab"] = tab
    c["iota_pc"] = (np.arange(8)[None, :] * 128.0 + np.arange(128)[:, None]).astype(np.float32)
    nb = (2 * S) // BS + NE
    c["blkthr"] = np.tile((np.arange(nb, dtype=np.float32) * float(BS))[None, :, None], (1, 1, NE)).reshape(1, nb * NE)
    return c


CONST_DT = {"ident_bf": BF16, "ident_f": F32, "blk64": BF16, "cmask": BF16, "negm": F32, "ones_f": F32,
            "ustrict": F32, "atab": F32, "blkthr": F32, "iota_pc": F32}


def build_nc(S=4096, stage=99, dbg=None):
    NT = S // 128
    NB5 = S // 512
    CAP = 2 * S + NE * BS
    nc = bass.Bass("TRN2", target_bir_lowering=False)
    consts = _mk_consts(S)

    def din(name, shape, dt=F32):
        return nc.dram_tensor(name, list(shape), dt, kind="ExternalInput").ap()

    x_d = din("x", [S, D])
    w_in = din("w_in", [D, IN_W])
    w_bda = din("w_branch_da", [512, D])
    w_bml = din("w_branch_ml", [512, D])
    w_gate = din("w_gate", [D, 2 * D])
    w_out = din("w_out", [D, D])
    w1 = din("w1", [NE, D, 512])
    w3 = din("w3", [NE, D, 512])
    w2 = din("w2", [NE, 512, D])
    wr_d = din("w_rt", [D, 36])
    g_attn = din("attn_norm_g", [1, D])
    g_ffn = din("ffn_norm_g", [1, D])
    g_dao = din("da_out_norm_g", [1, 128])
    g_mlo = din("ml_out_norm_g", [1, 512])
    b_rt = din("b_rt", [1, 36])
    b_if = din("b_if", [1, 8])
    lamv = din("lamv", [1, 256])
    gqk_col = din("gqk_col", [128, 2])
    cw_col = din("cw_col", [128, 8 * 4])
    cb_col = din("cb_col", [128, 8])
    bg_col = din("bg_col", [128, 16])
    cd = {k: din("c_" + k, list(v.shape), CONST_DT[k]) for k, v in consts.items()}
    out_d = nc.dram_tensor("out", [S, D], F32, kind="ExternalOutput").ap()
    dbg_d = {}
    if dbg:
        for k, (shape, dt) in dbg.items():
            dbg_d[k] = nc.dram_tensor("dbg_" + k, list(shape), dt, kind="ExternalOutput").ap()
    xs_d = nc.dram_tensor("xs_scr", [CAP, D], BF16).ap()
    ys_d = nc.dram_tensor("ys_scr", [CAP, D], F32).ap()
    hn_d = nc.dram_tensor("hn_scr", [S, D], BF16).ap()
    w1b_d = nc.dram_tensor("w1b_scr", [NE * 128, 4096], BF16).ap()
    w3b_d = nc.dram_tensor("w3b_scr", [NE * 128, 4096], BF16).ap()
    w2b_d = nc.dram_tensor("w2b_scr", [NE * 128, 4096], BF16).ap()

    es = ExitStack()
    with es:
        kb = KB(nc, es)

        def sb(stack, name, shape, dt):
            return stack.enter_context(nc.sbuf_tensor(name, list(shape), dt))

        def ps(stack, name, shape, dt=F32):
            return stack.enter_context(nc.psum_tensor(name, list(shape), dt))

        ident_bf = sb(es, "ident_bf", [128, 128], BF16)
        ident_f = sb(es, "ident_f", [128, 128], F32)
        ones_f = sb(es, "ones_f", [128, 128], F32)
        B_const = kb.buf("const")
        for t, k in ((ident_bf, "ident_bf"), (ident_f, "ident_f"), (ones_f, "ones_f")):
            kb.dma("sp", "const", lambda q, t=t, k=k: q.dma_start(out=t[:], in_=cd[k]), w=[B_const])
        zero_bf = sb(es, "zero_bf", [128, 2048], BF16)
        B_zero = kb.buf("zero")
        for zi in range(4):
            kb.op("pool", lambda e: e.memset(zero_bf[:, zi * 512:(zi + 1) * 512], 0.0), w=[B_zero])

        hT = sb(es, "hT", [128, 8, S], BF16)
        B_hT = kb.bufs(NT, "hT")
        es_mix = ExitStack()
        y_daT = sb(es_mix, "y_daT", [128, 4, S], BF16)
        B_ydaT = kb.bufs(NT * NH, "ydaT")
        B_ymlT = kb.bufs(NT * NH, "ymlT")

        B_xs = kb.buf()
        zf_rows = list(range(0, CAP, 256)) if stage >= 4 else []
        zf_per = (len(zf_rows) + NT - 1) // NT
        with ExitStack() as s1:
            gat = sb(s1, "gat", [128, D], F32)
            B_gat = kb.buf()
            kb.dma("sp", "const", lambda q: q.dma_start(out=gat[:], in_=g_attn.partition_broadcast(128)), w=[B_gat])
            xt = [sb(s1, f"xt{i}", [128, D], F32) for i in range(2)]
            xn = [sb(s1, f"xn{i}", [128, D], BF16) for i in range(2)]
            junk = sb(s1, "junk1", [128, D], F32)
            ssq = sb(s1, "ssq", [128, NT], F32)
            rst = sb(s1, "rst", [128, NT], F32)
            rsd = sb(s1, "rsd", [128, NT], F32)
            ssq2 = sb(s1, "ssq2", [128, 2 * NT], F32)
            pT = [ps(s1, f"pT{i}", [128, 8, 128], BF16) for i in range(2)]
            B_xt, B_xn, B_pT = kb.bufs(2), kb.bufs(2), kb.bufs(2)
            B_junk, B_ss = kb.buf(), kb.bufs(NT)
            for i in range(NT):
                j = i % 2
                kb.dma("sp", f"xt{j}", lambda q: q.dma_start(out=xt[j][:], in_=x_d[i * 128:(i + 1) * 128, :]), w=[B_xt[j]])
                for r0 in zf_rows[i * zf_per:(i + 1) * zf_per]:
                    kb.dma("sp", "xsz", lambda q: q.dma_start(out=xs_d[r0:r0 + 256, :].rearrange("(p a) d -> p (a d)", a=2), in_=zero_bf[:, 0:2048]),
                           r=[B_zero], w=[B_xs])
                for hf in range(2):
                    kb.op("act", lambda e: e.activation(out=junk[:, hf * 512:(hf + 1) * 512], in_=xt[j][:, hf * 512:(hf + 1) * 512],
                                                        func=AF.Square, accum_out=ssq2[:, 2 * i + hf:2 * i + hf + 1]),
                          r=[B_xt[j]], w=[B_junk, B_ss[i]])
                kb.op("act", lambda e: e.activation(out=rst[:, i:i + 1], in_=ssq2[:, 2 * i:2 * i + 1], func=AF.Identity,
                                                    bias=ssq2[:, 2 * i + 1:2 * i + 2]), r=[B_ss[i]], w=[B_ss[i]])
                kb.op("act", lambda e: e.activation(out=rst[:, i:i + 1], in_=rst[:, i:i + 1], func=AF.Sqrt, scale=1.0 / D, bias=EPS),
                      r=[B_ss[i]], w=[B_ss[i]])
                kb.op("dve", lambda e: e.reciprocal(out=rsd[:, i:i + 1], in_=rst[:, i:i + 1]), r=[B_ss[i]], w=[B_ss[i]])
                for hf in range(2):
                    kb.op("dve", lambda e: e.scalar_tensor_tensor(out=xn[j][:, hf * 512:(hf + 1) * 512], in0=xt[j][:, hf * 512:(hf + 1) * 512],
                                                                  scalar=rsd[:, i:i + 1], in1=gat[:, hf * 512:(hf + 1) * 512],
                                                                  op0=ALU.mult, op1=ALU.mult),
                          r=[B_xt[j], B_ss[i], B_gat], w=[B_xn[j]])
                for c in range(8):
                    kb.op("pe", lambda e: e.transpose(out=pT[j][:, c, :], in_=xn[j][:, c * 128:(c + 1) * 128], identity=ident_bf[:]),
                          r=[B_xn[j], B_const], w=[B_pT[j]])
                for hf in range(2):
                    kb.op("act", lambda e: e.copy(out=hT[:, hf * 4:hf * 4 + 4, i * 128:(i + 1) * 128], in_=pT[j][:, hf * 4:hf * 4 + 4, :]),
                          r=[B_pT[j]], w=[B_hT[i]])

        kb.barrier()
        if "hT" in dbg_d:
            kb.dma("sp", "dbg", lambda q: q.dma_start(out=dbg_d["hT"], in_=hT[:]), r=B_hT)

        B_wconv = kb.buf()
        conv_state = [0]

        def conv_next(n=1):
            if stage < 5:
                return
            for _ in range(n):
                e_ = conv_state[0]
                if e_ >= NE:
                    return
                conv_state[0] += 1
                for src_, dst_, c_ in ((w1, w1b_d, 8), (w3, w3b_d, 8), (w2, w2b_d, 4)):
                    kb.dma("pool", "wconv", lambda q: q.dma_start(out=dst_[e_ * 128:(e_ + 1) * 128, :],
                           in_=src_[e_].rearrange("(p c) f -> p (c f)", c=c_)), w=[B_wconv])
        if stage >= 2:
            _da_phase(nc, kb, S, hT, B_hT, y_daT, B_ydaT, w_in, cd, gqk_col, g_dao, lamv, ident_bf, zero_bf, B_const, B_zero, sb, ps, dbg_d, conv_next)
        conv_next(NE)
        y_mlT = sb(es_mix, "y_mlT", [128, 4, S], BF16)
        if stage >= 3:
            _ml_phase(nc, kb, S, hT, B_hT, y_mlT, B_ymlT, w_in, cd, cw_col, cb_col, g_mlo, b_if, ident_bf, ident_f, ones_f,
                      B_const, sb, ps, dbg_d)
        if stage >= 4:
            _merge_moe(nc, kb, S, hT, B_hT, y_daT, B_ydaT, y_mlT, B_ymlT, es_mix, x_d, out_d, w_bda, w_bml, w_gate, w_out,
                       bg_col, g_ffn, wr_d, b_rt, w1, w3, w2, xs_d, ys_d, hn_d, cd, ident_bf, ident_f, ones_f, zero_bf,
                       B_const, B_zero, sb, ps, dbg_d, stage, B_xs, (w1b_d, w3b_d, w2b_d, B_wconv))
        else:
            es_mix.close()

        allb = []
        for k in list(kb.dsem.keys()):
            b = Buf()
            b.w = (k, kb.dcnt[k])
            allb.append(b)
        for k in kb.sem:
            if kb.cnt[k] > 0:
                b = Buf()
                b.w = (k, kb.cnt[k])
                allb.append(b)
        kb._wait("sp", allb, [], True)
    return nc, consts


def _da_phase(nc, kb, S, hT, B_hT, y_daT, B_ydaT, w_in, cd, gqk_col, g_dao, lamv, ident_bf, zero_bf, B_const, B_zero, sb, ps, dbg_d, conv_next):
    NT = S // 128
    NB5 = S // 512
    with ExitStack() as s:
        blk64 = sb(s, "blk64", [128, 128], BF16)
        cmask = sb(s, "cmask", [128, 128], BF16)
        atab = sb(s, "atab", [128, NH * NOFF], F32)
        gqk = sb(s, "gqk", [128, 2], F32)
        gdo = sb(s, "gdo", [128, 128], F32)
        lam_t = sb(s, "lam_t", [128, 256], F32)
        B_c = kb.buf()
        for t, src in ((blk64, cd["blk64"]), (cmask, cd["cmask"]), (atab, cd["atab"]), (gqk, gqk_col),
                       (gdo, g_dao.partition_broadcast(128)), (lam_t, lamv.partition_broadcast(128))):
            kb.dma("sp", "const", lambda q, t=t, src=src: q.dma_start(out=t[:], in_=src), w=[B_c])
        lj = sb(s, "lj", [128, 128], F32)
        ls = sb(s, "ls", [128, 4], F32)
        neglam = sb(s, "neglam", [128, 1], F32)
        B_l = kb.buf()
        lv = lam_t[:].rearrange("p (a b d) -> p a b d", a=2, b=2)
        kb.op("dve", lambda e: e.tensor_tensor(out=lj[:].rearrange("p (a d) -> p a d", a=2), in0=lv[:, :, 0, :], in1=lv[:, :, 1, :],
                                               op=ALU.mult), r=[B_c], w=[B_l])
        kb.op("dve", lambda e: e.tensor_reduce(out=ls[:, 0:2], in_=lj[:].rearrange("p (a d) -> p a d", a=2), axis=AX.X, op=ALU.add),
              r=[B_l], w=[B_l])
        kb.op("act", lambda e: e.activation(out=ls[:, 2:4], in_=ls[:, 0:2], func=AF.Exp), r=[B_l], w=[B_l])
        kb.op("dve", lambda e: e.tensor_tensor(out=neglam[:], in0=ls[:, 3:4], in1=ls[:, 2:3], op=ALU.subtract), r=[B_l], w=[B_l])
        kb.op("dve", lambda e: e.tensor_scalar(out=neglam[:], in0=neglam[:], scalar1=-LAM_INIT, scalar2=None, op0=ALU.add),
              r=[B_l], w=[B_l])
        kb.op("dve", lambda e: e.tensor_scalar(out=gdo[:], in0=gdo[:], scalar1=1.0 - LAM_INIT, scalar2=None, op0=ALU.mult),
              r=[B_c], w=[B_c])

        P3 = [ps(s, f"daP{i}", [128, 512], F32) for i in range(3)]
        B_P3 = kb.bufs(3)
        acc4 = ps(s, "daAcc", [128, 4, 512], F32)
        B_acc = kb.buf()
        ptr = ps(s, "daPtr", [128, 8, 128], BF16)
        B_ptr = kb.buf()
        pcnt = [0]

        def nextP():
            i = pcnt[0] % 3
            pcnt[0] += 1
            return P3[i], B_P3[i]

        Vda = sb(s, "Vda", [128, NT, 4, 130], BF16)
        B_V = kb.bufs(NT)
        B_Vones = kb.buf()
        kb.op("pool", lambda e: e.memset(Vda[:, :, :, 128:130], 1.0), w=[B_Vones])
        with ExitStack() as sv:
            wv = sb(sv, "wv", [128, 8, 512], BF16)
            B_wv = kb.buf()
            kb.dma("pool", "wv", lambda q: q.dma_start(out=wv[:], in_=w_in[:, OFF_DA_V:OFF_DA_V + 512].rearrange("(c p) f -> p c f", p=128)),
                   w=[B_wv])
            for i in range(NT):
                P, BP = nextP()
                for c in range(8):
                    kb.op("pe", lambda e: e.matmul(P[:], lhsT=hT[:, c, i * 128:(i + 1) * 128], rhs=wv[:, c, :], start=(c == 0), stop=(c == 7)),
                          r=[B_hT[i], B_wv], w=[BP])
                kb.op("act", lambda e: e.copy(out=Vda[:, i, :, 0:128], in_=P[:].rearrange("p (h d) -> p h d", h=4)),
                      r=[BP, B_Vones], w=[B_V[i]])
        kb.barrier()

        wqk = [sb(s, f"wqk{i}", [128, 8, 256], BF16) for i in range(2)]
        B_wqk = kb.bufs(2)
        qkT = [sb(s, f"qkT{i}", [128, 2, S], BF16) for i in range(2)]
        B_qk = [kb.bufs(NB5 * 2) for _ in range(2)]
        sq_sb = [sb(s, f"sq_sb{i}", [128, 512], BF16) for i in range(2)]
        sd_sb = [sb(s, f"sd_sb{i}", [128, 512], F32) for i in range(2)]
        B_sq, B_sd = kb.bufs(2), kb.bufs(2)
        Et = [sb(s, f"Et{i}", [128, 512], BF16) for i in range(4)]
        B_Et = kb.bufs(4)
        ecnt = 0
        o_sb = sb(s, "o_sb", [128, 4, 128], F32)
        t_sb = sb(s, "t_sb", [128, 4, 128], F32)
        y_sb = sb(s, "y_sb", [128, 4, 128], BF16)
        rr = sb(s, "rr", [128, 16], F32)
        rra = sb(s, "rra", [128, 8], F32)
        B_o = kb.buf()
        B_accj = kb.bufs(4)

        def load_w(h):
            hb = h % 2
            kb.dma("pool", f"wqk{hb}", lambda q: q.dma_start(out=wqk[hb][:, :, 0:128],
                   in_=w_in[:, OFF_DA_Q + h * 128:OFF_DA_Q + (h + 1) * 128].rearrange("(c p) f -> p c f", p=128)), w=[B_wqk[hb]])
            kb.dma("pool", f"wqk{hb}", lambda q: q.dma_start(out=wqk[hb][:, :, 128:256],
                   in_=w_in[:, OFF_DA_K + h * 128:OFF_DA_K + (h + 1) * 128].rearrange("(c p) f -> p c f", p=128)), w=[B_wqk[hb]])

        load_w(0)
        for h in range(NH):
            hb = h % 2
            if h + 1 < NH:
                load_w(h + 1)
            slope = SLOPES[h]
            kb.barrier()
            pool7 = [(P3[i_][:], B_P3[i_]) for i_ in range(3)] + [(acc4[:, j_, :], B_accj[j_]) for j_ in range(4)]
            blocks = [(tb, which) for tb in range(NB5) for which in range(2)]
            bstate = {}

            def pfront(n):
                tb, which = blocks[n]
                Pa, Ba = pool7[(2 * n) % 7]
                Pb, Bb = pool7[(2 * n + 1) % 7]
                kk = n % 2
                for c in range(8):
                    kb.op("pe", lambda e: e.matmul(Pa, lhsT=wqk[hb][:, c, which * 128:(which + 1) * 128],
                                                   rhs=hT[:, c, tb * 512:(tb + 1) * 512], start=(c == 0), stop=(c == 7)),
                          r=B_hT[tb * 4:tb * 4 + 4] + [B_wqk[hb]], w=[Ba])
                kb.op("act", lambda e: e.activation(out=sq_sb[kk][:], in_=Pa, func=AF.Square), r=[Ba], w=[B_sq[kk]])
                kb.op("pe", lambda e: e.matmul(Pb, lhsT=blk64[:], rhs=sq_sb[kk][:], start=True, stop=True),
                      r=[B_sq[kk], B_c], w=[Bb])

            def pback(n):
                tb, which = blocks[n]
                Pa, Ba = pool7[(2 * n) % 7]
                Pb, Bb = pool7[(2 * n + 1) % 7]
                kk = n % 2
                kb.op("act", lambda e: e.activation(out=sd_sb[kk][:], in_=Pb, func=AF.Ln, bias=EPS), r=[Bb], w=[B_sd[kk]])
                kb.op("act", lambda e: e.activation(out=sd_sb[kk][:], in_=sd_sb[kk][:], func=AF.Exp, scale=-0.5), r=[B_sd[kk]], w=[B_sd[kk]])
                kb.op("dve", lambda e: e.scalar_tensor_tensor(out=qkT[hb][:, which, tb * 512:(tb + 1) * 512], in0=Pa,
                                                              scalar=gqk[:, which:which + 1], in1=sd_sb[kk][:],
                                                              op0=ALU.mult, op1=ALU.mult),
                      r=[Ba, B_sd[kk], B_c], w=[B_qk[hb][tb * 2 + which]])

            pfront(0)
            for n in range(len(blocks)):
                if n + 1 < len(blocks):
                    pfront(n + 1)
                pback(n)
            kb.barrier()
            if h == 0 and "wqk" in dbg_d:
                kb.dma("sp", "dbg", lambda q: q.dma_start(out=dbg_d["wqk"], in_=wqk[0][:]), r=[B_wqk[0]])
            if h == 0 and "sd" in dbg_d:
                kb.dma("sp", "dbg", lambda q: q.dma_start(out=dbg_d["sd"], in_=sd_sb[0][:]), r=[B_sd[0]])
                kb.dma("sp", "dbg", lambda q: q.dma_start(out=dbg_d["sq"], in_=sq_sb[0][:]), r=[B_sq[0]])
            if h == 0 and "qkT" in dbg_d:
                kb.dma("sp", "dbg", lambda q: q.dma_start(out=dbg_d["qkT"], in_=qkT[0][:]), r=B_qk[0])
            sub = 128 if slope * 511 > 32.0 else 512
            units = []
            for qb in range(NB5):
                kt_max_ = 4 * qb + 3
                kt_min_ = max(0, int(math.ceil((qb * 512 - SKIP_T / slope - 127) / 128.0)))
                for kt in range(kt_min_, kt_max_ + 1):
                    for m in range(2):
                        units.append((qb, kt, m, kt == kt_min_ and m == 0, kt == kt_max_ and m == 1))
            ustate = {}
            pending = []

            def emit_qk(u):
                nonlocal ecnt
                qb, kt, m, _, _ = u
                q0 = qb * 512
                jj = kt - 4 * qb
                j_lo = max(0, jj)
                P, BP = nextP()
                kb.op("pe", lambda e: e.matmul(P[:, j_lo * 128:512], lhsT=qkT[hb][m * 64:(m + 1) * 64, 1, kt * 128:(kt + 1) * 128],
                                               rhs=qkT[hb][m * 64:(m + 1) * 64, 0, q0 + j_lo * 128:q0 + 512], start=True, stop=True),
                      r=[B_qk[hb][(kt // 4) * 2 + 1], B_qk[hb][qb * 2]], w=[BP])
                ei = ecnt % 4
                ecnt += 1
                E, BE = Et[ei], B_Et[ei]
                if sub == 512:
                    col = h * NOFF + (kt - 4 * qb) + OFF0
                    kb.op("act", lambda e: e.activation(out=E[:, j_lo * 128:512], in_=P[:, j_lo * 128:512], func=AF.Exp,
                                                        scale=0.125, bias=atab[:, col:col + 1]), r=[BP, B_c], w=[BE])
                else:
                    for j in range(j_lo, 4):
                        col = h * NOFF + (kt - 4 * qb - j) + OFF0
                        kb.op("act", lambda e: e.activation(out=E[:, j * 128:(j + 1) * 128], in_=P[:, j * 128:(j + 1) * 128],
                                                            func=AF.Exp, scale=0.125, bias=atab[:, col:col + 1]),
                              r=[BP, B_c], w=[BE])
                if jj >= 0:
                    kb.op("pool", lambda e: e.tensor_tensor(out=E[:, jj * 128:(jj + 1) * 128], in0=E[:, jj * 128:(jj + 1) * 128],
                                                            in1=cmask[:], op=ALU.mult), r=[B_c], w=[BE])
                ustate[u] = (E, BE, j_lo)

            def emit_av(u):
                qb, kt, m, first, last = u
                q0 = qb * 512
                E, BE, j_lo = ustate.pop(u)
                if first:
                    for j in range(4):
                        kb.op("pe", lambda e: e.matmul(acc4[:, j, :], lhsT=zero_bf[0:1, 0:128], rhs=zero_bf[0:1, 0:512],
                                                       start=True, stop=True, skip_group_check=True), r=[B_zero], w=[B_acc])
                for j in range(j_lo, 4):
                    kb.op("pe", lambda e: e.matmul(acc4[:, j, m * 129:(m + 1) * 129], lhsT=E[:, j * 128:(j + 1) * 128],
                                                   rhs=Vda[:, kt, h, 0:129], start=False, stop=last, skip_group_check=True),
                          r=[BE, B_V[kt]], w=[B_acc])
                if last:
                    while pending:
                        kb.op(*pending.pop(0))
                    evac(qb, q0)
                    conv_next(1)

            def evac(qb, q0):
                kb.op("dve", lambda e: e.reciprocal(out=rr[:, 0:4], in_=acc4[:, :, 128:129]), r=[B_acc], w=[B_o])
                kb.op("dve", lambda e: e.reciprocal(out=rr[:, 4:8], in_=acc4[:, :, 257:258]), r=[B_acc], w=[B_o])
                kb.op("dve", lambda e: e.tensor_scalar(out=rr[:, 4:8], in0=rr[:, 4:8], scalar1=neglam[:, 0:1], scalar2=None, op0=ALU.mult),
                      r=[B_l], w=[B_o])
                kb.op("dve", lambda e: e.tensor_tensor(out=o_sb[:], in0=acc4[:, :, 0:128], in1=rr[:, 0:4].unsqueeze(2).to_broadcast([128, 4, 128]),
                                                       op=ALU.mult), r=[B_acc], w=[B_o])
                kb.op("dve", lambda e: e.tensor_tensor(out=t_sb[:], in0=acc4[:, :, 129:257], in1=rr[:, 4:8].unsqueeze(2).to_broadcast([128, 4, 128]),
                                                       op=ALU.mult), r=[B_acc], w=[B_o])
                rec_ = []
                kb.op = lambda e, fn, r=(), w=(), same=None: rec_.append((e, fn, list(r), list(w), same))
                try:
                    evac_tail(qb, q0)
                finally:
                    del kb.op
                pending.extend(rec_)

            def evac_tail(qb, q0):
                kb.op("dve", lambda e: e.tensor_tensor(out=o_sb[:], in0=o_sb[:], in1=t_sb[:], op=ALU.add), r=[B_o], w=[B_o])
                kb.op("dve", lambda e: e.tensor_tensor(out=t_sb[:], in0=o_sb[:], in1=o_sb[:], op=ALU.mult), r=[B_o], w=[B_o])
                kb.op("dve", lambda e: e.tensor_reduce(out=rr[:, 8:12], in_=t_sb[:], axis=AX.X, op=ALU.add), r=[B_o], w=[B_o])
                kb.op("act", lambda e: e.activation(out=rra[:, 0:4], in_=rr[:, 8:12], func=AF.Ln, scale=1.0 / 128, bias=EPS), r=[B_o], w=[B_o])
                kb.op("act", lambda e: e.activation(out=rra[:, 4:8], in_=rra[:, 0:4], func=AF.Exp, scale=-0.5), r=[B_o], w=[B_o])
                kb.op("dve", lambda e: e.tensor_tensor(out=t_sb[:], in0=o_sb[:], in1=rra[:, 4:8].unsqueeze(2).to_broadcast([128, 4, 128]),
                                                       op=ALU.mult), r=[B_o], w=[B_o])
                kb.op("dve", lambda e: e.tensor_tensor(out=y_sb[:], in0=t_sb[:], in1=gdo[:].unsqueeze(1).to_broadcast([128, 4, 128]),
                                                       op=ALU.mult), r=[B_o, B_c], w=[B_o])
                for j in range(4):
                    kb.op("pe", lambda e, j=j: e.transpose(out=ptr[:, j, :], in_=y_sb[:, j, :], identity=ident_bf[:]), r=[B_o, B_const], w=[B_ptr])
                kb.op("act", lambda e, h=h: e.copy(out=y_daT[:, h, q0:q0 + 512].rearrange("p (j t) -> p j t", j=4), in_=ptr[:, 0:4, :]),
                      r=[B_ptr], w=B_ydaT[h * NT + qb * 4:h * NT + qb * 4 + 4])

            emit_qk(units[0])
            for ui in range(len(units)):
                if ui + 1 < len(units):
                    emit_qk(units[ui + 1])
                emit_av(units[ui])
                if pending and not units[ui][4]:
                    kb.op(*pending.pop(0))
            while pending:
                kb.op(*pending.pop(0))
        if "y_daT" in dbg_d:
            kb.dma("sp", "dbg", lambda q: q.dma_start(out=dbg_d["y_daT"], in_=y_daT[:]), r=B_ydaT)
        kb.barrier()


def _ml_phase(nc, kb, S, hT, B_hT, y_mlT, B_ymlT, w_in, cd, cw_col, cb_col, g_mlo, b_if, ident_bf, ident_f, ones_f,
              B_const, sb, ps, dbg_d):
    NT = S // 128
    NB5 = S // 512
    NHC = NT * 4
    QS = 128.0 ** -0.5
    with ExitStack() as s:
        negm = sb(s, "negm", [128, 128], F32)
        cw = sb(s, "cw", [128, 32], F32)
        cb = sb(s, "cb", [128, 8], F32)
        gml = sb(s, "gml", [128, 512], F32)
        bif = sb(s, "bif", [128, 8], F32)
        wif = sb(s, "wif", [128, 8, 8], BF16)
        B_c = kb.buf()
        for t, src in ((negm, cd["negm"]), (cw, cw_col), (cb, cb_col), (gml, g_mlo.partition_broadcast(128)),
                       (bif, b_if.partition_broadcast(128))):
            kb.dma("sp", "const", lambda q, t=t, src=src: q.dma_start(out=t[:], in_=src), w=[B_c])
        kb.dma("pool", "wif", lambda q: q.dma_start(out=wif[:], in_=w_in[:, OFF_ML_I:OFF_ML_I + 8].rearrange("(c p) f -> p c f", p=128)), w=[B_c])

        PA = [ps(s, f"mlPA{i}", [128, 512], F32) for i in range(2)]
        B_PA = kb.bufs(2)
        pacnt = [0]

        def nextPA():
            i = pacnt[0] % 2
            pacnt[0] += 1
            return PA[i], B_PA[i]
        Pew2 = [ps(s, f"mlPew{i}", [128, 512], F32) for i in range(2)]
        B_Pew2 = kb.bufs(2)
        Pew, B_Pew = Pew2[0], B_Pew2[0]
        Po2 = [ps(s, f"mlPo{i}", [128, 512], F32) for i in range(2)]
        B_Po2 = kb.bufs(2)
        Po, B_Po = Po2[0], B_Po2[0]
        Pc = ps(s, "mlPc", [128, 512], F32)
        Ptk = ps(s, "mlPtk", [128, 8, 128], BF16)
        Pty = Ptk
        Pg = Po
        B_Pc, B_Ptk = kb.bufs(2)
        B_Pty = B_Ptk
        B_Pg = B_Po

        for i in range(NT):
            for c in range(8):
                kb.op("pe", lambda e: e.matmul(Pg[:, i * 8:(i + 1) * 8], lhsT=hT[:, c, i * 128:(i + 1) * 128], rhs=wif[:, c, :],
                                               start=(c == 0), stop=(c == 7)), r=[B_hT[i], B_c], w=[B_Pg])
        gsb = sb(s, "gsb", [128, NT, 8], F32)
        XC = sb(s, "XC", [128, 2, NT, 4], F32)
        tmpg = sb(s, "tmpg", [128, NT, 4], F32)
        B_g = kb.buf()
        kb.op("dve", lambda e: e.tensor_tensor(out=gsb[:], in0=Pg[:, 0:NT * 8].rearrange("p (i g) -> p i g", g=8),
                                               in1=bif[:].unsqueeze(1).to_broadcast([128, NT, 8]), op=ALU.add), r=[B_Pg, B_c], w=[B_g])
        kb.op("act", lambda e: e.activation(out=tmpg[:], in_=gsb[:, :, 4:8], func=AF.Exp, scale=-1.0), r=[B_g], w=[B_g])
        kb.op("act", lambda e: e.activation(out=tmpg[:], in_=tmpg[:], func=AF.Ln, bias=1.0), r=[B_g], w=[B_g])
        kb.op("dve", lambda e: e.tensor_scalar(out=XC[:, 1], in0=tmpg[:], scalar1=-1.0, scalar2=None, op0=ALU.mult), r=[B_g], w=[B_g])
        kb.op("dve", lambda e: e.tensor_copy(out=XC[:, 0], in_=gsb[:, :, 0:4]), r=[B_g], w=[B_g])
        RN = ["iR", "lfR", "bR", "betaR", "pmR", "mxR", "alphaR", "mrowR", "winterR", "emrR", "winR", "zR"]
        Rt = {n: sb(s, n, [128, 128], F32) for n in RN}
        B_R = kb.buf()
        for a, n in ((0, "iR"), (1, "lfR")):
            kb.op("pe", lambda e: e.transpose(out=Pew[0:NHC, a * 128:(a + 1) * 128], in_=XC[:, a].rearrange("p i h -> p (i h)"),
                                              identity=ident_f[:]), r=[B_g, B_const], w=[B_Pew])
            kb.op("dve", lambda e: e.tensor_copy(out=Rt[n][0:NHC, :], in_=Pew[0:NHC, a * 128:(a + 1) * 128]), r=[B_Pew], w=[B_R])
        R = {n: Rt[n][0:NHC, :] for n in RN}
        kb.op("pool", lambda e: e.memset(Rt["zR"][:], 0.0), w=[B_R])
        kb.op("dve", lambda e: e.tensor_tensor_scan(out=R["bR"], data0=R["lfR"], data1=R["zR"], initial=0.0, op0=ALU.add, op1=ALU.add),
              r=[B_R], w=[B_R])
        kb.op("dve", lambda e: e.tensor_tensor(out=R["betaR"], in0=R["iR"], in1=R["bR"], op=ALU.subtract), r=[B_R], w=[B_R])
        kb.op("dve", lambda e: e.tensor_tensor_scan(out=R["pmR"], data0=R["betaR"], data1=R["betaR"], initial=-1e30, op0=ALU.max, op1=ALU.max),
              r=[B_R], w=[B_R])
        c2 = sb(s, "c2", [128, 8], F32)
        rows = sb(s, "rows", [1, 4, 128], F32)
        drow = sb(s, "drow", [1, 128], F32)
        kb.op("dve", lambda e: e.tensor_copy(out=c2[0:NHC, 0:1], in_=R["bR"][:, 127:128]), r=[B_R], w=[B_R])
        kb.op("dve", lambda e: e.tensor_tensor(out=c2[0:NHC, 1:2], in0=R["bR"][:, 127:128], in1=R["pmR"][:, 127:128], op=ALU.add), r=[B_R], w=[B_R])
        for a in range(2):
            kb.op("pe", lambda e: e.transpose(out=Pew[0:1, a * 128:a * 128 + NHC], in_=c2[0:NHC, a:a + 1], identity=ident_f[0:NHC, 0:NHC]),
                  r=[B_R, B_const], w=[B_Pew])
        kb.op("dve", lambda e: e.tensor_copy(out=rows[:, 0:2, 0:NHC], in_=Pew[0:1, 0:256].rearrange("p (a n) -> p a n", a=2)[:, :, 0:NHC]),
              r=[B_Pew], w=[B_R])
        for h in range(4):
            v = lambda a: rows[:, a, 0:NHC].rearrange("p (i h) -> p h i", h=4)[:, h, :]
            kb.op("dve", lambda e: e.tensor_tensor_scan(out=v(2), data0=v(0), data1=v(1), initial=0.0, op0=ALU.add, op1=ALU.max),
                  r=[B_R], w=[B_R])
        kb.op("pool", lambda e: e.memset(rows[:, 3, 0:4], 0.0), r=[B_R], w=[B_R])
        if NHC > 4:
            kb.op("dve", lambda e: e.tensor_copy(out=rows[:, 3, 4:NHC], in_=rows[:, 2, 0:NHC - 4]), r=[B_R], w=[B_R])
        kb.op("pe", lambda e: e.matmul(Pew[0:NHC, 0:1], lhsT=rows[:, 3, 0:NHC], rhs=ones_f[0:1, 0:1], start=True, stop=True),
              r=[B_R, B_const], w=[B_Pew])
        kb.op("dve", lambda e: e.tensor_copy(out=c2[0:NHC, 2:3], in_=Pew[0:NHC, 0:1]), r=[B_Pew], w=[B_R])
        ms = c2[0:NHC, 2:3]
        kb.op("dve", lambda e: e.tensor_scalar(out=R["mxR"], in0=R["pmR"], scalar1=ms, scalar2=None, op0=ALU.max), r=[B_R], w=[B_R])
        kb.op("dve", lambda e: e.tensor_scalar(out=R["alphaR"], in0=R["mxR"], scalar1=-1.0, scalar2=None, op0=ALU.mult), r=[B_R], w=[B_R])
        kb.op("dve", lambda e: e.tensor_tensor(out=R["mrowR"], in0=R["bR"], in1=R["mxR"], op=ALU.add), r=[B_R], w=[B_R])
        kb.op("act", lambda e: e.activation(out=R["winterR"], in_=R["alphaR"], func=AF.Exp, bias=ms), r=[B_R], w=[B_R])
        kb.op("act", lambda e: e.activation(out=R["emrR"], in_=R["mrowR"], func=AF.Exp, scale=-1.0), r=[B_R], w=[B_R])
        kb.op("dve", lambda e: e.tensor_tensor(out=c2[0:NHC, 6:7], in0=ms, in1=R["pmR"][:, 127:128], op=ALU.max), r=[B_R], w=[B_R])
        kb.op("dve", lambda e: e.tensor_tensor(out=c2[0:NHC, 3:4], in0=c2[0:NHC, 6:7], in1=c2[0:NHC, 0:1], op=ALU.add), r=[B_R], w=[B_R])
        kb.op("dve", lambda e: e.tensor_tensor(out=c2[0:NHC, 5:6], in0=c2[0:NHC, 0:1], in1=c2[0:NHC, 3:4], op=ALU.subtract), r=[B_R], w=[B_R])
        kb.op("act", lambda e: e.activation(out=c2[0:NHC, 4:5], in_=ms, func=AF.Exp, bias=c2[0:NHC, 5:6]), r=[B_R], w=[B_R])
        kb.op("act", lambda e: e.activation(out=R["winR"], in_=R["betaR"], func=AF.Exp, bias=c2[0:NHC, 5:6]), r=[B_R], w=[B_R])
        CN = ["betaR", "alphaR", "winterR", "emrR", "winR"]
        Ct = {n: sb(s, "C_" + n, [128, 128], F32) for n in CN}
        dbc = sb(s, "dbc", [128, 128], F32)
        B_C = kb.buf()
        for n in CN:
            kb.op("pe", lambda e: e.transpose(out=Pew[:, 0:NHC], in_=R[n], identity=ident_f[0:NHC, 0:NHC]), r=[B_R, B_const], w=[B_Pew])
            kb.op("dve", lambda e: e.tensor_copy(out=Ct[n][:, 0:NHC], in_=Pew[:, 0:NHC]), r=[B_Pew], w=[B_C])
        kb.op("pe", lambda e: e.transpose(out=Pew[0:1, 0:NHC], in_=c2[0:NHC, 4:5], identity=ident_f[0:NHC, 0:NHC]), r=[B_R, B_const], w=[B_Pew])
        kb.op("dve", lambda e: e.tensor_copy(out=drow[:, 0:NHC], in_=Pew[0:1, 0:NHC]), r=[B_Pew], w=[B_R])
        kb.op("pe", lambda e: e.matmul(Pew[:, 0:NHC], lhsT=ones_f[0:1, :], rhs=drow[:, 0:NHC], start=True, stop=True), r=[B_R, B_const], w=[B_Pew])
        kb.op("dve", lambda e: e.tensor_copy(out=dbc[:, 0:NHC], in_=Pew[:, 0:NHC]), r=[B_Pew], w=[B_C])

        wq4 = sb(s, "wq4", [128, 8, 512], BF16)
        B_w = kb.buf()
        qkT = sb(s, "mlqkT", [128, 2, S], BF16)
        B_qk = [kb.bufs(NB5), kb.bufs(NB5)]
        Vml = sb(s, "Vml", [128, NT, 130], BF16)
        og = sb(s, "og", [128, NT, 128], BF16)
        B_vo = kb.bufs(NT)
        B_vones = kb.buf()
        kb.op("pool", lambda e: e.memset(Vml[:, :, 128:130], 1.0), w=[B_vones])
        U2 = [sb(s, f"U2{i}", [128, 515], F32) for i in range(2)]
        B_U = kb.bufs(2)
        acc = [sb(s, f"cacc{i}", [128, 512], F32) for i in range(2)]
        B_a = kb.bufs(2)
        Cf = sb(s, "Cf", [128, 130], F32)
        Cbf = [sb(s, f"Cbf{i}", [128, 130], BF16) for i in range(2)]
        B_Cf = kb.buf()
        B_Cbf = kb.bufs(2)
        dA = [sb(s, f"dA{i}", [128, 128], F32) for i in range(2)]
        dW = [sb(s, f"dW{i}", [128, 128], F32) for i in range(2)]
        Wt = [sb(s, f"Wt{i}", [128, 128], F32) for i in range(2)]
        Pt = [sb(s, f"Pt{i}", [128, 128], BF16) for i in range(2)]
        qs = [sb(s, f"qs{i}", [128, 128], BF16) for i in range(2)]
        kw = [sb(s, f"kw{i}", [128, 128], BF16) for i in range(2)]
        t1_2 = [sb(s, f"t1{i}", [128, 128], F32) for i in range(2)]
        yb_2 = [sb(s, f"yb{i}", [128, 128], BF16) for i in range(2)]
        jk_2 = [sb(s, f"jk{i}", [128, 128], F32) for i in range(2)]
        sc_2 = [sb(s, f"sc{i}", [128, 8], F32) for i in range(2)]
        sca_2 = [sb(s, f"sca{i}", [128, 8], F32) for i in range(2)]
        B_ch2 = kb.bufs(2)
        B_y2 = kb.bufs(2)
        B_dA, B_dW, B_Wt, B_Pt, B_qs, B_kw = [kb.bufs(2) for _ in range(6)]
        offs = [OFF_ML_QK, OFF_ML_QK + 512, OFF_ML_V, OFF_ML_O]
        for h in range(NH):
            for a in range(4):
                kb.dma("pool", "wq4", lambda q: q.dma_start(out=wq4[:, :, a * 128:(a + 1) * 128],
                       in_=w_in[:, offs[a] + h * 128:offs[a] + (h + 1) * 128].rearrange("(c p) f -> p c f", p=128)), w=[B_w])
            for i in range(NT):
                P, BP = nextPA()
                for c in range(8):
                    kb.op("pe", lambda e: e.matmul(P[:, 0:256], lhsT=hT[:, c, i * 128:(i + 1) * 128], rhs=wq4[:, c, 256:512],
                                                   start=(c == 0), stop=(c == 7)), r=[B_hT[i], B_w], w=[BP])
                kb.op("act", lambda e: e.copy(out=Vml[:, i, 0:128], in_=P[:, 0:128]), r=[BP, B_vones], w=[B_vo[i]])
                kb.op("act", lambda e: e.activation(out=og[:, i, :], in_=P[:, 128:256], func=AF.Sigmoid), r=[BP], w=[B_vo[i]])
            for which in range(2):
                cc = which * 4 + h
                for tb in range(NB5):
                    ub = tb % 2
                    P, BP = nextPA()
                    for c in range(8):
                        kb.op("pe", lambda e: e.matmul(P[:], lhsT=wq4[:, c, which * 128:(which + 1) * 128], rhs=hT[:, c, tb * 512:(tb + 1) * 512],
                                                       start=(c == 0), stop=(c == 7)), r=B_hT[tb * 4:tb * 4 + 4] + [B_w], w=[BP])
                    kb.op("act", lambda e: e.copy(out=U2[ub][:, 3:515], in_=P[:]), r=[BP], w=[B_U[ub]])
                    if tb == 0:
                        kb.op("pool", lambda e: e.memset(U2[ub][:, 0:3], 0.0), w=[B_U[ub]])
                    else:
                        kb.op("pool", lambda e: e.tensor_copy(out=U2[ub][:, 0:3], in_=U2[1 - ub][:, 512:515]), r=[B_U[1 - ub]], w=[B_U[ub]])
                    A_, BA = acc[ub], B_a[ub]
                    kb.op("dve", lambda e: e.tensor_scalar(out=A_[:], in0=U2[ub][:, 3:515], scalar1=cw[:, cc * 4 + 3:cc * 4 + 4],
                                                           scalar2=cb[:, cc:cc + 1], op0=ALU.mult, op1=ALU.add), r=[B_U[ub], B_c], w=[BA])
                    for j in range(3):
                        kb.op("dve", lambda e: e.scalar_tensor_tensor(out=A_[:], in0=U2[ub][:, j:j + 512], scalar=cw[:, cc * 4 + j:cc * 4 + j + 1],
                                                                      in1=A_[:], op0=ALU.mult, op1=ALU.add), r=[B_U[ub], B_c], w=[BA])
                    if which == 1:
                        kb.op("act", lambda e: e.activation(out=qkT[:, 1, tb * 512:(tb + 1) * 512], in_=A_[:], func=AF.Silu),
                              r=[BA], w=[B_qk[1][tb]])
                    else:
                        kb.op("act", lambda e: e.activation(out=A_[:], in_=A_[:], func=AF.Silu), r=[BA], w=[BA])
                        kb.op("pool", lambda e: e.tensor_scalar(out=qkT[:, 0, tb * 512:(tb + 1) * 512], in0=A_[:], scalar1=QS, scalar2=None,
                                                                op0=ALU.mult), r=[BA], w=[B_qk[0][tb]])
            kb.op("pool", lambda e: e.memset(Cf[:], 0.0), w=[B_Cf])
            kb.op("pool", lambda e: e.memset(Cbf[1][:], 0.0), w=[B_Cbf[1]])

            def pre(i):
                jj = i % 2
                hc = i * 4 + h
                tsl = slice(i * 128, (i + 1) * 128)
                tb = i // 4
                Ps_, BPs = PA[jj], B_PA[jj]
                Pw_, BPw = Pew2[jj], B_Pew2[jj]
                kb.op("pe", lambda e: e.matmul(Ps_[:, 0:128], lhsT=qkT[:, 1, tsl], rhs=qkT[:, 0, tsl], start=True, stop=True),
                      r=[B_qk[0][tb], B_qk[1][tb]], w=[BPs])
                kb.op("dve", lambda e: e.tensor_scalar(out=dA[jj][:], in0=ident_f[:], scalar1=Ct["alphaR"][:, hc:hc + 1], scalar2=None, op0=ALU.mult),
                      r=[B_C, B_const], w=[B_dA[jj]])
                kb.op("dve", lambda e: e.tensor_scalar(out=dW[jj][:], in0=ident_f[:], scalar1=Ct["winterR"][:, hc:hc + 1], scalar2=None, op0=ALU.mult),
                      r=[B_C, B_const], w=[B_dW[jj]])
                kb.op("pe", lambda e: e.matmul(Pw_[:, 0:128], lhsT=ones_f[:], rhs=dA[jj][:], start=True, stop=False), r=[B_dA[jj], B_const], w=[BPw])
                kb.op("pe", lambda e: e.matmul(Pw_[:, 0:128], lhsT=ident_f[:], rhs=negm[:], start=False, stop=True), r=[B_c, B_const], w=[BPw])
                kb.op("pe", lambda e: e.matmul(Pw_[:, 128:256], lhsT=ones_f[:], rhs=dW[jj][:], start=True, stop=True), r=[B_dW[jj], B_const], w=[BPw])
                kb.op("pe", lambda e: e.transpose(out=Ptk[:, jj, :], in_=qkT[:, 1, tsl], identity=ident_bf[:]), r=[B_qk[1][tb], B_const], w=[B_Ptk])

            def preB(i):
                jj = i % 2
                hc = i * 4 + h
                tsl = slice(i * 128, (i + 1) * 128)
                tb = i // 4
                Ps_, BPs = PA[jj], B_PA[jj]
                Pw_, BPw = Pew2[jj], B_Pew2[jj]
                kb.op("act", lambda e: e.activation(out=Wt[jj][:], in_=Pw_[:, 0:128], func=AF.Exp, bias=Ct["betaR"][:, hc:hc + 1]),
                      r=[BPw, B_C], w=[B_Wt[jj]])
                kb.op("dve", lambda e: e.tensor_tensor(out=qs[jj][:], in0=qkT[:, 0, tsl], in1=Pw_[:, 128:256], op=ALU.mult),
                      r=[BPw, B_qk[0][tb], B_Wt[jj]], w=[B_qs[jj]])
                kb.op("dve", lambda e: e.tensor_scalar(out=kw[jj][:], in0=Ptk[:, jj, :], scalar1=Ct["winR"][:, hc:hc + 1], scalar2=None, op0=ALU.mult),
                      r=[B_Ptk, B_C], w=[B_kw[jj]])
                kb.op("dve", lambda e: e.tensor_tensor(out=Pt[jj][:], in0=Ps_[:, 0:128], in1=Wt[jj][:], op=ALU.mult), r=[BPs, B_Wt[jj]], w=[B_Pt[jj]])

            def main(i):
                mainA(i)
                tail(i)

            def mainA(i):
                jj = i % 2
                hc = i * 4 + h
                tsl = slice(i * 128, (i + 1) * 128)
                Po, B_Po = Po2[jj], B_Po2[jj]
                kb.op("pe", lambda e: e.matmul(Pc[:, 0:129], lhsT=kw[jj][:], rhs=Vml[:, i, 0:129], start=True, stop=True), r=[B_kw[jj], B_vo[i]], w=[B_Pc])
                kb.op("pe", lambda e: e.matmul(Po[:, 0:129], lhsT=Pt[jj][:], rhs=Vml[:, i, 0:129], start=True, stop=False), r=[B_Pt[jj], B_vo[i]], w=[B_Po])
                kb.op("pe", lambda e: e.matmul(Po[:, 0:129], lhsT=qs[jj][:], rhs=Cbf[(i + 1) % 2][:, 0:129], start=False, stop=True),
                      r=[B_qs[jj], B_Cbf[(i + 1) % 2]], w=[B_Po])
                kb.op("dve", lambda e: e.scalar_tensor_tensor(out=Cf[:, 0:129], in0=Cf[:, 0:129], scalar=dbc[:, hc:hc + 1], in1=Pc[:, 0:129],
                                                              op0=ALU.mult, op1=ALU.add), r=[B_Pc, B_C], w=[B_Cf])
                kb.op("act", lambda e: e.copy(out=Cbf[jj][:, 0:129], in_=Cf[:, 0:129]), r=[B_Cf], w=[B_Cbf[jj]])

            def tail(i):
                jj = i % 2
                hc = i * 4 + h
                tsl = slice(i * 128, (i + 1) * 128)
                Po, B_Po = Po2[jj], B_Po2[jj]
                t1, yb, jk, sc, sca, B_ch, B_y = t1_2[jj], yb_2[jj], jk_2[jj], sc_2[jj], sca_2[jj], B_ch2[jj], B_y2[jj]
                kb.op("act", lambda e: e.activation(out=sca[:, 0:1], in_=Po[:, 128:129], func=AF.Abs), r=[B_Po], w=[B_ch])
                kb.op("dve", lambda e: e.tensor_tensor(out=sc[:, 0:1], in0=sca[:, 0:1], in1=Ct["emrR"][:, hc:hc + 1], op=ALU.max), r=[B_ch, B_C], w=[B_ch])
                kb.op("dve", lambda e: e.tensor_scalar(out=sc[:, 1:2], in0=sc[:, 0:1], scalar1=sc[:, 0:1], scalar2=EPS, op0=ALU.mult, op1=ALU.mult),
                      r=[B_ch], w=[B_ch])
                kb.op("act", lambda e: e.activation(out=jk[:], in_=Po[:, 0:128], func=AF.Square, accum_out=sca[:, 2:3]), r=[B_Po], w=[B_ch])
                kb.op("act", lambda e: e.activation(out=sca[:, 3:4], in_=sca[:, 2:3], func=AF.Ln, scale=1.0 / 128, bias=sc[:, 1:2]), r=[B_ch], w=[B_ch])
                kb.op("act", lambda e: e.activation(out=sca[:, 4:5], in_=sca[:, 3:4], func=AF.Exp, scale=-0.5), r=[B_ch], w=[B_ch])
                kb.op("dve", lambda e: e.scalar_tensor_tensor(out=t1[:], in0=Po[:, 0:128], scalar=sca[:, 4:5], in1=gml[:, h * 128:(h + 1) * 128],
                                                              op0=ALU.mult, op1=ALU.mult), r=[B_Po, B_ch, B_c], w=[B_ch])
                kb.op("pool", lambda e: e.tensor_tensor(out=yb[:], in0=t1[:], in1=og[:, i, :], op=ALU.mult), r=[B_ch, B_vo[i]], w=[B_y])
                kb.op("pe", lambda e: e.transpose(out=Pty[:, 2, :], in_=yb[:], identity=ident_bf[:]), r=[B_y, B_const], w=[B_Pty])
                kb.op("act", lambda e: e.copy(out=y_mlT[:, h, tsl], in_=Pty[:, 2, :]), r=[B_Pty], w=[B_ymlT[h * NT + i]])

            LOOKAHEAD = ML_LOOKAHEAD
            if LOOKAHEAD == 0:
                for i in range(NT):
                    pre(i)
                    preB(i)
                    main(i)
            elif LOOKAHEAD == 1:
                pre(0)
                for i in range(NT):
                    preB(i)
                    if i + 1 < NT:
                        pre(i + 1)
                    main(i)
            elif LOOKAHEAD == 3:
                def rec_ops(fns):
                    rec = []
                    kb.op = lambda e, fn, r=(), w=(), same=None: rec.append((e, fn, list(r), list(w), same))
                    try:
                        for f_ in fns:
                            f_()
                    finally:
                        del kb.op
                    return rec
                pre(0)
                for i in range(NT + 1):
                    fa = []
                    if i < NT:
                        fa.append(lambda i=i: preB(i))
                        if i + 1 < NT:
                            fa.append(lambda i=i: pre(i + 1))
                        fa.append(lambda i=i: mainA(i))
                    ra = rec_ops(fa)
                    rb = rec_ops([lambda i=i: tail(i - 1)]) if i >= 1 else []
                    ia = ib = 0
                    while ia < len(ra) or ib < len(rb):
                        for _ in range(2):
                            if ia < len(ra):
                                kb.op(*ra[ia])
                                ia += 1
                        if ib < len(rb):
                            kb.op(*rb[ib])
                            ib += 1
            else:
                pre(0)
                preB(0)
                for i in range(NT):
                    if i + 1 < NT:
                        pre(i + 1)
                        preB(i + 1)
                    main(i)
        if "y_mlT" in dbg_d:
            kb.dma("sp", "dbg", lambda q: q.dma_start(out=dbg_d["y_mlT"], in_=y_mlT[:]), r=B_ymlT)
        kb.barrier()


def _merge_moe(nc, kb, S, hT, B_hT, y_daT, B_ydaT, y_mlT, B_ymlT, es_mix, x_d, out_d, w_bda, w_bml, w_gate, w_out,
               bg_col, g_ffn, wr_d, b_rt, w1, w3, w2, xs_d, ys_d, hn_d, cd, ident_bf, ident_f, ones_f, zero_bf,
               B_const, B_zero, sb, ps, dbg_d, stage, B_xs, wconv):
    w1b_d, w3b_d, w2b_d, B_wconv = wconv
    NT = S // 128
    NB5 = S // 512
    CAP = 2 * S + NE * BS
    NBLK = CAP // BS
    NSUB = BS // 128
    with ExitStack() as s:
        wg = sb(s, "wg", [128, 8, 2048], BF16)
        wda = sb(s, "wda", [128, 4, 1024], BF16)
        wml = sb(s, "wml", [128, 4, 1024], BF16)
        bg = sb(s, "bg", [128, 16], F32)
        B_w = kb.buf()
        for k4 in range(4):
            kb.dma("pool", "mw", lambda q: q.dma_start(out=wg[:, :, k4 * 512:(k4 + 1) * 512],
                   in_=w_gate[:, k4 * 512:(k4 + 1) * 512].rearrange("(c p) f -> p c f", p=128)), w=[B_w])
        kb.dma("pool", "mw", lambda q: q.dma_start(out=wda[:], in_=w_bda.rearrange("(c p) f -> p c f", p=128)), w=[B_w])
        kb.dma("pool", "mw", lambda q: q.dma_start(out=wml[:], in_=w_bml.rearrange("(c p) f -> p c f", p=128)), w=[B_w])
        kb.dma("sp", "const", lambda q: q.dma_start(out=bg[:], in_=bg_col), w=[B_w])
        Pg = [ps(s, f"mPg{i}", [128, 512], F32) for i in range(2)]
        Pab = [ps(s, f"mPab{i}", [128, 512], F32) for i in range(2)]
        B_Pg, B_Pab = kb.bufs(2), kb.bufs(2)
        gs = [sb(s, f"gs{i}", [128, 512], F32) for i in range(2)]
        tt = [sb(s, f"tt{i}", [128, 512], F32) for i in range(2)]
        B_gs, B_tt = kb.bufs(2), kb.bufs(2)
        mixb = sb(s, "mixb", [128, 8, 512], BF16)
        B_mix = kb.bufs(8)
        for tb in range(NB5):
            tsl = slice(tb * 512, (tb + 1) * 512)
            hb = B_hT[tb * 4:tb * 4 + 4]
            for fc in range(8):
                for g in range(2):
                    kb_w = wda if g == 0 else wml
                    yT = y_daT if g == 0 else y_mlT
                    By = (B_ydaT if g == 0 else B_ymlT)
                    byl = [By[hh * NT + tb * 4 + t4] for hh in range(4) for t4 in range(4)]
                    for c in range(8):
                        kb.op("pe", lambda e: e.matmul(Pg[g][:], lhsT=wg[:, c, g * 1024 + fc * 128:g * 1024 + (fc + 1) * 128], rhs=hT[:, c, tsl],
                                                       start=(c == 0), stop=(c == 7)), r=hb + [B_w], w=[B_Pg[g]])
                    kb.op("act", lambda e: e.activation(out=gs[g][:], in_=Pg[g][:], func=AF.Sigmoid, bias=bg[:, g * 8 + fc:g * 8 + fc + 1]),
                          r=[B_Pg[g], B_w], w=[B_gs[g]])
                    for c in range(4):
                        kb.op("pe", lambda e: e.matmul(Pab[g][:], lhsT=kb_w[:, c, fc * 128:(fc + 1) * 128], rhs=yT[:, c, tsl],
                                                       start=(c == 0), stop=(c == 3)), r=byl + [B_w], w=[B_Pab[g]])
                    kb.op("dve", lambda e: e.tensor_tensor(out=tt[g][:], in0=gs[g][:], in1=Pab[g][:], op=ALU.mult),
                          r=[B_gs[g], B_Pab[g]], w=[B_tt[g]])
                kb.op("pool", lambda e: e.tensor_tensor(out=mixb[:, fc, :], in0=tt[0][:], in1=tt[1][:], op=ALU.add),
                      r=[B_tt[0], B_tt[1]], w=[B_mix[fc]])
            for fc in range(8):
                kb.op("pool", lambda e: e.tensor_copy(out=hT[:, fc, tsl], in_=mixb[:, fc, :]), r=[B_mix[fc]], w=hb)
        kb.barrier()
    es_mix.close()

    es2 = ExitStack()
    with es2:
        s = es2
        A1a = sb(s, "A1a", [128, NT, 32], F32)
        A2a = sb(s, "A2a", [128, NT, 32], F32)
        rka = sb(s, "rka", [128, NT, 32], F32)
        gta = sb(s, "gta", [128, NT, 2], F32)
        base = sb(s, "base", [128, 32], F32)
        d1i = sb(s, "d1i", [128, NT], I32)
        d2i = sb(s, "d2i", [128, NT], I32)
        bexp = sb(s, "bexp", [128, NBLK], I32)
        idxw = sb(s, "idxw", [128, NBLK], I32)
        B_rt = kb.buf()
        B_out = kb.bufs(NT)
        B_hn = kb.bufs(NT)
        with ExitStack() as sB:
            wo = sb(sB, "wo", [128, 8, 1024], BF16)
            gff = sb(sB, "gff", [128, 1024], F32)
            wr = sb(sB, "wr", [128, 8, 36], F32)
            brt = sb(sB, "brt", [128, 36], F32)
            ustr = sb(sB, "ustr", [128, 128], F32)
            B_w = kb.buf()
            for k2 in range(2):
                kb.dma("pool", "mw", lambda q: q.dma_start(out=wo[:, :, k2 * 512:(k2 + 1) * 512],
                       in_=w_out[:, k2 * 512:(k2 + 1) * 512].rearrange("(c p) f -> p c f", p=128)), w=[B_w])
            kb.dma("sp", "const", lambda q: q.dma_start(out=gff[:], in_=g_ffn.partition_broadcast(128)), w=[B_w])
            kb.dma("sp", "const", lambda q: q.dma_start(out=wr[:], in_=wr_d.rearrange("(c p) f -> p c f", p=128)), w=[B_w])
            kb.dma("sp", "const", lambda q: q.dma_start(out=brt[:], in_=b_rt.partition_broadcast(128)), w=[B_w])
            kb.dma("sp", "const", lambda q: q.dma_start(out=ustr[:], in_=cd["ustrict"]), w=[B_w])
            kb.op("pool", lambda e: e.memset(base[:], 0.0), w=[B_rt])
            Px = ps(sB, "bPx", [128, 2, 512], F32)
            PT = ps(sB, "bPT", [128, 8, 128], F32)
            Pr = ps(sB, "bPr", [128, 512], F32)
            B_Px, B_PT, B_Pr = kb.bufs(3)
            xt = [sb(sB, f"bxt{i}", [128, 1024], F32) for i in range(2)]
            x2 = [sb(sB, f"bx2{i}", [128, 1024], F32) for i in range(2)]
            hnf = sb(sB, "hnf", [128, 1024], F32)
            hnb = [sb(sB, f"hnb{i}", [128, 1024], BF16) for i in range(2)]
            hnT = sb(sB, "hnT", [128, 8, 128], F32)
            junk = sb(sB, "bjunk", [128, 512], F32)
            st = sb(sB, "bst", [128, 8], F32)
            sd_ = sb(sB, "bsd", [128, 16], F32)
            lg = sb(sB, "lg", [128, 36], F32)
            oh = sb(sB, "oh", [128, 4], F32)
            t48 = sb(sB, "t48", [128, 4, 8], F32)
            es8 = sb(sB, "es8", [128, 8], F32)
            e28 = sb(sB, "e28", [128, 8], F32)
            mk1 = sb(sB, "mk1", [128, 8], F32)
            mk2 = sb(sB, "mk2", [128, 8], F32)
            At = sb(sB, "At", [128, 32], F32)
            B_xt, B_x2, B_hnb = kb.bufs(2), kb.bufs(2), kb.bufs(2)
            B_hnf, B_hnT, B_r = kb.buf(), kb.buf(), kb.buf()
            B_rx = kb.buf()
            B_lg = kb.bufs(4)
            lg2 = [sb(sB, f"lg2{i}", [128, 36], F32) for i in range(4)]
            B_r2 = kb.bufs(2)
            sd2 = [sd_, sb(sB, "bsd_b", [128, 16], F32)]
            st2 = [st, sb(sB, "bst_b", [128, 8], F32)]
            oh2 = [oh, sb(sB, "oh_b", [128, 4], F32)]
            t482 = [t48, sb(sB, "t48_b", [128, 4, 8], F32)]
            es82 = [es8, sb(sB, "es8_b", [128, 8], F32)]
            e282 = [e28, sb(sB, "e28_b", [128, 8], F32)]
            mk12 = [mk1, sb(sB, "mk1_b", [128, 8], F32)]
            mk22 = [mk2, sb(sB, "mk2_b", [128, 8], F32)]
            stX = sb(sB, "bstX", [128, 8], F32)
            sdX = sb(sB, "bsdX", [128, 16], F32)
            junkR2 = [sb(sB, f"bjunkR{i}", [128, 8], F32) for i in range(2)]
            Pr2 = ps(sB, "bPr2", [128, 512], F32)
            B_Pr2 = kb.buf()
            hnf2 = [hnf, sb(sB, "hnf_b", [128, 1024], F32)]
            hnT2 = [hnT, sb(sB, "hnT_b", [128, 8, 128], F32)]
            stX2 = [stX, sb(sB, "bstX_b", [128, 8], F32)]
            junk2 = [junk, sb(sB, "bjunk_b", [128, 512], F32)]
            PT2 = [PT, ps(sB, "bPT_b", [128, 8, 128], F32)]
            Pr_2 = [Pr, Pr2]
            B_hnf2, B_hnT2, B_rx2, B_PT2 = [B_hnf, kb.buf()], [B_hnT, kb.buf()], [B_rx, kb.buf()], [B_PT, kb.buf()]
            B_Px2 = [B_Px, kb.buf()]
            B_Pr_2 = [B_Pr, B_Pr2]
            ingroup = [None]

            def gb():
                ingroup[0] = []

            def ge():
                g_, ingroup[0] = ingroup[0], None
                return g_

            def stageX(i):
                j = i % 2
                lgi = lg2[i % 4]
                hnf, hnT, stX, junk, PT, Pr = hnf2[j], hnT2[j], stX2[j], junk2[j], PT2[j], Pr_2[j]
                B_hnf, B_hnT, B_rx, B_PT, B_Pr, B_Px = B_hnf2[j], B_hnT2[j], B_rx2[j], B_PT2[j], B_Pr_2[j], B_Px2[j]
                rows = slice(i * 128, (i + 1) * 128)
                half = fc = hs = c = None
                kb.dma("sp", f"bxt{j}", lambda q: q.dma_start(out=xt[j][:], in_=x_d[rows, :]), w=[B_xt[j]])
                for half in range(2):
                    hs = slice(half * 512, (half + 1) * 512)
                    gb()
                    for fc in range(8):
                        kb.op("pe", lambda e, half=half, fc=fc, hs=hs, c=c: e.matmul(Px[:, j, :], lhsT=hT[:, fc, rows], rhs=wo[:, fc, half * 512:(half + 1) * 512],
                                                       start=(fc == 0), stop=(fc == 7)), r=[B_hT[i], B_w], w=[B_Px])
                    grp_done(ge())
                    kb.op("dve", lambda e, half=half, fc=fc, hs=hs, c=c: e.tensor_tensor(out=x2[j][:, hs], in0=xt[j][:, hs], in1=Px[:, j, :], op=ALU.add),
                          r=[B_xt[j], B_Px], w=[B_x2[j]])
                kb.dma("sp", "ost", lambda q: q.dma_start(out=out_d[rows, :], in_=x2[j][:]), r=[B_x2[j]], w=[B_out[i]])
                for half in range(2):
                    hs = slice(half * 512, (half + 1) * 512)
                    kb.op("act", lambda e, half=half, fc=fc, hs=hs, c=c: e.activation(out=junk[:], in_=x2[j][:, hs], func=AF.Square, accum_out=stX[:, half:half + 1]),
                          r=[B_x2[j]], w=[B_rx])
                kb.op("act", lambda e, half=half, fc=fc, hs=hs, c=c: e.activation(out=stX[:, 2:3], in_=stX[:, 0:1], func=AF.Identity, bias=stX[:, 1:2]), r=[B_rx], w=[B_rx])
                kb.op("act", lambda e, half=half, fc=fc, hs=hs, c=c: e.activation(out=stX[:, 3:4], in_=stX[:, 2:3], func=AF.Ln, scale=1.0 / D, bias=EPS), r=[B_rx], w=[B_rx])
                kb.op("act", lambda e, half=half, fc=fc, hs=hs, c=c: e.activation(out=stX[:, 6:7], in_=stX[:, 3:4], func=AF.Exp, scale=-0.5), r=[B_rx], w=[B_rx])
                for half in range(2):
                    hs = slice(half * 512, (half + 1) * 512)
                    kb.op("dve", lambda e, half=half, fc=fc, hs=hs, c=c: e.scalar_tensor_tensor(out=hnf[:, hs], in0=x2[j][:, hs], scalar=stX[:, 6:7], in1=gff[:, hs],
                                                                  op0=ALU.mult, op1=ALU.mult), r=[B_x2[j], B_rx, B_w], w=[B_hnf])
                    kb.op("pool", lambda e, half=half, fc=fc, hs=hs, c=c: e.tensor_copy(out=hnb[j][:, hs], in_=hnf[:, hs]), r=[B_hnf], w=[B_hnb[j]])
                kb.dma("sp", "hnst", lambda q: q.dma_start(out=hn_d[rows, :], in_=hnb[j][:]), r=[B_hnb[j]], w=[B_hn[i]])
                for c in range(8):
                    kb.op("pe", lambda e, half=half, fc=fc, hs=hs, c=c: e.transpose(out=PT[:, c, :], in_=hnf[:, c * 128:(c + 1) * 128], identity=ident_f[:]),
                          r=[B_hnf, B_const], w=[B_PT])
                for half in range(2):
                    kb.op("act", lambda e, half=half, fc=fc, hs=hs, c=c: e.copy(out=hnT[:, half * 4:half * 4 + 4, :], in_=PT[:, half * 4:half * 4 + 4, :]), r=[B_PT], w=[B_hnT])
                gb()
                for c in range(8):
                    kb.op("pe", lambda e, half=half, fc=fc, hs=hs, c=c: e.matmul(Pr[:, 0:36], lhsT=hnT[:, c, :], rhs=wr[:, c, :], start=(c == 0), stop=(c == 7)),
                          r=[B_hnT, B_w], w=[B_Pr])
                grp_done(ge())
                kb.op("dve", lambda e, half=half, fc=fc, hs=hs, c=c: e.tensor_tensor(out=lgi[:], in0=Pr[:, 0:36], in1=brt[:], op=ALU.add), r=[B_Pr, B_w], w=[B_lg[i % 4]])

            def stageR(i):
                lgi = lg2[i % 4]
                q = i % 2
                sd_, st, oh, t48, es8, e28, mk1, mk2, junkR = sd2[q], st2[q], oh2[q], t482[q], es82[q], e282[q], mk12[q], mk22[q], junkR2[q]
                R_ = [B_r2[q]]
                RL = [B_r2[q], B_lg[i % 4]]
                kb.op("dve", lambda e: e.tensor_reduce(out=sd_[:, 1:2], in_=lgi[:, 0:4], axis=AX.X, op=ALU.max), r=RL, w=R_)
                kb.op("dve", lambda e: e.tensor_scalar(out=sd_[:, 2:3], in0=sd_[:, 1:2], scalar1=-1.0, scalar2=None, op0=ALU.mult), r=R_, w=R_)
                kb.op("act", lambda e: e.activation(out=junkR[:, 0:4], in_=lgi[:, 0:4], func=AF.Exp, bias=sd_[:, 2:3], accum_out=st[:, 4:5]), r=RL, w=R_)
                kb.op("dve", lambda e: e.reciprocal(out=sd_[:, 3:4], in_=st[:, 4:5]), r=R_, w=R_)
                kb.op("dve", lambda e: e.tensor_scalar(out=oh[:], in0=lgi[:, 0:4], scalar1=sd_[:, 1:2], scalar2=None, op0=ALU.is_equal), r=RL, w=R_)
                kb.op("dve", lambda e: e.tensor_tensor(out=t48[:], in0=lgi[:, 4:36].rearrange("p (g j) -> p g j", g=4),
                                                       in1=oh[:].unsqueeze(2).to_broadcast([128, 4, 8]), op=ALU.mult), r=RL, w=R_)
                kb.op("dve", lambda e: e.tensor_reduce(out=es8[:], in_=t48[:].rearrange("p g j -> p j g"), axis=AX.X, op=ALU.add), r=R_, w=R_)
                kb.op("dve", lambda e: e.tensor_reduce(out=sd_[:, 4:5], in_=es8[:], axis=AX.X, op=ALU.max), r=R_, w=R_)
                kb.op("dve", lambda e: e.tensor_scalar(out=mk1[:], in0=es8[:], scalar1=sd_[:, 4:5], scalar2=None, op0=ALU.is_equal), r=R_, w=R_)
                kb.op("dve", lambda e: e.scalar_tensor_tensor(out=e28[:], in0=mk1[:], scalar=-1e30, in1=es8[:], op0=ALU.mult, op1=ALU.add), r=R_, w=R_)
                kb.op("dve", lambda e: e.tensor_reduce(out=sd_[:, 5:6], in_=e28[:], axis=AX.X, op=ALU.max), r=R_, w=R_)
                kb.op("dve", lambda e: e.tensor_scalar(out=mk2[:], in0=e28[:], scalar1=sd_[:, 5:6], scalar2=None, op0=ALU.is_equal), r=R_, w=R_)
                kb.op("dve", lambda e: e.tensor_tensor(out=sd_[:, 6:7], in0=sd_[:, 5:6], in1=sd_[:, 4:5], op=ALU.subtract), r=R_, w=R_)
                kb.op("act", lambda e: e.activation(out=st[:, 5:6], in_=sd_[:, 6:7], func=AF.Exp), r=R_, w=R_)
                kb.op("dve", lambda e: e.tensor_scalar(out=sd_[:, 7:8], in0=st[:, 5:6], scalar1=1.0, scalar2=None, op0=ALU.add), r=R_, w=R_)
                kb.op("dve", lambda e: e.reciprocal(out=sd_[:, 8:9], in_=sd_[:, 7:8]), r=R_, w=R_)
                kb.op("dve", lambda e: e.tensor_tensor(out=gta[:, i, 0:1], in0=sd_[:, 3:4], in1=sd_[:, 8:9], op=ALU.mult), r=R_, w=R_ + [B_rt])
                kb.op("dve", lambda e: e.tensor_tensor(out=gta[:, i, 1:2], in0=gta[:, i, 0:1], in1=st[:, 5:6], op=ALU.mult), r=R_, w=R_ + [B_rt])
                kb.op("dve", lambda e: e.tensor_tensor(out=A1a[:, i, :].rearrange("p (g j) -> p g j", g=4),
                                                       in0=oh[:].unsqueeze(2).to_broadcast([128, 4, 8]),
                                                       in1=mk1[:].unsqueeze(1).to_broadcast([128, 4, 8]), op=ALU.mult), r=R_, w=R_ + [B_rt])
                kb.op("dve", lambda e: e.tensor_tensor(out=A2a[:, i, :].rearrange("p (g j) -> p g j", g=4),
                                                       in0=oh[:].unsqueeze(2).to_broadcast([128, 4, 8]),
                                                       in1=mk2[:].unsqueeze(1).to_broadcast([128, 4, 8]), op=ALU.mult), r=R_, w=R_ + [B_rt])
            cur_rec = [None]

            def grp_done(g_):
                cur_rec[0].append(("grp", g_))

            def record(fn_stage, i):
                rec = []
                cur_rec[0] = rec

                def rop(e, fn, r=(), w=(), same=None):
                    (ingroup[0] if ingroup[0] is not None else rec).append((e, fn, list(r), list(w), same))
                kb.op = rop
                kb.dma = lambda q, sname, fn, r=(), w=(): rec.append(("dma", q, sname, fn, list(r), list(w)))
                try:
                    fn_stage(i)
                finally:
                    del kb.op
                    del kb.dma
                return rec

            def replay(t):
                if t[0] == "grp":
                    for t2 in t[1]:
                        kb.op(*t2)
                elif t[0] == "dma":
                    kb.dma(*t[1:])
                else:
                    kb.op(*t)

            def emit_x_pair(i2a):
                xa = record(stageX, i2a) if i2a < NT else []
                xb_ = record(stageX, i2a + 1) if i2a + 1 < NT else []
                for k_ in range(max(len(xa), len(xb_))):
                    if k_ < len(xa):
                        replay(xa[k_])
                    if k_ < len(xb_):
                        replay(xb_[k_])

            emit_x_pair(0)
            for i in range(0, NT, 2):
                emit_x_pair(i + 2)
                ra = record(stageR, i)
                rb = record(stageR, i + 1) if i + 1 < NT else []
                for k_ in range(max(len(ra), len(rb))):
                    if k_ < len(ra):
                        replay(ra[k_])
                    if k_ < len(rb):
                        replay(rb[k_])
            Ata = sb(sB, "Ata", [128, NT, 32], F32)
            B_Ata = kb.buf()
            for i0_ in range(0, NT, 16):
                n_ = min(16, NT - i0_)
                kb.op("dve", lambda e: e.tensor_tensor(out=Ata[:, i0_:i0_ + n_, :], in0=A1a[:, i0_:i0_ + n_, :], in1=A2a[:, i0_:i0_ + n_, :], op=ALU.add),
                      r=[B_rt], w=[B_Ata])
            Apre = sb(sB, "Apre", [128, NT, 32], F32)
            kb.op("pool", lambda e: e.memset(Apre[:, 0, :], 0.0), w=[B_Ata])
            for i in range(1, NT):
                kb.op("dve", lambda e: e.tensor_tensor(out=Apre[:, i, :], in0=Apre[:, i - 1, :], in1=Ata[:, i - 1, :], op=ALU.add),
                      r=[B_Ata], w=[B_Ata])
            for i in range(NT):
                bank, col = i // 16, (i % 16) * 32
                kb.op("pe", lambda e: e.matmul(Px[:, bank, col:col + 32], lhsT=ustr[:], rhs=Ata[:, i, :], start=True, stop=False),
                      r=[B_Ata, B_w], w=[B_Px2[bank]])
                kb.op("pe", lambda e: e.matmul(Px[:, bank, col:col + 32], lhsT=ones_f[:], rhs=Apre[:, i, :], start=False, stop=True),
                      r=[B_Ata, B_const], w=[B_Px2[bank]])
            for i in range(NT):
                kb.op("pe", lambda e: e.matmul(Pr2[:, 0:32], lhsT=ones_f[:], rhs=Ata[:, i, :], start=(i == 0), stop=(i == NT - 1)),
                      r=[B_Ata, B_const], w=[B_Pr2])
            for i0_ in range(0, NT, 16):
                n_ = min(16, NT - i0_)
                kb.op("dve", lambda e: e.tensor_copy(out=rka[:, i0_:i0_ + n_, :], in_=Px[:, i0_ // 16, 0:n_ * 32].rearrange("p (i e) -> p i e", e=32)),
                      r=[B_Px2[i0_ // 16]], w=[B_rt])
            kb.op("dve", lambda e: e.tensor_copy(out=base[:], in_=Pr2[:, 0:32]), r=[B_Pr2], w=[B_rt])
            kb.barrier()
        with ExitStack() as sC:
            thr = sb(sC, "thr", [128, NBLK, 32], F32)
            cmp_ = sb(sC, "cmp", [128, 16, 32], F32)
            nbk = sb(sC, "nbk", [128, 32], F32)
            tmp32 = sb(sC, "tmp32", [128, 32], F32)
            pend = sb(sC, "pend", [128, 32], F32)
            pst = sb(sC, "pst", [128, 32], F32)
            zer = sb(sC, "zer", [128, 32], F32)
            bef = sb(sC, "bef", [128, NBLK], F32)
            dst = sb(sC, "dst", [128, NT, 32], F32)
            prod = sb(sC, "prod", [128, 16, 32], F32)
            d1f = sb(sC, "d1f", [128, NT], F32)
            d2f = sb(sC, "d2f", [128, NT], F32)
            Q = [B_rt]
            kb.dma("sp", "const", lambda q: q.dma_start(out=thr[:].rearrange("p b e -> p (b e)"), in_=cd["blkthr"].partition_broadcast(128)), w=Q)
            kb.op("pool", lambda e: e.memset(nbk[:], 0.0), w=Q)
            kb.op("pool", lambda e: e.memset(zer[:], 0.0), w=Q)
            for b0 in range(0, NBLK, 16):
                nb_ = min(16, NBLK - b0)
                kb.op("dve", lambda e: e.tensor_tensor(out=cmp_[:, 0:nb_, :], in0=base[:].unsqueeze(1).to_broadcast([128, nb_, 32]),
                                                       in1=thr[:, b0:b0 + nb_, :], op=ALU.is_gt), r=Q, w=Q)
                kb.op("dve", lambda e: e.tensor_reduce(out=tmp32[:], in_=cmp_[:, 0:nb_, :].rearrange("p b e -> p e b"), axis=AX.X, op=ALU.add), r=Q, w=Q)
                kb.op("dve", lambda e: e.tensor_tensor(out=nbk[:], in0=nbk[:], in1=tmp32[:], op=ALU.add), r=Q, w=Q)
            kb.op("dve", lambda e: e.tensor_scalar(out=nbk[:], in0=nbk[:], scalar1=float(BS), scalar2=None, op0=ALU.mult), r=Q, w=Q)
            kb.op("dve", lambda e: e.tensor_tensor_scan(out=pend[:], data0=nbk[:], data1=zer[:], initial=0.0, op0=ALU.add, op1=ALU.add), r=Q, w=Q)
            kb.op("dve", lambda e: e.tensor_tensor(out=pst[:], in0=pend[:], in1=nbk[:], op=ALU.subtract), r=Q, w=Q)
            for b0 in range(0, NBLK, 16):
                nb_ = min(16, NBLK - b0)
                kb.op("dve", lambda e: e.tensor_tensor(out=cmp_[:, 0:nb_, :], in0=pend[:].unsqueeze(1).to_broadcast([128, nb_, 32]),
                                                       in1=thr[:, b0:b0 + nb_, :], op=ALU.is_le), r=Q, w=Q)
                kb.op("dve", lambda e: e.tensor_reduce(out=bef[:, b0:b0 + nb_], in_=cmp_[:, 0:nb_, :], axis=AX.X, op=ALU.add), r=Q, w=Q)
            kb.op("dve", lambda e: e.tensor_scalar(out=bef[:], in0=bef[:], scalar1=float(NE - 1), scalar2=None, op0=ALU.min), r=Q, w=Q)
            kb.op("dve", lambda e: e.tensor_copy(out=bexp[:], in_=bef[:]), r=Q, w=Q)
            iot = sb(sC, "iot", [128, 8], F32)
            idf = sb(sC, "idf", [128, NBLK], F32)
            kb.dma("sp", "const", lambda q: q.dma_start(out=iot[:], in_=cd["iota_pc"]), w=Q)
            kb.op("dve", lambda e: e.scalar_tensor_tensor(out=idf[:], in0=bef[:], scalar=128.0, in1=iot[:, 0:1].to_broadcast([128, NBLK]),
                                                          op0=ALU.mult, op1=ALU.add), r=Q, w=Q)
            kb.op("dve", lambda e: e.tensor_copy(out=idxw[:], in_=idf[:]), r=Q, w=Q)
            for i0_ in range(0, NT, 16):
                n_ = min(16, NT - i0_)
                kb.op("dve", lambda e: e.tensor_tensor(out=dst[:, i0_:i0_ + n_, :], in0=rka[:, i0_:i0_ + n_, :],
                                                       in1=pst[:].unsqueeze(1).to_broadcast([128, n_, 32]), op=ALU.add), r=Q, w=Q)
                for Aa, df in ((A1a, d1f), (A2a, d2f)):
                    kb.op("dve", lambda e: e.tensor_tensor(out=prod[:, 0:n_, :], in0=dst[:, i0_:i0_ + n_, :], in1=Aa[:, i0_:i0_ + n_, :], op=ALU.mult), r=Q, w=Q)
                    kb.op("dve", lambda e: e.tensor_reduce(out=df[:, i0_:i0_ + n_], in_=prod[:, 0:n_, :], axis=AX.X, op=ALU.add), r=Q, w=Q)
            kb.op("dve", lambda e: e.tensor_copy(out=d1i[:], in_=d1f[:]), r=Q, w=Q)
            kb.op("dve", lambda e: e.tensor_copy(out=d2i[:], in_=d2f[:]), r=Q, w=Q)
            kb.barrier()
        if "d1i" in dbg_d:
            kb.dma("sp", "dbg", lambda q: q.dma_start(out=dbg_d["d1i"], in_=d1i[:]), r=[B_rt])
            kb.dma("sp", "dbg", lambda q: q.dma_start(out=dbg_d["d2i"], in_=d2i[:]), r=[B_rt])
            kb.dma("sp", "dbg", lambda q: q.dma_start(out=dbg_d["bexp"], in_=bexp[:]), r=[B_rt])
            kb.dma("sp", "dbg", lambda q: q.dma_start(out=dbg_d["gta"], in_=gta[:]), r=[B_rt])
        if stage < 5:
            return
        with ExitStack() as sD:
            hb_ = [sb(sD, f"dhb{i}", [128, 1024], BF16) for i in range(4)]
            B_hb = kb.bufs(4)
            for i in range(NT):
                j = i % 4
                kb.dma("sp", f"dhb{j}", lambda q: q.dma_start(out=hb_[j][:], in_=hn_d[i * 128:(i + 1) * 128, :]), r=[B_hn[i]], w=[B_hb[j]])
                for di in (d1i, d2i):
                    kb.dma("pool", "disp", lambda q: q.indirect_dma_start(out=xs_d[:, :], out_offset=bass.IndirectOffsetOnAxis(ap=di[:, i:i + 1], axis=0),
                                                                           in_=hb_[j][:], in_offset=None), r=[B_hb[j], B_rt], w=[B_xs])
            kb.barrier()
        B_ys = kb.buf()
        with ExitStack() as sE:
            NWB = 3
            W1 = [sb(sE, f"eW1{i}", [128, 8, 512], BF16) for i in range(NWB)]
            W3 = [sb(sE, f"eW3{i}", [128, 8, 512], BF16) for i in range(NWB)]
            W2 = [sb(sE, f"eW2{i}", [128, 4, 1024], BF16) for i in range(NWB)]
            B_W = kb.bufs(NWB)
            xb = [sb(sE, f"exb{i}", [128, 1024], BF16) for i in range(2)]
            B_xb = kb.bufs(2)
            xT = [sb(sE, f"exT{i}", [128, 8, 128], BF16) for i in range(2)]
            sl = [sb(sE, f"esl{i}", [128, 512], F32) for i in range(2)]
            hid = [sb(sE, f"ehid{i}", [128, 512], BF16) for i in range(2)]
            hidT = [sb(sE, f"ehidT{i}", [128, 4, 128], BF16) for i in range(2)]
            ysb = [sb(sE, f"eys{i}", [128, 1024], F32) for i in range(2)]
            B_ysb = kb.bufs(2)
            B_xT, B_sl, B_hid, B_hidT = kb.bufs(2), kb.bufs(2), kb.bufs(2), kb.bufs(2)
            PTb = [ps(sE, f"ePT{i}", [128, 8, 128], BF16) for i in range(2)]
            Ph1 = [ps(sE, f"ePh1{i}", [128, 512], F32) for i in range(2)]
            Ph3 = [ps(sE, f"ePh3{i}", [128, 512], F32) for i in range(2)]
            Py = ps(sE, "ePy", [128, 2, 512], F32)
            B_PTb, B_Ph1, B_Ph3 = kb.bufs(2), kb.bufs(2), kb.bufs(2)
            B_Py = kb.buf()
            w1r, w3r, w2r = w1b_d, w3b_d, w2b_d
            B_xT2 = [kb.bufs(2), kb.bufs(2)]
            B_ysb2 = [kb.bufs(2), kb.bufs(2)]
            NB128 = NBLK * NSUB

            def load_w(sbi):
                k = sbi % NWB
                for Wt_, wr_ in ((W1, w1r), (W3, w3r), (W2, w2r)):
                    kb.dma("pool", f"ew{k}", lambda q: q.indirect_dma_start(out=Wt_[k][:].rearrange("p c f -> p (c f)"), out_offset=None, in_=wr_[:, :],
                           in_offset=bass.IndirectOffsetOnAxis(ap=idxw[:, sbi:sbi + 1], axis=0)), r=[B_rt, B_wconv], w=[B_W[k]])

            def load_x(b):
                j = b % 2
                kb.dma("sp", f"exb{j}", lambda q: q.dma_start(out=xb[j][:], in_=xs_d[b * 128:(b + 1) * 128, :]), r=[B_xs], w=[B_xb[j]])

            def stageA(b):
                j = b % 2
                k = (b // NSUB) % NWB
                for c in range(8):
                    kb.op("pe", lambda e: e.transpose(out=PTb[j][:, c, :], in_=xb[j][:].rearrange("s (p c) -> s c p", c=8)[:, c, :], identity=ident_bf[:]),
                          r=[B_xb[j], B_const], w=[B_PTb[j]])
                kb.op("dve", lambda e: e.tensor_copy(out=xT[j][:, 0:4, :], in_=PTb[j][:, 0:4, :]), r=[B_PTb[j]], w=[B_xT2[j][0]])
                kb.op("dve", lambda e: e.tensor_copy(out=xT[j][:, 4:8, :], in_=PTb[j][:, 4:8, :]), r=[B_PTb[j]], w=[B_xT2[j][1]])
                for c in range(8):
                    kb.op("pe", lambda e: e.matmul(Ph1[j][:], lhsT=xT[j][:, c, :], rhs=W1[k][:, c, :], start=(c == 0), stop=(c == 7)),
                          r=[B_xT2[j][c // 4], B_W[k]], w=[B_Ph1[j]])
                for c in range(8):
                    kb.op("pe", lambda e: e.matmul(Ph3[j][:], lhsT=xT[j][:, c, :], rhs=W3[k][:, c, :], start=(c == 0), stop=(c == 7)),
                          r=[B_xT2[j][c // 4], B_W[k]], w=[B_Ph3[j]])
                kb.op("act", lambda e: e.activation(out=sl[j][:], in_=Ph1[j][:], func=AF.Silu), r=[B_Ph1[j]], w=[B_sl[j]])
                kb.op("dve", lambda e: e.tensor_tensor(out=hid[j][:], in0=sl[j][:], in1=Ph3[j][:], op=ALU.mult), r=[B_sl[j], B_Ph3[j]], w=[B_hid[j]])

            def stageB(b):
                j = b % 2
                k = (b // NSUB) % NWB
                for c in range(4):
                    kb.op("pe", lambda e: e.transpose(out=PTb[j][:, c, :], in_=hid[j][:].rearrange("s (p c) -> s c p", c=4)[:, c, :], identity=ident_bf[:]),
                          r=[B_hid[j], B_const], w=[B_PTb[j]])
                kb.op("act", lambda e: e.copy(out=hidT[j][:], in_=PTb[j][:, 0:4, :]), r=[B_PTb[j]], w=[B_hidT[j]])
                for half in range(2):
                    for c in range(4):
                        kb.op("pe", lambda e: e.matmul(Py[:, half, :], lhsT=hidT[j][:, c, :], rhs=W2[k][:, c, half * 512:(half + 1) * 512],
                                                       start=(c == 0), stop=(c == 3)), r=[B_hidT[j], B_W[k]], w=[B_Py])
                kb.op("act", lambda e: e.copy(out=ysb[j][:, 0:512], in_=Py[:, 0, :]), r=[B_Py], w=[B_ysb2[j][0]])
                kb.op("dve", lambda e: e.tensor_copy(out=ysb[j][:, 512:1024], in_=Py[:, 1, :]), r=[B_Py], w=[B_ysb2[j][1]])
                kb.dma("sp", "yst", lambda q: q.dma_start(out=ys_d[b * 128:(b + 1) * 128, :], in_=ysb[j][:]), r=B_ysb2[j], w=[B_ys])

            for s0 in range(min(NWB, NBLK)):
                load_w(s0)
            load_x(0)
            for b in range(NB128 + 1):
                if b + 1 < NB128:
                    load_x(b + 1)
                if b < NB128:
                    stageA(b)
                if b >= 1:
                    stageB(b - 1)
                    if (b - 1) % NSUB == NSUB - 1:
                        nxt = (b - 1) // NSUB + NWB
                        if nxt < NBLK:
                            load_w(nxt)
            kb.barrier()
        with ExitStack() as sF:
            NCB = 4
            y1 = [sb(sF, f"cy1{i}", [128, 1024], F32) for i in range(NCB)]
            y2 = [sb(sF, f"cy2{i}", [128, 1024], F32) for i in range(NCB)]
            xr = [sb(sF, f"cxr{i}", [128, 1024], F32) for i in range(NCB)]
            B_y1, B_y2, B_xr = kb.bufs(NCB), kb.bufs(NCB), kb.bufs(NCB)

            def cload(i):
                j = i % NCB
                rows = slice(i * 128, (i + 1) * 128)
                kb.dma("pool", f"cg1{j}", lambda q: q.indirect_dma_start(out=y1[j][:], out_offset=None, in_=ys_d[:, :],
                       in_offset=bass.IndirectOffsetOnAxis(ap=d1i[:, i:i + 1], axis=0)), r=[B_ys, B_rt], w=[B_y1[j]])
                kb.dma("pool", f"cg2{j}", lambda q: q.indirect_dma_start(out=y2[j][:], out_offset=None, in_=ys_d[:, :],
                       in_offset=bass.IndirectOffsetOnAxis(ap=d2i[:, i:i + 1], axis=0)), r=[B_ys, B_rt], w=[B_y2[j]])
                kb.dma("act", f"cxr{j}", lambda q: q.dma_start(out=xr[j][:], in_=out_d[rows, :]), r=[B_out[i]], w=[B_xr[j]])

            for i in range(min(NCB - 1, NT)):
                cload(i)
            for i in range(NT):
                j = i % NCB
                rows = slice(i * 128, (i + 1) * 128)
                if i + NCB - 1 < NT:
                    cload(i + NCB - 1)
                for half in range(2):
                    hs = slice(half * 512, (half + 1) * 512)
                    kb.op("dve", lambda e: e.scalar_tensor_tensor(out=xr[j][:, hs], in0=y1[j][:, hs], scalar=gta[:, i, 0:1], in1=xr[j][:, hs],
                                                                  op0=ALU.mult, op1=ALU.add), r=[B_y1[j], B_rt], w=[B_xr[j]])
                    kb.op("dve", lambda e: e.scalar_tensor_tensor(out=xr[j][:, hs], in0=y2[j][:, hs], scalar=gta[:, i, 1:2], in1=xr[j][:, hs],
                                                                  op0=ALU.mult, op1=ALU.add), r=[B_y2[j], B_rt], w=[B_xr[j]])
                kb.dma("sp", "ost2", lambda q: q.dma_start(out=out_d[rows, :], in_=xr[j][:]), r=[B_xr[j]], w=[B_out[i]])
            kb.barrier()


def _shared_maps(inp, consts):
    f = np.float32
    g = lambda k: np.ascontiguousarray(np.asarray(inp[k], dtype=f)[0])
    m = {}
    m["w_in"] = g("w_in")
    m["w_branch_da"] = g("w_branch_da")
    m["w_branch_ml"] = g("w_branch_ml")
    m["w_gate"] = g("w_gate")
    m["w_out"] = g("w_out")
    m["w1"] = g("w1")
    m["w3"] = g("w3")
    m["w2"] = g("w2")
    m["w_rt"] = np.ascontiguousarray(np.concatenate([g("w_group"), g("w_router")], axis=1))
    m["attn_norm_g"] = g("attn_norm_g")[None, :]
    m["ffn_norm_g"] = g("ffn_norm_g")[None, :]
    m["da_out_norm_g"] = g("da_out_norm_g")[None, :]
    m["ml_out_norm_g"] = g("ml_out_norm_g")[None, :]
    m["b_rt"] = np.concatenate([g("b_group"), g("b_router")])[None, :]
    m["b_if"] = np.concatenate([g("ml_i_bias"), g("ml_f_bias")])[None, :]
    m["lamv"] = np.concatenate([g("da_lambda_q1"), g("da_lambda_k1"), g("da_lambda_q2"), g("da_lambda_k2")])[None, :]
    m["gqk_col"] = np.ascontiguousarray(np.stack([np.tile(g("da_q_norm_g"), 2), np.tile(g("da_k_norm_g"), 2)], axis=1))
    cw = g("ml_conv_w")
    m["cw_col"] = np.ascontiguousarray(cw.reshape(4, 8, 128).transpose(2, 1, 0).reshape(128, 32))
    m["cb_col"] = np.ascontiguousarray(g("ml_conv_b").reshape(8, 128).T)
    m["bg_col"] = np.ascontiguousarray(g("b_gate").reshape(16, 128).T)
    for k, v in consts.items():
        m["c_" + k] = v
    return m


_CACHE = {}


def kernel(**inputs):
    x = np.asarray(inputs["x"], dtype=np.float32)
    B, S, _ = x.shape
    key = (S,)
    if key not in _CACHE:
        _CACHE[key] = build_nc(S)
    nc, consts = _CACHE[key]
    shared = _shared_maps(inputs, consts)
    in_maps = []
    for b in range(B):
        m = dict(shared)
        m["x"] = np.ascontiguousarray(x[b])
        in_maps.append(m)
    res = run_bass_kernel_spmd(nc, in_maps, core_ids=list(range(B)))
    return np.stack([np.asarray(r["out"], dtype=np.float32) for r in res.results], axis=0)
```

```python
import math
from contextlib import ExitStack
import numpy as np
import ml_dtypes
import concourse.bass as bass
import concourse.mybir as mybir
from concourse.bass_utils import run_bass_kernel_spmd

F32 = mybir.dt.float32
BF16 = mybir.dt.bfloat16
I32 = mybir.dt.int32
AF = mybir.ActivationFunctionType
ALU = mybir.AluOpType
AX = mybir.AxisListType

D = 1024
NH = 4
IN_W = 3592
OFF_DA_Q, OFF_DA_K, OFF_DA_V = 0, 512, 1024
OFF_ML_QK, OFF_ML_V, OFF_ML_O, OFF_ML_I = 1536, 2560, 3072, 3584
NE = 32
EPS = 1e-6
LAM_INIT = 0.8 - 0.6 * math.exp(-0.3 * 0)
SLOPES = [2.0 ** (-8.0 * (i + 1) / NH) for i in range(NH)]
SKIP_T = 60.0
NOFF = 36
OFF0 = 31
ML_LOOKAHEAD = 3
BS = 256


class Buf:
    __slots__ = ("w", "r", "name")

    def __init__(self, name=""):
        self.w = None
        self.r = {}
        self.name = name


class KB:
    def __init__(self, nc, es):
        self.nc = nc
        self.es = es
        self.eng = {"pe": nc.tensor, "act": nc.scalar, "dve": nc.vector, "pool": nc.gpsimd, "sp": nc.sync}
        self.sem = {k: es.enter_context(nc.semaphore("sem_" + k)) for k in self.eng}
        self.cnt = {k: 0 for k in self.eng}
        self.seen = {k: {} for k in self.eng}
        self.dsem = {}
        self.dcnt = {}
        self.nbuf = 0

    def buf(self, name=""):
        self.nbuf += 1
        return Buf(name)

    def bufs(self, n, name=""):
        return [self.buf(name + str(i)) for i in range(n)]

    def dma_sem(self, name):
        if name not in self.dsem:
            self.dsem[name] = self.es.enter_context(self.nc.semaphore("dsem_" + name))
            self.dcnt[name] = 0
        return name

    def _semh(self, key):
        return self.sem[key] if key in self.sem else self.dsem[key]

    def _wait(self, e, reads, writes, same=True, skip=None):
        deps = {}
        for b in reads:
            if b.w is not None:
                k, v = b.w
                deps[k] = max(deps.get(k, 0), v)
        for b in writes:
            if b.w is not None:
                k, v = b.w
                deps[k] = max(deps.get(k, 0), v)
            for k, v in b.r.items():
                deps[k] = max(deps.get(k, 0), v)
        for k, v in deps.items():
            if k == e and not same:
                continue
            if k == skip:
                continue
            if k in self.dcnt:
                v = self.dcnt[k]
            if self.seen[e].get(k, 0) >= v:
                continue
            self.eng[e].wait_ge(self._semh(k), v)
            self.seen[e][k] = v

    def op(self, e, fn, r=(), w=(), same=None):
        if same is None:
            same = (e != "pe")
        self._wait(e, r, w, same)
        inst = fn(self.eng[e])
        self.cnt[e] += 1
        inst.then_inc(self.sem[e], 1)
        ev = (e, self.cnt[e])
        for b in w:
            b.w = ev
            b.r = {}
        for b in r:
            if b not in w:
                b.r[e] = self.cnt[e]
        return inst

    def dma(self, q, sname, fn, r=(), w=()):
        self.dma_sem(sname)
        self._wait(q, r, w, True, skip=sname)
        inst = fn(self.eng[q])
        self.dcnt[sname] += 16
        inst.then_inc(self.dsem[sname], 16)
        ev = (sname, self.dcnt[sname])
        for b in w:
            b.w = ev
            b.r = {}
        for b in r:
            if b not in w:
                b.r[sname] = self.dcnt[sname]
        return inst

    def barrier(self):
        evs = [(k, self.cnt[k]) for k in self.sem if self.cnt[k] > 0] + [(k, v) for k, v in self.dcnt.items() if v > 0]
        for e in self.eng:
            for k, v in evs:
                if k == e:
                    continue
                if self.seen[e].get(k, 0) >= v:
                    continue
                self.eng[e].wait_ge(self._semh(k), v)
                self.seen[e][k] = v

    def wait_all(self, e, bufs):
        self._wait(e, bufs, bufs, True)


def _mk_consts(S):
    c = {}
    bf = ml_dtypes.bfloat16
    c["ident_bf"] = np.eye(128, dtype=np.float32).astype(bf)
    c["ident_f"] = np.eye(128, dtype=np.float32)
    blk = np.zeros((128, 128), np.float32)
    blk[:64, :64] = 1.0 / 64
    blk[64:, 64:] = 1.0 / 64
    c["blk64"] = blk.astype(bf)
    k = np.arange(128)[:, None]
    q = np.arange(128)[None, :]
    c["cmask"] = (k <= q).astype(np.float32).astype(bf)
    c["negm"] = np.where(k <= q, 0.0, -30000.0).astype(np.float32)
    c["ones_f"] = np.ones((128, 128), np.float32)
    c["ustrict"] = (k < q).astype(np.float32)
    tab = np.zeros((128, NH * NOFF), np.float32)
    for h in range(NH):
        for j in range(NOFF):
            tab[:, h * NOFF + j] = SLOPES[h] * (np.arange(128) + 128.0 * (j - OFF0))
    c["atab"] = tab
    c["iota_pc"] = (np.arange(8)[None, :] * 128.0 + np.arange(128)[:, None]).astype(np.float32)
    nb = (2 * S) // BS + NE
    c["blkthr"] = np.tile((np.arange(nb, dtype=np.float32) * float(BS))[None, :, None], (1, 1, NE)).reshape(1, nb * NE)
    return c


CONST_DT = {"ident_bf": BF16, "ident_f": F32, "blk64": BF16, "cmask": BF16, "negm": F32, "ones_f": F32,
            "ustrict": F32, "atab": F32, "blkthr": F32, "iota_pc": F32}


def build_nc(S=4096, stage=99, dbg=None):
    NT = S // 128
    NB5 = S // 512
    CAP = 2 * S + NE * BS
    nc = bass.Bass("TRN2", target_bir_lowering=False)
    consts = _mk_consts(S)

    def din(name, shape, dt=F32):
        return nc.dram_tensor(name, list(shape), dt, kind="ExternalInput").ap()

    x_d = din("x", [S, D])
    w_in = din("w_in", [D, IN_W])
    w_bda = din("w_branch_da", [512, D])
    w_bml = din("w_branch_ml", [512, D])
    w_gate = din("w_gate", [D, 2 * D])
    w_out = din("w_out", [D, D])
    w1 = din("w1", [NE, D, 512])
    w3 = din("w3", [NE, D, 512])
    w2 = din("w2", [NE, 512, D])
    wr_d = din("w_rt", [D, 36])
    g_attn = din("attn_norm_g", [1, D])
    g_ffn = din("ffn_norm_g", [1, D])
    g_dao = din("da_out_norm_g", [1, 128])
    g_mlo = din("ml_out_norm_g", [1, 512])
    b_rt = din("b_rt", [1, 36])
    b_if = din("b_if", [1, 8])
    lamv = din("lamv", [1, 256])
    gqk_col = din("gqk_col", [128, 2])
    cw_col = din("cw_col", [128, 8 * 4])
    cb_col = din("cb_col", [128, 8])
    bg_col = din("bg_col", [128, 16])
    cd = {k: din("c_" + k, list(v.shape), CONST_DT[k]) for k, v in consts.items()}
    out_d = nc.dram_tensor("out", [S, D], F32, kind="ExternalOutput").ap()
    dbg_d = {}
    if dbg:
        for k, (shape, dt) in dbg.items():
            dbg_d[k] = nc.dram_tensor("dbg_" + k, list(shape), dt, kind="ExternalOutput").ap()
    xs_d = nc.dram_tensor("xs_scr", [CAP, D], BF16).ap()
    ys_d = nc.dram_tensor("ys_scr", [CAP, D], F32).ap()
    hn_d = nc.dram_tensor("hn_scr", [S, D], BF16).ap()
    w1b_d = nc.dram_tensor("w1b_scr", [NE * 128, 4096], BF16).ap()
    w3b_d = nc.dram_tensor("w3b_scr", [NE * 128, 4096], BF16).ap()
    w2b_d = nc.dram_tensor("w2b_scr", [NE * 128, 4096], BF16).ap()

    es = ExitStack()
    with es:
        kb = KB(nc, es)

        def sb(stack, name, shape, dt):
            return stack.enter_context(nc.sbuf_tensor(name, list(shape), dt))

        def ps(stack, name, shape, dt=F32):
            return stack.enter_context(nc.psum_tensor(name, list(shape), dt))

        ident_bf = sb(es, "ident_bf", [128, 128], BF16)
        ident_f = sb(es, "ident_f", [128, 128], F32)
        ones_f = sb(es, "ones_f", [128, 128], F32)
        B_const = kb.buf("const")
        for t, k in ((ident_bf, "ident_bf"), (ident_f, "ident_f"), (ones_f, "ones_f")):
            kb.dma("sp", "const", lambda q, t=t, k=k: q.dma_start(out=t[:], in_=cd[k]), w=[B_const])
        zero_bf = sb(es, "zero_bf", [128, 2048], BF16)
        B_zero = kb.buf("zero")
        for zi in range(4):
            kb.op("pool", lambda e: e.memset(zero_bf[:, zi * 512:(zi + 1) * 512], 0.0), w=[B_zero])

        hT = sb(es, "hT", [128, 8, S], BF16)
        B_hT = kb.bufs(NT, "hT")
        es_mix = ExitStack()
        y_daT = sb(es_mix, "y_daT", [128, 4, S], BF16)
        B_ydaT = kb.bufs(NT * NH, "ydaT")
        B_ymlT = kb.bufs(NT * NH, "ymlT")

        B_xs = kb.buf()
        zf_rows = list(range(0, CAP, 256)) if stage >= 4 else []
        zf_per = (len(zf_rows) + NT - 1) // NT
        with ExitStack() as s1:
            gat = sb(s1, "gat", [128, D], F32)
            B_gat = kb.buf()
            kb.dma("sp", "const", lambda q: q.dma_start(out=gat[:], in_=g_attn.partition_broadcast(128)), w=[B_gat])
            xt = [sb(s1, f"xt{i}", [128, D], F32) for i in range(2)]
            xn = [sb(s1, f"xn{i}", [128, D], BF16) for i in range(2)]
            junk = sb(s1, "junk1", [128, D], F32)
            ssq = sb(s1, "ssq", [128, NT], F32)
            rst = sb(s1, "rst", [128, NT], F32)
            rsd = sb(s1, "rsd", [128, NT], F32)
            ssq2 = sb(s1, "ssq2", [128, 2 * NT], F32)
            pT = [ps(s1, f"pT{i}", [128, 8, 128], BF16) for i in range(2)]
            B_xt, B_xn, B_pT = kb.bufs(2), kb.bufs(2), kb.bufs(2)
            B_junk, B_ss = kb.buf(), kb.bufs(NT)
            for i in range(NT):
                j = i % 2
                kb.dma("sp", f"xt{j}", lambda q: q.dma_start(out=xt[j][:], in_=x_d[i * 128:(i + 1) * 128, :]), w=[B_xt[j]])
                for r0 in zf_rows[i * zf_per:(i + 1) * zf_per]:
                    kb.dma("sp", "xsz", lambda q: q.dma_start(out=xs_d[r0:r0 + 256, :].rearrange("(p a) d -> p (a d)", a=2), in_=zero_bf[:, 0:2048]),
                           r=[B_zero], w=[B_xs])
                for hf in range(2):
                    kb.op("act", lambda e: e.activation(out=junk[:, hf * 512:(hf + 1) * 512], in_=xt[j][:, hf * 512:(hf + 1) * 512],
                                                        func=AF.Square, accum_out=ssq2[:, 2 * i + hf:2 * i + hf + 1]),
                          r=[B_xt[j]], w=[B_junk, B_ss[i]])
                kb.op("act", lambda e: e.activation(out=rst[:, i:i + 1], in_=ssq2[:, 2 * i:2 * i + 1], func=AF.Identity,
                                                    bias=ssq2[:, 2 * i + 1:2 * i + 2]), r=[B_ss[i]], w=[B_ss[i]])
                kb.op("act", lambda e: e.activation(out=rst[:, i:i + 1], in_=rst[:, i:i + 1], func=AF.Sqrt, scale=1.0 / D, bias=EPS),
                      r=[B_ss[i]], w=[B_ss[i]])
                kb.op("dve", lambda e: e.reciprocal(out=rsd[:, i:i + 1], in_=rst[:, i:i + 1]), r=[B_ss[i]], w=[B_ss[i]])
                for hf in range(2):
                    kb.op("dve", lambda e: e.scalar_tensor_tensor(out=xn[j][:, hf * 512:(hf + 1) * 512], in0=xt[j][:, hf * 512:(hf + 1) * 512],
                                                                  scalar=rsd[:, i:i + 1], in1=gat[:, hf * 512:(hf + 1) * 512],
                                                                  op0=ALU.mult, op1=ALU.mult),
                          r=[B_xt[j], B_ss[i], B_gat], w=[B_xn[j]])
                for c in range(8):
                    kb.op("pe", lambda e: e.transpose(out=pT[j][:, c, :], in_=xn[j][:, c * 128:(c + 1) * 128], identity=ident_bf[:]),
                          r=[B_xn[j], B_const], w=[B_pT[j]])
                for hf in range(2):
                    kb.op("act", lambda e: e.copy(out=hT[:, hf * 4:hf * 4 + 4, i * 128:(i + 1) * 128], in_=pT[j][:, hf * 4:hf * 4 + 4, :]),
                          r=[B_pT[j]], w=[B_hT[i]])

        kb.barrier()
        if "hT" in dbg_d:
            kb.dma("sp", "dbg", lambda q: q.dma_start(out=dbg_d["hT"], in_=hT[:]), r=B_hT)

        B_wconv = kb.buf()
        conv_state = [0]

        def conv_next(n=1):
            if stage < 5:
                return
            for _ in range(n):
                e_ = conv_state[0]
                if e_ >= NE:
                    return
                conv_state[0] += 1
                for src_, dst_, c_ in ((w1, w1b_d, 8), (w3, w3b_d, 8), (w2, w2b_d, 4)):
                    kb.dma("pool", "wconv", lambda q: q.dma_start(out=dst_[e_ * 128:(e_ + 1) * 128, :],
                           in_=src_[e_].rearrange("(p c) f -> p (c f)", c=c_)), w=[B_wconv])
        if stage >= 2:
            _da_phase(nc, kb, S, hT, B_hT, y_daT, B_ydaT, w_in, cd, gqk_col, g_dao, lamv, ident_bf, zero_bf, B_const, B_zero, sb, ps, dbg_d, conv_next)
        conv_next(NE)
        y_mlT = sb(es_mix, "y_mlT", [128, 4, S], BF16)
        if stage >= 3:
            _ml_phase(nc, kb, S, hT, B_hT, y_mlT, B_ymlT, w_in, cd, cw_col, cb_col, g_mlo, b_if, ident_bf, ident_f, ones_f,
                      B_const, sb, ps, dbg_d)
        if stage >= 4:
            _merge_moe(nc, kb, S, hT, B_hT, y_daT, B_ydaT, y_mlT, B_ymlT, es_mix, x_d, out_d, w_bda, w_bml, w_gate, w_out,
                       bg_col, g_ffn, wr_d, b_rt, w1, w3, w2, xs_d, ys_d, hn_d, cd, ident_bf, ident_f, ones_f, zero_bf,
                       B_const, B_zero, sb, ps, dbg_d, stage, B_xs, (w1b_d, w3b_d, w2b_d, B_wconv))
        else:
            es_mix.close()

        allb = []
        for k in list(kb.dsem.keys()):
            b = Buf()
            b.w = (k, kb.dcnt[k])
            allb.append(b)
        for k in kb.sem:
            if kb.cnt[k] > 0:
                b = Buf()
                b.w = (k, kb.cnt[k])
                allb.append(b)
        kb._wait("sp", allb, [], True)
    return nc, consts


def _da_phase(nc, kb, S, hT, B_hT, y_daT, B_ydaT, w_in, cd, gqk_col, g_dao, lamv, ident_bf, zero_bf, B_const, B_zero, sb, ps, dbg_d, conv_next):
    NT = S // 128
    NB5 = S // 512
    with ExitStack() as s:
        blk64 = sb(s, "blk64", [128, 128], BF16)
        cmask = sb(s, "cmask", [128, 128], BF16)
        atab = sb(s, "atab", [128, NH * NOFF], F32)
        gqk = sb(s, "gqk", [128, 2], F32)
        gdo = sb(s, "gdo", [128, 128], F32)
        lam_t = sb(s, "lam_t", [128, 256], F32)
        B_c = kb.buf()
        for t, src in ((blk64, cd["blk64"]), (cmask, cd["cmask"]), (atab, cd["atab"]), (gqk, gqk_col),
                       (gdo, g_dao.partition_broadcast(128)), (lam_t, lamv.partition_broadcast(128))):
            kb.dma("sp", "const", lambda q, t=t, src=src: q.dma_start(out=t[:], in_=src), w=[B_c])
        lj = sb(s, "lj", [128, 128], F32)
        ls = sb(s, "ls", [128, 4], F32)
        neglam = sb(s, "neglam", [128, 1], F32)
        B_l = kb.buf()
        lv = lam_t[:].rearrange("p (a b d) -> p a b d", a=2, b=2)
        kb.op("dve", lambda e: e.tensor_tensor(out=lj[:].rearrange("p (a d) -> p a d", a=2), in0=lv[:, :, 0, :], in1=lv[:, :, 1, :],
                                               op=ALU.mult), r=[B_c], w=[B_l])
        kb.op("dve", lambda e: e.tensor_reduce(out=ls[:, 0:2], in_=lj[:].rearrange("p (a d) -> p a d", a=2), axis=AX.X, op=ALU.add),
              r=[B_l], w=[B_l])
        kb.op("act", lambda e: e.activation(out=ls[:, 2:4], in_=ls[:, 0:2], func=AF.Exp), r=[B_l], w=[B_l])
        kb.op("dve", lambda e: e.tensor_tensor(out=neglam[:], in0=ls[:, 3:4], in1=ls[:, 2:3], op=ALU.subtract), r=[B_l], w=[B_l])
        kb.op("dve", lambda e: e.tensor_scalar(out=neglam[:], in0=neglam[:], scalar1=-LAM_INIT, scalar2=None, op0=ALU.add),
              r=[B_l], w=[B_l])
        kb.op("dve", lambda e: e.tensor_scalar(out=gdo[:], in0=gdo[:], scalar1=1.0 - LAM_INIT, scalar2=None, op0=ALU.mult),
              r=[B_c], w=[B_c])

        P3 = [ps(s, f"daP{i}", [128, 512], F32) for i in range(3)]
        B_P3 = kb.bufs(3)
        acc4 = ps(s, "daAcc", [128, 4, 512], F32)
        B_acc = kb.buf()
        ptr = ps(s, "daPtr", [128, 8, 128], BF16)
        B_ptr = kb.buf()
        pcnt = [0]

        def nextP():
            i = pcnt[0] % 3
            pcnt[0] += 1
            return P3[i], B_P3[i]

        Vda = sb(s, "Vda", [128, NT, 4, 130], BF16)
        B_V = kb.bufs(NT)
        B_Vones = kb.buf()
        kb.op("pool", lambda e: e.memset(Vda[:, :, :, 128:130], 1.0), w=[B_Vones])
        with ExitStack() as sv:
            wv = sb(sv, "wv", [128, 8, 512], BF16)
            B_wv = kb.buf()
            kb.dma("pool", "wv", lambda q: q.dma_start(out=wv[:], in_=w_in[:, OFF_DA_V:OFF_DA_V + 512].rearrange("(c p) f -> p c f", p=128)),
                   w=[B_wv])
            for i in range(NT):
                P, BP = nextP()
                for c in range(8):
                    kb.op("pe", lambda e: e.matmul(P[:], lhsT=hT[:, c, i * 128:(i + 1) * 128], rhs=wv[:, c, :], start=(c == 0), stop=(c == 7)),
                          r=[B_hT[i], B_wv], w=[BP])
                kb.op("act", lambda e: e.copy(out=Vda[:, i, :, 0:128], in_=P[:].rearrange("p (h d) -> p h d", h=4)),
                      r=[BP, B_Vones], w=[B_V[i]])
        kb.barrier()

        wqk = [sb(s, f"wqk{i}", [128, 8, 256], BF16) for i in range(2)]
        B_wqk = kb.bufs(2)
        qkT = [sb(s, f"qkT{i}", [128, 2, S], BF16) for i in range(2)]
        B_qk = [kb.bufs(NB5 * 2) for _ in range(2)]
        sq_sb = [sb(s, f"sq_sb{i}", [128, 512], BF16) for i in range(2)]
        sd_sb = [sb(s, f"sd_sb{i}", [128, 512], F32) for i in range(2)]
        B_sq, B_sd = kb.bufs(2), kb.bufs(2)
        Et = [sb(s, f"Et{i}", [128, 512], BF16) for i in range(4)]
        B_Et = kb.bufs(4)
        ecnt = 0
        o_sb = sb(s, "o_sb", [128, 4, 128], F32)
        t_sb = sb(s, "t_sb", [128, 4, 128], F32)
        y_sb = sb(s, "y_sb", [128, 4, 128], BF16)
        rr = sb(s, "rr", [128, 16], F32)
        rra = sb(s, "rra", [128, 8], F32)
        B_o = kb.buf()

        def load_w(h):
            hb = h % 2
            kb.dma("pool", f"wqk{hb}", lambda q: q.dma_start(out=wqk[hb][:, :, 0:128],
                   in_=w_in[:, OFF_DA_Q + h * 128:OFF_DA_Q + (h + 1) * 128].rearrange("(c p) f -> p c f", p=128)), w=[B_wqk[hb]])
            kb.dma("pool", f"wqk{hb}", lambda q: q.dma_start(out=wqk[hb][:, :, 128:256],
                   in_=w_in[:, OFF_DA_K + h * 128:OFF_DA_K + (h + 1) * 128].rearrange("(c p) f -> p c f", p=128)), w=[B_wqk[hb]])

        load_w(0)
        for h in range(NH):
            hb = h % 2
            if h + 1 < NH:
                load_w(h + 1)
            slope = SLOPES[h]
            k2 = 0
            for tb in range(NB5):
                for which in range(2):
                    P, BP = nextP()
                    for c in range(8):
                        kb.op("pe", lambda e: e.matmul(P[:], lhsT=wqk[hb][:, c, which * 128:(which + 1) * 128],
                                                       rhs=hT[:, c, tb * 512:(tb + 1) * 512], start=(c == 0), stop=(c == 7)),
                              r=B_hT[tb * 4:tb * 4 + 4] + [B_wqk[hb]], w=[BP])
                    kk = k2 % 2
                    k2 += 1
                    kb.op("act", lambda e: e.activation(out=sq_sb[kk][:], in_=P[:], func=AF.Square), r=[BP], w=[B_sq[kk]])
                    P2, BP2 = nextP()
                    kb.op("pe", lambda e: e.matmul(P2[:], lhsT=blk64[:], rhs=sq_sb[kk][:], start=True, stop=True),
                          r=[B_sq[kk], B_c], w=[BP2])
                    kb.op("act", lambda e: e.activation(out=sd_sb[kk][:], in_=P2[:], func=AF.Ln, bias=EPS), r=[BP2], w=[B_sd[kk]])
                    kb.op("act", lambda e: e.activation(out=sd_sb[kk][:], in_=sd_sb[kk][:], func=AF.Exp, scale=-0.5), r=[B_sd[kk]], w=[B_sd[kk]])
                    kb.op("dve", lambda e: e.scalar_tensor_tensor(out=qkT[hb][:, which, tb * 512:(tb + 1) * 512], in0=P[:],
                                                                  scalar=gqk[:, which:which + 1], in1=sd_sb[kk][:],
                                                                  op0=ALU.mult, op1=ALU.mult),
                          r=[BP, B_sd[kk], B_c], w=[B_qk[hb][tb * 2 + which]])
            if h == 0 and "wqk" in dbg_d:
                kb.dma("sp", "dbg", lambda q: q.dma_start(out=dbg_d["wqk"], in_=wqk[0][:]), r=[B_wqk[0]])
            if h == 0 and "sd" in dbg_d:
                kb.dma("sp", "dbg", lambda q: q.dma_start(out=dbg_d["sd"], in_=sd_sb[0][:]), r=[B_sd[0]])
                kb.dma("sp", "dbg", lambda q: q.dma_start(out=dbg_d["sq"], in_=sq_sb[0][:]), r=[B_sq[0]])
            if h == 0 and "qkT" in dbg_d:
                kb.dma("sp", "dbg", lambda q: q.dma_start(out=dbg_d["qkT"], in_=qkT[0][:]), r=B_qk[0])
            sub = 128 if slope * 511 > 32.0 else 512
            units = []
            for qb in range(NB5):
                kt_max_ = 4 * qb + 3
                kt_min_ = max(0, int(math.ceil((qb * 512 - SKIP_T / slope - 127) / 128.0)))
                for kt in range(kt_min_, kt_max_ + 1):
                    for m in range(2):
                        units.append((qb, kt, m, kt == kt_min_ and m == 0, kt == kt_max_ and m == 1))
            ustate = {}
            pending = []

            def emit_qk(u):
                nonlocal ecnt
                qb, kt, m, _, _ = u
                q0 = qb * 512
                jj = kt - 4 * qb
                j_lo = max(0, jj)
                P, BP = nextP()
                kb.op("pe", lambda e: e.matmul(P[:, j_lo * 128:512], lhsT=qkT[hb][m * 64:(m + 1) * 64, 1, kt * 128:(kt + 1) * 128],
                                               rhs=qkT[hb][m * 64:(m + 1) * 64, 0, q0 + j_lo * 128:q0 + 512], start=True, stop=True),
                      r=[B_qk[hb][(kt // 4) * 2 + 1], B_qk[hb][qb * 2]], w=[BP])
                ei = ecnt % 4
                ecnt += 1
                E, BE = Et[ei], B_Et[ei]
                if sub == 512:
                    col = h * NOFF + (kt - 4 * qb) + OFF0
                    kb.op("act", lambda e: e.activation(out=E[:, j_lo * 128:512], in_=P[:, j_lo * 128:512], func=AF.Exp,
                                                        scale=0.125, bias=atab[:, col:col + 1]), r=[BP, B_c], w=[BE])
                else:
                    for j in range(j_lo, 4):
                        col = h * NOFF + (kt - 4 * qb - j) + OFF0
                        kb.op("act", lambda e: e.activation(out=E[:, j * 128:(j + 1) * 128], in_=P[:, j * 128:(j + 1) * 128],
                                                            func=AF.Exp, scale=0.125, bias=atab[:, col:col + 1]),
                              r=[BP, B_c], w=[BE])
                if jj >= 0:
                    kb.op("pool", lambda e: e.tensor_tensor(out=E[:, jj * 128:(jj + 1) * 128], in0=E[:, jj * 128:(jj + 1) * 128],
                                                            in1=cmask[:], op=ALU.mult), r=[B_c], w=[BE])
                ustate[u] = (E, BE, j_lo)

            def emit_av(u):
                qb, kt, m, first, last = u
                q0 = qb * 512
                E, BE, j_lo = ustate.pop(u)
                if first:
                    for j in range(4):
                        kb.op("pe", lambda e: e.matmul(acc4[:, j, :], lhsT=zero_bf[0:1, 0:128], rhs=zero_bf[0:1, 0:512],
                                                       start=True, stop=True, skip_group_check=True), r=[B_zero], w=[B_acc])
                for j in range(j_lo, 4):
                    kb.op("pe", lambda e: e.matmul(acc4[:, j, m * 129:(m + 1) * 129], lhsT=E[:, j * 128:(j + 1) * 128],
                                                   rhs=Vda[:, kt, h, 0:129], start=False, stop=last, skip_group_check=True),
                          r=[BE, B_V[kt]], w=[B_acc])
                if last:
                    while pending:
                        kb.op(*pending.pop(0))
                    evac(qb, q0)
                    conv_next(1)

            def evac(qb, q0):
                kb.op("dve", lambda e: e.reciprocal(out=rr[:, 0:4], in_=acc4[:, :, 128:129]), r=[B_acc], w=[B_o])
                kb.op("dve", lambda e: e.reciprocal(out=rr[:, 4:8], in_=acc4[:, :, 257:258]), r=[B_acc], w=[B_o])
                kb.op("dve", lambda e: e.tensor_tensor(out=o_sb[:], in0=acc4[:, :, 0:128], in1=rr[:, 0:4].unsqueeze(2).to_broadcast([128, 4, 128]),
                                                       op=ALU.mult), r=[B_acc], w=[B_o])
                kb.op("dve", lambda e: e.tensor_tensor(out=t_sb[:], in0=acc4[:, :, 129:257], in1=rr[:, 4:8].unsqueeze(2).to_broadcast([128, 4, 128]),
                                                       op=ALU.mult), r=[B_acc], w=[B_o])
                rec_ = []
                kb.op = lambda e, fn, r=(), w=(), same=None: rec_.append((e, fn, list(r), list(w), same))
                try:
                    evac_tail(qb, q0)
                finally:
                    del kb.op
                pending.extend(rec_)

            def evac_tail(qb, q0):
                kb.op("dve", lambda e: e.scalar_tensor_tensor(out=o_sb[:], in0=t_sb[:], scalar=neglam[:, 0:1], in1=o_sb[:],
                                                              op0=ALU.mult, op1=ALU.add), r=[B_o, B_l], w=[B_o])
                kb.op("dve", lambda e: e.tensor_tensor(out=t_sb[:], in0=o_sb[:], in1=o_sb[:], op=ALU.mult), r=[B_o], w=[B_o])
                kb.op("dve", lambda e: e.tensor_reduce(out=rr[:, 8:12], in_=t_sb[:], axis=AX.X, op=ALU.add), r=[B_o], w=[B_o])
                kb.op("act", lambda e: e.activation(out=rra[:, 0:4], in_=rr[:, 8:12], func=AF.Ln, scale=1.0 / 128, bias=EPS), r=[B_o], w=[B_o])
                kb.op("act", lambda e: e.activation(out=rra[:, 4:8], in_=rra[:, 0:4], func=AF.Exp, scale=-0.5), r=[B_o], w=[B_o])
                kb.op("dve", lambda e: e.tensor_tensor(out=t_sb[:], in0=o_sb[:], in1=rra[:, 4:8].unsqueeze(2).to_broadcast([128, 4, 128]),
                                                       op=ALU.mult), r=[B_o], w=[B_o])
                kb.op("dve", lambda e: e.tensor_tensor(out=y_sb[:], in0=t_sb[:], in1=gdo[:].unsqueeze(1).to_broadcast([128, 4, 128]),
                                                       op=ALU.mult), r=[B_o, B_c], w=[B_o])
                for j in range(4):
                    kb.op("pe", lambda e, j=j: e.transpose(out=ptr[:, j, :], in_=y_sb[:, j, :], identity=ident_bf[:]), r=[B_o, B_const], w=[B_ptr])
                kb.op("act", lambda e, h=h: e.copy(out=y_daT[:, h, q0:q0 + 512].rearrange("p (j t) -> p j t", j=4), in_=ptr[:, 0:4, :]),
                      r=[B_ptr], w=B_ydaT[h * NT + qb * 4:h * NT + qb * 4 + 4])

            emit_qk(units[0])
            if len(units) > 1:
                emit_qk(units[1])
            for ui in range(len(units)):
                if ui + 2 < len(units):
                    emit_qk(units[ui + 2])
                emit_av(units[ui])
                if pending and not units[ui][4]:
                    kb.op(*pending.pop(0))
            while pending:
                kb.op(*pending.pop(0))
        if "y_daT" in dbg_d:
            kb.dma("sp", "dbg", lambda q: q.dma_start(out=dbg_d["y_daT"], in_=y_daT[:]), r=B_ydaT)
        kb.barrier()


def _ml_phase(nc, kb, S, hT, B_hT, y_mlT, B_ymlT, w_in, cd, cw_col, cb_col, g_mlo, b_if, ident_bf, ident_f, ones_f,
              B_const, sb, ps, dbg_d):
    NT = S // 128
    NB5 = S // 512
    NHC = NT * 4
    QS = 128.0 ** -0.5
    with ExitStack() as s:
        negm = sb(s, "negm", [128, 128], F32)
        cw = sb(s, "cw", [128, 32], F32)
        cb = sb(s, "cb", [128, 8], F32)
        gml = sb(s, "gml", [128, 512], F32)
        bif = sb(s, "bif", [128, 8], F32)
        wif = sb(s, "wif", [128, 8, 8], BF16)
        B_c = kb.buf()
        for t, src in ((negm, cd["negm"]), (cw, cw_col), (cb, cb_col), (gml, g_mlo.partition_broadcast(128)),
                       (bif, b_if.partition_broadcast(128))):
            kb.dma("sp", "const", lambda q, t=t, src=src: q.dma_start(out=t[:], in_=src), w=[B_c])
        kb.dma("pool", "wif", lambda q: q.dma_start(out=wif[:], in_=w_in[:, OFF_ML_I:OFF_ML_I + 8].rearrange("(c p) f -> p c f", p=128)), w=[B_c])

        PA = [ps(s, f"mlPA{i}", [128, 512], F32) for i in range(2)]
        B_PA = kb.bufs(2)
        pacnt = [0]

        def nextPA():
            i = pacnt[0] % 2
            pacnt[0] += 1
            return PA[i], B_PA[i]
        Pew2 = [ps(s, f"mlPew{i}", [128, 512], F32) for i in range(2)]
        B_Pew2 = kb.bufs(2)
        Pew, B_Pew = Pew2[0], B_Pew2[0]
        Po2 = [ps(s, f"mlPo{i}", [128, 512], F32) for i in range(2)]
        B_Po2 = kb.bufs(2)
        Po, B_Po = Po2[0], B_Po2[0]
        Pc = ps(s, "mlPc", [128, 512], F32)
        Ptk = ps(s, "mlPtk", [128, 8, 128], BF16)
        Pty = Ptk
        Pg = Po
        B_Pc, B_Ptk = kb.bufs(2)
        B_Pty = B_Ptk
        B_Pg = B_Po

        for i in range(NT):
            for c in range(8):
                kb.op("pe", lambda e: e.matmul(Pg[:, i * 8:(i + 1) * 8], lhsT=hT[:, c, i * 128:(i + 1) * 128], rhs=wif[:, c, :],
                                               start=(c == 0), stop=(c == 7)), r=[B_hT[i], B_c], w=[B_Pg])
        gsb = sb(s, "gsb", [128, NT, 8], F32)
        XC = sb(s, "XC", [128, 2, NT, 4], F32)
        tmpg = sb(s, "tmpg", [128, NT, 4], F32)
        B_g = kb.buf()
        kb.op("dve", lambda e: e.tensor_tensor(out=gsb[:], in0=Pg[:, 0:NT * 8].rearrange("p (i g) -> p i g", g=8),
                                               in1=bif[:].unsqueeze(1).to_broadcast([128, NT, 8]), op=ALU.add), r=[B_Pg, B_c], w=[B_g])
        kb.op("act", lambda e: e.activation(out=tmpg[:], in_=gsb[:, :, 4:8], func=AF.Exp, scale=-1.0), r=[B_g], w=[B_g])
        kb.op("act", lambda e: e.activation(out=tmpg[:], in_=tmpg[:], func=AF.Ln, bias=1.0), r=[B_g], w=[B_g])
        kb.op("dve", lambda e: e.tensor_scalar(out=XC[:, 1], in0=tmpg[:], scalar1=-1.0, scalar2=None, op0=ALU.mult), r=[B_g], w=[B_g])
        kb.op("dve", lambda e: e.tensor_copy(out=XC[:, 0], in_=gsb[:, :, 0:4]), r=[B_g], w=[B_g])
        RN = ["iR", "lfR", "bR", "betaR", "pmR", "mxR", "alphaR", "mrowR", "winterR", "emrR", "winR", "zR"]
        Rt = {n: sb(s, n, [128, 128], F32) for n in RN}
        B_R = kb.buf()
        for a, n in ((0, "iR"), (1, "lfR")):
            kb.op("pe", lambda e: e.transpose(out=Pew[0:NHC, a * 128:(a + 1) * 128], in_=XC[:, a].rearrange("p i h -> p (i h)"),
                                              identity=ident_f[:]), r=[B_g, B_const], w=[B_Pew])
            kb.op("dve", lambda e: e.tensor_copy(out=Rt[n][0:NHC, :], in_=Pew[0:NHC, a * 128:(a + 1) * 128]), r=[B_Pew], w=[B_R])
        R = {n: Rt[n][0:NHC, :] for n in RN}
        kb.op("pool", lambda e: e.memset(Rt["zR"][:], 0.0), w=[B_R])
        kb.op("dve", lambda e: e.tensor_tensor_scan(out=R["bR"], data0=R["lfR"], data1=R["zR"], initial=0.0, op0=ALU.add, op1=ALU.add),
              r=[B_R], w=[B_R])
        kb.op("dve", lambda e: e.tensor_tensor(out=R["betaR"], in0=R["iR"], in1=R["bR"], op=ALU.subtract), r=[B_R], w=[B_R])
        kb.op("dve", lambda e: e.tensor_tensor_scan(out=R["pmR"], data0=R["betaR"], data1=R["betaR"], initial=-1e30, op0=ALU.max, op1=ALU.max),
              r=[B_R], w=[B_R])
        c2 = sb(s, "c2", [128, 8], F32)
        rows = sb(s, "rows", [1, 4, 128], F32)
        drow = sb(s, "drow", [1, 128], F32)
        kb.op("dve", lambda e: e.tensor_copy(out=c2[0:NHC, 0:1], in_=R["bR"][:, 127:128]), r=[B_R], w=[B_R])
        kb.op("dve", lambda e: e.tensor_tensor(out=c2[0:NHC, 1:2], in0=R["bR"][:, 127:128], in1=R["pmR"][:, 127:128], op=ALU.add), r=[B_R], w=[B_R])
        for a in range(2):
            kb.op("pe", lambda e: e.transpose(out=Pew[0:1, a * 128:a * 128 + NHC], in_=c2[0:NHC, a:a + 1], identity=ident_f[0:NHC, 0:NHC]),
                  r=[B_R, B_const], w=[B_Pew])
        kb.op("dve", lambda e: e.tensor_copy(out=rows[:, 0:2, 0:NHC], in_=Pew[0:1, 0:256].rearrange("p (a n) -> p a n", a=2)[:, :, 0:NHC]),
              r=[B_Pew], w=[B_R])
        for h in range(4):
            v = lambda a: rows[:, a, 0:NHC].rearrange("p (i h) -> p h i", h=4)[:, h, :]
            kb.op("dve", lambda e: e.tensor_tensor_scan(out=v(2), data0=v(0), data1=v(1), initial=0.0, op0=ALU.add, op1=ALU.max),
                  r=[B_R], w=[B_R])
        kb.op("pool", lambda e: e.memset(rows[:, 3, 0:4], 0.0), r=[B_R], w=[B_R])
        if NHC > 4:
            kb.op("dve", lambda e: e.tensor_copy(out=rows[:, 3, 4:NHC], in_=rows[:, 2, 0:NHC - 4]), r=[B_R], w=[B_R])
        kb.op("pe", lambda e: e.matmul(Pew[0:NHC, 0:1], lhsT=rows[:, 3, 0:NHC], rhs=ones_f[0:1, 0:1], start=True, stop=True),
              r=[B_R, B_const], w=[B_Pew])
        kb.op("dve", lambda e: e.tensor_copy(out=c2[0:NHC, 2:3], in_=Pew[0:NHC, 0:1]), r=[B_Pew], w=[B_R])
        ms = c2[0:NHC, 2:3]
        kb.op("dve", lambda e: e.tensor_scalar(out=R["mxR"], in0=R["pmR"], scalar1=ms, scalar2=None, op0=ALU.max), r=[B_R], w=[B_R])
        kb.op("dve", lambda e: e.tensor_scalar(out=R["alphaR"], in0=R["mxR"], scalar1=-1.0, scalar2=None, op0=ALU.mult), r=[B_R], w=[B_R])
        kb.op("dve", lambda e: e.tensor_tensor(out=R["mrowR"], in0=R["bR"], in1=R["mxR"], op=ALU.add), r=[B_R], w=[B_R])
        kb.op("act", lambda e: e.activation(out=R["winterR"], in_=R["alphaR"], func=AF.Exp, bias=ms), r=[B_R], w=[B_R])
        kb.op("act", lambda e: e.activation(out=R["emrR"], in_=R["mrowR"], func=AF.Exp, scale=-1.0), r=[B_R], w=[B_R])
        kb.op("dve", lambda e: e.tensor_tensor(out=c2[0:NHC, 6:7], in0=ms, in1=R["pmR"][:, 127:128], op=ALU.max), r=[B_R], w=[B_R])
        kb.op("dve", lambda e: e.tensor_tensor(out=c2[0:NHC, 3:4], in0=c2[0:NHC, 6:7], in1=c2[0:NHC, 0:1], op=ALU.add), r=[B_R], w=[B_R])
        kb.op("dve", lambda e: e.tensor_tensor(out=c2[0:NHC, 5:6], in0=c2[0:NHC, 0:1], in1=c2[0:NHC, 3:4], op=ALU.subtract), r=[B_R], w=[B_R])
        kb.op("act", lambda e: e.activation(out=c2[0:NHC, 4:5], in_=ms, func=AF.Exp, bias=c2[0:NHC, 5:6]), r=[B_R], w=[B_R])
        kb.op("act", lambda e: e.activation(out=R["winR"], in_=R["betaR"], func=AF.Exp, bias=c2[0:NHC, 5:6]), r=[B_R], w=[B_R])
        CN = ["betaR", "alphaR", "winterR", "emrR", "winR"]
        Ct = {n: sb(s, "C_" + n, [128, 128], F32) for n in CN}
        dbc = sb(s, "dbc", [128, 128], F32)
        B_C = kb.buf()
        for n in CN:
            kb.op("pe", lambda e: e.transpose(out=Pew[:, 0:NHC], in_=R[n], identity=ident_f[0:NHC, 0:NHC]), r=[B_R, B_const], w=[B_Pew])
            kb.op("dve", lambda e: e.tensor_copy(out=Ct[n][:, 0:NHC], in_=Pew[:, 0:NHC]), r=[B_Pew], w=[B_C])
        kb.op("pe", lambda e: e.transpose(out=Pew[0:1, 0:NHC], in_=c2[0:NHC, 4:5], identity=ident_f[0:NHC, 0:NHC]), r=[B_R, B_const], w=[B_Pew])
        kb.op("dve", lambda e: e.tensor_copy(out=drow[:, 0:NHC], in_=Pew[0:1, 0:NHC]), r=[B_Pew], w=[B_R])
        kb.op("pe", lambda e: e.matmul(Pew[:, 0:NHC], lhsT=ones_f[0:1, :], rhs=drow[:, 0:NHC], start=True, stop=True), r=[B_R, B_const], w=[B_Pew])
        kb.op("dve", lambda e: e.tensor_copy(out=dbc[:, 0:NHC], in_=Pew[:, 0:NHC]), r=[B_Pew], w=[B_C])

        wq4 = sb(s, "wq4", [128, 8, 512], BF16)
        B_w = kb.buf()
        qkT = sb(s, "mlqkT", [128, 2, S], BF16)
        B_qk = [kb.bufs(NB5), kb.bufs(NB5)]
        Vml = sb(s, "Vml", [128, NT, 130], BF16)
        og = sb(s, "og", [128, NT, 128], BF16)
        B_vo = kb.bufs(NT)
        B_vones = kb.buf()
        kb.op("pool", lambda e: e.memset(Vml[:, :, 128:130], 1.0), w=[B_vones])
        U2 = [sb(s, f"U2{i}", [128, 515], F32) for i in range(2)]
        B_U = kb.bufs(2)
        acc = [sb(s, f"cacc{i}", [128, 512], F32) for i in range(2)]
        B_a = kb.bufs(2)
        Cf = sb(s, "Cf", [128, 130], F32)
        Cbf = [sb(s, f"Cbf{i}", [128, 130], BF16) for i in range(2)]
        B_Cf = kb.buf()
        B_Cbf = kb.bufs(2)
        dA = [sb(s, f"dA{i}", [128, 128], F32) for i in range(2)]
        dW = [sb(s, f"dW{i}", [128, 128], F32) for i in range(2)]
        Wt = [sb(s, f"Wt{i}", [128, 128], F32) for i in range(2)]
        Pt = [sb(s, f"Pt{i}", [128, 128], BF16) for i in range(2)]
        qs = [sb(s, f"qs{i}", [128, 128], BF16) for i in range(2)]
        kw = [sb(s, f"kw{i}", [128, 128], BF16) for i in range(2)]
        t1_2 = [sb(s, f"t1{i}", [128, 128], F32) for i in range(2)]
        yb_2 = [sb(s, f"yb{i}", [128, 128], BF16) for i in range(2)]
        jk_2 = [sb(s, f"jk{i}", [128, 128], F32) for i in range(2)]
        sc_2 = [sb(s, f"sc{i}", [128, 8], F32) for i in range(2)]
        sca_2 = [sb(s, f"sca{i}", [128, 8], F32) for i in range(2)]
        B_ch2 = kb.bufs(2)
        B_y2 = kb.bufs(2)
        B_dA, B_dW, B_Wt, B_Pt, B_qs, B_kw = [kb.bufs(2) for _ in range(6)]
        offs = [OFF_ML_QK, OFF_ML_QK + 512, OFF_ML_V, OFF_ML_O]
        for h in range(NH):
            for a in range(4):
                kb.dma("pool", "wq4", lambda q: q.dma_start(out=wq4[:, :, a * 128:(a + 1) * 128],
                       in_=w_in[:, offs[a] + h * 128:offs[a] + (h + 1) * 128].rearrange("(c p) f -> p c f", p=128)), w=[B_w])
            for i in range(NT):
                P, BP = nextPA()
                for c in range(8):
                    kb.op("pe", lambda e: e.matmul(P[:, 0:256], lhsT=hT[:, c, i * 128:(i + 1) * 128], rhs=wq4[:, c, 256:512],
                                                   start=(c == 0), stop=(c == 7)), r=[B_hT[i], B_w], w=[BP])
                kb.op("act", lambda e: e.copy(out=Vml[:, i, 0:128], in_=P[:, 0:128]), r=[BP, B_vones], w=[B_vo[i]])
                kb.op("act", lambda e: e.activation(out=og[:, i, :], in_=P[:, 128:256], func=AF.Sigmoid), r=[BP], w=[B_vo[i]])
            for which in range(2):
                cc = which * 4 + h
                for tb in range(NB5):
                    ub = tb % 2
                    P, BP = nextPA()
                    for c in range(8):
                        kb.op("pe", lambda e: e.matmul(P[:], lhsT=wq4[:, c, which * 128:(which + 1) * 128], rhs=hT[:, c, tb * 512:(tb + 1) * 512],
                                                       start=(c == 0), stop=(c == 7)), r=B_hT[tb * 4:tb * 4 + 4] + [B_w], w=[BP])
                    kb.op("act", lambda e: e.copy(out=U2[ub][:, 3:515], in_=P[:]), r=[BP], w=[B_U[ub]])
                    if tb == 0:
                        kb.op("pool", lambda e: e.memset(U2[ub][:, 0:3], 0.0), w=[B_U[ub]])
                    else:
                        kb.op("pool", lambda e: e.tensor_copy(out=U2[ub][:, 0:3], in_=U2[1 - ub][:, 512:515]), r=[B_U[1 - ub]], w=[B_U[ub]])
                    A_, BA = acc[ub], B_a[ub]
                    kb.op("dve", lambda e: e.tensor_scalar(out=A_[:], in0=U2[ub][:, 3:515], scalar1=cw[:, cc * 4 + 3:cc * 4 + 4],
                                                           scalar2=cb[:, cc:cc + 1], op0=ALU.mult, op1=ALU.add), r=[B_U[ub], B_c], w=[BA])
                    for j in range(3):
                        kb.op("dve", lambda e: e.scalar_tensor_tensor(out=A_[:], in0=U2[ub][:, j:j + 512], scalar=cw[:, cc * 4 + j:cc * 4 + j + 1],
                                                                      in1=A_[:], op0=ALU.mult, op1=ALU.add), r=[B_U[ub], B_c], w=[BA])
                    if which == 1:
                        kb.op("act", lambda e: e.activation(out=qkT[:, 1, tb * 512:(tb + 1) * 512], in_=A_[:], func=AF.Silu),
                              r=[BA], w=[B_qk[1][tb]])
                    else:
                        kb.op("act", lambda e: e.activation(out=A_[:], in_=A_[:], func=AF.Silu), r=[BA], w=[BA])
                        kb.op("pool", lambda e: e.tensor_scalar(out=qkT[:, 0, tb * 512:(tb + 1) * 512], in0=A_[:], scalar1=QS, scalar2=None,
                                                                op0=ALU.mult), r=[BA], w=[B_qk[0][tb]])
            kb.op("pool", lambda e: e.memset(Cf[:], 0.0), w=[B_Cf])
            kb.op("pool", lambda e: e.memset(Cbf[1][:], 0.0), w=[B_Cbf[1]])

            def pre(i):
                jj = i % 2
                hc = i * 4 + h
                tsl = slice(i * 128, (i + 1) * 128)
                tb = i // 4
                Ps_, BPs = PA[jj], B_PA[jj]
                Pw_, BPw = Pew2[jj], B_Pew2[jj]
                kb.op("pe", lambda e: e.matmul(Ps_[:, 0:128], lhsT=qkT[:, 1, tsl], rhs=qkT[:, 0, tsl], start=True, stop=True),
                      r=[B_qk[0][tb], B_qk[1][tb]], w=[BPs])
                kb.op("dve", lambda e: e.tensor_scalar(out=dA[jj][:], in0=ident_f[:], scalar1=Ct["alphaR"][:, hc:hc + 1], scalar2=None, op0=ALU.mult),
                      r=[B_C, B_const], w=[B_dA[jj]])
                kb.op("dve", lambda e: e.tensor_scalar(out=dW[jj][:], in0=ident_f[:], scalar1=Ct["winterR"][:, hc:hc + 1], scalar2=None, op0=ALU.mult),
                      r=[B_C, B_const], w=[B_dW[jj]])
                kb.op("pe", lambda e: e.matmul(Pw_[:, 0:128], lhsT=ones_f[:], rhs=dA[jj][:], start=True, stop=False), r=[B_dA[jj], B_const], w=[BPw])
                kb.op("pe", lambda e: e.matmul(Pw_[:, 0:128], lhsT=ident_f[:], rhs=negm[:], start=False, stop=True), r=[B_c, B_const], w=[BPw])
                kb.op("pe", lambda e: e.matmul(Pw_[:, 128:256], lhsT=ones_f[:], rhs=dW[jj][:], start=True, stop=True), r=[B_dW[jj], B_const], w=[BPw])
                kb.op("pe", lambda e: e.transpose(out=Ptk[:, jj, :], in_=qkT[:, 1, tsl], identity=ident_bf[:]), r=[B_qk[1][tb], B_const], w=[B_Ptk])

            def preB(i):
                jj = i % 2
                hc = i * 4 + h
                tsl = slice(i * 128, (i + 1) * 128)
                tb = i // 4
                Ps_, BPs = PA[jj], B_PA[jj]
                Pw_, BPw = Pew2[jj], B_Pew2[jj]
                kb.op("act", lambda e: e.activation(out=Wt[jj][:], in_=Pw_[:, 0:128], func=AF.Exp, bias=Ct["betaR"][:, hc:hc + 1]),
                      r=[BPw, B_C], w=[B_Wt[jj]])
                kb.op("dve", lambda e: e.tensor_tensor(out=qs[jj][:], in0=qkT[:, 0, tsl], in1=Pw_[:, 128:256], op=ALU.mult),
                      r=[BPw, B_qk[0][tb], B_Wt[jj]], w=[B_qs[jj]])
                kb.op("dve", lambda e: e.tensor_scalar(out=kw[jj][:], in0=Ptk[:, jj, :], scalar1=Ct["winR"][:, hc:hc + 1], scalar2=None, op0=ALU.mult),
                      r=[B_Ptk, B_C], w=[B_kw[jj]])
                kb.op("dve", lambda e: e.tensor_tensor(out=Pt[jj][:], in0=Ps_[:, 0:128], in1=Wt[jj][:], op=ALU.mult), r=[BPs, B_Wt[jj]], w=[B_Pt[jj]])

            def main(i):
                mainA(i)
                tail(i)

            def mainA(i):
                jj = i % 2
                hc = i * 4 + h
                tsl = slice(i * 128, (i + 1) * 128)
                Po, B_Po = Po2[jj], B_Po2[jj]
                kb.op("pe", lambda e: e.matmul(Pc[:, 0:129], lhsT=kw[jj][:], rhs=Vml[:, i, 0:129], start=True, stop=True), r=[B_kw[jj], B_vo[i]], w=[B_Pc])
                kb.op("pe", lambda e: e.matmul(Po[:, 0:129], lhsT=Pt[jj][:], rhs=Vml[:, i, 0:129], start=True, stop=False), r=[B_Pt[jj], B_vo[i]], w=[B_Po])
                kb.op("pe", lambda e: e.matmul(Po[:, 0:129], lhsT=qs[jj][:], rhs=Cbf[(i + 1) % 2][:, 0:129], start=False, stop=True),
                      r=[B_qs[jj], B_Cbf[(i + 1) % 2]], w=[B_Po])
                kb.op("dve", lambda e: e.scalar_tensor_tensor(out=Cf[:, 0:129], in0=Cf[:, 0:129], scalar=dbc[:, hc:hc + 1], in1=Pc[:, 0:129],
                                                              op0=ALU.mult, op1=ALU.add), r=[B_Pc, B_C], w=[B_Cf])
                kb.op("act", lambda e: e.copy(out=Cbf[jj][:, 0:129], in_=Cf[:, 0:129]), r=[B_Cf], w=[B_Cbf[jj]])

            def tail(i):
                jj = i % 2
                hc = i * 4 + h
                tsl = slice(i * 128, (i + 1) * 128)
                Po, B_Po = Po2[jj], B_Po2[jj]
                t1, yb, jk, sc, sca, B_ch, B_y = t1_2[jj], yb_2[jj], jk_2[jj], sc_2[jj], sca_2[jj], B_ch2[jj], B_y2[jj]
                kb.op("act", lambda e: e.activation(out=sca[:, 0:1], in_=Po[:, 128:129], func=AF.Abs), r=[B_Po], w=[B_ch])
                kb.op("dve", lambda e: e.tensor_tensor(out=sc[:, 0:1], in0=sca[:, 0:1], in1=Ct["emrR"][:, hc:hc + 1], op=ALU.max), r=[B_ch, B_C], w=[B_ch])
                kb.op("dve", lambda e: e.tensor_scalar(out=sc[:, 1:2], in0=sc[:, 0:1], scalar1=sc[:, 0:1], scalar2=EPS, op0=ALU.mult, op1=ALU.mult),
                      r=[B_ch], w=[B_ch])
                kb.op("act", lambda e: e.activation(out=jk[:], in_=Po[:, 0:128], func=AF.Square, accum_out=sca[:, 2:3]), r=[B_Po], w=[B_ch])
                kb.op("act", lambda e: e.activation(out=sca[:, 3:4], in_=sca[:, 2:3], func=AF.Ln, scale=1.0 / 128, bias=sc[:, 1:2]), r=[B_ch], w=[B_ch])
                kb.op("act", lambda e: e.activation(out=sca[:, 4:5], in_=sca[:, 3:4], func=AF.Exp, scale=-0.5), r=[B_ch], w=[B_ch])
                kb.op("dve", lambda e: e.scalar_tensor_tensor(out=t1[:], in0=Po[:, 0:128], scalar=sca[:, 4:5], in1=gml[:, h * 128:(h + 1) * 128],
                                                              op0=ALU.mult, op1=ALU.mult), r=[B_Po, B_ch, B_c], w=[B_ch])
                kb.op("pool", lambda e: e.tensor_tensor(out=yb[:], in0=t1[:], in1=og[:, i, :], op=ALU.mult), r=[B_ch, B_vo[i]], w=[B_y])
                kb.op("pe", lambda e: e.transpose(out=Pty[:, 2, :], in_=yb[:], identity=ident_bf[:]), r=[B_y, B_const], w=[B_Pty])
                kb.op("act", lambda e: e.copy(out=y_mlT[:, h, tsl], in_=Pty[:, 2, :]), r=[B_Pty], w=[B_ymlT[h * NT + i]])

            LOOKAHEAD = ML_LOOKAHEAD
            if LOOKAHEAD == 0:
                for i in range(NT):
                    pre(i)
                    preB(i)
                    main(i)
            elif LOOKAHEAD == 1:
                pre(0)
                for i in range(NT):
                    preB(i)
                    if i + 1 < NT:
                        pre(i + 1)
                    main(i)
            elif LOOKAHEAD == 3:
                def rec_ops(fns):
                    rec = []
                    kb.op = lambda e, fn, r=(), w=(), same=None: rec.append((e, fn, list(r), list(w), same))
                    try:
                        for f_ in fns:
                            f_()
                    finally:
                        del kb.op
                    return rec
                pre(0)
                for i in range(NT + 1):
                    fa = []
                    if i < NT:
                        fa.append(lambda i=i: preB(i))
                        if i + 1 < NT:
                            fa.append(lambda i=i: pre(i + 1))
                        fa.append(lambda i=i: mainA(i))
                    ra = rec_ops(fa)
                    rb = rec_ops([lambda i=i: tail(i - 1)]) if i >= 1 else []
                    ia = ib = 0
                    while ia < len(ra) or ib < len(rb):
                        for _ in range(2):
                            if ia < len(ra):
                                kb.op(*ra[ia])
                                ia += 1
                        if ib < len(rb):
                            kb.op(*rb[ib])
                            ib += 1
            else:
                pre(0)
                preB(0)
                for i in range(NT):
                    if i + 1 < NT:
                        pre(i + 1)
                        preB(i + 1)
                    main(i)
        if "y_mlT" in dbg_d:
            kb.dma("sp", "dbg", lambda q: q.dma_start(out=dbg_d["y_mlT"], in_=y_mlT[:]), r=B_ymlT)
        kb.barrier()


def _merge_moe(nc, kb, S, hT, B_hT, y_daT, B_ydaT, y_mlT, B_ymlT, es_mix, x_d, out_d, w_bda, w_bml, w_gate, w_out,
               bg_col, g_ffn, wr_d, b_rt, w1, w3, w2, xs_d, ys_d, hn_d, cd, ident_bf, ident_f, ones_f, zero_bf,
               B_const, B_zero, sb, ps, dbg_d, stage, B_xs, wconv):
    w1b_d, w3b_d, w2b_d, B_wconv = wconv
    NT = S // 128
    NB5 = S // 512
    CAP = 2 * S + NE * BS
    NBLK = CAP // BS
    NSUB = BS // 128
    with ExitStack() as s:
        wg = sb(s, "wg", [128, 8, 2048], BF16)
        wda = sb(s, "wda", [128, 4, 1024], BF16)
        wml = sb(s, "wml", [128, 4, 1024], BF16)
        bg = sb(s, "bg", [128, 16], F32)
        B_w = kb.buf()
        for k4 in range(4):
            kb.dma("pool", "mw", lambda q: q.dma_start(out=wg[:, :, k4 * 512:(k4 + 1) * 512],
                   in_=w_gate[:, k4 * 512:(k4 + 1) * 512].rearrange("(c p) f -> p c f", p=128)), w=[B_w])
        kb.dma("pool", "mw", lambda q: q.dma_start(out=wda[:], in_=w_bda.rearrange("(c p) f -> p c f", p=128)), w=[B_w])
        kb.dma("pool", "mw", lambda q: q.dma_start(out=wml[:], in_=w_bml.rearrange("(c p) f -> p c f", p=128)), w=[B_w])
        kb.dma("sp", "const", lambda q: q.dma_start(out=bg[:], in_=bg_col), w=[B_w])
        Pg = [ps(s, f"mPg{i}", [128, 512], F32) for i in range(2)]
        Pab = [ps(s, f"mPab{i}", [128, 512], F32) for i in range(2)]
        B_Pg, B_Pab = kb.bufs(2), kb.bufs(2)
        gs = [sb(s, f"gs{i}", [128, 512], F32) for i in range(2)]
        tt = [sb(s, f"tt{i}", [128, 512], F32) for i in range(2)]
        B_gs, B_tt = kb.bufs(2), kb.bufs(2)
        mixb = sb(s, "mixb", [128, 8, 512], BF16)
        B_mix = kb.bufs(8)
        for tb in range(NB5):
            tsl = slice(tb * 512, (tb + 1) * 512)
            hb = B_hT[tb * 4:tb * 4 + 4]
            for fc in range(8):
                for g in range(2):
                    kb_w = wda if g == 0 else wml
                    yT = y_daT if g == 0 else y_mlT
                    By = (B_ydaT if g == 0 else B_ymlT)
                    byl = [By[hh * NT + tb * 4 + t4] for hh in range(4) for t4 in range(4)]
                    for c in range(8):
                        kb.op("pe", lambda e: e.matmul(Pg[g][:], lhsT=wg[:, c, g * 1024 + fc * 128:g * 1024 + (fc + 1) * 128], rhs=hT[:, c, tsl],
                                                       start=(c == 0), stop=(c == 7)), r=hb + [B_w], w=[B_Pg[g]])
                    kb.op("act", lambda e: e.activation(out=gs[g][:], in_=Pg[g][:], func=AF.Sigmoid, bias=bg[:, g * 8 + fc:g * 8 + fc + 1]),
                          r=[B_Pg[g], B_w], w=[B_gs[g]])
                    for c in range(4):
                        kb.op("pe", lambda e: e.matmul(Pab[g][:], lhsT=kb_w[:, c, fc * 128:(fc + 1) * 128], rhs=yT[:, c, tsl],
                                                       start=(c == 0), stop=(c == 3)), r=byl + [B_w], w=[B_Pab[g]])
                    kb.op("dve", lambda e: e.tensor_tensor(out=tt[g][:], in0=gs[g][:], in1=Pab[g][:], op=ALU.mult),
                          r=[B_gs[g], B_Pab[g]], w=[B_tt[g]])
                kb.op("pool", lambda e: e.tensor_tensor(out=mixb[:, fc, :], in0=tt[0][:], in1=tt[1][:], op=ALU.add),
                      r=[B_tt[0], B_tt[1]], w=[B_mix[fc]])
            for fc in range(8):
                kb.op("pool", lambda e: e.tensor_copy(out=hT[:, fc, tsl], in_=mixb[:, fc, :]), r=[B_mix[fc]], w=hb)
        kb.barrier()
    es_mix.close()

    es2 = ExitStack()
    with es2:
        s = es2
        A1a = sb(s, "A1a", [128, NT, 32], F32)
        A2a = sb(s, "A2a", [128, NT, 32], F32)
        rka = sb(s, "rka", [128, NT, 32], F32)
        gta = sb(s, "gta", [128, NT, 2], F32)
        base = sb(s, "base", [128, 32], F32)
        d1i = sb(s, "d1i", [128, NT], I32)
        d2i = sb(s, "d2i", [128, NT], I32)
        bexp = sb(s, "bexp", [128, NBLK], I32)
        idxw = sb(s, "idxw", [128, NBLK], I32)
        B_rt = kb.buf()
        B_out = kb.bufs(NT)
        B_hn = kb.bufs(NT)
        with ExitStack() as sB:
            wo = sb(sB, "wo", [128, 8, 1024], BF16)
            gff = sb(sB, "gff", [128, 1024], F32)
            wr = sb(sB, "wr", [128, 8, 36], F32)
            brt = sb(sB, "brt", [128, 36], F32)
            ustr = sb(sB, "ustr", [128, 128], F32)
            B_w = kb.buf()
            for k2 in range(2):
                kb.dma("pool", "mw", lambda q: q.dma_start(out=wo[:, :, k2 * 512:(k2 + 1) * 512],
                       in_=w_out[:, k2 * 512:(k2 + 1) * 512].rearrange("(c p) f -> p c f", p=128)), w=[B_w])
            kb.dma("sp", "const", lambda q: q.dma_start(out=gff[:], in_=g_ffn.partition_broadcast(128)), w=[B_w])
            kb.dma("sp", "const", lambda q: q.dma_start(out=wr[:], in_=wr_d.rearrange("(c p) f -> p c f", p=128)), w=[B_w])
            kb.dma("sp", "const", lambda q: q.dma_start(out=brt[:], in_=b_rt.partition_broadcast(128)), w=[B_w])
            kb.dma("sp", "const", lambda q: q.dma_start(out=ustr[:], in_=cd["ustrict"]), w=[B_w])
            kb.op("pool", lambda e: e.memset(base[:], 0.0), w=[B_rt])
            Px = ps(sB, "bPx", [128, 2, 512], F32)
            PT = ps(sB, "bPT", [128, 8, 128], F32)
            Pr = ps(sB, "bPr", [128, 512], F32)
            B_Px, B_PT, B_Pr = kb.bufs(3)
            xt = [sb(sB, f"bxt{i}", [128, 1024], F32) for i in range(2)]
            x2 = [sb(sB, f"bx2{i}", [128, 1024], F32) for i in range(2)]
            hnf = sb(sB, "hnf", [128, 1024], F32)
            hnb = [sb(sB, f"hnb{i}", [128, 1024], BF16) for i in range(2)]
            hnT = sb(sB, "hnT", [128, 8, 128], F32)
            junk = sb(sB, "bjunk", [128, 512], F32)
            st = sb(sB, "bst", [128, 8], F32)
            sd_ = sb(sB, "bsd", [128, 16], F32)
            lg = sb(sB, "lg", [128, 36], F32)
            oh = sb(sB, "oh", [128, 4], F32)
            t48 = sb(sB, "t48", [128, 4, 8], F32)
            es8 = sb(sB, "es8", [128, 8], F32)
            e28 = sb(sB, "e28", [128, 8], F32)
            mk1 = sb(sB, "mk1", [128, 8], F32)
            mk2 = sb(sB, "mk2", [128, 8], F32)
            At = sb(sB, "At", [128, 32], F32)
            B_xt, B_x2, B_hnb = kb.bufs(2), kb.bufs(2), kb.bufs(2)
            B_hnf, B_hnT, B_r = kb.buf(), kb.buf(), kb.buf()
            B_rx = kb.buf()
            B_lg = kb.bufs(4)
            lg2 = [sb(sB, f"lg2{i}", [128, 36], F32) for i in range(4)]
            B_r2 = kb.bufs(2)
            sd2 = [sd_, sb(sB, "bsd_b", [128, 16], F32)]
            st2 = [st, sb(sB, "bst_b", [128, 8], F32)]
            oh2 = [oh, sb(sB, "oh_b", [128, 4], F32)]
            t482 = [t48, sb(sB, "t48_b", [128, 4, 8], F32)]
            es82 = [es8, sb(sB, "es8_b", [128, 8], F32)]
            e282 = [e28, sb(sB, "e28_b", [128, 8], F32)]
            mk12 = [mk1, sb(sB, "mk1_b", [128, 8], F32)]
            mk22 = [mk2, sb(sB, "mk2_b", [128, 8], F32)]
            stX = sb(sB, "bstX", [128, 8], F32)
            sdX = sb(sB, "bsdX", [128, 16], F32)
            junkR2 = [sb(sB, f"bjunkR{i}", [128, 8], F32) for i in range(2)]
            Pr2 = ps(sB, "bPr2", [128, 512], F32)
            B_Pr2 = kb.buf()
            hnf2 = [hnf, sb(sB, "hnf_b", [128, 1024], F32)]
            hnT2 = [hnT, sb(sB, "hnT_b", [128, 8, 128], F32)]
            stX2 = [stX, sb(sB, "bstX_b", [128, 8], F32)]
            junk2 = [junk, sb(sB, "bjunk_b", [128, 512], F32)]
            PT2 = [PT, ps(sB, "bPT_b", [128, 8, 128], F32)]
            Pr_2 = [Pr, Pr2]
            B_hnf2, B_hnT2, B_rx2, B_PT2 = [B_hnf, kb.buf()], [B_hnT, kb.buf()], [B_rx, kb.buf()], [B_PT, kb.buf()]
            B_Px2 = [B_Px, kb.buf()]
            B_Pr_2 = [B_Pr, B_Pr2]
            ingroup = [None]

            def gb():
                ingroup[0] = []

            def ge():
                g_, ingroup[0] = ingroup[0], None
                return g_

            def stageX(i):
                j = i % 2
                lgi = lg2[i % 4]
                hnf, hnT, stX, junk, PT, Pr = hnf2[j], hnT2[j], stX2[j], junk2[j], PT2[j], Pr_2[j]
                B_hnf, B_hnT, B_rx, B_PT, B_Pr, B_Px = B_hnf2[j], B_hnT2[j], B_rx2[j], B_PT2[j], B_Pr_2[j], B_Px2[j]
                rows = slice(i * 128, (i + 1) * 128)
                half = fc = hs = c = None
                kb.dma("sp", f"bxt{j}", lambda q: q.dma_start(out=xt[j][:], in_=x_d[rows, :]), w=[B_xt[j]])
                for half in range(2):
                    hs = slice(half * 512, (half + 1) * 512)
                    gb()
                    for fc in range(8):
                        kb.op("pe", lambda e, half=half, fc=fc, hs=hs, c=c: e.matmul(Px[:, j, :], lhsT=hT[:, fc, rows], rhs=wo[:, fc, half * 512:(half + 1) * 512],
                                                       start=(fc == 0), stop=(fc == 7)), r=[B_hT[i], B_w], w=[B_Px])
                    grp_done(ge())
                    kb.op("dve", lambda e, half=half, fc=fc, hs=hs, c=c: e.tensor_tensor(out=x2[j][:, hs], in0=xt[j][:, hs], in1=Px[:, j, :], op=ALU.add),
                          r=[B_xt[j], B_Px], w=[B_x2[j]])
                kb.dma("sp", "ost", lambda q: q.dma_start(out=out_d[rows, :], in_=x2[j][:]), r=[B_x2[j]], w=[B_out[i]])
                for half in range(2):
                    hs = slice(half * 512, (half + 1) * 512)
                    kb.op("act", lambda e, half=half, fc=fc, hs=hs, c=c: e.activation(out=junk[:], in_=x2[j][:, hs], func=AF.Square, accum_out=stX[:, half:half + 1]),
                          r=[B_x2[j]], w=[B_rx])
                kb.op("act", lambda e, half=half, fc=fc, hs=hs, c=c: e.activation(out=stX[:, 2:3], in_=stX[:, 0:1], func=AF.Identity, bias=stX[:, 1:2]), r=[B_rx], w=[B_rx])
                kb.op("act", lambda e, half=half, fc=fc, hs=hs, c=c: e.activation(out=stX[:, 3:4], in_=stX[:, 2:3], func=AF.Ln, scale=1.0 / D, bias=EPS), r=[B_rx], w=[B_rx])
                kb.op("act", lambda e, half=half, fc=fc, hs=hs, c=c: e.activation(out=stX[:, 6:7], in_=stX[:, 3:4], func=AF.Exp, scale=-0.5), r=[B_rx], w=[B_rx])
                for half in range(2):
                    hs = slice(half * 512, (half + 1) * 512)
                    kb.op("dve", lambda e, half=half, fc=fc, hs=hs, c=c: e.scalar_tensor_tensor(out=hnf[:, hs], in0=x2[j][:, hs], scalar=stX[:, 6:7], in1=gff[:, hs],
                                                                  op0=ALU.mult, op1=ALU.mult), r=[B_x2[j], B_rx, B_w], w=[B_hnf])
                    kb.op("pool", lambda e, half=half, fc=fc, hs=hs, c=c: e.tensor_copy(out=hnb[j][:, hs], in_=hnf[:, hs]), r=[B_hnf], w=[B_hnb[j]])
                kb.dma("sp", "hnst", lambda q: q.dma_start(out=hn_d[rows, :], in_=hnb[j][:]), r=[B_hnb[j]], w=[B_hn[i]])
                for c in range(8):
                    kb.op("pe", lambda e, half=half, fc=fc, hs=hs, c=c: e.transpose(out=PT[:, c, :], in_=hnf[:, c * 128:(c + 1) * 128], identity=ident_f[:]),
                          r=[B_hnf, B_const], w=[B_PT])
                for half in range(2):
                    kb.op("act", lambda e, half=half, fc=fc, hs=hs, c=c: e.copy(out=hnT[:, half * 4:half * 4 + 4, :], in_=PT[:, half * 4:half * 4 + 4, :]), r=[B_PT], w=[B_hnT])
                gb()
                for c in range(8):
                    kb.op("pe", lambda e, half=half, fc=fc, hs=hs, c=c: e.matmul(Pr[:, 0:36], lhsT=hnT[:, c, :], rhs=wr[:, c, :], start=(c == 0), stop=(c == 7)),
                          r=[B_hnT, B_w], w=[B_Pr])
                grp_done(ge())
                kb.op("dve", lambda e, half=half, fc=fc, hs=hs, c=c: e.tensor_tensor(out=lgi[:], in0=Pr[:, 0:36], in1=brt[:], op=ALU.add), r=[B_Pr, B_w], w=[B_lg[i % 4]])

            def stageR(i):
                lgi = lg2[i % 4]
                q = i % 2
                sd_, st, oh, t48, es8, e28, mk1, mk2, junkR = sd2[q], st2[q], oh2[q], t482[q], es82[q], e282[q], mk12[q], mk22[q], junkR2[q]
                R_ = [B_r2[q]]
                RL = [B_r2[q], B_lg[i % 4]]
                kb.op("dve", lambda e: e.tensor_reduce(out=sd_[:, 1:2], in_=lgi[:, 0:4], axis=AX.X, op=ALU.max), r=RL, w=R_)
                kb.op("dve", lambda e: e.tensor_scalar(out=sd_[:, 2:3], in0=sd_[:, 1:2], scalar1=-1.0, scalar2=None, op0=ALU.mult), r=R_, w=R_)
                kb.op("act", lambda e: e.activation(out=junkR[:, 0:4], in_=lgi[:, 0:4], func=AF.Exp, bias=sd_[:, 2:3], accum_out=st[:, 4:5]), r=RL, w=R_)
                kb.op("dve", lambda e: e.reciprocal(out=sd_[:, 3:4], in_=st[:, 4:5]), r=R_, w=R_)
                kb.op("dve", lambda e: e.tensor_scalar(out=oh[:], in0=lgi[:, 0:4], scalar1=sd_[:, 1:2], scalar2=None, op0=ALU.is_equal), r=RL, w=R_)
                kb.op("dve", lambda e: e.tensor_tensor(out=t48[:], in0=lgi[:, 4:36].rearrange("p (g j) -> p g j", g=4),
                                                       in1=oh[:].unsqueeze(2).to_broadcast([128, 4, 8]), op=ALU.mult), r=RL, w=R_)
                kb.op("dve", lambda e: e.tensor_reduce(out=es8[:], in_=t48[:].rearrange("p g j -> p j g"), axis=AX.X, op=ALU.add), r=R_, w=R_)
                kb.op("dve", lambda e: e.tensor_reduce(out=sd_[:, 4:5], in_=es8[:], axis=AX.X, op=ALU.max), r=R_, w=R_)
                kb.op("dve", lambda e: e.tensor_scalar(out=mk1[:], in0=es8[:], scalar1=sd_[:, 4:5], scalar2=None, op0=ALU.is_equal), r=R_, w=R_)
                kb.op("dve", lambda e: e.scalar_tensor_tensor(out=e28[:], in0=mk1[:], scalar=-1e30, in1=es8[:], op0=ALU.mult, op1=ALU.add), r=R_, w=R_)
                kb.op("dve", lambda e: e.tensor_reduce(out=sd_[:, 5:6], in_=e28[:], axis=AX.X, op=ALU.max), r=R_, w=R_)
                kb.op("dve", lambda e: e.tensor_scalar(out=mk2[:], in0=e28[:], scalar1=sd_[:, 5:6], scalar2=None, op0=ALU.is_equal), r=R_, w=R_)
                kb.op("dve", lambda e: e.tensor_tensor(out=sd_[:, 6:7], in0=sd_[:, 5:6], in1=sd_[:, 4:5], op=ALU.subtract), r=R_, w=R_)
                kb.op("act", lambda e: e.activation(out=st[:, 5:6], in_=sd_[:, 6:7], func=AF.Exp), r=R_, w=R_)
                kb.op("dve", lambda e: e.tensor_scalar(out=sd_[:, 7:8], in0=st[:, 5:6], scalar1=1.0, scalar2=None, op0=ALU.add), r=R_, w=R_)
                kb.op("dve", lambda e: e.reciprocal(out=sd_[:, 8:9], in_=sd_[:, 7:8]), r=R_, w=R_)
                kb.op("dve", lambda e: e.tensor_tensor(out=gta[:, i, 0:1], in0=sd_[:, 3:4], in1=sd_[:, 8:9], op=ALU.mult), r=R_, w=R_ + [B_rt])
                kb.op("dve", lambda e: e.tensor_tensor(out=gta[:, i, 1:2], in0=gta[:, i, 0:1], in1=st[:, 5:6], op=ALU.mult), r=R_, w=R_ + [B_rt])
                kb.op("dve", lambda e: e.tensor_tensor(out=A1a[:, i, :].rearrange("p (g j) -> p g j", g=4),
                                                       in0=oh[:].unsqueeze(2).to_broadcast([128, 4, 8]),
                                                       in1=mk1[:].unsqueeze(1).to_broadcast([128, 4, 8]), op=ALU.mult), r=R_, w=R_ + [B_rt])
                kb.op("dve", lambda e: e.tensor_tensor(out=A2a[:, i, :].rearrange("p (g j) -> p g j", g=4),
                                                       in0=oh[:].unsqueeze(2).to_broadcast([128, 4, 8]),
                                                       in1=mk2[:].unsqueeze(1).to_broadcast([128, 4, 8]), op=ALU.mult), r=R_, w=R_ + [B_rt])
            cur_rec = [None]

            def grp_done(g_):
                cur_rec[0].append(("grp", g_))

            def record(fn_stage, i):
                rec = []
                cur_rec[0] = rec

                def rop(e, fn, r=(), w=(), same=None):
                    (ingroup[0] if ingroup[0] is not None else rec).append((e, fn, list(r), list(w), same))
                kb.op = rop
                kb.dma = lambda q, sname, fn, r=(), w=(): rec.append(("dma", q, sname, fn, list(r), list(w)))
                try:
                    fn_stage(i)
                finally:
                    del kb.op
                    del kb.dma
                return rec

            def replay(t):
                if t[0] == "grp":
                    for t2 in t[1]:
                        kb.op(*t2)
                elif t[0] == "dma":
                    kb.dma(*t[1:])
                else:
                    kb.op(*t)

            def emit_x_pair(i2a):
                xa = record(stageX, i2a) if i2a < NT else []
                xb_ = record(stageX, i2a + 1) if i2a + 1 < NT else []
                for k_ in range(max(len(xa), len(xb_))):
                    if k_ < len(xa):
                        replay(xa[k_])
                    if k_ < len(xb_):
                        replay(xb_[k_])

            emit_x_pair(0)
            for i in range(0, NT, 2):
                emit_x_pair(i + 2)
                ra = record(stageR, i)
                rb = record(stageR, i + 1) if i + 1 < NT else []
                for k_ in range(max(len(ra), len(rb))):
                    if k_ < len(ra):
                        replay(ra[k_])
                    if k_ < len(rb):
                        replay(rb[k_])
            Ata = sb(sB, "Ata", [128, NT, 32], F32)
            B_Ata = kb.buf()
            for i0_ in range(0, NT, 16):
                n_ = min(16, NT - i0_)
                kb.op("dve", lambda e: e.tensor_tensor(out=Ata[:, i0_:i0_ + n_, :], in0=A1a[:, i0_:i0_ + n_, :], in1=A2a[:, i0_:i0_ + n_, :], op=ALU.add),
                      r=[B_rt], w=[B_Ata])
            Apre = sb(sB, "Apre", [128, NT, 32], F32)
            kb.op("pool", lambda e: e.memset(Apre[:, 0, :], 0.0), w=[B_Ata])
            for i in range(1, NT):
                kb.op("dve", lambda e: e.tensor_tensor(out=Apre[:, i, :], in0=Apre[:, i - 1, :], in1=Ata[:, i - 1, :], op=ALU.add),
                      r=[B_Ata], w=[B_Ata])
            for i in range(NT):
                bank, col = i // 16, (i % 16) * 32
                kb.op("pe", lambda e: e.matmul(Px[:, bank, col:col + 32], lhsT=ustr[:], rhs=Ata[:, i, :], start=True, stop=False),
                      r=[B_Ata, B_w], w=[B_Px2[bank]])
                kb.op("pe", lambda e: e.matmul(Px[:, bank, col:col + 32], lhsT=ones_f[:], rhs=Apre[:, i, :], start=False, stop=True),
                      r=[B_Ata, B_const], w=[B_Px2[bank]])
            for i in range(NT):
                kb.op("pe", lambda e: e.matmul(Pr2[:, 0:32], lhsT=ones_f[:], rhs=Ata[:, i, :], start=(i == 0), stop=(i == NT - 1)),
                      r=[B_Ata, B_const], w=[B_Pr2])
            for i0_ in range(0, NT, 16):
                n_ = min(16, NT - i0_)
                kb.op("dve", lambda e: e.tensor_copy(out=rka[:, i0_:i0_ + n_, :], in_=Px[:, i0_ // 16, 0:n_ * 32].rearrange("p (i e) -> p i e", e=32)),
                      r=[B_Px2[i0_ // 16]], w=[B_rt])
            kb.op("dve", lambda e: e.tensor_copy(out=base[:], in_=Pr2[:, 0:32]), r=[B_Pr2], w=[B_rt])
            kb.barrier()
        with ExitStack() as sC:
            thr = sb(sC, "thr", [128, NBLK, 32], F32)
            cmp_ = sb(sC, "cmp", [128, 16, 32], F32)
            nbk = sb(sC, "nbk", [128, 32], F32)
            tmp32 = sb(sC, "tmp32", [128, 32], F32)
            pend = sb(sC, "pend", [128, 32], F32)
            pst = sb(sC, "pst", [128, 32], F32)
            zer = sb(sC, "zer", [128, 32], F32)
            bef = sb(sC, "bef", [128, NBLK], F32)
            dst = sb(sC, "dst", [128, NT, 32], F32)
            prod = sb(sC, "prod", [128, 16, 32], F32)
            d1f = sb(sC, "d1f", [128, NT], F32)
            d2f = sb(sC, "d2f", [128, NT], F32)
            Q = [B_rt]
            kb.dma("sp", "const", lambda q: q.dma_start(out=thr[:].rearrange("p b e -> p (b e)"), in_=cd["blkthr"].partition_broadcast(128)), w=Q)
            kb.op("pool", lambda e: e.memset(nbk[:], 0.0), w=Q)
            kb.op("pool", lambda e: e.memset(zer[:], 0.0), w=Q)
            for b0 in range(0, NBLK, 16):
                nb_ = min(16, NBLK - b0)
                kb.op("dve", lambda e: e.tensor_tensor(out=cmp_[:, 0:nb_, :], in0=base[:].unsqueeze(1).to_broadcast([128, nb_, 32]),
                                                       in1=thr[:, b0:b0 + nb_, :], op=ALU.is_gt), r=Q, w=Q)
                kb.op("dve", lambda e: e.tensor_reduce(out=tmp32[:], in_=cmp_[:, 0:nb_, :].rearrange("p b e -> p e b"), axis=AX.X, op=ALU.add), r=Q, w=Q)
                kb.op("dve", lambda e: e.tensor_tensor(out=nbk[:], in0=nbk[:], in1=tmp32[:], op=ALU.add), r=Q, w=Q)
            kb.op("dve", lambda e: e.tensor_scalar(out=nbk[:], in0=nbk[:], scalar1=float(BS), scalar2=None, op0=ALU.mult), r=Q, w=Q)
            kb.op("dve", lambda e: e.tensor_tensor_scan(out=pend[:], data0=nbk[:], data1=zer[:], initial=0.0, op0=ALU.add, op1=ALU.add), r=Q, w=Q)
            kb.op("dve", lambda e: e.tensor_tensor(out=pst[:], in0=pend[:], in1=nbk[:], op=ALU.subtract), r=Q, w=Q)
            for b0 in range(0, NBLK, 16):
                nb_ = min(16, NBLK - b0)
                kb.op("dve", lambda e: e.tensor_tensor(out=cmp_[:, 0:nb_, :], in0=pend[:].unsqueeze(1).to_broadcast([128, nb_, 32]),
                                                       in1=thr[:, b0:b0 + nb_, :], op=ALU.is_le), r=Q, w=Q)
                kb.op("dve", lambda e: e.tensor_reduce(out=bef[:, b0:b0 + nb_], in_=cmp_[:, 0:nb_, :], axis=AX.X, op=ALU.add), r=Q, w=Q)
            kb.op("dve", lambda e: e.tensor_scalar(out=bef[:], in0=bef[:], scalar1=float(NE - 1), scalar2=None, op0=ALU.min), r=Q, w=Q)
            kb.op("dve", lambda e: e.tensor_copy(out=bexp[:], in_=bef[:]), r=Q, w=Q)
            iot = sb(sC, "iot", [128, 8], F32)
            idf = sb(sC, "idf", [128, NBLK], F32)
            kb.dma("sp", "const", lambda q: q.dma_start(out=iot[:], in_=cd["iota_pc"]), w=Q)
            kb.op("dve", lambda e: e.scalar_tensor_tensor(out=idf[:], in0=bef[:], scalar=128.0, in1=iot[:, 0:1].to_broadcast([128, NBLK]),
                                                          op0=ALU.mult, op1=ALU.add), r=Q, w=Q)
            kb.op("dve", lambda e: e.tensor_copy(out=idxw[:], in_=idf[:]), r=Q, w=Q)
            for i0_ in range(0, NT, 16):
                n_ = min(16, NT - i0_)
                kb.op("dve", lambda e: e.tensor_tensor(out=dst[:, i0_:i0_ + n_, :], in0=rka[:, i0_:i0_ + n_, :],
                                                       in1=pst[:].unsqueeze(1).to_broadcast([128, n_, 32]), op=ALU.add), r=Q, w=Q)
                for Aa, df in ((A1a, d1f), (A2a, d2f)):
                    kb.op("dve", lambda e: e.tensor_tensor(out=prod[:, 0:n_, :], in0=dst[:, i0_:i0_ + n_, :], in1=Aa[:, i0_:i0_ + n_, :], op=ALU.mult), r=Q, w=Q)
                    kb.op("dve", lambda e: e.tensor_reduce(out=df[:, i0_:i0_ + n_], in_=prod[:, 0:n_, :], axis=AX.X, op=ALU.add), r=Q, w=Q)
            kb.op("dve", lambda e: e.tensor_copy(out=d1i[:], in_=d1f[:]), r=Q, w=Q)
            kb.op("dve", lambda e: e.tensor_copy(out=d2i[:], in_=d2f[:]), r=Q, w=Q)
            kb.barrier()
        if "d1i" in dbg_d:
            kb.dma("sp", "dbg", lambda q: q.dma_start(out=dbg_d["d1i"], in_=d1i[:]), r=[B_rt])
            kb.dma("sp", "dbg", lambda q: q.dma_start(out=dbg_d["d2i"], in_=d2i[:]), r=[B_rt])
            kb.dma("sp", "dbg", lambda q: q.dma_start(out=dbg_d["bexp"], in_=bexp[:]), r=[B_rt])
            kb.dma("sp", "dbg", lambda q: q.dma_start(out=dbg_d["gta"], in_=gta[:]), r=[B_rt])
        if stage < 5:
            return
        with ExitStack() as sD:
            hb_ = [sb(sD, f"dhb{i}", [128, 1024], BF16) for i in range(4)]
            B_hb = kb.bufs(4)
            for i in range(NT):
                j = i % 4
                kb.dma("sp", f"dhb{j}", lambda q: q.dma_start(out=hb_[j][:], in_=hn_d[i * 128:(i + 1) * 128, :]), r=[B_hn[i]], w=[B_hb[j]])
                for di in (d1i, d2i):
                    kb.dma("pool", "disp", lambda q: q.indirect_dma_start(out=xs_d[:, :], out_offset=bass.IndirectOffsetOnAxis(ap=di[:, i:i + 1], axis=0),
                                                                           in_=hb_[j][:], in_offset=None), r=[B_hb[j], B_rt], w=[B_xs])
            kb.barrier()
        B_ys = kb.buf()
        with ExitStack() as sE:
            NWB = 3
            W1 = [sb(sE, f"eW1{i}", [128, 8, 512], BF16) for i in range(NWB)]
            W3 = [sb(sE, f"eW3{i}", [128, 8, 512], BF16) for i in range(NWB)]
            W2 = [sb(sE, f"eW2{i}", [128, 4, 1024], BF16) for i in range(NWB)]
            B_W = kb.bufs(NWB)
            xb = [sb(sE, f"exb{i}", [128, 1024], BF16) for i in range(2)]
            B_xb = kb.bufs(2)
            xT = [sb(sE, f"exT{i}", [128, 8, 128], BF16) for i in range(2)]
            sl = [sb(sE, f"esl{i}", [128, 512], F32) for i in range(2)]
            hid = [sb(sE, f"ehid{i}", [128, 512], BF16) for i in range(2)]
            hidT = [sb(sE, f"ehidT{i}", [128, 4, 128], BF16) for i in range(2)]
            ysb = [sb(sE, f"eys{i}", [128, 1024], F32) for i in range(2)]
            B_ysb = kb.bufs(2)
            B_xT, B_sl, B_hid, B_hidT = kb.bufs(2), kb.bufs(2), kb.bufs(2), kb.bufs(2)
            PTb = [ps(sE, f"ePT{i}", [128, 8, 128], BF16) for i in range(2)]
            Ph1 = [ps(sE, f"ePh1{i}", [128, 512], F32) for i in range(2)]
            Ph3 = [ps(sE, f"ePh3{i}", [128, 512], F32) for i in range(2)]
            Py = ps(sE, "ePy", [128, 2, 512], F32)
            B_PTb, B_Ph1, B_Ph3 = kb.bufs(2), kb.bufs(2), kb.bufs(2)
            B_Py = kb.buf()
            w1r, w3r, w2r = w1b_d, w3b_d, w2b_d
            B_xT2 = [kb.bufs(2), kb.bufs(2)]
            B_ysb2 = [kb.bufs(2), kb.bufs(2)]
            NB128 = NBLK * NSUB

            def load_w(sbi):
                k = sbi % NWB
                for Wt_, wr_ in ((W1, w1r), (W3, w3r), (W2, w2r)):
                    kb.dma("pool", f"ew{k}", lambda q: q.indirect_dma_start(out=Wt_[k][:].rearrange("p c f -> p (c f)"), out_offset=None, in_=wr_[:, :],
                           in_offset=bass.IndirectOffsetOnAxis(ap=idxw[:, sbi:sbi + 1], axis=0)), r=[B_rt, B_wconv], w=[B_W[k]])

            def load_x(b):
                j = b % 2
                kb.dma("sp", f"exb{j}", lambda q: q.dma_start(out=xb[j][:], in_=xs_d[b * 128:(b + 1) * 128, :]), r=[B_xs], w=[B_xb[j]])

            def stageA(b):
                j = b % 2
                k = (b // NSUB) % NWB
                for c in range(8):
                    kb.op("pe", lambda e: e.transpose(out=PTb[j][:, c, :], in_=xb[j][:].rearrange("s (p c) -> s c p", c=8)[:, c, :], identity=ident_bf[:]),
                          r=[B_xb[j], B_const], w=[B_PTb[j]])
                kb.op("dve", lambda e: e.tensor_copy(out=xT[j][:, 0:4, :], in_=PTb[j][:, 0:4, :]), r=[B_PTb[j]], w=[B_xT2[j][0]])
                kb.op("dve", lambda e: e.tensor_copy(out=xT[j][:, 4:8, :], in_=PTb[j][:, 4:8, :]), r=[B_PTb[j]], w=[B_xT2[j][1]])
                for c in range(8):
                    kb.op("pe", lambda e: e.matmul(Ph1[j][:], lhsT=xT[j][:, c, :], rhs=W1[k][:, c, :], start=(c == 0), stop=(c == 7)),
                          r=[B_xT2[j][c // 4], B_W[k]], w=[B_Ph1[j]])
                for c in range(8):
                    kb.op("pe", lambda e: e.matmul(Ph3[j][:], lhsT=xT[j][:, c, :], rhs=W3[k][:, c, :], start=(c == 0), stop=(c == 7)),
                          r=[B_xT2[j][c // 4], B_W[k]], w=[B_Ph3[j]])
                kb.op("act", lambda e: e.activation(out=sl[j][:], in_=Ph1[j][:], func=AF.Silu), r=[B_Ph1[j]], w=[B_sl[j]])
                kb.op("dve", lambda e: e.tensor_tensor(out=hid[j][:], in0=sl[j][:], in1=Ph3[j][:], op=ALU.mult), r=[B_sl[j], B_Ph3[j]], w=[B_hid[j]])

            def stageB(b):
                j = b % 2
                k = (b // NSUB) % NWB
                for c in range(4):
                    kb.op("pe", lambda e: e.transpose(out=PTb[j][:, c, :], in_=hid[j][:].rearrange("s (p c) -> s c p", c=4)[:, c, :], identity=ident_bf[:]),
                          r=[B_hid[j], B_const], w=[B_PTb[j]])
                kb.op("act", lambda e: e.copy(out=hidT[j][:], in_=PTb[j][:, 0:4, :]), r=[B_PTb[j]], w=[B_hidT[j]])
                for half in range(2):
                    for c in range(4):
                        kb.op("pe", lambda e: e.matmul(Py[:, half, :], lhsT=hidT[j][:, c, :], rhs=W2[k][:, c, half * 512:(half + 1) * 512],
                                                       start=(c == 0), stop=(c == 3)), r=[B_hidT[j], B_W[k]], w=[B_Py])
                kb.op("act", lambda e: e.copy(out=ysb[j][:, 0:512], in_=Py[:, 0, :]), r=[B_Py], w=[B_ysb2[j][0]])
                kb.op("dve", lambda e: e.tensor_copy(out=ysb[j][:, 512:1024], in_=Py[:, 1, :]), r=[B_Py], w=[B_ysb2[j][1]])
                kb.dma("sp", "yst", lambda q: q.dma_start(out=ys_d[b * 128:(b + 1) * 128, :], in_=ysb[j][:]), r=B_ysb2[j], w=[B_ys])

            for s0 in range(min(NWB, NBLK)):
                load_w(s0)
            load_x(0)
            for b in range(NB128 + 1):
                if b + 1 < NB128:
                    load_x(b + 1)
                if b < NB128:
                    stageA(b)
                if b >= 1:
                    stageB(b - 1)
                    if (b - 1) % NSUB == NSUB - 1:
                        nxt = (b - 1) // NSUB + NWB
                        if nxt < NBLK:
                            load_w(nxt)
            kb.barrier()
        with ExitStack() as sF:
            NCB = 4
            y1 = [sb(sF, f"cy1{i}", [128, 1024], F32) for i in range(NCB)]
            y2 = [sb(sF, f"cy2{i}", [128, 1024], F32) for i in range(NCB)]
            xr = [sb(sF, f"cxr{i}", [128, 1024], F32) for i in range(NCB)]
            B_y1, B_y2, B_xr = kb.bufs(NCB), kb.bufs(NCB), kb.bufs(NCB)

            def cload(i):
                j = i % NCB
                rows = slice(i * 128, (i + 1) * 128)
                kb.dma("pool", f"cg1{j}", lambda q: q.indirect_dma_start(out=y1[j][:], out_offset=None, in_=ys_d[:, :],
                       in_offset=bass.IndirectOffsetOnAxis(ap=d1i[:, i:i + 1], axis=0)), r=[B_ys, B_rt], w=[B_y1[j]])
                kb.dma("pool", f"cg2{j}", lambda q: q.indirect_dma_start(out=y2[j][:], out_offset=None, in_=ys_d[:, :],
                       in_offset=bass.IndirectOffsetOnAxis(ap=d2i[:, i:i + 1], axis=0)), r=[B_ys, B_rt], w=[B_y2[j]])
                kb.dma("act", f"cxr{j}", lambda q: q.dma_start(out=xr[j][:], in_=out_d[rows, :]), r=[B_out[i]], w=[B_xr[j]])

            for i in range(min(NCB - 1, NT)):
                cload(i)
            for i in range(NT):
                j = i % NCB
                rows = slice(i * 128, (i + 1) * 128)
                if i + NCB - 1 < NT:
                    cload(i + NCB - 1)
                for half in range(2):
                    hs = slice(half * 512, (half + 1) * 512)
                    kb.op("dve", lambda e: e.scalar_tensor_tensor(out=xr[j][:, hs], in0=y1[j][:, hs], scalar=gta[:, i, 0:1], in1=xr[j][:, hs],
                                                                  op0=ALU.mult, op1=ALU.add), r=[B_y1[j], B_rt], w=[B_xr[j]])
                    kb.op("dve", lambda e: e.scalar_tensor_tensor(out=xr[j][:, hs], in0=y2[j][:, hs], scalar=gta[:, i, 1:2], in1=xr[j][:, hs],
                                                                  op0=ALU.mult, op1=ALU.add), r=[B_y2[j], B_rt], w=[B_xr[j]])
                kb.dma("sp", "ost2", lambda q: q.dma_start(out=out_d[rows, :], in_=xr[j][:]), r=[B_xr[j]], w=[B_out[i]])
            kb.barrier()


def _shared_maps(inp, consts):
    f = np.float32
    g = lambda k: np.ascontiguousarray(np.asarray(inp[k], dtype=f)[0])
    m = {}
    m["w_in"] = g("w_in")
    m["w_branch_da"] = g("w_branch_da")
    m["w_branch_ml"] = g("w_branch_ml")
    m["w_gate"] = g("w_gate")
    m["w_out"] = g("w_out")
    m["w1"] = g("w1")
    m["w3"] = g("w3")
    m["w2"] = g("w2")
    m["w_rt"] = np.ascontiguousarray(np.concatenate([g("w_group"), g("w_router")], axis=1))
    m["attn_norm_g"] = g("attn_norm_g")[None, :]
    m["ffn_norm_g"] = g("ffn_norm_g")[None, :]
    m["da_out_norm_g"] = g("da_out_norm_g")[None, :]
    m["ml_out_norm_g"] = g("ml_out_norm_g")[None, :]
    m["b_rt"] = np.concatenate([g("b_group"), g("b_router")])[None, :]
    m["b_if"] = np.concatenate([g("ml_i_bias"), g("ml_f_bias")])[None, :]
    m["lamv"] = np.concatenate([g("da_lambda_q1"), g("da_lambda_k1"), g("da_lambda_q2"), g("da_lambda_k2")])[None, :]
    m["gqk_col"] = np.ascontiguousarray(np.stack([np.tile(g("da_q_norm_g"), 2), np.tile(g("da_k_norm_g"), 2)], axis=1))
    cw = g("ml_conv_w")
    m["cw_col"] = np.ascontiguousarray(cw.reshape(4, 8, 128).transpose(2, 1, 0).reshape(128, 32))
    m["cb_col"] = np.ascontiguousarray(g("ml_conv_b").reshape(8, 128).T)
    m["bg_col"] = np.ascontiguousarray(g("b_gate").reshape(16, 128).T)
    for k, v in consts.items():
        m["c_" + k] = v
    return m


_CACHE = {}


def kernel(**inputs):
    x = np.asarray(inputs["x"], dtype=np.float32)
    B, S, _ = x.shape
    key = (S,)
    if key not in _CACHE:
        _CACHE[key] = build_nc(S)
    nc, consts = _CACHE[key]
    shared = _shared_maps(inputs, consts)
    in_maps = []
    for b in range(B):
        m = dict(shared)
        m["x"] = np.ascontiguousarray(x[b])
        in_maps.append(m)
    res = run_bass_kernel_spmd(nc, in_maps, core_ids=list(range(B)))
    return np.stack([np.asarray(r["out"], dtype=np.float32) for r in res.results], axis=0)
```

```python
import math
from contextlib import ExitStack
import numpy as np
import ml_dtypes
import concourse.bass as bass
import concourse.mybir as mybir
from concourse.bass_utils import run_bass_kernel_spmd

F32 = mybir.dt.float32
BF16 = mybir.dt.bfloat16
I32 = mybir.dt.int32
AF = mybir.ActivationFunctionType
ALU = mybir.AluOpType
AX = mybir.AxisListType

D = 1024
NH = 4
IN_W = 3592
OFF_DA_Q, OFF_DA_K, OFF_DA_V = 0, 512, 1024
OFF_ML_QK, OFF_ML_V, OFF_ML_O, OFF_ML_I = 1536, 2560, 3072, 3584
NE = 32
EPS = 1e-6
LAM_INIT = 0.8 - 0.6 * math.exp(-0.3 * 0)
SLOPES = [2.0 ** (-8.0 * (i + 1) / NH) for i in range(NH)]
SKIP_T = 60.0
NOFF = 36
OFF0 = 31
ML_LOOKAHEAD = 3
BS = 256


class Buf:
    __slots__ = ("w", "r", "name")

    def __init__(self, name=""):
        self.w = None
        self.r = {}
        self.name = name


class KB:
    def __init__(self, nc, es):
        self.nc = nc
        self.es = es
        self.eng = {"pe": nc.tensor, "act": nc.scalar, "dve": nc.vector, "pool": nc.gpsimd, "sp": nc.sync}
        self.sem = {k: es.enter_context(nc.semaphore("sem_" + k)) for k in self.eng}
        self.cnt = {k: 0 for k in self.eng}
        self.seen = {k: {} for k in self.eng}
        self.dsem = {}
        self.dcnt = {}
        self.nbuf = 0

    def buf(self, name=""):
        self.nbuf += 1
        return Buf(name)

    def bufs(self, n, name=""):
        return [self.buf(name + str(i)) for i in range(n)]

    def dma_sem(self, name):
        if name not in self.dsem:
            self.dsem[name] = self.es.enter_context(self.nc.semaphore("dsem_" + name))
            self.dcnt[name] = 0
        return name

    def _semh(self, key):
        return self.sem[key] if key in self.sem else self.dsem[key]

    def _wait(self, e, reads, writes, same=True, skip=None):
        deps = {}
        for b in reads:
            if b.w is not None:
                k, v = b.w
                deps[k] = max(deps.get(k, 0), v)
        for b in writes:
            if b.w is not None:
                k, v = b.w
                deps[k] = max(deps.get(k, 0), v)
            for k, v in b.r.items():
                deps[k] = max(deps.get(k, 0), v)
        for k, v in deps.items():
            if k == e and not same:
                continue
            if k == skip:
                continue
            if k in self.dcnt:
                v = self.dcnt[k]
            if self.seen[e].get(k, 0) >= v:
                continue
            self.eng[e].wait_ge(self._semh(k), v)
            self.seen[e][k] = v

    def op(self, e, fn, r=(), w=(), same=None):
        if same is None:
            same = (e != "pe")
        self._wait(e, r, w, same)
        inst = fn(self.eng[e])
        self.cnt[e] += 1
        inst.then_inc(self.sem[e], 1)
        ev = (e, self.cnt[e])
        for b in w:
            b.w = ev
            b.r = {}
        for b in r:
            if b not in w:
                b.r[e] = self.cnt[e]
        return inst

    def dma(self, q, sname, fn, r=(), w=()):
        self.dma_sem(sname)
        self._wait(q, r, w, True, skip=sname)
        inst = fn(self.eng[q])
        self.dcnt[sname] += 16
        inst.then_inc(self.dsem[sname], 16)
        ev = (sname, self.dcnt[sname])
        for b in w:
            b.w = ev
            b.r = {}
        for b in r:
            if b not in w:
                b.r[sname] = self.dcnt[sname]
        return inst

    def barrier(self):
        evs = [(k, self.cnt[k]) for k in self.sem if self.cnt[k] > 0] + [(k, v) for k, v in self.dcnt.items() if v > 0]
        for e in self.eng:
            for k, v in evs:
                if k == e:
                    continue
                if self.seen[e].get(k, 0) >= v:
                    continue
                self.eng[e].wait_ge(self._semh(k), v)
                self.seen[e][k] = v

    def wait_all(self, e, bufs):
        self._wait(e, bufs, bufs, True)


def _mk_consts(S):
    c = {}
    bf = ml_dtypes.bfloat16
    c["ident_bf"] = np.eye(128, dtype=np.float32).astype(bf)
    c["ident_f"] = np.eye(128, dtype=np.float32)
    blk = np.zeros((128, 128), np.float32)
    blk[:64, :64] = 1.0 / 64
    blk[64:, 64:] = 1.0 / 64
    c["blk64"] = blk.astype(bf)
    k = np.arange(128)[:, None]
    q = np.arange(128)[None, :]
    c["cmask"] = (k <= q).astype(np.float32).astype(bf)
    c["negm"] = np.where(k <= q, 0.0, -30000.0).astype(np.float32)
    c["ones_f"] = np.ones((128, 128), np.float32)
    c["ustrict"] = (k < q).astype(np.float32)
    tab = np.zeros((128, NH * NOFF), np.float32)
    for h in range(NH):
        for j in range(NOFF):
            tab[:, h * NOFF + j] = SLOPES[h] * (np.arange(128) + 128.0 * (j - OFF0))
    c["atab"] = tab
    c["iota_pc"] = (np.arange(8)[None, :] * 128.0 + np.arange(128)[:, None]).astype(np.float32)
    nb = (2 * S) // BS + NE
    c["blkthr"] = np.tile((np.arange(nb, dtype=np.float32) * float(BS))[None, :, None], (1, 1, NE)).reshape(1, nb * NE)
    return c


CONST_DT = {"ident_bf": BF16, "ident_f": F32, "blk64": BF16, "cmask": BF16, "negm": F32, "ones_f": F32,
            "ustrict": F32, "atab": F32, "blkthr": F32, "iota_pc": F32}


def build_nc(S=4096, stage=99, dbg=None):
    NT = S // 128
    NB5 = S // 512
    CAP = 2 * S + NE * BS
    nc = bass.Bass("TRN2", target_bir_lowering=False)
    consts = _mk_consts(S)

    def din(name, shape, dt=F32):
        return nc.dram_tensor(name, list(shape), dt, kind="ExternalInput").ap()

    x_d = din("x", [S, D])
    w_in = din("w_in", [D, IN_W])
    w_bda = din("w_branch_da", [512, D])
    w_bml = din("w_branch_ml", [512, D])
    w_gate = din("w_gate", [D, 2 * D])
    w_out = din("w_out", [D, D])
    w1 = din("w1", [NE, D, 512])
    w3 = din("w3", [NE, D, 512])
    w2 = din("w2", [NE, 512, D])
    wr_d = din("w_rt", [D, 36])
    g_attn = din("attn_norm_g", [1, D])
    g_ffn = din("ffn_norm_g", [1, D])
    g_dao = din("da_out_norm_g", [1, 128])
    g_mlo = din("ml_out_norm_g", [1, 512])
    b_rt = din("b_rt", [1, 36])
    b_if = din("b_if", [1, 8])
    lamv = din("lamv", [1, 256])
    gqk_col = din("gqk_col", [128, 2])
    cw_col = din("cw_col", [128, 8 * 4])
    cb_col = din("cb_col", [128, 8])
    bg_col = din("bg_col", [128, 16])
    cd = {k: din("c_" + k, list(v.shape), CONST_DT[k]) for k, v in consts.items()}
    out_d = nc.dram_tensor("out", [S, D], F32, kind="ExternalOutput").ap()
    dbg_d = {}
    if dbg:
        for k, (shape, dt) in dbg.items():
            dbg_d[k] = nc.dram_tensor("dbg_" + k, list(shape), dt, kind="ExternalOutput").ap()
    xs_d = nc.dram_tensor("xs_scr", [CAP, D], BF16).ap()
    ys_d = nc.dram_tensor("ys_scr", [CAP, D], F32).ap()
    hn_d = nc.dram_tensor("hn_scr", [S, D], BF16).ap()
    w1b_d = nc.dram_tensor("w1b_scr", [NE * 128, 4096], BF16).ap()
    w3b_d = nc.dram_tensor("w3b_scr", [NE * 128, 4096], BF16).ap()
    w2b_d = nc.dram_tensor("w2b_scr", [NE * 128, 4096], BF16).ap()

    es = ExitStack()
    with es:
        kb = KB(nc, es)

        def sb(stack, name, shape, dt):
            return stack.enter_context(nc.sbuf_tensor(name, list(shape), dt))

        def ps(stack, name, shape, dt=F32):
            return stack.enter_context(nc.psum_tensor(name, list(shape), dt))

        ident_bf = sb(es, "ident_bf", [128, 128], BF16)
        ident_f = sb(es, "ident_f", [128, 128], F32)
        ones_f = sb(es, "ones_f", [128, 128], F32)
        B_const = kb.buf("const")
        for t, k in ((ident_bf, "ident_bf"), (ident_f, "ident_f"), (ones_f, "ones_f")):
            kb.dma("sp", "const", lambda q, t=t, k=k: q.dma_start(out=t[:], in_=cd[k]), w=[B_const])
        zero_bf = sb(es, "zero_bf", [128, 2048], BF16)
        B_zero = kb.buf("zero")
        for zi in range(4):
            kb.op("pool", lambda e: e.memset(zero_bf[:, zi * 512:(zi + 1) * 512], 0.0), w=[B_zero])

        hT = sb(es, "hT", [128, 8, S], BF16)
        B_hT = kb.bufs(NT, "hT")
        es_mix = ExitStack()
        y_daT = sb(es_mix, "y_daT", [128, 4, S], BF16)
        B_ydaT = kb.bufs(NT * NH, "ydaT")
        B_ymlT = kb.bufs(NT * NH, "ymlT")

        B_xs = kb.buf()
        zf_rows = list(range(0, CAP, 256)) if stage >= 4 else []
        zf_per = (len(zf_rows) + NT - 1) // NT
        with ExitStack() as s1:
            gat = sb(s1, "gat", [128, D], F32)
            B_gat = kb.buf()
            kb.dma("sp", "const", lambda q: q.dma_start(out=gat[:], in_=g_attn.partition_broadcast(128)), w=[B_gat])
            xt = [sb(s1, f"xt{i}", [128, D], F32) for i in range(2)]
            xn = [sb(s1, f"xn{i}", [128, D], BF16) for i in range(2)]
            junk = sb(s1, "junk1", [128, D], F32)
            ssq = sb(s1, "ssq", [128, NT], F32)
            rst = sb(s1, "rst", [128, NT], F32)
            rsd = sb(s1, "rsd", [128, NT], F32)
            ssq2 = sb(s1, "ssq2", [128, 2 * NT], F32)
            pT = [ps(s1, f"pT{i}", [128, 8, 128], BF16) for i in range(2)]
            B_xt, B_xn, B_pT = kb.bufs(2), kb.bufs(2), kb.bufs(2)
            B_junk, B_ss = kb.buf(), kb.bufs(NT)
            for i in range(NT):
                j = i % 2
                kb.dma("sp", f"xt{j}", lambda q: q.dma_start(out=xt[j][:], in_=x_d[i * 128:(i + 1) * 128, :]), w=[B_xt[j]])
                for r0 in zf_rows[i * zf_per:(i + 1) * zf_per]:
                    kb.dma("sp", "xsz", lambda q: q.dma_start(out=xs_d[r0:r0 + 256, :].rearrange("(p a) d -> p (a d)", a=2), in_=zero_bf[:, 0:2048]),
                           r=[B_zero], w=[B_xs])
                for hf in range(2):
                    kb.op("act", lambda e: e.activation(out=junk[:, hf * 512:(hf + 1) * 512], in_=xt[j][:, hf * 512:(hf + 1) * 512],
                                                        func=AF.Square, accum_out=ssq2[:, 2 * i + hf:2 * i + hf + 1]),
                          r=[B_xt[j]], w=[B_junk, B_ss[i]])
                kb.op("act", lambda e: e.activation(out=rst[:, i:i + 1], in_=ssq2[:, 2 * i:2 * i + 1], func=AF.Identity,
                                                    bias=ssq2[:, 2 * i + 1:2 * i + 2]), r=[B_ss[i]], w=[B_ss[i]])
                kb.op("act", lambda e: e.activation(out=rst[:, i:i + 1], in_=rst[:, i:i + 1], func=AF.Sqrt, scale=1.0 / D, bias=EPS),
                      r=[B_ss[i]], w=[B_ss[i]])
                kb.op("dve", lambda e: e.reciprocal(out=rsd[:, i:i + 1], in_=rst[:, i:i + 1]), r=[B_ss[i]], w=[B_ss[i]])
                for hf in range(2):
                    kb.op("dve", lambda e: e.scalar_tensor_tensor(out=xn[j][:, hf * 512:(hf + 1) * 512], in0=xt[j][:, hf * 512:(hf + 1) * 512],
                                                                  scalar=rsd[:, i:i + 1], in1=gat[:, hf * 512:(hf + 1) * 512],
                                                                  op0=ALU.mult, op1=ALU.mult),
                          r=[B_xt[j], B_ss[i], B_gat], w=[B_xn[j]])
                for c in range(8):
                    kb.op("pe", lambda e: e.transpose(out=pT[j][:, c, :], in_=xn[j][:, c * 128:(c + 1) * 128], identity=ident_bf[:]),
                          r=[B_xn[j], B_const], w=[B_pT[j]])
                for hf in range(2):
                    kb.op("act", lambda e: e.copy(out=hT[:, hf * 4:hf * 4 + 4, i * 128:(i + 1) * 128], in_=pT[j][:, hf * 4:hf * 4 + 4, :]),
                          r=[B_pT[j]], w=[B_hT[i]])

        kb.barrier()
        if "hT" in dbg_d:
            kb.dma("sp", "dbg", lambda q: q.dma_start(out=dbg_d["hT"], in_=hT[:]), r=B_hT)

        B_wconv = kb.buf()
        conv_state = [0]

        def conv_next(n=1):
            if stage < 5:
                return
            for _ in range(n):
                e_ = conv_state[0]
                if e_ >= NE:
                    return
                conv_state[0] += 1
                for src_, dst_, c_ in ((w1, w1b_d, 8), (w3, w3b_d, 8), (w2, w2b_d, 4)):
                    kb.dma("pool", "wconv", lambda q: q.dma_start(out=dst_[e_ * 128:(e_ + 1) * 128, :],
                           in_=src_[e_].rearrange("(p c) f -> p (c f)", c=c_)), w=[B_wconv])
        if stage >= 2:
            _da_phase(nc, kb, S, hT, B_hT, y_daT, B_ydaT, w_in, cd, gqk_col, g_dao, lamv, ident_bf, zero_bf, B_const, B_zero, sb, ps, dbg_d, conv_next)
        conv_next(NE)
        y_mlT = sb(es_mix, "y_mlT", [128, 4, S], BF16)
        if stage >= 3:
            _ml_phase(nc, kb, S, hT, B_hT, y_mlT, B_ymlT, w_in, cd, cw_col, cb_col, g_mlo, b_if, ident_bf, ident_f, ones_f,
                      B_const, sb, ps, dbg_d)
        if stage >= 4:
            _merge_moe(nc, kb, S, hT, B_hT, y_daT, B_ydaT, y_mlT, B_ymlT, es_mix, x_d, out_d, w_bda, w_bml, w_gate, w_out,
                       bg_col, g_ffn, wr_d, b_rt, w1, w3, w2, xs_d, ys_d, hn_d, cd, ident_bf, ident_f, ones_f, zero_bf,
                       B_const, B_zero, sb, ps, dbg_d, stage, B_xs, (w1b_d, w3b_d, w2b_d, B_wconv))
        else:
            es_mix.close()

        allb = []
        for k in list(kb.dsem.keys()):
            b = Buf()
            b.w = (k, kb.dcnt[k])
            allb.append(b)
        for k in kb.sem:
            if kb.cnt[k] > 0:
                b = Buf()
                b.w = (k, kb.cnt[k])
                allb.append(b)
        kb._wait("sp", allb, [], True)
    return nc, consts


def _da_phase(nc, kb, S, hT, B_hT, y_daT, B_ydaT, w_in, cd, gqk_col, g_dao, lamv, ident_bf, zero_bf, B_const, B_zero, sb, ps, dbg_d, conv_next):
    NT = S // 128
    NB5 = S // 512
    with ExitStack() as s:
        blk64 = sb(s, "blk64", [128, 128], BF16)
        cmask = sb(s, "cmask", [128, 128], BF16)
        atab = sb(s, "atab", [128, NH * NOFF], F32)
        gqk = sb(s, "gqk", [128, 2], F32)
        gdo = sb(s, "gdo", [128, 128], F32)
        lam_t = sb(s, "lam_t", [128, 256], F32)
        B_c = kb.buf()
        for t, src in ((blk64, cd["blk64"]), (cmask, cd["cmask"]), (atab, cd["atab"]), (gqk, gqk_col),
                       (gdo, g_dao.partition_broadcast(128)), (lam_t, lamv.partition_broadcast(128))):
            kb.dma("sp", "const", lambda q, t=t, src=src: q.dma_start(out=t[:], in_=src), w=[B_c])
        lj = sb(s, "lj", [128, 128], F32)
        ls = sb(s, "ls", [128, 4], F32)
        neglam = sb(s, "neglam", [128, 1], F32)
        B_l = kb.buf()
        lv = lam_t[:].rearrange("p (a b d) -> p a b d", a=2, b=2)
        kb.op("dve", lambda e: e.tensor_tensor(out=lj[:].rearrange("p (a d) -> p a d", a=2), in0=lv[:, :, 0, :], in1=lv[:, :, 1, :],
                                               op=ALU.mult), r=[B_c], w=[B_l])
        kb.op("dve", lambda e: e.tensor_reduce(out=ls[:, 0:2], in_=lj[:].rearrange("p (a d) -> p a d", a=2), axis=AX.X, op=ALU.add),
              r=[B_l], w=[B_l])
        kb.op("act", lambda e: e.activation(out=ls[:, 2:4], in_=ls[:, 0:2], func=AF.Exp), r=[B_l], w=[B_l])
        kb.op("dve", lambda e: e.tensor_tensor(out=neglam[:], in0=ls[:, 3:4], in1=ls[:, 2:3], op=ALU.subtract), r=[B_l], w=[B_l])
        kb.op("dve", lambda e: e.tensor_scalar(out=neglam[:], in0=neglam[:], scalar1=-LAM_INIT, scalar2=None, op0=ALU.add),
              r=[B_l], w=[B_l])
        kb.op("dve", lambda e: e.tensor_scalar(out=gdo[:], in0=gdo[:], scalar1=1.0 - LAM_INIT, scalar2=None, op0=ALU.mult),
              r=[B_c], w=[B_c])

        P3 = [ps(s, f"daP{i}", [128, 512], F32) for i in range(3)]
        B_P3 = kb.bufs(3)
        acc4 = ps(s, "daAcc", [128, 4, 512], F32)
        B_acc = kb.buf()
        ptr = ps(s, "daPtr", [128, 8, 128], BF16)
        B_ptr = kb.buf()
        pcnt = [0]

        def nextP():
            i = pcnt[0] % 3
            pcnt[0] += 1
            return P3[i], B_P3[i]

        Vda = sb(s, "Vda", [128, NT, 4, 130], BF16)
        B_V = kb.bufs(NT)
        B_Vones = kb.buf()
        kb.op("pool", lambda e: e.memset(Vda[:, :, :, 128:130], 1.0), w=[B_Vones])
        with ExitStack() as sv:
            wv = sb(sv, "wv", [128, 8, 512], BF16)
            B_wv = kb.buf()
            kb.dma("pool", "wv", lambda q: q.dma_start(out=wv[:], in_=w_in[:, OFF_DA_V:OFF_DA_V + 512].rearrange("(c p) f -> p c f", p=128)),
                   w=[B_wv])
            for i in range(NT):
                P, BP = nextP()
                for c in range(8):
                    kb.op("pe", lambda e: e.matmul(P[:], lhsT=hT[:, c, i * 128:(i + 1) * 128], rhs=wv[:, c, :], start=(c == 0), stop=(c == 7)),
                          r=[B_hT[i], B_wv], w=[BP])
                kb.op("act", lambda e: e.copy(out=Vda[:, i, :, 0:128], in_=P[:].rearrange("p (h d) -> p h d", h=4)),
                      r=[BP, B_Vones], w=[B_V[i]])
        kb.barrier()

        wqk = [sb(s, f"wqk{i}", [128, 8, 256], BF16) for i in range(2)]
        B_wqk = kb.bufs(2)
        qkT = [sb(s, f"qkT{i}", [128, 2, S], BF16) for i in range(2)]
        B_qk = [kb.bufs(NB5 * 2) for _ in range(2)]
        sq_sb = [sb(s, f"sq_sb{i}", [128, 512], BF16) for i in range(2)]
        sd_sb = [sb(s, f"sd_sb{i}", [128, 512], F32) for i in range(2)]
        B_sq, B_sd = kb.bufs(2), kb.bufs(2)
        Et = [sb(s, f"Et{i}", [128, 512], BF16) for i in range(4)]
        B_Et = kb.bufs(4)
        ecnt = 0
        o_sb = sb(s, "o_sb", [128, 4, 128], F32)
        t_sb = sb(s, "t_sb", [128, 4, 128], F32)
        y_sb = sb(s, "y_sb", [128, 4, 128], BF16)
        rr = sb(s, "rr", [128, 16], F32)
        rra = sb(s, "rra", [128, 8], F32)
        B_o = kb.buf()

        def load_w(h):
            hb = h % 2
            kb.dma("pool", f"wqk{hb}", lambda q: q.dma_start(out=wqk[hb][:, :, 0:128],
                   in_=w_in[:, OFF_DA_Q + h * 128:OFF_DA_Q + (h + 1) * 128].rearrange("(c p) f -> p c f", p=128)), w=[B_wqk[hb]])
            kb.dma("pool", f"wqk{hb}", lambda q: q.dma_start(out=wqk[hb][:, :, 128:256],
                   in_=w_in[:, OFF_DA_K + h * 128:OFF_DA_K + (h + 1) * 128].rearrange("(c p) f -> p c f", p=128)), w=[B_wqk[hb]])

        load_w(0)
        for h in range(NH):
            hb = h % 2
            if h + 1 < NH:
                load_w(h + 1)
            slope = SLOPES[h]
            k2 = 0
            for tb in range(NB5):
                for which in range(2):
                    P, BP = nextP()
                    for c in range(8):
                        kb.op("pe", lambda e: e.matmul(P[:], lhsT=wqk[hb][:, c, which * 128:(which + 1) * 128],
                                                       rhs=hT[:, c, tb * 512:(tb + 1) * 512], start=(c == 0), stop=(c == 7)),
                              r=B_hT[tb * 4:tb * 4 + 4] + [B_wqk[hb]], w=[BP])
                    kk = k2 % 2
                    k2 += 1
                    kb.op("act", lambda e: e.activation(out=sq_sb[kk][:], in_=P[:], func=AF.Square), r=[BP], w=[B_sq[kk]])
                    P2, BP2 = nextP()
                    kb.op("pe", lambda e: e.matmul(P2[:], lhsT=blk64[:], rhs=sq_sb[kk][:], start=True, stop=True),
                          r=[B_sq[kk], B_c], w=[BP2])
                    kb.op("act", lambda e: e.activation(out=sd_sb[kk][:], in_=P2[:], func=AF.Ln, bias=EPS), r=[BP2], w=[B_sd[kk]])
                    kb.op("act", lambda e: e.activation(out=sd_sb[kk][:], in_=sd_sb[kk][:], func=AF.Exp, scale=-0.5), r=[B_sd[kk]], w=[B_sd[kk]])
                    kb.op("dve", lambda e: e.scalar_tensor_tensor(out=qkT[hb][:, which, tb * 512:(tb + 1) * 512], in0=P[:],
                                                                  scalar=gqk[:, which:which + 1], in1=sd_sb[kk][:],
                                                                  op0=ALU.mult, op1=ALU.mult),
                          r=[BP, B_sd[kk], B_c], w=[B_qk[hb][tb * 2 + which]])
            if h == 0 and "wqk" in dbg_d:
                kb.dma("sp", "dbg", lambda q: q.dma_start(out=dbg_d["wqk"], in_=wqk[0][:]), r=[B_wqk[0]])
            if h == 0 and "sd" in dbg_d:
                kb.dma("sp", "dbg", lambda q: q.dma_start(out=dbg_d["sd"], in_=sd_sb[0][:]), r=[B_sd[0]])
                kb.dma("sp", "dbg", lambda q: q.dma_start(out=dbg_d["sq"], in_=sq_sb[0][:]), r=[B_sq[0]])
            if h == 0 and "qkT" in dbg_d:
                kb.dma("sp", "dbg", lambda q: q.dma_start(out=dbg_d["qkT"], in_=qkT[0][:]), r=B_qk[0])
            sub = 128 if slope * 511 > 32.0 else 512
            units = []
            for qb in range(NB5):
                kt_max_ = 4 * qb + 3
                kt_min_ = max(0, int(math.ceil((qb * 512 - SKIP_T / slope - 127) / 128.0)))
                for kt in range(kt_min_, kt_max_ + 1):
                    for m in range(2):
                        units.append((qb, kt, m, kt == kt_min_ and m == 0, kt == kt_max_ and m == 1))
            ustate = {}
            pending = []

            def emit_qk(u):
                nonlocal ecnt
                qb, kt, m, _, _ = u
                q0 = qb * 512
                jj = kt - 4 * qb
                j_lo = max(0, jj)
                P, BP = nextP()
                kb.op("pe", lambda e: e.matmul(P[:, j_lo * 128:512], lhsT=qkT[hb][m * 64:(m + 1) * 64, 1, kt * 128:(kt + 1) * 128],
                                               rhs=qkT[hb][m * 64:(m + 1) * 64, 0, q0 + j_lo * 128:q0 + 512], start=True, stop=True),
                      r=[B_qk[hb][(kt // 4) * 2 + 1], B_qk[hb][qb * 2]], w=[BP])
                ei = ecnt % 4
                ecnt += 1
                E, BE = Et[ei], B_Et[ei]
                if sub == 512:
                    col = h * NOFF + (kt - 4 * qb) + OFF0
                    kb.op("act", lambda e: e.activation(out=E[:, j_lo * 128:512], in_=P[:, j_lo * 128:512], func=AF.Exp,
                                                        scale=0.125, bias=atab[:, col:col + 1]), r=[BP, B_c], w=[BE])
                else:
                    for j in range(j_lo, 4):
                        col = h * NOFF + (kt - 4 * qb - j) + OFF0
                        kb.op("act", lambda e: e.activation(out=E[:, j * 128:(j + 1) * 128], in_=P[:, j * 128:(j + 1) * 128],
                                                            func=AF.Exp, scale=0.125, bias=atab[:, col:col + 1]),
                              r=[BP, B_c], w=[BE])
                if jj >= 0:
                    kb.op("pool", lambda e: e.tensor_tensor(out=E[:, jj * 128:(jj + 1) * 128], in0=E[:, jj * 128:(jj + 1) * 128],
                                                            in1=cmask[:], op=ALU.mult), r=[B_c], w=[BE])
                ustate[u] = (E, BE, j_lo)

            def emit_av(u):
                qb, kt, m, first, last = u
                q0 = qb * 512
                E, BE, j_lo = ustate.pop(u)
                if first:
                    for j in range(4):
                        kb.op("pe", lambda e: e.matmul(acc4[:, j, :], lhsT=zero_bf[0:1, 0:128], rhs=zero_bf[0:1, 0:512],
                                                       start=True, stop=True, skip_group_check=True), r=[B_zero], w=[B_acc])
                for j in range(j_lo, 4):
                    kb.op("pe", lambda e: e.matmul(acc4[:, j, m * 129:(m + 1) * 129], lhsT=E[:, j * 128:(j + 1) * 128],
                                                   rhs=Vda[:, kt, h, 0:129], start=False, stop=last, skip_group_check=True),
                          r=[BE, B_V[kt]], w=[B_acc])
                if last:
                    while pending:
                        kb.op(*pending.pop(0))
                    evac(qb, q0)
                    conv_next(1)

            def evac(qb, q0):
                kb.op("dve", lambda e: e.reciprocal(out=rr[:, 0:4], in_=acc4[:, :, 128:129]), r=[B_acc], w=[B_o])
                kb.op("dve", lambda e: e.reciprocal(out=rr[:, 4:8], in_=acc4[:, :, 257:258]), r=[B_acc], w=[B_o])
                kb.op("dve", lambda e: e.tensor_tensor(out=o_sb[:], in0=acc4[:, :, 0:128], in1=rr[:, 0:4].unsqueeze(2).to_broadcast([128, 4, 128]),
                                                       op=ALU.mult), r=[B_acc], w=[B_o])
                kb.op("dve", lambda e: e.tensor_tensor(out=t_sb[:], in0=acc4[:, :, 129:257], in1=rr[:, 4:8].unsqueeze(2).to_broadcast([128, 4, 128]),
                                                       op=ALU.mult), r=[B_acc], w=[B_o])
                rec_ = []
                kb.op = lambda e, fn, r=(), w=(), same=None: rec_.append((e, fn, list(r), list(w), same))
                try:
                    evac_tail(qb, q0)
                finally:
                    del kb.op
                pending.extend(rec_)

            def evac_tail(qb, q0):
                kb.op("dve", lambda e: e.scalar_tensor_tensor(out=o_sb[:], in0=t_sb[:], scalar=neglam[:, 0:1], in1=o_sb[:],
                                                              op0=ALU.mult, op1=ALU.add), r=[B_o, B_l], w=[B_o])
                kb.op("dve", lambda e: e.tensor_tensor(out=t_sb[:], in0=o_sb[:], in1=o_sb[:], op=ALU.mult), r=[B_o], w=[B_o])
                kb.op("dve", lambda e: e.tensor_reduce(out=rr[:, 8:12], in_=t_sb[:], axis=AX.X, op=ALU.add), r=[B_o], w=[B_o])
                kb.op("act", lambda e: e.activation(out=rra[:, 0:4], in_=rr[:, 8:12], func=AF.Ln, scale=1.0 / 128, bias=EPS), r=[B_o], w=[B_o])
                kb.op("act", lambda e: e.activation(out=rra[:, 4:8], in_=rra[:, 0:4], func=AF.Exp, scale=-0.5), r=[B_o], w=[B_o])
                kb.op("dve", lambda e: e.tensor_tensor(out=t_sb[:], in0=o_sb[:], in1=rra[:, 4:8].unsqueeze(2).to_broadcast([128, 4, 128]),
                                                       op=ALU.mult), r=[B_o], w=[B_o])
                kb.op("dve", lambda e: e.tensor_tensor(out=y_sb[:], in0=t_sb[:], in1=gdo[:].unsqueeze(1).to_broadcast([128, 4, 128]),
                                                       op=ALU.mult), r=[B_o, B_c], w=[B_o])
                for j in range(4):
                    kb.op("pe", lambda e, j=j: e.transpose(out=ptr[:, j, :], in_=y_sb[:, j, :], identity=ident_bf[:]), r=[B_o, B_const], w=[B_ptr])
                kb.op("act", lambda e, h=h: e.copy(out=y_daT[:, h, q0:q0 + 512].rearrange("p (j t) -> p j t", j=4), in_=ptr[:, 0:4, :]),
                      r=[B_ptr], w=B_ydaT[h * NT + qb * 4:h * NT + qb * 4 + 4])

            emit_qk(units[0])
            if len(units) > 1:
                emit_qk(units[1])
            for ui in range(len(units)):
                if ui + 2 < len(units):
                    emit_qk(units[ui + 2])
                emit_av(units[ui])
                if pending and not units[ui][4]:
                    kb.op(*pending.pop(0))
            while pending:
                kb.op(*pending.pop(0))
        if "y_daT" in dbg_d:
            kb.dma("sp", "dbg", lambda q: q.dma_start(out=dbg_d["y_daT"], in_=y_daT[:]), r=B_ydaT)
        kb.barrier()


def _ml_phase(nc, kb, S, hT, B_hT, y_mlT, B_ymlT, w_in, cd, cw_col, cb_col, g_mlo, b_if, ident_bf, ident_f, ones_f,
              B_const, sb, ps, dbg_d):
    NT = S // 128
    NB5 = S // 512
    NHC = NT * 4
    QS = 128.0 ** -0.5
    with ExitStack() as s:
        negm = sb(s, "negm", [128, 128], F32)
        cw = sb(s, "cw", [128, 32], F32)
        cb = sb(s, "cb", [128, 8], F32)
        gml = sb(s, "gml", [128, 512], F32)
        bif = sb(s, "bif", [128, 8], F32)
        wif = sb(s, "wif", [128, 8, 8], BF16)
        B_c = kb.buf()
        for t, src in ((negm, cd["negm"]), (cw, cw_col), (cb, cb_col), (gml, g_mlo.partition_broadcast(128)),
                       (bif, b_if.partition_broadcast(128))):
            kb.dma("sp", "const", lambda q, t=t, src=src: q.dma_start(out=t[:], in_=src), w=[B_c])
        kb.dma("pool", "wif", lambda q: q.dma_start(out=wif[:], in_=w_in[:, OFF_ML_I:OFF_ML_I + 8].rearrange("(c p) f -> p c f", p=128)), w=[B_c])

        PA = [ps(s, f"mlPA{i}", [128, 512], F32) for i in range(2)]
        B_PA = kb.bufs(2)
        pacnt = [0]

        def nextPA():
            i = pacnt[0] % 2
            pacnt[0] += 1
            return PA[i], B_PA[i]
        Pew2 = [ps(s, f"mlPew{i}", [128, 512], F32) for i in range(2)]
        B_Pew2 = kb.bufs(2)
        Pew, B_Pew = Pew2[0], B_Pew2[0]
        Po2 = [ps(s, f"mlPo{i}", [128, 512], F32) for i in range(2)]
        B_Po2 = kb.bufs(2)
        Po, B_Po = Po2[0], B_Po2[0]
        Pc = ps(s, "mlPc", [128, 512], F32)
        Ptk = ps(s, "mlPtk", [128, 8, 128], BF16)
        Pty = Ptk
        Pg = Po
        B_Pc, B_Ptk = kb.bufs(2)
        B_Pty = B_Ptk
        B_Pg = B_Po

        for i in range(NT):
            for c in range(8):
                kb.op("pe", lambda e: e.matmul(Pg[:, i * 8:(i + 1) * 8], lhsT=hT[:, c, i * 128:(i + 1) * 128], rhs=wif[:, c, :],
                                               start=(c == 0), stop=(c == 7)), r=[B_hT[i], B_c], w=[B_Pg])
        gsb = sb(s, "gsb", [128, NT, 8], F32)
        XC = sb(s, "XC", [128, 2, NT, 4], F32)
        tmpg = sb(s, "tmpg", [128, NT, 4], F32)
        B_g = kb.buf()
        kb.op("dve", lambda e: e.tensor_tensor(out=gsb[:], in0=Pg[:, 0:NT * 8].rearrange("p (i g) -> p i g", g=8),
                                               in1=bif[:].unsqueeze(1).to_broadcast([128, NT, 8]), op=ALU.add), r=[B_Pg, B_c], w=[B_g])
        kb.op("act", lambda e: e.activation(out=tmpg[:], in_=gsb[:, :, 4:8], func=AF.Exp, scale=-1.0), r=[B_g], w=[B_g])
        kb.op("act", lambda e: e.activation(out=tmpg[:], in_=tmpg[:], func=AF.Ln, bias=1.0), r=[B_g], w=[B_g])
        kb.op("dve", lambda e: e.tensor_scalar(out=XC[:, 1], in0=tmpg[:], scalar1=-1.0, scalar2=None, op0=ALU.mult), r=[B_g], w=[B_g])
        kb.op("dve", lambda e: e.tensor_copy(out=XC[:, 0], in_=gsb[:, :, 0:4]), r=[B_g], w=[B_g])
        RN = ["iR", "lfR", "bR", "betaR", "pmR", "mxR", "alphaR", "mrowR", "winterR", "emrR", "winR", "zR"]
        Rt = {n: sb(s, n, [128, 128], F32) for n in RN}
        B_R = kb.buf()
        for a, n in ((0, "iR"), (1, "lfR")):
            kb.op("pe", lambda e: e.transpose(out=Pew[0:NHC, a * 128:(a + 1) * 128], in_=XC[:, a].rearrange("p i h -> p (i h)"),
                                              identity=ident_f[:]), r=[B_g, B_const], w=[B_Pew])
            kb.op("dve", lambda e: e.tensor_copy(out=Rt[n][0:NHC, :], in_=Pew[0:NHC, a * 128:(a + 1) * 128]), r=[B_Pew], w=[B_R])
        R = {n: Rt[n][0:NHC, :] for n in RN}
        kb.op("pool", lambda e: e.memset(Rt["zR"][:], 0.0), w=[B_R])
        kb.op("dve", lambda e: e.tensor_tensor_scan(out=R["bR"], data0=R["lfR"], data1=R["zR"], initial=0.0, op0=ALU.add, op1=ALU.add),
              r=[B_R], w=[B_R])
        kb.op("dve", lambda e: e.tensor_tensor(out=R["betaR"], in0=R["iR"], in1=R["bR"], op=ALU.subtract), r=[B_R], w=[B_R])
        kb.op("dve", lambda e: e.tensor_tensor_scan(out=R["pmR"], data0=R["betaR"], data1=R["betaR"], initial=-1e30, op0=ALU.max, op1=ALU.max),
              r=[B_R], w=[B_R])
        c2 = sb(s, "c2", [128, 8], F32)
        rows = sb(s, "rows", [1, 4, 128], F32)
        drow = sb(s, "drow", [1, 128], F32)
        kb.op("dve", lambda e: e.tensor_copy(out=c2[0:NHC, 0:1], in_=R["bR"][:, 127:128]), r=[B_R], w=[B_R])
        kb.op("dve", lambda e: e.tensor_tensor(out=c2[0:NHC, 1:2], in0=R["bR"][:, 127:128], in1=R["pmR"][:, 127:128], op=ALU.add), r=[B_R], w=[B_R])
        for a in range(2):
            kb.op("pe", lambda e: e.transpose(out=Pew[0:1, a * 128:a * 128 + NHC], in_=c2[0:NHC, a:a + 1], identity=ident_f[0:NHC, 0:NHC]),
                  r=[B_R, B_const], w=[B_Pew])
        kb.op("dve", lambda e: e.tensor_copy(out=rows[:, 0:2, 0:NHC], in_=Pew[0:1, 0:256].rearrange("p (a n) -> p a n", a=2)[:, :, 0:NHC]),
              r=[B_Pew], w=[B_R])
        for h in range(4):
            v = lambda a: rows[:, a, 0:NHC].rearrange("p (i h) -> p h i", h=4)[:, h, :]
            kb.op("dve", lambda e: e.tensor_tensor_scan(out=v(2), data0=v(0), data1=v(1), initial=0.0, op0=ALU.add, op1=ALU.max),
                  r=[B_R], w=[B_R])
        kb.op("pool", lambda e: e.memset(rows[:, 3, 0:4], 0.0), r=[B_R], w=[B_R])
        if NHC > 4:
            kb.op("dve", lambda e: e.tensor_copy(out=rows[:, 3, 4:NHC], in_=rows[:, 2, 0:NHC - 4]), r=[B_R], w=[B_R])
        kb.op("pe", lambda e: e.matmul(Pew[0:NHC, 0:1], lhsT=rows[:, 3, 0:NHC], rhs=ones_f[0:1, 0:1], start=True, stop=True),
              r=[B_R, B_const], w=[B_Pew])
        kb.op("dve", lambda e: e.tensor_copy(out=c2[0:NHC, 2:3], in_=Pew[0:NHC, 0:1]), r=[B_Pew], w=[B_R])
        ms = c2[0:NHC, 2:3]
        kb.op("dve", lambda e: e.tensor_scalar(out=R["mxR"], in0=R["pmR"], scalar1=ms, scalar2=None, op0=ALU.max), r=[B_R], w=[B_R])
        kb.op("dve", lambda e: e.tensor_scalar(out=R["alphaR"], in0=R["mxR"], scalar1=-1.0, scalar2=None, op0=ALU.mult), r=[B_R], w=[B_R])
        kb.op("dve", lambda e: e.tensor_tensor(out=R["mrowR"], in0=R["bR"], in1=R["mxR"], op=ALU.add), r=[B_R], w=[B_R])
        kb.op("act", lambda e: e.activation(out=R["winterR"], in_=R["alphaR"], func=AF.Exp, bias=ms), r=[B_R], w=[B_R])
        kb.op("act", lambda e: e.activation(out=R["emrR"], in_=R["mrowR"], func=AF.Exp, scale=-1.0), r=[B_R], w=[B_R])
        kb.op("dve", lambda e: e.tensor_tensor(out=c2[0:NHC, 6:7], in0=ms, in1=R["pmR"][:, 127:128], op=ALU.max), r=[B_R], w=[B_R])
        kb.op("dve", lambda e: e.tensor_tensor(out=c2[0:NHC, 3:4], in0=c2[0:NHC, 6:7], in1=c2[0:NHC, 0:1], op=ALU.add), r=[B_R], w=[B_R])
        kb.op("dve", lambda e: e.tensor_tensor(out=c2[0:NHC, 5:6], in0=c2[0:NHC, 0:1], in1=c2[0:NHC, 3:4], op=ALU.subtract), r=[B_R], w=[B_R])
        kb.op("act", lambda e: e.activation(out=c2[0:NHC, 4:5], in_=ms, func=AF.Exp, bias=c2[0:NHC, 5:6]), r=[B_R], w=[B_R])
        kb.op("act", lambda e: e.activation(out=R["winR"], in_=R["betaR"], func=AF.Exp, bias=c2[0:NHC, 5:6]), r=[B_R], w=[B_R])
        CN = ["betaR", "alphaR", "winterR", "emrR", "winR"]
        Ct = {n: sb(s, "C_" + n, [128, 128], F32) for n in CN}
        dbc = sb(s, "dbc", [128, 128], F32)
        B_C = kb.buf()
        for n in CN:
            kb.op("pe", lambda e: e.transpose(out=Pew[:, 0:NHC], in_=R[n], identity=ident_f[0:NHC, 0:NHC]), r=[B_R, B_const], w=[B_Pew])
            kb.op("dve", lambda e: e.tensor_copy(out=Ct[n][:, 0:NHC], in_=Pew[:, 0:NHC]), r=[B_Pew], w=[B_C])
        kb.op("pe", lambda e: e.transpose(out=Pew[0:1, 0:NHC], in_=c2[0:NHC, 4:5], identity=ident_f[0:NHC, 0:NHC]), r=[B_R, B_const], w=[B_Pew])
        kb.op("dve", lambda e: e.tensor_copy(out=drow[:, 0:NHC], in_=Pew[0:1, 0:NHC]), r=[B_Pew], w=[B_R])
        kb.op("pe", lambda e: e.matmul(Pew[:, 0:NHC], lhsT=ones_f[0:1, :], rhs=drow[:, 0:NHC], start=True, stop=True), r=[B_R, B_const], w=[B_Pew])
        kb.op("dve", lambda e: e.tensor_copy(out=dbc[:, 0:NHC], in_=Pew[:, 0:NHC]), r=[B_Pew], w=[B_C])

        wq4 = sb(s, "wq4", [128, 8, 512], BF16)
        B_w = kb.buf()
        qkT = sb(s, "mlqkT", [128, 2, S], BF16)
        B_qk = [kb.bufs(NB5), kb.bufs(NB5)]
        Vml = sb(s, "Vml", [128, NT, 130], BF16)
        og = sb(s, "og", [128, NT, 128], BF16)
        B_vo = kb.bufs(NT)
        B_vones = kb.buf()
        kb.op("pool", lambda e: e.memset(Vml[:, :, 128:130], 1.0), w=[B_vones])
        U2 = [sb(s, f"U2{i}", [128, 515], F32) for i in range(2)]
        B_U = kb.bufs(2)
        acc = [sb(s, f"cacc{i}", [128, 512], F32) for i in range(2)]
        B_a = kb.bufs(2)
        Cf = sb(s, "Cf", [128, 130], F32)
        Cbf = [sb(s, f"Cbf{i}", [128, 130], BF16) for i in range(2)]
        B_Cf = kb.buf()
        B_Cbf = kb.bufs(2)
        dA = [sb(s, f"dA{i}", [128, 128], F32) for i in range(2)]
        dW = [sb(s, f"dW{i}", [128, 128], F32) for i in range(2)]
        Wt = [sb(s, f"Wt{i}", [128, 128], F32) for i in range(2)]
        Pt = [sb(s, f"Pt{i}", [128, 128], BF16) for i in range(2)]
        qs = [sb(s, f"qs{i}", [128, 128], BF16) for i in range(2)]
        kw = [sb(s, f"kw{i}", [128, 128], BF16) for i in range(2)]
        t1_2 = [sb(s, f"t1{i}", [128, 128], F32) for i in range(2)]
        yb_2 = [sb(s, f"yb{i}", [128, 128], BF16) for i in range(2)]
        jk_2 = [sb(s, f"jk{i}", [128, 128], F32) for i in range(2)]
        sc_2 = [sb(s, f"sc{i}", [128, 8], F32) for i in range(2)]
        sca_2 = [sb(s, f"sca{i}", [128, 8], F32) for i in range(2)]
        B_ch2 = kb.bufs(2)
        B_y2 = kb.bufs(2)
        B_dA, B_dW, B_Wt, B_Pt, B_qs, B_kw = [kb.bufs(2) for _ in range(6)]
        offs = [OFF_ML_QK, OFF_ML_QK + 512, OFF_ML_V, OFF_ML_O]
        for h in range(NH):
            for a in range(4):
                kb.dma("pool", "wq4", lambda q: q.dma_start(out=wq4[:, :, a * 128:(a + 1) * 128],
                       in_=w_in[:, offs[a] + h * 128:offs[a] + (h + 1) * 128].rearrange("(c p) f -> p c f", p=128)), w=[B_w])
            for i in range(NT):
                P, BP = nextPA()
                for c in range(8):
                    kb.op("pe", lambda e: e.matmul(P[:, 0:256], lhsT=hT[:, c, i * 128:(i + 1) * 128], rhs=wq4[:, c, 256:512],
                                                   start=(c == 0), stop=(c == 7)), r=[B_hT[i], B_w], w=[BP])
                kb.op("act", lambda e: e.copy(out=Vml[:, i, 0:128], in_=P[:, 0:128]), r=[BP, B_vones], w=[B_vo[i]])
                kb.op("act", lambda e: e.activation(out=og[:, i, :], in_=P[:, 128:256], func=AF.Sigmoid), r=[BP], w=[B_vo[i]])
                kb.op("pool", lambda e: e.tensor_tensor(out=og[:, i, :], in0=og[:, i, :], in1=gml[:, h * 128:(h + 1) * 128], op=ALU.mult),
                      r=[B_c], w=[B_vo[i]])
            for which in range(2):
                cc = which * 4 + h
                for tb in range(NB5):
                    ub = tb % 2
                    P, BP = nextPA()
                    for c in range(8):
                        kb.op("pe", lambda e: e.matmul(P[:], lhsT=wq4[:, c, which * 128:(which + 1) * 128], rhs=hT[:, c, tb * 512:(tb + 1) * 512],
                                                       start=(c == 0), stop=(c == 7)), r=B_hT[tb * 4:tb * 4 + 4] + [B_w], w=[BP])
                    kb.op("act", lambda e: e.copy(out=U2[ub][:, 3:515], in_=P[:]), r=[BP], w=[B_U[ub]])
                    if tb == 0:
                        kb.op("pool", lambda e: e.memset(U2[ub][:, 0:3], 0.0), w=[B_U[ub]])
                    else:
                        kb.op("pool", lambda e: e.tensor_copy(out=U2[ub][:, 0:3], in_=U2[1 - ub][:, 512:515]), r=[B_U[1 - ub]], w=[B_U[ub]])
                    A_, BA = acc[ub], B_a[ub]
                    kb.op("dve", lambda e: e.tensor_scalar(out=A_[:], in0=U2[ub][:, 3:515], scalar1=cw[:, cc * 4 + 3:cc * 4 + 4],
                                                           scalar2=cb[:, cc:cc + 1], op0=ALU.mult, op1=ALU.add), r=[B_U[ub], B_c], w=[BA])
                    for j in range(3):
                        kb.op("dve", lambda e: e.scalar_tensor_tensor(out=A_[:], in0=U2[ub][:, j:j + 512], scalar=cw[:, cc * 4 + j:cc * 4 + j + 1],
                                                                      in1=A_[:], op0=ALU.mult, op1=ALU.add), r=[B_U[ub], B_c], w=[BA])
                    if which == 1:
                        kb.op("act", lambda e: e.activation(out=qkT[:, 1, tb * 512:(tb + 1) * 512], in_=A_[:], func=AF.Silu),
                              r=[BA], w=[B_qk[1][tb]])
                    else:
                        kb.op("act", lambda e: e.activation(out=A_[:], in_=A_[:], func=AF.Silu), r=[BA], w=[BA])
                        kb.op("pool", lambda e: e.tensor_scalar(out=qkT[:, 0, tb * 512:(tb + 1) * 512], in0=A_[:], scalar1=QS, scalar2=None,
                                                                op0=ALU.mult), r=[BA], w=[B_qk[0][tb]])
            kb.op("pool", lambda e: e.memset(Cf[:], 0.0), w=[B_Cf])
            kb.op("pool", lambda e: e.memset(Cbf[1][:], 0.0), w=[B_Cbf[1]])

            def pre(i):
                jj = i % 2
                hc = i * 4 + h
                tsl = slice(i * 128, (i + 1) * 128)
                tb = i // 4
                Ps_, BPs = PA[jj], B_PA[jj]
                Pw_, BPw = Pew2[jj], B_Pew2[jj]
                kb.op("pe", lambda e: e.matmul(Ps_[:, 0:128], lhsT=qkT[:, 1, tsl], rhs=qkT[:, 0, tsl], start=True, stop=True),
                      r=[B_qk[0][tb], B_qk[1][tb]], w=[BPs])
                kb.op("dve", lambda e: e.tensor_scalar(out=dA[jj][:], in0=ident_f[:], scalar1=Ct["alphaR"][:, hc:hc + 1], scalar2=None, op0=ALU.mult),
                      r=[B_C, B_const], w=[B_dA[jj]])
                kb.op("dve", lambda e: e.tensor_scalar(out=dW[jj][:], in0=ident_f[:], scalar1=Ct["winterR"][:, hc:hc + 1], scalar2=None, op0=ALU.mult),
                      r=[B_C, B_const], w=[B_dW[jj]])
                kb.op("pe", lambda e: e.matmul(Pw_[:, 0:128], lhsT=ones_f[:], rhs=dA[jj][:], start=True, stop=False), r=[B_dA[jj], B_const], w=[BPw])
                kb.op("pe", lambda e: e.matmul(Pw_[:, 0:128], lhsT=ident_f[:], rhs=negm[:], start=False, stop=True), r=[B_c, B_const], w=[BPw])
                kb.op("pe", lambda e: e.matmul(Pw_[:, 128:256], lhsT=ones_f[:], rhs=dW[jj][:], start=True, stop=True), r=[B_dW[jj], B_const], w=[BPw])
                kb.op("pe", lambda e: e.transpose(out=Ptk[:, jj, :], in_=qkT[:, 1, tsl], identity=ident_bf[:]), r=[B_qk[1][tb], B_const], w=[B_Ptk])

            def preB(i):
                jj = i % 2
                hc = i * 4 + h
                tsl = slice(i * 128, (i + 1) * 128)
                tb = i // 4
                Ps_, BPs = PA[jj], B_PA[jj]
                Pw_, BPw = Pew2[jj], B_Pew2[jj]
                kb.op("act", lambda e: e.activation(out=Wt[jj][:], in_=Pw_[:, 0:128], func=AF.Exp, bias=Ct["betaR"][:, hc:hc + 1]),
                      r=[BPw, B_C], w=[B_Wt[jj]])
                kb.op("dve", lambda e: e.tensor_tensor(out=qs[jj][:], in0=qkT[:, 0, tsl], in1=Pw_[:, 128:256], op=ALU.mult),
                      r=[BPw, B_qk[0][tb], B_Wt[jj]], w=[B_qs[jj]])
                kb.op("dve", lambda e: e.tensor_scalar(out=kw[jj][:], in0=Ptk[:, jj, :], scalar1=Ct["winR"][:, hc:hc + 1], scalar2=None, op0=ALU.mult),
                      r=[B_Ptk, B_C], w=[B_kw[jj]])
                kb.op("dve", lambda e: e.tensor_tensor(out=Pt[jj][:], in0=Ps_[:, 0:128], in1=Wt[jj][:], op=ALU.mult), r=[BPs, B_Wt[jj]], w=[B_Pt[jj]])

            def main(i):
                mainA(i)
                tail(i)

            def mainA(i):
                jj = i % 2
                hc = i * 4 + h
                tsl = slice(i * 128, (i + 1) * 128)
                Po, B_Po = Po2[jj], B_Po2[jj]
                kb.op("pe", lambda e: e.matmul(Pc[:, 0:129], lhsT=kw[jj][:], rhs=Vml[:, i, 0:129], start=True, stop=True), r=[B_kw[jj], B_vo[i]], w=[B_Pc])
                kb.op("pe", lambda e: e.matmul(Po[:, 0:129], lhsT=Pt[jj][:], rhs=Vml[:, i, 0:129], start=True, stop=False), r=[B_Pt[jj], B_vo[i]], w=[B_Po])
                kb.op("pe", lambda e: e.matmul(Po[:, 0:129], lhsT=qs[jj][:], rhs=Cbf[(i + 1) % 2][:, 0:129], start=False, stop=True),
                      r=[B_qs[jj], B_Cbf[(i + 1) % 2]], w=[B_Po])
                kb.op("dve", lambda e: e.scalar_tensor_tensor(out=Cf[:, 0:129], in0=Cf[:, 0:129], scalar=dbc[:, hc:hc + 1], in1=Pc[:, 0:129],
                                                              op0=ALU.mult, op1=ALU.add), r=[B_Pc, B_C], w=[B_Cf])
                kb.op("act", lambda e: e.copy(out=Cbf[jj][:, 0:129], in_=Cf[:, 0:129]), r=[B_Cf], w=[B_Cbf[jj]])

            def tail(i):
                jj = i % 2
                hc = i * 4 + h
                tsl = slice(i * 128, (i + 1) * 128)
                Po, B_Po = Po2[jj], B_Po2[jj]
                t1, yb, jk, sc, sca, B_ch, B_y = t1_2[jj], yb_2[jj], jk_2[jj], sc_2[jj], sca_2[jj], B_ch2[jj], B_y2[jj]
                kb.op("act", lambda e: e.activation(out=sca[:, 0:1], in_=Po[:, 128:129], func=AF.Abs), r=[B_Po], w=[B_ch])
                kb.op("dve", lambda e: e.tensor_tensor(out=sc[:, 0:1], in0=sca[:, 0:1], in1=Ct["emrR"][:, hc:hc + 1], op=ALU.max), r=[B_ch, B_C], w=[B_ch])
                kb.op("dve", lambda e: e.tensor_scalar(out=sc[:, 1:2], in0=sc[:, 0:1], scalar1=sc[:, 0:1], scalar2=EPS, op0=ALU.mult, op1=ALU.mult),
                      r=[B_ch], w=[B_ch])
                kb.op("act", lambda e: e.activation(out=jk[:], in_=Po[:, 0:128], func=AF.Square, accum_out=sca[:, 2:3]), r=[B_Po], w=[B_ch])
                kb.op("act", lambda e: e.activation(out=sca[:, 3:4], in_=sca[:, 2:3], func=AF.Ln, scale=1.0 / 128, bias=sc[:, 1:2]), r=[B_ch], w=[B_ch])
                kb.op("act", lambda e: e.activation(out=sca[:, 4:5], in_=sca[:, 3:4], func=AF.Exp, scale=-0.5), r=[B_ch], w=[B_ch])
                kb.op("dve", lambda e: e.scalar_tensor_tensor(out=yb[:], in0=Po[:, 0:128], scalar=sca[:, 4:5], in1=og[:, i, :],
                                                              op0=ALU.mult, op1=ALU.mult), r=[B_Po, B_ch, B_vo[i]], w=[B_y])
                kb.op("pe", lambda e: e.transpose(out=Pty[:, 2, :], in_=yb[:], identity=ident_bf[:]), r=[B_y, B_const], w=[B_Pty])
                kb.op("act", lambda e: e.copy(out=y_mlT[:, h, tsl], in_=Pty[:, 2, :]), r=[B_Pty], w=[B_ymlT[h * NT + i]])

            LOOKAHEAD = ML_LOOKAHEAD
            if LOOKAHEAD == 0:
                for i in range(NT):
                    pre(i)
                    preB(i)
                    main(i)
            elif LOOKAHEAD == 1:
                pre(0)
                for i in range(NT):
                    preB(i)
                    if i + 1 < NT:
                        pre(i + 1)
                    main(i)
            elif LOOKAHEAD == 3:
                def rec_ops(fns):
                    rec = []
                    kb.op = lambda e, fn, r=(), w=(), same=None: rec.append((e, fn, list(r), list(w), same))
                    try:
                        for f_ in fns:
                            f_()
                    finally:
                        del kb.op
                    return rec
                pre(0)
                for i in range(NT + 1):
                    fa = []
                    if i < NT:
                        fa.append(lambda i=i: preB(i))
                        if i + 1 < NT:
                            fa.append(lambda i=i: pre(i + 1))
                        fa.append(lambda i=i: mainA(i))
                    ra = rec_ops(fa)
                    rb = rec_ops([lambda i=i: tail(i - 1)]) if i >= 1 else []
                    ia = ib = 0
                    while ia < len(ra) or ib < len(rb):
                        for _ in range(2):
                            if ia < len(ra):
                                kb.op(*ra[ia])
                                ia += 1
                        if ib < len(rb):
                            kb.op(*rb[ib])
                            ib += 1
            else:
                pre(0)
                preB(0)
                for i in range(NT):
                    if i + 1 < NT:
                        pre(i + 1)
                        preB(i + 1)
                    main(i)
        if "y_mlT" in dbg_d:
            kb.dma("sp", "dbg", lambda q: q.dma_start(out=dbg_d["y_mlT"], in_=y_mlT[:]), r=B_ymlT)
        kb.barrier()


def _merge_moe(nc, kb, S, hT, B_hT, y_daT, B_ydaT, y_mlT, B_ymlT, es_mix, x_d, out_d, w_bda, w_bml, w_gate, w_out,
               bg_col, g_ffn, wr_d, b_rt, w1, w3, w2, xs_d, ys_d, hn_d, cd, ident_bf, ident_f, ones_f, zero_bf,
               B_const, B_zero, sb, ps, dbg_d, stage, B_xs, wconv):
    w1b_d, w3b_d, w2b_d, B_wconv = wconv
    NT = S // 128
    NB5 = S // 512
    CAP = 2 * S + NE * BS
    NBLK = CAP // BS
    NSUB = BS // 128
    with ExitStack() as s:
        wg = sb(s, "wg", [128, 8, 2048], BF16)
        wda = sb(s, "wda", [128, 4, 1024], BF16)
        wml = sb(s, "wml", [128, 4, 1024], BF16)
        bg = sb(s, "bg", [128, 16], F32)
        B_w = kb.buf()
        for k4 in range(4):
            kb.dma("pool", "mw", lambda q: q.dma_start(out=wg[:, :, k4 * 512:(k4 + 1) * 512],
                   in_=w_gate[:, k4 * 512:(k4 + 1) * 512].rearrange("(c p) f -> p c f", p=128)), w=[B_w])
        kb.dma("pool", "mw", lambda q: q.dma_start(out=wda[:], in_=w_bda.rearrange("(c p) f -> p c f", p=128)), w=[B_w])
        kb.dma("pool", "mw", lambda q: q.dma_start(out=wml[:], in_=w_bml.rearrange("(c p) f -> p c f", p=128)), w=[B_w])
        kb.dma("sp", "const", lambda q: q.dma_start(out=bg[:], in_=bg_col), w=[B_w])
        Pg = [ps(s, f"mPg{i}", [128, 512], F32) for i in range(2)]
        Pab = [ps(s, f"mPab{i}", [128, 512], F32) for i in range(2)]
        B_Pg, B_Pab = kb.bufs(2), kb.bufs(2)
        gs = [sb(s, f"gs{i}", [128, 512], F32) for i in range(2)]
        tt = [sb(s, f"tt{i}", [128, 512], F32) for i in range(2)]
        B_gs, B_tt = kb.bufs(2), kb.bufs(2)
        mixb = sb(s, "mixb", [128, 8, 512], BF16)
        B_mix = kb.bufs(8)
        for tb in range(NB5):
            tsl = slice(tb * 512, (tb + 1) * 512)
            hb = B_hT[tb * 4:tb * 4 + 4]
            for fc in range(8):
                for g in range(2):
                    kb_w = wda if g == 0 else wml
                    yT = y_daT if g == 0 else y_mlT
                    By = (B_ydaT if g == 0 else B_ymlT)
                    byl = [By[hh * NT + tb * 4 + t4] for hh in range(4) for t4 in range(4)]
                    for c in range(8):
                        kb.op("pe", lambda e: e.matmul(Pg[g][:], lhsT=wg[:, c, g * 1024 + fc * 128:g * 1024 + (fc + 1) * 128], rhs=hT[:, c, tsl],
                                                       start=(c == 0), stop=(c == 7)), r=hb + [B_w], w=[B_Pg[g]])
                    kb.op("act", lambda e: e.activation(out=gs[g][:], in_=Pg[g][:], func=AF.Sigmoid, bias=bg[:, g * 8 + fc:g * 8 + fc + 1]),
                          r=[B_Pg[g], B_w], w=[B_gs[g]])
                    for c in range(4):
                        kb.op("pe", lambda e: e.matmul(Pab[g][:], lhsT=kb_w[:, c, fc * 128:(fc + 1) * 128], rhs=yT[:, c, tsl],
                                                       start=(c == 0), stop=(c == 3)), r=byl + [B_w], w=[B_Pab[g]])
                    kb.op("dve", lambda e: e.tensor_tensor(out=tt[g][:], in0=gs[g][:], in1=Pab[g][:], op=ALU.mult),
                          r=[B_gs[g], B_Pab[g]], w=[B_tt[g]])
                kb.op("pool", lambda e: e.tensor_tensor(out=mixb[:, fc, :], in0=tt[0][:], in1=tt[1][:], op=ALU.add),
                      r=[B_tt[0], B_tt[1]], w=[B_mix[fc]])
            for fc in range(8):
                kb.op("pool", lambda e: e.tensor_copy(out=hT[:, fc, tsl], in_=mixb[:, fc, :]), r=[B_mix[fc]], w=hb)
        kb.barrier()
    es_mix.close()

    es2 = ExitStack()
    with es2:
        s = es2
        A1a = sb(s, "A1a", [128, NT, 32], F32)
        A2a = sb(s, "A2a", [128, NT, 32], F32)
        rka = sb(s, "rka", [128, NT, 32], F32)
        gta = sb(s, "gta", [128, NT, 2], F32)
        base = sb(s, "base", [128, 32], F32)
        d1i = sb(s, "d1i", [128, NT], I32)
        d2i = sb(s, "d2i", [128, NT], I32)
        bexp = sb(s, "bexp", [128, NBLK], I32)
        idxw = sb(s, "idxw", [128, NBLK], I32)
        B_rt = kb.buf()
        B_out = kb.bufs(NT)
        B_hn = kb.bufs(NT)
        with ExitStack() as sB:
            wo = sb(sB, "wo", [128, 8, 1024], BF16)
            gff = sb(sB, "gff", [128, 1024], F32)
            wr = sb(sB, "wr", [128, 8, 36], F32)
            brt = sb(sB, "brt", [128, 36], F32)
            ustr = sb(sB, "ustr", [128, 128], F32)
            B_w = kb.buf()
            for k2 in range(2):
                kb.dma("pool", "mw", lambda q: q.dma_start(out=wo[:, :, k2 * 512:(k2 + 1) * 512],
                       in_=w_out[:, k2 * 512:(k2 + 1) * 512].rearrange("(c p) f -> p c f", p=128)), w=[B_w])
            kb.dma("sp", "const", lambda q: q.dma_start(out=gff[:], in_=g_ffn.partition_broadcast(128)), w=[B_w])
            kb.dma("sp", "const", lambda q: q.dma_start(out=wr[:], in_=wr_d.rearrange("(c p) f -> p c f", p=128)), w=[B_w])
            kb.dma("sp", "const", lambda q: q.dma_start(out=brt[:], in_=b_rt.partition_broadcast(128)), w=[B_w])
            kb.dma("sp", "const", lambda q: q.dma_start(out=ustr[:], in_=cd["ustrict"]), w=[B_w])
            kb.op("pool", lambda e: e.memset(base[:], 0.0), w=[B_rt])
            Px = ps(sB, "bPx", [128, 2, 512], F32)
            PT = ps(sB, "bPT", [128, 8, 128], F32)
            Pr = ps(sB, "bPr", [128, 512], F32)
            B_Px, B_PT, B_Pr = kb.bufs(3)
            xt = [sb(sB, f"bxt{i}", [128, 1024], F32) for i in range(2)]
            x2 = [sb(sB, f"bx2{i}", [128, 1024], F32) for i in range(2)]
            hnf = sb(sB, "hnf", [128, 1024], F32)
            hnb = [sb(sB, f"hnb{i}", [128, 1024], BF16) for i in range(2)]
            hnT = sb(sB, "hnT", [128, 8, 128], F32)
            junk = sb(sB, "bjunk", [128, 512], F32)
            st = sb(sB, "bst", [128, 8], F32)
            sd_ = sb(sB, "bsd", [128, 16], F32)
            lg = sb(sB, "lg", [128, 36], F32)
            oh = sb(sB, "oh", [128, 4], F32)
            t48 = sb(sB, "t48", [128, 4, 8], F32)
            es8 = sb(sB, "es8", [128, 8], F32)
            e28 = sb(sB, "e28", [128, 8], F32)
            mk1 = sb(sB, "mk1", [128, 8], F32)
            mk2 = sb(sB, "mk2", [128, 8], F32)
            At = sb(sB, "At", [128, 32], F32)
            B_xt, B_x2, B_hnb = kb.bufs(2), kb.bufs(2), kb.bufs(2)
            B_hnf, B_hnT, B_r = kb.buf(), kb.buf(), kb.buf()
            B_rx = kb.buf()
            B_lg = kb.bufs(4)
            lg2 = [sb(sB, f"lg2{i}", [128, 36], F32) for i in range(4)]
            B_r2 = kb.bufs(2)
            sd2 = [sd_, sb(sB, "bsd_b", [128, 16], F32)]
            st2 = [st, sb(sB, "bst_b", [128, 8], F32)]
            oh2 = [oh, sb(sB, "oh_b", [128, 4], F32)]
            t482 = [t48, sb(sB, "t48_b", [128, 4, 8], F32)]
            es82 = [es8, sb(sB, "es8_b", [128, 8], F32)]
            e282 = [e28, sb(sB, "e28_b", [128, 8], F32)]
            mk12 = [mk1, sb(sB, "mk1_b", [128, 8], F32)]
            mk22 = [mk2, sb(sB, "mk2_b", [128, 8], F32)]
            stX = sb(sB, "bstX", [128, 8], F32)
            sdX = sb(sB, "bsdX", [128, 16], F32)
            junkR2 = [sb(sB, f"bjunkR{i}", [128, 8], F32) for i in range(2)]
            Pr2 = ps(sB, "bPr2", [128, 512], F32)
            B_Pr2 = kb.buf()
            hnf2 = [hnf, sb(sB, "hnf_b", [128, 1024], F32)]
            hnT2 = [hnT, sb(sB, "hnT_b", [128, 8, 128], F32)]
            stX2 = [stX, sb(sB, "bstX_b", [128, 8], F32)]
            junk2 = [junk, sb(sB, "bjunk_b", [128, 512], F32)]
            PT2 = [PT, ps(sB, "bPT_b", [128, 8, 128], F32)]
            Pr_2 = [Pr, Pr2]
            B_hnf2, B_hnT2, B_rx2, B_PT2 = [B_hnf, kb.buf()], [B_hnT, kb.buf()], [B_rx, kb.buf()], [B_PT, kb.buf()]
            B_Px2 = [B_Px, kb.buf()]
            B_Pr_2 = [B_Pr, B_Pr2]
            ingroup = [None]

            def gb():
                ingroup[0] = []

            def ge():
                g_, ingroup[0] = ingroup[0], None
                return g_

            def stageX(i):
                j = i % 2
                lgi = lg2[i % 4]
                hnf, hnT, stX, junk, PT, Pr = hnf2[j], hnT2[j], stX2[j], junk2[j], PT2[j], Pr_2[j]
                B_hnf, B_hnT, B_rx, B_PT, B_Pr, B_Px = B_hnf2[j], B_hnT2[j], B_rx2[j], B_PT2[j], B_Pr_2[j], B_Px2[j]
                rows = slice(i * 128, (i + 1) * 128)
                half = fc = hs = c = None
                kb.dma("sp", f"bxt{j}", lambda q: q.dma_start(out=xt[j][:], in_=x_d[rows, :]), w=[B_xt[j]])
                for half in range(2):
                    hs = slice(half * 512, (half + 1) * 512)
                    gb()
                    for fc in range(8):
                        kb.op("pe", lambda e, half=half, fc=fc, hs=hs, c=c: e.matmul(Px[:, j, :], lhsT=hT[:, fc, rows], rhs=wo[:, fc, half * 512:(half + 1) * 512],
                                                       start=(fc == 0), stop=(fc == 7)), r=[B_hT[i], B_w], w=[B_Px])
                    grp_done(ge())
                    kb.op("dve", lambda e, half=half, fc=fc, hs=hs, c=c: e.tensor_tensor(out=x2[j][:, hs], in0=xt[j][:, hs], in1=Px[:, j, :], op=ALU.add),
                          r=[B_xt[j], B_Px], w=[B_x2[j]])
                kb.dma("sp", "ost", lambda q: q.dma_start(out=out_d[rows, :], in_=x2[j][:]), r=[B_x2[j]], w=[B_out[i]])
                for half in range(2):
                    hs = slice(half * 512, (half + 1) * 512)
                    kb.op("act", lambda e, half=half, fc=fc, hs=hs, c=c: e.activation(out=junk[:], in_=x2[j][:, hs], func=AF.Square, accum_out=stX[:, half:half + 1]),
                          r=[B_x2[j]], w=[B_rx])
                kb.op("act", lambda e, half=half, fc=fc, hs=hs, c=c: e.activation(out=stX[:, 2:3], in_=stX[:, 0:1], func=AF.Identity, bias=stX[:, 1:2]), r=[B_rx], w=[B_rx])
                kb.op("act", lambda e, half=half, fc=fc, hs=hs, c=c: e.activation(out=stX[:, 3:4], in_=stX[:, 2:3], func=AF.Ln, scale=1.0 / D, bias=EPS), r=[B_rx], w=[B_rx])
                kb.op("act", lambda e, half=half, fc=fc, hs=hs, c=c: e.activation(out=stX[:, 6:7], in_=stX[:, 3:4], func=AF.Exp, scale=-0.5), r=[B_rx], w=[B_rx])
                for half in range(2):
                    hs = slice(half * 512, (half + 1) * 512)
                    kb.op("dve", lambda e, half=half, fc=fc, hs=hs, c=c: e.scalar_tensor_tensor(out=hnf[:, hs], in0=x2[j][:, hs], scalar=stX[:, 6:7], in1=gff[:, hs],
                                                                  op0=ALU.mult, op1=ALU.mult), r=[B_x2[j], B_rx, B_w], w=[B_hnf])
                    kb.op("pool", lambda e, half=half, fc=fc, hs=hs, c=c: e.tensor_copy(out=hnb[j][:, hs], in_=hnf[:, hs]), r=[B_hnf], w=[B_hnb[j]])
                kb.dma("sp", "hnst", lambda q: q.dma_start(out=hn_d[rows, :], in_=hnb[j][:]), r=[B_hnb[j]], w=[B_hn[i]])
                for c in range(8):
                    kb.op("pe", lambda e, half=half, fc=fc, hs=hs, c=c: e.transpose(out=PT[:, c, :], in_=hnf[:, c * 128:(c + 1) * 128], identity=ident_f[:]),
                          r=[B_hnf, B_const], w=[B_PT])
                for half in range(2):
                    kb.op("act", lambda e, half=half, fc=fc, hs=hs, c=c: e.copy(out=hnT[:, half * 4:half * 4 + 4, :], in_=PT[:, half * 4:half * 4 + 4, :]), r=[B_PT], w=[B_hnT])
                gb()
                for c in range(8):
                    kb.op("pe", lambda e, half=half, fc=fc, hs=hs, c=c: e.matmul(Pr[:, 0:36], lhsT=hnT[:, c, :], rhs=wr[:, c, :], start=(c == 0), stop=(c == 7)),
                          r=[B_hnT, B_w], w=[B_Pr])
                grp_done(ge())
                kb.op("dve", lambda e, half=half, fc=fc, hs=hs, c=c: e.tensor_tensor(out=lgi[:], in0=Pr[:, 0:36], in1=brt[:], op=ALU.add), r=[B_Pr, B_w], w=[B_lg[i % 4]])

            def stageR(i):
                lgi = lg2[i % 4]
                q = i % 2
                sd_, st, oh, t48, es8, e28, mk1, mk2, junkR = sd2[q], st2[q], oh2[q], t482[q], es82[q], e282[q], mk12[q], mk22[q], junkR2[q]
                R_ = [B_r2[q]]
                RL = [B_r2[q], B_lg[i % 4]]
                kb.op("dve", lambda e: e.tensor_reduce(out=sd_[:, 1:2], in_=lgi[:, 0:4], axis=AX.X, op=ALU.max), r=RL, w=R_)
                kb.op("dve", lambda e: e.tensor_scalar(out=sd_[:, 2:3], in0=sd_[:, 1:2], scalar1=-1.0, scalar2=None, op0=ALU.mult), r=R_, w=R_)
                kb.op("act", lambda e: e.activation(out=junkR[:, 0:4], in_=lgi[:, 0:4], func=AF.Exp, bias=sd_[:, 2:3], accum_out=st[:, 4:5]), r=RL, w=R_)
                kb.op("dve", lambda e: e.reciprocal(out=sd_[:, 3:4], in_=st[:, 4:5]), r=R_, w=R_)
                kb.op("dve", lambda e: e.tensor_scalar(out=oh[:], in0=lgi[:, 0:4], scalar1=sd_[:, 1:2], scalar2=None, op0=ALU.is_equal), r=RL, w=R_)
                kb.op("dve", lambda e: e.tensor_tensor(out=t48[:], in0=lgi[:, 4:36].rearrange("p (g j) -> p g j", g=4),
                                                       in1=oh[:].unsqueeze(2).to_broadcast([128, 4, 8]), op=ALU.mult), r=RL, w=R_)
                kb.op("dve", lambda e: e.tensor_reduce(out=es8[:], in_=t48[:].rearrange("p g j -> p j g"), axis=AX.X, op=ALU.add), r=R_, w=R_)
                kb.op("dve", lambda e: e.tensor_reduce(out=sd_[:, 4:5], in_=es8[:], axis=AX.X, op=ALU.max), r=R_, w=R_)
                kb.op("dve", lambda e: e.tensor_scalar(out=mk1[:], in0=es8[:], scalar1=sd_[:, 4:5], scalar2=None, op0=ALU.is_equal), r=R_, w=R_)
                kb.op("dve", lambda e: e.scalar_tensor_tensor(out=e28[:], in0=mk1[:], scalar=-1e30, in1=es8[:], op0=ALU.mult, op1=ALU.add), r=R_, w=R_)
                kb.op("dve", lambda e: e.tensor_reduce(out=sd_[:, 5:6], in_=e28[:], axis=AX.X, op=ALU.max), r=R_, w=R_)
                kb.op("dve", lambda e: e.tensor_scalar(out=mk2[:], in0=e28[:], scalar1=sd_[:, 5:6], scalar2=None, op0=ALU.is_equal), r=R_, w=R_)
                kb.op("dve", lambda e: e.tensor_tensor(out=sd_[:, 6:7], in0=sd_[:, 5:6], in1=sd_[:, 4:5], op=ALU.subtract), r=R_, w=R_)
                kb.op("act", lambda e: e.activation(out=st[:, 5:6], in_=sd_[:, 6:7], func=AF.Exp), r=R_, w=R_)
                kb.op("dve", lambda e: e.tensor_scalar(out=sd_[:, 7:8], in0=st[:, 5:6], scalar1=1.0, scalar2=None, op0=ALU.add), r=R_, w=R_)
                kb.op("dve", lambda e: e.reciprocal(out=sd_[:, 8:9], in_=sd_[:, 7:8]), r=R_, w=R_)
                kb.op("dve", lambda e: e.tensor_tensor(out=gta[:, i, 0:1], in0=sd_[:, 3:4], in1=sd_[:, 8:9], op=ALU.mult), r=R_, w=R_ + [B_rt])
                kb.op("dve", lambda e: e.tensor_tensor(out=gta[:, i, 1:2], in0=gta[:, i, 0:1], in1=st[:, 5:6], op=ALU.mult), r=R_, w=R_ + [B_rt])
                kb.op("dve", lambda e: e.tensor_tensor(out=A1a[:, i, :].rearrange("p (g j) -> p g j", g=4),
                                                       in0=oh[:].unsqueeze(2).to_broadcast([128, 4, 8]),
                                                       in1=mk1[:].unsqueeze(1).to_broadcast([128, 4, 8]), op=ALU.mult), r=R_, w=R_ + [B_rt])
                kb.op("dve", lambda e: e.tensor_tensor(out=A2a[:, i, :].rearrange("p (g j) -> p g j", g=4),
                                                       in0=oh[:].unsqueeze(2).to_broadcast([128, 4, 8]),
                                                       in1=mk2[:].unsqueeze(1).to_broadcast([128, 4, 8]), op=ALU.mult), r=R_, w=R_ + [B_rt])
            cur_rec = [None]

            def grp_done(g_):
                cur_rec[0].append(("grp", g_))

            def record(fn_stage, i):
                rec = []
                cur_rec[0] = rec

                def rop(e, fn, r=(), w=(), same=None):
                    (ingroup[0] if ingroup[0] is not None else rec).append((e, fn, list(r), list(w), same))
                kb.op = rop
                kb.dma = lambda q, sname, fn, r=(), w=(): rec.append(("dma", q, sname, fn, list(r), list(w)))
                try:
                    fn_stage(i)
                finally:
                    del kb.op
                    del kb.dma
                return rec

            def replay(t):
                if t[0] == "grp":
                    for t2 in t[1]:
                        kb.op(*t2)
                elif t[0] == "dma":
                    kb.dma(*t[1:])
                else:
                    kb.op(*t)

            def emit_x_pair(i2a):
                xa = record(stageX, i2a) if i2a < NT else []
                xb_ = record(stageX, i2a + 1) if i2a + 1 < NT else []
                for k_ in range(max(len(xa), len(xb_))):
                    if k_ < len(xa):
                        replay(xa[k_])
                    if k_ < len(xb_):
                        replay(xb_[k_])

            emit_x_pair(0)
            for i in range(0, NT, 2):
                emit_x_pair(i + 2)
                ra = record(stageR, i)
                rb = record(stageR, i + 1) if i + 1 < NT else []
                for k_ in range(max(len(ra), len(rb))):
                    if k_ < len(ra):
                        replay(ra[k_])
                    if k_ < len(rb):
                        replay(rb[k_])
            Ata = sb(sB, "Ata", [128, NT, 32], F32)
            B_Ata = kb.buf()
            for i0_ in range(0, NT, 16):
                n_ = min(16, NT - i0_)
                kb.op("dve", lambda e: e.tensor_tensor(out=Ata[:, i0_:i0_ + n_, :], in0=A1a[:, i0_:i0_ + n_, :], in1=A2a[:, i0_:i0_ + n_, :], op=ALU.add),
                      r=[B_rt], w=[B_Ata])
            Apre = sb(sB, "Apre", [128, NT, 32], F32)
            kb.op("pool", lambda e: e.memset(Apre[:, 0, :], 0.0), w=[B_Ata])
            for i in range(1, NT):
                kb.op("dve", lambda e: e.tensor_tensor(out=Apre[:, i, :], in0=Apre[:, i - 1, :], in1=Ata[:, i - 1, :], op=ALU.add),
                      r=[B_Ata], w=[B_Ata])
            for i in range(NT):
                bank, col = i // 16, (i % 16) * 32
                kb.op("pe", lambda e: e.matmul(Px[:, bank, col:col + 32], lhsT=ustr[:], rhs=Ata[:, i, :], start=True, stop=False),
                      r=[B_Ata, B_w], w=[B_Px2[bank]])
                kb.op("pe", lambda e: e.matmul(Px[:, bank, col:col + 32], lhsT=ones_f[:], rhs=Apre[:, i, :], start=False, stop=True),
                      r=[B_Ata, B_const], w=[B_Px2[bank]])
            for i in range(NT):
                kb.op("pe", lambda e: e.matmul(Pr2[:, 0:32], lhsT=ones_f[:], rhs=Ata[:, i, :], start=(i == 0), stop=(i == NT - 1)),
                      r=[B_Ata, B_const], w=[B_Pr2])
            for i0_ in range(0, NT, 16):
                n_ = min(16, NT - i0_)
                kb.op("dve", lambda e: e.tensor_copy(out=rka[:, i0_:i0_ + n_, :], in_=Px[:, i0_ // 16, 0:n_ * 32].rearrange("p (i e) -> p i e", e=32)),
                      r=[B_Px2[i0_ // 16]], w=[B_rt])
            kb.op("dve", lambda e: e.tensor_copy(out=base[:], in_=Pr2[:, 0:32]), r=[B_Pr2], w=[B_rt])
            kb.barrier()
        with ExitStack() as sC:
            thr = sb(sC, "thr", [128, NBLK, 32], F32)
            cmp_ = sb(sC, "cmp", [128, 16, 32], F32)
            nbk = sb(sC, "nbk", [128, 32], F32)
            tmp32 = sb(sC, "tmp32", [128, 32], F32)
            pend = sb(sC, "pend", [128, 32], F32)
            pst = sb(sC, "pst", [128, 32], F32)
            zer = sb(sC, "zer", [128, 32], F32)
            bef = sb(sC, "bef", [128, NBLK], F32)
            dst = sb(sC, "dst", [128, NT, 32], F32)
            prod = sb(sC, "prod", [128, 16, 32], F32)
            d1f = sb(sC, "d1f", [128, NT], F32)
            d2f = sb(sC, "d2f", [128, NT], F32)
            Q = [B_rt]
            kb.dma("sp", "const", lambda q: q.dma_start(out=thr[:].rearrange("p b e -> p (b e)"), in_=cd["blkthr"].partition_broadcast(128)), w=Q)
            kb.op("pool", lambda e: e.memset(nbk[:], 0.0), w=Q)
            kb.op("pool", lambda e: e.memset(zer[:], 0.0), w=Q)
            for b0 in range(0, NBLK, 16):
                nb_ = min(16, NBLK - b0)
                kb.op("dve", lambda e: e.tensor_tensor(out=cmp_[:, 0:nb_, :], in0=base[:].unsqueeze(1).to_broadcast([128, nb_, 32]),
                                                       in1=thr[:, b0:b0 + nb_, :], op=ALU.is_gt), r=Q, w=Q)
                kb.op("dve", lambda e: e.tensor_reduce(out=tmp32[:], in_=cmp_[:, 0:nb_, :].rearrange("p b e -> p e b"), axis=AX.X, op=ALU.add), r=Q, w=Q)
                kb.op("dve", lambda e: e.tensor_tensor(out=nbk[:], in0=nbk[:], in1=tmp32[:], op=ALU.add), r=Q, w=Q)
            kb.op("dve", lambda e: e.tensor_scalar(out=nbk[:], in0=nbk[:], scalar1=float(BS), scalar2=None, op0=ALU.mult), r=Q, w=Q)
            kb.op("dve", lambda e: e.tensor_tensor_scan(out=pend[:], data0=nbk[:], data1=zer[:], initial=0.0, op0=ALU.add, op1=ALU.add), r=Q, w=Q)
            kb.op("dve", lambda e: e.tensor_tensor(out=pst[:], in0=pend[:], in1=nbk[:], op=ALU.subtract), r=Q, w=Q)
            for b0 in range(0, NBLK, 16):
                nb_ = min(16, NBLK - b0)
                kb.op("dve", lambda e: e.tensor_tensor(out=cmp_[:, 0:nb_, :], in0=pend[:].unsqueeze(1).to_broadcast([128, nb_, 32]),
                                                       in1=thr[:, b0:b0 + nb_, :], op=ALU.is_le), r=Q, w=Q)
                kb.op("dve", lambda e: e.tensor_reduce(out=bef[:, b0:b0 + nb_], in_=cmp_[:, 0:nb_, :], axis=AX.X, op=ALU.add), r=Q, w=Q)
            kb.op("dve", lambda e: e.tensor_scalar(out=bef[:], in0=bef[:], scalar1=float(NE - 1), scalar2=None, op0=ALU.min), r=Q, w=Q)
            kb.op("dve", lambda e: e.tensor_copy(out=bexp[:], in_=bef[:]), r=Q, w=Q)
            iot = sb(sC, "iot", [128, 8], F32)
            idf = sb(sC, "idf", [128, NBLK], F32)
            kb.dma("sp", "const", lambda q: q.dma_start(out=iot[:], in_=cd["iota_pc"]), w=Q)
            kb.op("dve", lambda e: e.scalar_tensor_tensor(out=idf[:], in0=bef[:], scalar=128.0, in1=iot[:, 0:1].to_broadcast([128, NBLK]),
                                                          op0=ALU.mult, op1=ALU.add), r=Q, w=Q)
            kb.op("dve", lambda e: e.tensor_copy(out=idxw[:], in_=idf[:]), r=Q, w=Q)
            for i0_ in range(0, NT, 16):
                n_ = min(16, NT - i0_)
                kb.op("dve", lambda e: e.tensor_tensor(out=dst[:, i0_:i0_ + n_, :], in0=rka[:, i0_:i0_ + n_, :],
                                                       in1=pst[:].unsqueeze(1).to_broadcast([128, n_, 32]), op=ALU.add), r=Q, w=Q)
                for Aa, df in ((A1a, d1f), (A2a, d2f)):
                    kb.op("dve", lambda e: e.tensor_tensor(out=prod[:, 0:n_, :], in0=dst[:, i0_:i0_ + n_, :], in1=Aa[:, i0_:i0_ + n_, :], op=ALU.mult), r=Q, w=Q)
                    kb.op("dve", lambda e: e.tensor_reduce(out=df[:, i0_:i0_ + n_], in_=prod[:, 0:n_, :], axis=AX.X, op=ALU.add), r=Q, w=Q)
            kb.op("dve", lambda e: e.tensor_copy(out=d1i[:], in_=d1f[:]), r=Q, w=Q)
            kb.op("dve", lambda e: e.tensor_copy(out=d2i[:], in_=d2f[:]), r=Q, w=Q)
            kb.barrier()
        if "d1i" in dbg_d:
            kb.dma("sp", "dbg", lambda q: q.dma_start(out=dbg_d["d1i"], in_=d1i[:]), r=[B_rt])
            kb.dma("sp", "dbg", lambda q: q.dma_start(out=dbg_d["d2i"], in_=d2i[:]), r=[B_rt])
            kb.dma("sp", "dbg", lambda q: q.dma_start(out=dbg_d["bexp"], in_=bexp[:]), r=[B_rt])
            kb.dma("sp", "dbg", lambda q: q.dma_start(out=dbg_d["gta"], in_=gta[:]), r=[B_rt])
        if stage < 5:
            return
        with ExitStack() as sD:
            hb_ = [sb(sD, f"dhb{i}", [128, 1024], BF16) for i in range(4)]
            B_hb = kb.bufs(4)
            for i in range(NT):
                j = i % 4
                kb.dma("sp", f"dhb{j}", lambda q: q.dma_start(out=hb_[j][:], in_=hn_d[i * 128:(i + 1) * 128, :]), r=[B_hn[i]], w=[B_hb[j]])
                for di in (d1i, d2i):
                    kb.dma("pool", "disp", lambda q: q.indirect_dma_start(out=xs_d[:, :], out_offset=bass.IndirectOffsetOnAxis(ap=di[:, i:i + 1], axis=0),
                                                                           in_=hb_[j][:], in_offset=None), r=[B_hb[j], B_rt], w=[B_xs])
            kb.barrier()
        B_ys = kb.buf()
        with ExitStack() as sE:
            NWB = 3
            W1 = [sb(sE, f"eW1{i}", [128, 8, 512], BF16) for i in range(NWB)]
            W3 = [sb(sE, f"eW3{i}", [128, 8, 512], BF16) for i in range(NWB)]
            W2 = [sb(sE, f"eW2{i}", [128, 4, 1024], BF16) for i in range(NWB)]
            B_W = kb.bufs(NWB)
            xb = [sb(sE, f"exb{i}", [128, 1024], BF16) for i in range(2)]
            B_xb = kb.bufs(2)
            xT = [sb(sE, f"exT{i}", [128, 8, 128], BF16) for i in range(2)]
            sl = [sb(sE, f"esl{i}", [128, 512], F32) for i in range(2)]
            hid = [sb(sE, f"ehid{i}", [128, 512], BF16) for i in range(2)]
            hidT = [sb(sE, f"ehidT{i}", [128, 4, 128], BF16) for i in range(2)]
            ysb = [sb(sE, f"eys{i}", [128, 1024], F32) for i in range(2)]
            B_ysb = kb.bufs(2)
            B_xT, B_sl, B_hid, B_hidT = kb.bufs(2), kb.bufs(2), kb.bufs(2), kb.bufs(2)
            PTb = [ps(sE, f"ePT{i}", [128, 8, 128], BF16) for i in range(2)]
            Ph1 = [ps(sE, f"ePh1{i}", [128, 512], F32) for i in range(2)]
            Ph3 = [ps(sE, f"ePh3{i}", [128, 512], F32) for i in range(2)]
            Py = ps(sE, "ePy", [128, 2, 512], F32)
            B_PTb, B_Ph1, B_Ph3 = kb.bufs(2), kb.bufs(2), kb.bufs(2)
            B_Py = kb.buf()
            w1r, w3r, w2r = w1b_d, w3b_d, w2b_d
            B_xT2 = [kb.bufs(2), kb.bufs(2)]
            B_ysb2 = [kb.bufs(2), kb.bufs(2)]
            NB128 = NBLK * NSUB

            def load_w(sbi):
                k = sbi % NWB
                for Wt_, wr_ in ((W1, w1r), (W3, w3r), (W2, w2r)):
                    kb.dma("pool", f"ew{k}", lambda q: q.indirect_dma_start(out=Wt_[k][:].rearrange("p c f -> p (c f)"), out_offset=None, in_=wr_[:, :],
                           in_offset=bass.IndirectOffsetOnAxis(ap=idxw[:, sbi:sbi + 1], axis=0)), r=[B_rt, B_wconv], w=[B_W[k]])

            def load_x(b):
                j = b % 2
                kb.dma("sp", f"exb{j}", lambda q: q.dma_start(out=xb[j][:], in_=xs_d[b * 128:(b + 1) * 128, :]), r=[B_xs], w=[B_xb[j]])

            def stageA(b):
                j = b % 2
                k = (b // NSUB) % NWB
                for c in range(8):
                    kb.op("pe", lambda e: e.transpose(out=PTb[j][:, c, :], in_=xb[j][:].rearrange("s (p c) -> s c p", c=8)[:, c, :], identity=ident_bf[:]),
                          r=[B_xb[j], B_const], w=[B_PTb[j]])
                kb.op("dve", lambda e: e.tensor_copy(out=xT[j][:, 0:4, :], in_=PTb[j][:, 0:4, :]), r=[B_PTb[j]], w=[B_xT2[j][0]])
                kb.op("dve", lambda e: e.tensor_copy(out=xT[j][:, 4:8, :], in_=PTb[j][:, 4:8, :]), r=[B_PTb[j]], w=[B_xT2[j][1]])
                for c in range(8):
                    kb.op("pe", lambda e: e.matmul(Ph1[j][:], lhsT=xT[j][:, c, :], rhs=W1[k][:, c, :], start=(c == 0), stop=(c == 7)),
                          r=[B_xT2[j][c // 4], B_W[k]], w=[B_Ph1[j]])
                for c in range(8):
                    kb.op("pe", lambda e: e.matmul(Ph3[j][:], lhsT=xT[j][:, c, :], rhs=W3[k][:, c, :], start=(c == 0), stop=(c == 7)),
                          r=[B_xT2[j][c // 4], B_W[k]], w=[B_Ph3[j]])
                kb.op("act", lambda e: e.activation(out=sl[j][:], in_=Ph1[j][:], func=AF.Silu), r=[B_Ph1[j]], w=[B_sl[j]])
                kb.op("dve", lambda e: e.tensor_tensor(out=hid[j][:], in0=sl[j][:], in1=Ph3[j][:], op=ALU.mult), r=[B_sl[j], B_Ph3[j]], w=[B_hid[j]])

            def stageB(b):
                j = b % 2
                k = (b // NSUB) % NWB
                for c in range(4):
                    kb.op("pe", lambda e: e.transpose(out=PTb[j][:, c, :], in_=hid[j][:].rearrange("s (p c) -> s c p", c=4)[:, c, :], identity=ident_bf[:]),
                          r=[B_hid[j], B_const], w=[B_PTb[j]])
                kb.op("act", lambda e: e.copy(out=hidT[j][:], in_=PTb[j][:, 0:4, :]), r=[B_PTb[j]], w=[B_hidT[j]])
                for half in range(2):
                    for c in range(4):
                        kb.op("pe", lambda e: e.matmul(Py[:, half, :], lhsT=hidT[j][:, c, :], rhs=W2[k][:, c, half * 512:(half + 1) * 512],
                                                       start=(c == 0), stop=(c == 3)), r=[B_hidT[j], B_W[k]], w=[B_Py])
                kb.op("act", lambda e: e.copy(out=ysb[j][:, 0:512], in_=Py[:, 0, :]), r=[B_Py], w=[B_ysb2[j][0]])
                kb.op("dve", lambda e: e.tensor_copy(out=ysb[j][:, 512:1024], in_=Py[:, 1, :]), r=[B_Py], w=[B_ysb2[j][1]])
                kb.dma("sp", "yst", lambda q: q.dma_start(out=ys_d[b * 128:(b + 1) * 128, :], in_=ysb[j][:]), r=B_ysb2[j], w=[B_ys])

            for s0 in range(min(NWB, NBLK)):
                load_w(s0)
            load_x(0)
            for b in range(NB128 + 1):
                if b + 1 < NB128:
                    load_x(b + 1)
                if b < NB128:
                    stageA(b)
                if b >= 1:
                    stageB(b - 1)
                    if (b - 1) % NSUB == NSUB - 1:
                        nxt = (b - 1) // NSUB + NWB
                        if nxt < NBLK:
                            load_w(nxt)
            kb.barrier()
        with ExitStack() as sF:
            NCB = 4
            y1 = [sb(sF, f"cy1{i}", [128, 1024], F32) for i in range(NCB)]
            y2 = [sb(sF, f"cy2{i}", [128, 1024], F32) for i in range(NCB)]
            xr = [sb(sF, f"cxr{i}", [128, 1024], F32) for i in range(NCB)]
            B_y1, B_y2, B_xr = kb.bufs(NCB), kb.bufs(NCB), kb.bufs(NCB)

            def cload(i):
                j = i % NCB
                rows = slice(i * 128, (i + 1) * 128)
                kb.dma("pool", f"cg1{j}", lambda q: q.indirect_dma_start(out=y1[j][:], out_offset=None, in_=ys_d[:, :],
                       in_offset=bass.IndirectOffsetOnAxis(ap=d1i[:, i:i + 1], axis=0)), r=[B_ys, B_rt], w=[B_y1[j]])
                kb.dma("pool", f"cg2{j}", lambda q: q.indirect_dma_start(out=y2[j][:], out_offset=None, in_=ys_d[:, :],
                       in_offset=bass.IndirectOffsetOnAxis(ap=d2i[:, i:i + 1], axis=0)), r=[B_ys, B_rt], w=[B_y2[j]])
                kb.dma("act", f"cxr{j}", lambda q: q.dma_start(out=xr[j][:], in_=out_d[rows, :]), r=[B_out[i]], w=[B_xr[j]])

            for i in range(min(NCB - 1, NT)):
                cload(i)
            for i in range(NT):
                j = i % NCB
                rows = slice(i * 128, (i + 1) * 128)
                if i + NCB - 1 < NT:
                    cload(i + NCB - 1)
                for half in range(2):
                    hs = slice(half * 512, (half + 1) * 512)
                    kb.op("dve", lambda e: e.scalar_tensor_tensor(out=xr[j][:, hs], in0=y1[j][:, hs], scalar=gta[:, i, 0:1], in1=xr[j][:, hs],
                                                                  op0=ALU.mult, op1=ALU.add), r=[B_y1[j], B_rt], w=[B_xr[j]])
                    kb.op("dve", lambda e: e.scalar_tensor_tensor(out=xr[j][:, hs], in0=y2[j][:, hs], scalar=gta[:, i, 1:2], in1=xr[j][:, hs],
                                                                  op0=ALU.mult, op1=ALU.add), r=[B_y2[j], B_rt], w=[B_xr[j]])
                kb.dma("sp", "ost2", lambda q: q.dma_start(out=out_d[rows, :], in_=xr[j][:]), r=[B_xr[j]], w=[B_out[i]])
            kb.barrier()


def _shared_maps(inp, consts):
    f = np.float32
    g = lambda k: np.ascontiguousarray(np.asarray(inp[k], dtype=f)[0])
    m = {}
    m["w_in"] = g("w_in")
    m["w_branch_da"] = g("w_branch_da")
    m["w_branch_ml"] = g("w_branch_ml")
    m["w_gate"] = g("w_gate")
    m["w_out"] = g("w_out")
    m["w1"] = g("w1")
    m["w3"] = g("w3")
    m["w2"] = g("w2")
    m["w_rt"] = np.ascontiguousarray(np.concatenate([g("w_group"), g("w_router")], axis=1))
    m["attn_norm_g"] = g("attn_norm_g")[None, :]
    m["ffn_norm_g"] = g("ffn_norm_g")[None, :]
    m["da_out_norm_g"] = g("da_out_norm_g")[None, :]
    m["ml_out_norm_g"] = g("ml_out_norm_g")[None, :]
    m["b_rt"] = np.concatenate([g("b_group"), g("b_router")])[None, :]
    m["b_if"] = np.concatenate([g("ml_i_bias"), g("ml_f_bias")])[None, :]
    m["lamv"] = np.concatenate([g("da_lambda_q1"), g("da_lambda_k1"), g("da_lambda_q2"), g("da_lambda_k2")])[None, :]
    m["gqk_col"] = np.ascontiguousarray(np.stack([np.tile(g("da_q_norm_g"), 2), np.tile(g("da_k_norm_g"), 2)], axis=1))
    cw = g("ml_conv_w")
    m["cw_col"] = np.ascontiguousarray(cw.reshape(4, 8, 128).transpose(2, 1, 0).reshape(128, 32))
    m["cb_col"] = np.ascontiguousarray(g("ml_conv_b").reshape(8, 128).T)
    m["bg_col"] = np.ascontiguousarray(g("b_gate").reshape(16, 128).T)
    for k, v in consts.items():
        m["c_" + k] = v
    return m


_CACHE = {}


def kernel(**inputs):
    x = np.asarray(inputs["x"], dtype=np.float32)
    B, S, _ = x.shape
    key = (S,)
    if key not in _CACHE:
        _CACHE[key] = build_nc(S)
    nc, consts = _CACHE[key]
    shared = _shared_maps(inputs, consts)
    in_maps = []
    for b in range(B):
        m = dict(shared)
        m["x"] = np.ascontiguousarray(x[b])
        in_maps.append(m)
    res = run_bass_kernel_spmd(nc, in_maps, core_ids=list(range(B)))
    return np.stack([np.asarray(r["out"], dtype=np.float32) for r in res.results], axis=0)
```

```python
import math
from contextlib import ExitStack
import numpy as np
import ml_dtypes
import concourse.bass as bass
import concourse.mybir as mybir
from concourse.bass_utils import run_bass_kernel_spmd

F32 = mybir.dt.float32
BF16 = mybir.dt.bfloat16
I32 = mybir.dt.int32
AF = mybir.ActivationFunctionType
ALU = mybir.AluOpType
AX = mybir.AxisListType

D = 1024
NH = 4
IN_W = 3592
OFF_DA_Q, OFF_DA_K, OFF_DA_V = 0, 512, 1024
OFF_ML_QK, OFF_ML_V, OFF_ML_O, OFF_ML_I = 1536, 2560, 3072, 3584
NE = 32
EPS = 1e-6
LAM_INIT = 0.8 - 0.6 * math.exp(-0.3 * 0)
SLOPES = [2.0 ** (-8.0 * (i + 1) / NH) for i in range(NH)]
SKIP_T = 60.0
NOFF = 36
OFF0 = 31
ML_LOOKAHEAD = 3
BS = 256


class Buf:
    __slots__ = ("w", "r", "name")

    def __init__(self, name=""):
        self.w = None
        self.r = {}
        self.name = name


class KB:
    def __init__(self, nc, es):
        self.nc = nc
        self.es = es
        self.eng = {"pe": nc.tensor, "act": nc.scalar, "dve": nc.vector, "pool": nc.gpsimd, "sp": nc.sync}
        self.sem = {k: es.enter_context(nc.semaphore("sem_" + k)) for k in self.eng}
        self.cnt = {k: 0 for k in self.eng}
        self.seen = {k: {} for k in self.eng}
        self.dsem = {}
        self.dcnt = {}
        self.nbuf = 0

    def buf(self, name=""):
        self.nbuf += 1
        return Buf(name)

    def bufs(self, n, name=""):
        return [self.buf(name + str(i)) for i in range(n)]

    def dma_sem(self, name):
        if name not in self.dsem:
            self.dsem[name] = self.es.enter_context(self.nc.semaphore("dsem_" + name))
            self.dcnt[name] = 0
        return name

    def _semh(self, key):
        return self.sem[key] if key in self.sem else self.dsem[key]

    def _wait(self, e, reads, writes, same=True, skip=None):
        deps = {}
        for b in reads:
            if b.w is not None:
                k, v = b.w
                deps[k] = max(deps.get(k, 0), v)
        for b in writes:
            if b.w is not None:
                k, v = b.w
                deps[k] = max(deps.get(k, 0), v)
            for k, v in b.r.items():
                deps[k] = max(deps.get(k, 0), v)
        for k, v in deps.items():
            if k == e and not same:
                continue
            if k == skip:
                continue
            if k in self.dcnt:
                v = self.dcnt[k]
            if self.seen[e].get(k, 0) >= v:
                continue
            self.eng[e].wait_ge(self._semh(k), v)
            self.seen[e][k] = v

    def op(self, e, fn, r=(), w=(), same=None):
        if same is None:
            same = (e != "pe")
        self._wait(e, r, w, same)
        inst = fn(self.eng[e])
        self.cnt[e] += 1
        inst.then_inc(self.sem[e], 1)
        ev = (e, self.cnt[e])
        for b in w:
            b.w = ev
            b.r = {}
        for b in r:
            if b not in w:
                b.r[e] = self.cnt[e]
        return inst

    def dma(self, q, sname, fn, r=(), w=()):
        self.dma_sem(sname)
        self._wait(q, r, w, True, skip=sname)
        inst = fn(self.eng[q])
        self.dcnt[sname] += 16
        inst.then_inc(self.dsem[sname], 16)
        ev = (sname, self.dcnt[sname])
        for b in w:
            b.w = ev
            b.r = {}
        for b in r:
            if b not in w:
                b.r[sname] = self.dcnt[sname]
        return inst

    def barrier(self):
        evs = [(k, self.cnt[k]) for k in self.sem if self.cnt[k] > 0] + [(k, v) for k, v in self.dcnt.items() if v > 0]
        for e in self.eng:
            for k, v in evs:
                if k == e:
                    continue
                if self.seen[e].get(k, 0) >= v:
                    continue
                self.eng[e].wait_ge(self._semh(k), v)
                self.seen[e][k] = v

    def wait_all(self, e, bufs):
        self._wait(e, bufs, bufs, True)


def _mk_consts(S):
    c = {}
    bf = ml_dtypes.bfloat16
    c["ident_bf"] = np.eye(128, dtype=np.float32).astype(bf)
    c["ident_f"] = np.eye(128, dtype=np.float32)
    blk = np.zeros((128, 128), np.float32)
    blk[:64, :64] = 1.0 / 64
    blk[64:, 64:] = 1.0 / 64
    c["blk64"] = blk.astype(bf)
    k = np.arange(128)[:, None]
    q = np.arange(128)[None, :]
    c["cmask"] = (k <= q).astype(np.float32).astype(bf)
    c["negm"] = np.where(k <= q, 0.0, -30000.0).astype(np.float32)
    c["ones_f"] = np.ones((128, 128), np.float32)
    c["ustrict"] = (k < q).astype(np.float32)
    tab = np.zeros((128, NH * NOFF), np.float32)
    for h in range(NH):
        for j in range(NOFF):
            tab[:, h * NOFF + j] = SLOPES[h] * (np.arange(128) + 128.0 * (j - OFF0))
    c["atab"] = tab
    c["iota_pc"] = (np.arange(8)[None, :] * 128.0 + np.arange(128)[:, None]).astype(np.float32)
    nb = (2 * S) // BS + NE
    c["blkthr"] = np.tile((np.arange(nb, dtype=np.float32) * float(BS))[None, :, None], (1, 1, NE)).reshape(1, nb * NE)
    return c


CONST_DT = {"ident_bf": BF16, "ident_f": F32, "blk64": BF16, "cmask": BF16, "negm": F32, "ones_f": F32,
            "ustrict": F32, "atab": F32, "blkthr": F32, "iota_pc": F32}


def build_nc(S=4096, stage=99, dbg=None):
    NT = S // 128
    NB5 = S // 512
    CAP = 2 * S + NE * BS
    nc = bass.Bass("TRN2", target_bir_lowering=False)
    consts = _mk_consts(S)

    def din(name, shape, dt=F32):
        return nc.dram_tensor(name, list(shape), dt, kind="ExternalInput").ap()

    x_d = din("x", [S, D])
    w_in = din("w_in", [D, IN_W])
    w_bda = din("w_branch_da", [512, D])
    w_bml = din("w_branch_ml", [512, D])
    w_gate = din("w_gate", [D, 2 * D])
    w_out = din("w_out", [D, D])
    w1 = din("w1", [NE, D, 512])
    w3 = din("w3", [NE, D, 512])
    w2 = din("w2", [NE, 512, D])
    wr_d = din("w_rt", [D, 36])
    g_attn = din("attn_norm_g", [1, D])
    g_ffn = din("ffn_norm_g", [1, D])
    g_dao = din("da_out_norm_g", [1, 128])
    g_mlo = din("ml_out_norm_g", [1, 512])
    b_rt = din("b_rt", [1, 36])
    b_if = din("b_if", [1, 8])
    lamv = din("lamv", [1, 256])
    gqk_col = din("gqk_col", [128, 2])
    cw_col = din("cw_col", [128, 8 * 4])
    cb_col = din("cb_col", [128, 8])
    bg_col = din("bg_col", [128, 16])
    cd = {k: din("c_" + k, list(v.shape), CONST_DT[k]) for k, v in consts.items()}
    out_d = nc.dram_tensor("out", [S, D], F32, kind="ExternalOutput").ap()
    dbg_d = {}
    if dbg:
        for k, (shape, dt) in dbg.items():
            dbg_d[k] = nc.dram_tensor("dbg_" + k, list(shape), dt, kind="ExternalOutput").ap()
    xs_d = nc.dram_tensor("xs_scr", [CAP, D], BF16).ap()
    ys_d = nc.dram_tensor("ys_scr", [CAP, D], F32).ap()
    hn_d = nc.dram_tensor("hn_scr", [S, D], BF16).ap()
    w1b_d = nc.dram_tensor("w1b_scr", [NE * 128, 4096], BF16).ap()
    w3b_d = nc.dram_tensor("w3b_scr", [NE * 128, 4096], BF16).ap()
    w2b_d = nc.dram_tensor("w2b_scr", [NE * 128, 4096], BF16).ap()

    es = ExitStack()
    with es:
        kb = KB(nc, es)

        def sb(stack, name, shape, dt):
            return stack.enter_context(nc.sbuf_tensor(name, list(shape), dt))

        def ps(stack, name, shape, dt=F32):
            return stack.enter_context(nc.psum_tensor(name, list(shape), dt))

        ident_bf = sb(es, "ident_bf", [128, 128], BF16)
        ident_f = sb(es, "ident_f", [128, 128], F32)
        ones_f = sb(es, "ones_f", [128, 128], F32)
        B_const = kb.buf("const")
        for t, k in ((ident_bf, "ident_bf"), (ident_f, "ident_f"), (ones_f, "ones_f")):
            kb.dma("sp", "const", lambda q, t=t, k=k: q.dma_start(out=t[:], in_=cd[k]), w=[B_const])
        zero_bf = sb(es, "zero_bf", [128, 2048], BF16)
        B_zero = kb.buf("zero")
        for zi in range(4):
            kb.op("pool", lambda e: e.memset(zero_bf[:, zi * 512:(zi + 1) * 512], 0.0), w=[B_zero])

        hT = sb(es, "hT", [128, 8, S], BF16)
        B_hT = kb.bufs(NT, "hT")
        es_mix = ExitStack()
        y_daT = sb(es_mix, "y_daT", [128, 4, S], BF16)
        B_ydaT = kb.bufs(NT * NH, "ydaT")
        B_ymlT = kb.bufs(NT * NH, "ymlT")

        B_xs = kb.buf()
        zf_rows = list(range(0, CAP, 256)) if stage >= 4 else []
        zf_per = (len(zf_rows) + NT - 1) // NT
        with ExitStack() as s1:
            gat = sb(s1, "gat", [128, D], F32)
            B_gat = kb.buf()
            kb.dma("sp", "const", lambda q: q.dma_start(out=gat[:], in_=g_attn.partition_broadcast(128)), w=[B_gat])
            xt = [sb(s1, f"xt{i}", [128, D], F32) for i in range(2)]
            xn = [sb(s1, f"xn{i}", [128, D], BF16) for i in range(2)]
            junk = sb(s1, "junk1", [128, D], F32)
            ssq = sb(s1, "ssq", [128, NT], F32)
            rst = sb(s1, "rst", [128, NT], F32)
            rsd = sb(s1, "rsd", [128, NT], F32)
            ssq2 = sb(s1, "ssq2", [128, 2 * NT], F32)
            pT = [ps(s1, f"pT{i}", [128, 8, 128], BF16) for i in range(2)]
            B_xt, B_xn, B_pT = kb.bufs(2), kb.bufs(2), kb.bufs(2)
            B_junk, B_ss = kb.buf(), kb.bufs(NT)
            for i in range(NT):
                j = i % 2
                kb.dma("sp", f"xt{j}", lambda q: q.dma_start(out=xt[j][:], in_=x_d[i * 128:(i + 1) * 128, :]), w=[B_xt[j]])
                for r0 in zf_rows[i * zf_per:(i + 1) * zf_per]:
                    kb.dma("sp", "xsz", lambda q: q.dma_start(out=xs_d[r0:r0 + 256, :].rearrange("(p a) d -> p (a d)", a=2), in_=zero_bf[:, 0:2048]),
                           r=[B_zero], w=[B_xs])
                for hf in range(2):
                    kb.op("act", lambda e: e.activation(out=junk[:, hf * 512:(hf + 1) * 512], in_=xt[j][:, hf * 512:(hf + 1) * 512],
                                                        func=AF.Square, accum_out=ssq2[:, 2 * i + hf:2 * i + hf + 1]),
                          r=[B_xt[j]], w=[B_junk, B_ss[i]])
                kb.op("act", lambda e: e.activation(out=rst[:, i:i + 1], in_=ssq2[:, 2 * i:2 * i + 1], func=AF.Identity,
                                                    bias=ssq2[:, 2 * i + 1:2 * i + 2]), r=[B_ss[i]], w=[B_ss[i]])
                kb.op("act", lambda e: e.activation(out=rst[:, i:i + 1], in_=rst[:, i:i + 1], func=AF.Sqrt, scale=1.0 / D, bias=EPS),
                      r=[B_ss[i]], w=[B_ss[i]])
                kb.op("dve", lambda e: e.reciprocal(out=rsd[:, i:i + 1], in_=rst[:, i:i + 1]), r=[B_ss[i]], w=[B_ss[i]])
                for hf in range(2):
                    kb.op("dve", lambda e: e.scalar_tensor_tensor(out=xn[j][:, hf * 512:(hf + 1) * 512], in0=xt[j][:, hf * 512:(hf + 1) * 512],
                                                                  scalar=rsd[:, i:i + 1], in1=gat[:, hf * 512:(hf + 1) * 512],
                                                                  op0=ALU.mult, op1=ALU.mult),
                          r=[B_xt[j], B_ss[i], B_gat], w=[B_xn[j]])
                for c in range(8):
                    kb.op("pe", lambda e: e.transpose(out=pT[j][:, c, :], in_=xn[j][:, c * 128:(c + 1) * 128], identity=ident_bf[:]),
                          r=[B_xn[j], B_const], w=[B_pT[j]])
                for hf in range(2):
                    kb.op("act", lambda e: e.copy(out=hT[:, hf * 4:hf * 4 + 4, i * 128:(i + 1) * 128], in_=pT[j][:, hf * 4:hf * 4 + 4, :]),
                          r=[B_pT[j]], w=[B_hT[i]])

        kb.barrier()
        if "hT" in dbg_d:
            kb.dma("sp", "dbg", lambda q: q.dma_start(out=dbg_d["hT"], in_=hT[:]), r=B_hT)

        B_wconv = kb.buf()
        conv_state = [0]

        def conv_next(n=1):
            if stage < 5:
                return
            for _ in range(n):
                e_ = conv_state[0]
                if e_ >= NE:
                    return
                conv_state[0] += 1
                for src_, dst_, c_ in ((w1, w1b_d, 8), (w3, w3b_d, 8), (w2, w2b_d, 4)):
                    kb.dma("pool", "wconv", lambda q: q.dma_start(out=dst_[e_ * 128:(e_ + 1) * 128, :],
                           in_=src_[e_].rearrange("(p c) f -> p (c f)", c=c_)), w=[B_wconv])
        if stage >= 2:
            _da_phase(nc, kb, S, hT, B_hT, y_daT, B_ydaT, w_in, cd, gqk_col, g_dao, lamv, ident_bf, zero_bf, B_const, B_zero, sb, ps, dbg_d, conv_next)
        conv_next(NE)
        y_mlT = sb(es_mix, "y_mlT", [128, 4, S], BF16)
        if stage >= 3:
            _ml_phase(nc, kb, S, hT, B_hT, y_mlT, B_ymlT, w_in, cd, cw_col, cb_col, g_mlo, b_if, ident_bf, ident_f, ones_f,
                      B_const, sb, ps, dbg_d)
        if stage >= 4:
            _merge_moe(nc, kb, S, hT, B_hT, y_daT, B_ydaT, y_mlT, B_ymlT, es_mix, x_d, out_d, w_bda, w_bml, w_gate, w_out,
                       bg_col, g_ffn, wr_d, b_rt, w1, w3, w2, xs_d, ys_d, hn_d, cd, ident_bf, ident_f, ones_f, zero_bf,
                       B_const, B_zero, sb, ps, dbg_d, stage, B_xs, (w1b_d, w3b_d, w2b_d, B_wconv))
        else:
            es_mix.close()

        allb = []
        for k in list(kb.dsem.keys()):
            b = Buf()
            b.w = (k, kb.dcnt[k])
            allb.append(b)
        for k in kb.sem:
            if kb.cnt[k] > 0:
                b = Buf()
                b.w = (k, kb.cnt[k])
                allb.append(b)
        kb._wait("sp", allb, [], True)
    return nc, consts


def _da_phase(nc, kb, S, hT, B_hT, y_daT, B_ydaT, w_in, cd, gqk_col, g_dao, lamv, ident_bf, zero_bf, B_const, B_zero, sb, ps, dbg_d, conv_next):
    NT = S // 128
    NB5 = S // 512
    with ExitStack() as s:
        blk64 = sb(s, "blk64", [128, 128], BF16)
        cmask = sb(s, "cmask", [128, 128], BF16)
        atab = sb(s, "atab", [128, NH * NOFF], F32)
        gqk = sb(s, "gqk", [128, 2], F32)
        gdo = sb(s, "gdo", [128, 128], F32)
        lam_t = sb(s, "lam_t", [128, 256], F32)
        B_c = kb.buf()
        for t, src in ((blk64, cd["blk64"]), (cmask, cd["cmask"]), (atab, cd["atab"]), (gqk, gqk_col),
                       (gdo, g_dao.partition_broadcast(128)), (lam_t, lamv.partition_broadcast(128))):
            kb.dma("sp", "const", lambda q, t=t, src=src: q.dma_start(out=t[:], in_=src), w=[B_c])
        lj = sb(s, "lj", [128, 128], F32)
        ls = sb(s, "ls", [128, 4], F32)
        neglam = sb(s, "neglam", [128, 1], F32)
        B_l = kb.buf()
        lv = lam_t[:].rearrange("p (a b d) -> p a b d", a=2, b=2)
        kb.op("dve", lambda e: e.tensor_tensor(out=lj[:].rearrange("p (a d) -> p a d", a=2), in0=lv[:, :, 0, :], in1=lv[:, :, 1, :],
                                               op=ALU.mult), r=[B_c], w=[B_l])
        kb.op("dve", lambda e: e.tensor_reduce(out=ls[:, 0:2], in_=lj[:].rearrange("p (a d) -> p a d", a=2), axis=AX.X, op=ALU.add),
              r=[B_l], w=[B_l])
        kb.op("act", lambda e: e.activation(out=ls[:, 2:4], in_=ls[:, 0:2], func=AF.Exp), r=[B_l], w=[B_l])
        kb.op("dve", lambda e: e.tensor_tensor(out=neglam[:], in0=ls[:, 3:4], in1=ls[:, 2:3], op=ALU.subtract), r=[B_l], w=[B_l])
        kb.op("dve", lambda e: e.tensor_scalar(out=neglam[:], in0=neglam[:], scalar1=-LAM_INIT, scalar2=None, op0=ALU.add),
              r=[B_l], w=[B_l])
        kb.op("dve", lambda e: e.tensor_scalar(out=gdo[:], in0=gdo[:], scalar1=1.0 - LAM_INIT, scalar2=None, op0=ALU.mult),
              r=[B_c], w=[B_c])

        P3 = [ps(s, f"daP{i}", [128, 512], F32) for i in range(3)]
        B_P3 = kb.bufs(3)
        acc4 = ps(s, "daAcc", [128, 4, 512], F32)
        B_acc = kb.buf()
        ptr = ps(s, "daPtr", [128, 8, 128], BF16)
        B_ptr = kb.buf()
        pcnt = [0]

        def nextP():
            i = pcnt[0] % 3
            pcnt[0] += 1
            return P3[i], B_P3[i]

        Vda = sb(s, "Vda", [128, NT, 4, 130], BF16)
        B_V = kb.bufs(NT)
        B_Vones = kb.buf()
        kb.op("pool", lambda e: e.memset(Vda[:, :, :, 128:130], 1.0), w=[B_Vones])
        with ExitStack() as sv:
            wv = sb(sv, "wv", [128, 8, 512], BF16)
            B_wv = kb.buf()
            kb.dma("pool", "wv", lambda q: q.dma_start(out=wv[:], in_=w_in[:, OFF_DA_V:OFF_DA_V + 512].rearrange("(c p) f -> p c f", p=128)),
                   w=[B_wv])
            for i in range(NT):
                P, BP = nextP()
                for c in range(8):
                    kb.op("pe", lambda e: e.matmul(P[:], lhsT=hT[:, c, i * 128:(i + 1) * 128], rhs=wv[:, c, :], start=(c == 0), stop=(c == 7)),
                          r=[B_hT[i], B_wv], w=[BP])
                kb.op("act", lambda e: e.copy(out=Vda[:, i, :, 0:128], in_=P[:].rearrange("p (h d) -> p h d", h=4)),
                      r=[BP, B_Vones], w=[B_V[i]])
        kb.barrier()

        wqk = [sb(s, f"wqk{i}", [128, 8, 256], BF16) for i in range(2)]
        B_wqk = kb.bufs(2)
        qkT = [sb(s, f"qkT{i}", [128, 2, S], BF16) for i in range(2)]
        B_qk = [kb.bufs(NB5 * 2) for _ in range(2)]
        sq_sb = [sb(s, f"sq_sb{i}", [128, 512], BF16) for i in range(2)]
        sd_sb = [sb(s, f"sd_sb{i}", [128, 512], F32) for i in range(2)]
        B_sq, B_sd = kb.bufs(2), kb.bufs(2)
        Et = [sb(s, f"Et{i}", [128, 512], BF16) for i in range(4)]
        B_Et = kb.bufs(4)
        ecnt = 0
        o_sb = sb(s, "o_sb", [128, 4, 128], F32)
        t_sb = sb(s, "t_sb", [128, 4, 128], F32)
        y_sb = sb(s, "y_sb", [128, 4, 128], BF16)
        rr = sb(s, "rr", [128, 16], F32)
        rra = sb(s, "rra", [128, 8], F32)
        B_o = kb.buf()

        def load_w(h):
            hb = h % 2
            kb.dma("pool", f"wqk{hb}", lambda q: q.dma_start(out=wqk[hb][:, :, 0:128],
                   in_=w_in[:, OFF_DA_Q + h * 128:OFF_DA_Q + (h + 1) * 128].rearrange("(c p) f -> p c f", p=128)), w=[B_wqk[hb]])
            kb.dma("pool", f"wqk{hb}", lambda q: q.dma_start(out=wqk[hb][:, :, 128:256],
                   in_=w_in[:, OFF_DA_K + h * 128:OFF_DA_K + (h + 1) * 128].rearrange("(c p) f -> p c f", p=128)), w=[B_wqk[hb]])

        load_w(0)
        for h in range(NH):
            hb = h % 2
            if h + 1 < NH:
                load_w(h + 1)
            slope = SLOPES[h]
            k2 = 0
            for tb in range(NB5):
                for which in range(2):
                    P, BP = nextP()
                    for c in range(8):
                        kb.op("pe", lambda e: e.matmul(P[:], lhsT=wqk[hb][:, c, which * 128:(which + 1) * 128],
                                                       rhs=hT[:, c, tb * 512:(tb + 1) * 512], start=(c == 0), stop=(c == 7)),
                              r=B_hT[tb * 4:tb * 4 + 4] + [B_wqk[hb]], w=[BP])
                    kk = k2 % 2
                    k2 += 1
                    kb.op("act", lambda e: e.activation(out=sq_sb[kk][:], in_=P[:], func=AF.Square), r=[BP], w=[B_sq[kk]])
                    P2, BP2 = nextP()
                    kb.op("pe", lambda e: e.matmul(P2[:], lhsT=blk64[:], rhs=sq_sb[kk][:], start=True, stop=True),
                          r=[B_sq[kk], B_c], w=[BP2])
                    kb.op("act", lambda e: e.activation(out=sd_sb[kk][:], in_=P2[:], func=AF.Ln, bias=EPS), r=[BP2], w=[B_sd[kk]])
                    kb.op("act", lambda e: e.activation(out=sd_sb[kk][:], in_=sd_sb[kk][:], func=AF.Exp, scale=-0.5), r=[B_sd[kk]], w=[B_sd[kk]])
                    kb.op("dve", lambda e: e.scalar_tensor_tensor(out=qkT[hb][:, which, tb * 512:(tb + 1) * 512], in0=P[:],
                                                                  scalar=gqk[:, which:which + 1], in1=sd_sb[kk][:],
                                                                  op0=ALU.mult, op1=ALU.mult),
                          r=[BP, B_sd[kk], B_c], w=[B_qk[hb][tb * 2 + which]])
            if h == 0 and "wqk" in dbg_d:
                kb.dma("sp", "dbg", lambda q: q.dma_start(out=dbg_d["wqk"], in_=wqk[0][:]), r=[B_wqk[0]])
            if h == 0 and "sd" in dbg_d:
                kb.dma("sp", "dbg", lambda q: q.dma_start(out=dbg_d["sd"], in_=sd_sb[0][:]), r=[B_sd[0]])
                kb.dma("sp", "dbg", lambda q: q.dma_start(out=dbg_d["sq"], in_=sq_sb[0][:]), r=[B_sq[0]])
            if h == 0 and "qkT" in dbg_d:
                kb.dma("sp", "dbg", lambda q: q.dma_start(out=dbg_d["qkT"], in_=qkT[0][:]), r=B_qk[0])
            sub = 128 if slope * 511 > 32.0 else 512
            units = []
            for qb in range(NB5):
                kt_max_ = 4 * qb + 3
                kt_min_ = max(0, int(math.ceil((qb * 512 - SKIP_T / slope - 127) / 128.0)))
                for kt in range(kt_min_, kt_max_ + 1):
                    for m in range(2):
                        units.append((qb, kt, m, kt == kt_min_ and m == 0, kt == kt_max_ and m == 1))
            ustate = {}
            pending = []

            def emit_qk(u):
                nonlocal ecnt
                qb, kt, m, _, _ = u
                q0 = qb * 512
                jj = kt - 4 * qb
                j_lo = max(0, jj)
                P, BP = nextP()
                kb.op("pe", lambda e: e.matmul(P[:, j_lo * 128:512], lhsT=qkT[hb][m * 64:(m + 1) * 64, 1, kt * 128:(kt + 1) * 128],
                                               rhs=qkT[hb][m * 64:(m + 1) * 64, 0, q0 + j_lo * 128:q0 + 512], start=True, stop=True),
                      r=[B_qk[hb][(kt // 4) * 2 + 1], B_qk[hb][qb * 2]], w=[BP])
                ei = ecnt % 4
                ecnt += 1
                E, BE = Et[ei], B_Et[ei]
                if sub == 512:
                    col = h * NOFF + (kt - 4 * qb) + OFF0
                    kb.op("act", lambda e: e.activation(out=E[:, j_lo * 128:512], in_=P[:, j_lo * 128:512], func=AF.Exp,
                                                        scale=0.125, bias=atab[:, col:col + 1]), r=[BP, B_c], w=[BE])
                else:
                    for j in range(j_lo, 4):
                        col = h * NOFF + (kt - 4 * qb - j) + OFF0
                        kb.op("act", lambda e: e.activation(out=E[:, j * 128:(j + 1) * 128], in_=P[:, j * 128:(j + 1) * 128],
                                                            func=AF.Exp, scale=0.125, bias=atab[:, col:col + 1]),
                              r=[BP, B_c], w=[BE])
                if jj >= 0:
                    kb.op("pool", lambda e: e.tensor_tensor(out=E[:, jj * 128:(jj + 1) * 128], in0=E[:, jj * 128:(jj + 1) * 128],
                                                            in1=cmask[:], op=ALU.mult), r=[B_c], w=[BE])
                ustate[u] = (E, BE, j_lo)

            def emit_av(u):
                qb, kt, m, first, last = u
                q0 = qb * 512
                E, BE, j_lo = ustate.pop(u)
                if first:
                    for j in range(4):
                        kb.op("pe", lambda e: e.matmul(acc4[:, j, :], lhsT=zero_bf[0:1, 0:128], rhs=zero_bf[0:1, 0:512],
                                                       start=True, stop=True, skip_group_check=True), r=[B_zero], w=[B_acc])
                for j in range(j_lo, 4):
                    kb.op("pe", lambda e: e.matmul(acc4[:, j, m * 129:(m + 1) * 129], lhsT=E[:, j * 128:(j + 1) * 128],
                                                   rhs=Vda[:, kt, h, 0:129], start=False, stop=last, skip_group_check=True),
                          r=[BE, B_V[kt]], w=[B_acc])
                if last:
                    while pending:
                        kb.op(*pending.pop(0))
                    evac(qb, q0)
                    conv_next(1)

            def evac(qb, q0):
                kb.op("dve", lambda e: e.reciprocal(out=rr[:, 0:4], in_=acc4[:, :, 128:129]), r=[B_acc], w=[B_o])
                kb.op("dve", lambda e: e.reciprocal(out=rr[:, 4:8], in_=acc4[:, :, 257:258]), r=[B_acc], w=[B_o])
                kb.op("dve", lambda e: e.tensor_tensor(out=o_sb[:], in0=acc4[:, :, 0:128], in1=rr[:, 0:4].unsqueeze(2).to_broadcast([128, 4, 128]),
                                                       op=ALU.mult), r=[B_acc], w=[B_o])
                kb.op("dve", lambda e: e.tensor_tensor(out=t_sb[:], in0=acc4[:, :, 129:257], in1=rr[:, 4:8].unsqueeze(2).to_broadcast([128, 4, 128]),
                                                       op=ALU.mult), r=[B_acc], w=[B_o])
                rec_ = []
                kb.op = lambda e, fn, r=(), w=(), same=None: rec_.append((e, fn, list(r), list(w), same))
                try:
                    evac_tail(qb, q0)
                finally:
                    del kb.op
                pending.extend(rec_)

            def evac_tail(qb, q0):
                kb.op("dve", lambda e: e.scalar_tensor_tensor(out=o_sb[:], in0=t_sb[:], scalar=neglam[:, 0:1], in1=o_sb[:],
                                                              op0=ALU.mult, op1=ALU.add), r=[B_o, B_l], w=[B_o])
                kb.op("dve", lambda e: e.tensor_tensor(out=t_sb[:], in0=o_sb[:], in1=o_sb[:], op=ALU.mult), r=[B_o], w=[B_o])
                kb.op("dve", lambda e: e.tensor_reduce(out=rr[:, 8:12], in_=t_sb[:], axis=AX.X, op=ALU.add), r=[B_o], w=[B_o])
                kb.op("act", lambda e: e.activation(out=rra[:, 0:4], in_=rr[:, 8:12], func=AF.Ln, scale=1.0 / 128, bias=EPS), r=[B_o], w=[B_o])
                kb.op("act", lambda e: e.activation(out=rra[:, 4:8], in_=rra[:, 0:4], func=AF.Exp, scale=-0.5), r=[B_o], w=[B_o])
                kb.op("dve", lambda e: e.tensor_tensor(out=t_sb[:], in0=o_sb[:], in1=rra[:, 4:8].unsqueeze(2).to_broadcast([128, 4, 128]),
                                                       op=ALU.mult), r=[B_o], w=[B_o])
                kb.op("dve", lambda e: e.tensor_tensor(out=y_sb[:], in0=t_sb[:], in1=gdo[:].unsqueeze(1).to_broadcast([128, 4, 128]),
                                                       op=ALU.mult), r=[B_o, B_c], w=[B_o])
                for j in range(4):
                    kb.op("pe", lambda e, j=j: e.transpose(out=ptr[:, j, :], in_=y_sb[:, j, :], identity=ident_bf[:]), r=[B_o, B_const], w=[B_ptr])
                kb.op("act", lambda e, h=h: e.copy(out=y_daT[:, h, q0:q0 + 512].rearrange("p (j t) -> p j t", j=4), in_=ptr[:, 0:4, :]),
                      r=[B_ptr], w=B_ydaT[h * NT + qb * 4:h * NT + qb * 4 + 4])

            emit_qk(units[0])
            if len(units) > 1:
                emit_qk(units[1])
            for ui in range(len(units)):
                if ui + 2 < len(units):
                    emit_qk(units[ui + 2])
                emit_av(units[ui])
                if pending and not units[ui][4]:
                    kb.op(*pending.pop(0))
            while pending:
                kb.op(*pending.pop(0))
        if "y_daT" in dbg_d:
            kb.dma("sp", "dbg", lambda q: q.dma_start(out=dbg_d["y_daT"], in_=y_daT[:]), r=B_ydaT)
        kb.barrier()


def _ml_phase(nc, kb, S, hT, B_hT, y_mlT, B_ymlT, w_in, cd, cw_col, cb_col, g_mlo, b_if, ident_bf, ident_f, ones_f,
              B_const, sb, ps, dbg_d):
    NT = S // 128
    NB5 = S // 512
    NHC = NT * 4
    QS = 128.0 ** -0.5
    with ExitStack() as s:
        negm = sb(s, "negm", [128, 128], F32)
        cw = sb(s, "cw", [128, 32], F32)
        cb = sb(s, "cb", [128, 8], F32)
        gml = sb(s, "gml", [128, 512], F32)
        bif = sb(s, "bif", [128, 8], F32)
        wif = sb(s, "wif", [128, 8, 8], BF16)
        B_c = kb.buf()
        for t, src in ((negm, cd["negm"]), (cw, cw_col), (cb, cb_col), (gml, g_mlo.partition_broadcast(128)),
                       (bif, b_if.partition_broadcast(128))):
            kb.dma("sp", "const", lambda q, t=t, src=src: q.dma_start(out=t[:], in_=src), w=[B_c])
        kb.dma("pool", "wif", lambda q: q.dma_start(out=wif[:], in_=w_in[:, OFF_ML_I:OFF_ML_I + 8].rearrange("(c p) f -> p c f", p=128)), w=[B_c])

        PA = [ps(s, f"mlPA{i}", [128, 512], F32) for i in range(2)]
        B_PA = kb.bufs(2)
        pacnt = [0]

        def nextPA():
            i = pacnt[0] % 2
            pacnt[0] += 1
            return PA[i], B_PA[i]
        Pew2 = [ps(s, f"mlPew{i}", [128, 512], F32) for i in range(2)]
        B_Pew2 = kb.bufs(2)
        Pew, B_Pew = Pew2[0], B_Pew2[0]
        Po2 = [ps(s, f"mlPo{i}", [128, 512], F32) for i in range(2)]
        B_Po2 = kb.bufs(2)
        Po, B_Po = Po2[0], B_Po2[0]
        Pc = ps(s, "mlPc", [128, 512], F32)
        Ptk = ps(s, "mlPtk", [128, 8, 128], BF16)
        Pty = Ptk
        Pg = Po
        B_Pc, B_Ptk = kb.bufs(2)
        B_Pty = B_Ptk
        B_Pg = B_Po

        for i in range(NT):
            for c in range(8):
                kb.op("pe", lambda e: e.matmul(Pg[:, i * 8:(i + 1) * 8], lhsT=hT[:, c, i * 128:(i + 1) * 128], rhs=wif[:, c, :],
                                               start=(c == 0), stop=(c == 7)), r=[B_hT[i], B_c], w=[B_Pg])
        gsb = sb(s, "gsb", [128, NT, 8], F32)
        XC = sb(s, "XC", [128, 2, NT, 4], F32)
        tmpg = sb(s, "tmpg", [128, NT, 4], F32)
        B_g = kb.buf()
        kb.op("dve", lambda e: e.tensor_tensor(out=gsb[:], in0=Pg[:, 0:NT * 8].rearrange("p (i g) -> p i g", g=8),
                                               in1=bif[:].unsqueeze(1).to_broadcast([128, NT, 8]), op=ALU.add), r=[B_Pg, B_c], w=[B_g])
        kb.op("act", lambda e: e.activation(out=tmpg[:], in_=gsb[:, :, 4:8], func=AF.Exp, scale=-1.0), r=[B_g], w=[B_g])
        kb.op("act", lambda e: e.activation(out=tmpg[:], in_=tmpg[:], func=AF.Ln, bias=1.0), r=[B_g], w=[B_g])
        kb.op("dve", lambda e: e.tensor_scalar(out=XC[:, 1], in0=tmpg[:], scalar1=-1.0, scalar2=None, op0=ALU.mult), r=[B_g], w=[B_g])
        kb.op("dve", lambda e: e.tensor_copy(out=XC[:, 0], in_=gsb[:, :, 0:4]), r=[B_g], w=[B_g])
        RN = ["iR", "lfR", "bR", "betaR", "pmR", "mxR", "alphaR", "mrowR", "winterR", "emrR", "winR", "zR"]
        Rt = {n: sb(s, n, [128, 128], F32) for n in RN}
        B_R = kb.buf()
        for a, n in ((0, "iR"), (1, "lfR")):
            kb.op("pe", lambda e: e.transpose(out=Pew[0:NHC, a * 128:(a + 1) * 128], in_=XC[:, a].rearrange("p i h -> p (i h)"),
                                              identity=ident_f[:]), r=[B_g, B_const], w=[B_Pew])
            kb.op("dve", lambda e: e.tensor_copy(out=Rt[n][0:NHC, :], in_=Pew[0:NHC, a * 128:(a + 1) * 128]), r=[B_Pew], w=[B_R])
        R = {n: Rt[n][0:NHC, :] for n in RN}
        kb.op("pool", lambda e: e.memset(Rt["zR"][:], 0.0), w=[B_R])
        kb.op("dve", lambda e: e.tensor_tensor_scan(out=R["bR"], data0=R["lfR"], data1=R["zR"], initial=0.0, op0=ALU.add, op1=ALU.add),
              r=[B_R], w=[B_R])
        kb.op("dve", lambda e: e.tensor_tensor(out=R["betaR"], in0=R["iR"], in1=R["bR"], op=ALU.subtract), r=[B_R], w=[B_R])
        kb.op("dve", lambda e: e.tensor_tensor_scan(out=R["pmR"], data0=R["betaR"], data1=R["betaR"], initial=-1e30, op0=ALU.max, op1=ALU.max),
              r=[B_R], w=[B_R])
        c2 = sb(s, "c2", [128, 8], F32)
        rows = sb(s, "rows", [1, 4, 128], F32)
        drow = sb(s, "drow", [1, 128], F32)
        kb.op("dve", lambda e: e.tensor_copy(out=c2[0:NHC, 0:1], in_=R["bR"][:, 127:128]), r=[B_R], w=[B_R])
        kb.op("dve", lambda e: e.tensor_tensor(out=c2[0:NHC, 1:2], in0=R["bR"][:, 127:128], in1=R["pmR"][:, 127:128], op=ALU.add), r=[B_R], w=[B_R])
        for a in range(2):
            kb.op("pe", lambda e: e.transpose(out=Pew[0:1, a * 128:a * 128 + NHC], in_=c2[0:NHC, a:a + 1], identity=ident_f[0:NHC, 0:NHC]),
                  r=[B_R, B_const], w=[B_Pew])
        kb.op("dve", lambda e: e.tensor_copy(out=rows[:, 0:2, 0:NHC], in_=Pew[0:1, 0:256].rearrange("p (a n) -> p a n", a=2)[:, :, 0:NHC]),
              r=[B_Pew], w=[B_R])
        for h in range(4):
            v = lambda a: rows[:, a, 0:NHC].rearrange("p (i h) -> p h i", h=4)[:, h, :]
            kb.op("dve", lambda e: e.tensor_tensor_scan(out=v(2), data0=v(0), data1=v(1), initial=0.0, op0=ALU.add, op1=ALU.max),
                  r=[B_R], w=[B_R])
        kb.op("pool", lambda e: e.memset(rows[:, 3, 0:4], 0.0), r=[B_R], w=[B_R])
        if NHC > 4:
            kb.op("dve", lambda e: e.tensor_copy(out=rows[:, 3, 4:NHC], in_=rows[:, 2, 0:NHC - 4]), r=[B_R], w=[B_R])
        kb.op("pe", lambda e: e.matmul(Pew[0:NHC, 0:1], lhsT=rows[:, 3, 0:NHC], rhs=ones_f[0:1, 0:1], start=True, stop=True),
              r=[B_R, B_const], w=[B_Pew])
        kb.op("dve", lambda e: e.tensor_copy(out=c2[0:NHC, 2:3], in_=Pew[0:NHC, 0:1]), r=[B_Pew], w=[B_R])
        ms = c2[0:NHC, 2:3]
        kb.op("dve", lambda e: e.tensor_scalar(out=R["mxR"], in0=R["pmR"], scalar1=ms, scalar2=None, op0=ALU.max), r=[B_R], w=[B_R])
        kb.op("dve", lambda e: e.tensor_scalar(out=R["alphaR"], in0=R["mxR"], scalar1=-1.0, scalar2=None, op0=ALU.mult), r=[B_R], w=[B_R])
        kb.op("dve", lambda e: e.tensor_tensor(out=R["mrowR"], in0=R["bR"], in1=R["mxR"], op=ALU.add), r=[B_R], w=[B_R])
        kb.op("act", lambda e: e.activation(out=R["winterR"], in_=R["alphaR"], func=AF.Exp, bias=ms), r=[B_R], w=[B_R])
        kb.op("act", lambda e: e.activation(out=R["emrR"], in_=R["mrowR"], func=AF.Exp, scale=-2.0), r=[B_R], w=[B_R])
        kb.op("dve", lambda e: e.tensor_tensor(out=c2[0:NHC, 6:7], in0=ms, in1=R["pmR"][:, 127:128], op=ALU.max), r=[B_R], w=[B_R])
        kb.op("dve", lambda e: e.tensor_tensor(out=c2[0:NHC, 3:4], in0=c2[0:NHC, 6:7], in1=c2[0:NHC, 0:1], op=ALU.add), r=[B_R], w=[B_R])
        kb.op("dve", lambda e: e.tensor_tensor(out=c2[0:NHC, 5:6], in0=c2[0:NHC, 0:1], in1=c2[0:NHC, 3:4], op=ALU.subtract), r=[B_R], w=[B_R])
        kb.op("act", lambda e: e.activation(out=c2[0:NHC, 4:5], in_=ms, func=AF.Exp, bias=c2[0:NHC, 5:6]), r=[B_R], w=[B_R])
        kb.op("act", lambda e: e.activation(out=R["winR"], in_=R["betaR"], func=AF.Exp, bias=c2[0:NHC, 5:6]), r=[B_R], w=[B_R])
        CN = ["betaR", "alphaR", "winterR", "emrR", "winR"]
        Ct = {n: sb(s, "C_" + n, [128, 128], F32) for n in CN}
        dbc = sb(s, "dbc", [128, 128], F32)
        B_C = kb.buf()
        for n in CN:
            kb.op("pe", lambda e: e.transpose(out=Pew[:, 0:NHC], in_=R[n], identity=ident_f[0:NHC, 0:NHC]), r=[B_R, B_const], w=[B_Pew])
            kb.op("dve", lambda e: e.tensor_copy(out=Ct[n][:, 0:NHC], in_=Pew[:, 0:NHC]), r=[B_Pew], w=[B_C])
        kb.op("pe", lambda e: e.transpose(out=Pew[0:1, 0:NHC], in_=c2[0:NHC, 4:5], identity=ident_f[0:NHC, 0:NHC]), r=[B_R, B_const], w=[B_Pew])
        kb.op("dve", lambda e: e.tensor_copy(out=drow[:, 0:NHC], in_=Pew[0:1, 0:NHC]), r=[B_Pew], w=[B_R])
        kb.op("pe", lambda e: e.matmul(Pew[:, 0:NHC], lhsT=ones_f[0:1, :], rhs=drow[:, 0:NHC], start=True, stop=True), r=[B_R, B_const], w=[B_Pew])
        kb.op("dve", lambda e: e.tensor_copy(out=dbc[:, 0:NHC], in_=Pew[:, 0:NHC]), r=[B_Pew], w=[B_C])

        wq4 = sb(s, "wq4", [128, 8, 512], BF16)
        B_w = kb.buf()
        qkT = sb(s, "mlqkT", [128, 2, S], BF16)
        B_qk = [kb.bufs(NB5), kb.bufs(NB5)]
        Vml = sb(s, "Vml", [128, NT, 130], BF16)
        og = sb(s, "og", [128, NT, 128], BF16)
        B_vo = kb.bufs(NT)
        B_vones = kb.buf()
        kb.op("pool", lambda e: e.memset(Vml[:, :, 128:130], 1.0), w=[B_vones])
        U2 = [sb(s, f"U2{i}", [128, 515], F32) for i in range(2)]
        B_U = kb.bufs(2)
        acc = [sb(s, f"cacc{i}", [128, 512], F32) for i in range(2)]
        B_a = kb.bufs(2)
        Cf = sb(s, "Cf", [128, 130], F32)
        Cbf = [sb(s, f"Cbf{i}", [128, 130], BF16) for i in range(2)]
        B_Cf = kb.buf()
        B_Cbf = kb.bufs(2)
        dA = [sb(s, f"dA{i}", [128, 128], F32) for i in range(2)]
        dW = [sb(s, f"dW{i}", [128, 128], F32) for i in range(2)]
        Wt = [sb(s, f"Wt{i}", [128, 128], F32) for i in range(2)]
        Pt = [sb(s, f"Pt{i}", [128, 128], BF16) for i in range(2)]
        qs = [sb(s, f"qs{i}", [128, 128], BF16) for i in range(2)]
        kw = [sb(s, f"kw{i}", [128, 128], BF16) for i in range(2)]
        t1_2 = [sb(s, f"t1{i}", [128, 128], F32) for i in range(2)]
        yb_2 = [sb(s, f"yb{i}", [128, 128], BF16) for i in range(2)]
        jk_2 = [sb(s, f"jk{i}", [128, 128], F32) for i in range(2)]
        sc_2 = [sb(s, f"sc{i}", [128, 8], F32) for i in range(2)]
        sca_2 = [sb(s, f"sca{i}", [128, 8], F32) for i in range(2)]
        B_ch2 = kb.bufs(2)
        B_y2 = kb.bufs(2)
        B_dA, B_dW, B_Wt, B_Pt, B_qs, B_kw = [kb.bufs(2) for _ in range(6)]
        offs = [OFF_ML_QK, OFF_ML_QK + 512, OFF_ML_V, OFF_ML_O]
        for h in range(NH):
            for a in range(4):
                kb.dma("pool", "wq4", lambda q: q.dma_start(out=wq4[:, :, a * 128:(a + 1) * 128],
                       in_=w_in[:, offs[a] + h * 128:offs[a] + (h + 1) * 128].rearrange("(c p) f -> p c f", p=128)), w=[B_w])
            for i in range(NT):
                P, BP = nextPA()
                for c in range(8):
                    kb.op("pe", lambda e: e.matmul(P[:, 0:256], lhsT=hT[:, c, i * 128:(i + 1) * 128], rhs=wq4[:, c, 256:512],
                                                   start=(c == 0), stop=(c == 7)), r=[B_hT[i], B_w], w=[BP])
                kb.op("act", lambda e: e.copy(out=Vml[:, i, 0:128], in_=P[:, 0:128]), r=[BP, B_vones], w=[B_vo[i]])
                kb.op("act", lambda e: e.activation(out=og[:, i, :], in_=P[:, 128:256], func=AF.Sigmoid), r=[BP], w=[B_vo[i]])
                kb.op("pool", lambda e: e.tensor_tensor(out=og[:, i, :], in0=og[:, i, :], in1=gml[:, h * 128:(h + 1) * 128], op=ALU.mult),
                      r=[B_c], w=[B_vo[i]])
            for which in range(2):
                cc = which * 4 + h
                for tb in range(NB5):
                    ub = tb % 2
                    P, BP = nextPA()
                    for c in range(8):
                        kb.op("pe", lambda e: e.matmul(P[:], lhsT=wq4[:, c, which * 128:(which + 1) * 128], rhs=hT[:, c, tb * 512:(tb + 1) * 512],
                                                       start=(c == 0), stop=(c == 7)), r=B_hT[tb * 4:tb * 4 + 4] + [B_w], w=[BP])
                    kb.op("act", lambda e: e.copy(out=U2[ub][:, 3:515], in_=P[:]), r=[BP], w=[B_U[ub]])
                    if tb == 0:
                        kb.op("pool", lambda e: e.memset(U2[ub][:, 0:3], 0.0), w=[B_U[ub]])
                    else:
                        kb.op("pool", lambda e: e.tensor_copy(out=U2[ub][:, 0:3], in_=U2[1 - ub][:, 512:515]), r=[B_U[1 - ub]], w=[B_U[ub]])
                    A_, BA = acc[ub], B_a[ub]
                    kb.op("dve", lambda e: e.tensor_scalar(out=A_[:], in0=U2[ub][:, 3:515], scalar1=cw[:, cc * 4 + 3:cc * 4 + 4],
                                                           scalar2=cb[:, cc:cc + 1], op0=ALU.mult, op1=ALU.add), r=[B_U[ub], B_c], w=[BA])
                    for j in range(3):
                        kb.op("dve", lambda e: e.scalar_tensor_tensor(out=A_[:], in0=U2[ub][:, j:j + 512], scalar=cw[:, cc * 4 + j:cc * 4 + j + 1],
                                                                      in1=A_[:], op0=ALU.mult, op1=ALU.add), r=[B_U[ub], B_c], w=[BA])
                    if which == 1:
                        kb.op("act", lambda e: e.activation(out=qkT[:, 1, tb * 512:(tb + 1) * 512], in_=A_[:], func=AF.Silu),
                              r=[BA], w=[B_qk[1][tb]])
                    else:
                        kb.op("act", lambda e: e.activation(out=A_[:], in_=A_[:], func=AF.Silu), r=[BA], w=[BA])
                        kb.op("pool", lambda e: e.tensor_scalar(out=qkT[:, 0, tb * 512:(tb + 1) * 512], in0=A_[:], scalar1=QS, scalar2=None,
                                                                op0=ALU.mult), r=[BA], w=[B_qk[0][tb]])
            kb.op("pool", lambda e: e.memset(Cf[:], 0.0), w=[B_Cf])
            kb.op("pool", lambda e: e.memset(Cbf[1][:], 0.0), w=[B_Cbf[1]])

            def pre(i):
                jj = i % 2
                hc = i * 4 + h
                tsl = slice(i * 128, (i + 1) * 128)
                tb = i // 4
                Ps_, BPs = PA[jj], B_PA[jj]
                Pw_, BPw = Pew2[jj], B_Pew2[jj]
                kb.op("pe", lambda e: e.matmul(Ps_[:, 0:128], lhsT=qkT[:, 1, tsl], rhs=qkT[:, 0, tsl], start=True, stop=True),
                      r=[B_qk[0][tb], B_qk[1][tb]], w=[BPs])
                kb.op("dve", lambda e: e.tensor_scalar(out=dA[jj][:], in0=ident_f[:], scalar1=Ct["alphaR"][:, hc:hc + 1], scalar2=None, op0=ALU.mult),
                      r=[B_C, B_const], w=[B_dA[jj]])
                kb.op("dve", lambda e: e.tensor_scalar(out=dW[jj][:], in0=ident_f[:], scalar1=Ct["winterR"][:, hc:hc + 1], scalar2=None, op0=ALU.mult),
                      r=[B_C, B_const], w=[B_dW[jj]])
                kb.op("pe", lambda e: e.matmul(Pw_[:, 0:128], lhsT=ones_f[:], rhs=dA[jj][:], start=True, stop=False), r=[B_dA[jj], B_const], w=[BPw])
                kb.op("pe", lambda e: e.matmul(Pw_[:, 0:128], lhsT=ident_f[:], rhs=negm[:], start=False, stop=True), r=[B_c, B_const], w=[BPw])
                kb.op("pe", lambda e: e.matmul(Pw_[:, 128:256], lhsT=ones_f[:], rhs=dW[jj][:], start=True, stop=True), r=[B_dW[jj], B_const], w=[BPw])
                kb.op("pe", lambda e: e.transpose(out=Ptk[:, jj, :], in_=qkT[:, 1, tsl], identity=ident_bf[:]), r=[B_qk[1][tb], B_const], w=[B_Ptk])

            def preB(i):
                jj = i % 2
                hc = i * 4 + h
                tsl = slice(i * 128, (i + 1) * 128)
                tb = i // 4
                Ps_, BPs = PA[jj], B_PA[jj]
                Pw_, BPw = Pew2[jj], B_Pew2[jj]
                kb.op("act", lambda e: e.activation(out=Wt[jj][:], in_=Pw_[:, 0:128], func=AF.Exp, bias=Ct["betaR"][:, hc:hc + 1]),
                      r=[BPw, B_C], w=[B_Wt[jj]])
                kb.op("dve", lambda e: e.tensor_tensor(out=qs[jj][:], in0=qkT[:, 0, tsl], in1=Pw_[:, 128:256], op=ALU.mult),
                      r=[BPw, B_qk[0][tb], B_Wt[jj]], w=[B_qs[jj]])
                kb.op("dve", lambda e: e.tensor_scalar(out=kw[jj][:], in0=Ptk[:, jj, :], scalar1=Ct["winR"][:, hc:hc + 1], scalar2=None, op0=ALU.mult),
                      r=[B_Ptk, B_C], w=[B_kw[jj]])
                kb.op("dve", lambda e: e.tensor_tensor(out=Pt[jj][:], in0=Ps_[:, 0:128], in1=Wt[jj][:], op=ALU.mult), r=[BPs, B_Wt[jj]], w=[B_Pt[jj]])

            def main(i):
                mainA(i)
                tail(i)

            def mainA(i):
                jj = i % 2
                hc = i * 4 + h
                tsl = slice(i * 128, (i + 1) * 128)
                Po, B_Po = Po2[jj], B_Po2[jj]
                kb.op("pe", lambda e: e.matmul(Pc[:, 0:129], lhsT=kw[jj][:], rhs=Vml[:, i, 0:129], start=True, stop=True), r=[B_kw[jj], B_vo[i]], w=[B_Pc])
                kb.op("pe", lambda e: e.matmul(Po[:, 0:129], lhsT=Pt[jj][:], rhs=Vml[:, i, 0:129], start=True, stop=False), r=[B_Pt[jj], B_vo[i]], w=[B_Po])
                kb.op("pe", lambda e: e.matmul(Po[:, 0:129], lhsT=qs[jj][:], rhs=Cbf[(i + 1) % 2][:, 0:129], start=False, stop=True),
                      r=[B_qs[jj], B_Cbf[(i + 1) % 2]], w=[B_Po])
                kb.op("dve", lambda e: e.scalar_tensor_tensor(out=Cf[:, 0:129], in0=Cf[:, 0:129], scalar=dbc[:, hc:hc + 1], in1=Pc[:, 0:129],
                                                              op0=ALU.mult, op1=ALU.add), r=[B_Pc, B_C], w=[B_Cf])
                kb.op("act", lambda e: e.copy(out=Cbf[jj][:, 0:129], in_=Cf[:, 0:129]), r=[B_Cf], w=[B_Cbf[jj]])

            def tail(i):
                jj = i % 2
                hc = i * 4 + h
                tsl = slice(i * 128, (i + 1) * 128)
                Po, B_Po = Po2[jj], B_Po2[jj]
                t1, yb, jk, sc, sca, B_ch, B_y = t1_2[jj], yb_2[jj], jk_2[jj], sc_2[jj], sca_2[jj], B_ch2[jj], B_y2[jj]
                kb.op("act", lambda e: e.activation(out=sca[:, 0:1], in_=Po[:, 128:129], func=AF.Square), r=[B_Po], w=[B_ch])
                kb.op("dve", lambda e: e.tensor_scalar(out=sc[:, 1:2], in0=sca[:, 0:1], scalar1=Ct["emrR"][:, hc:hc + 1], scalar2=EPS,
                                                       op0=ALU.max, op1=ALU.mult), r=[B_ch, B_C], w=[B_ch])
                kb.op("act", lambda e: e.activation(out=jk[:], in_=Po[:, 0:128], func=AF.Square, accum_out=sca[:, 2:3]), r=[B_Po], w=[B_ch])
                kb.op("act", lambda e: e.activation(out=sca[:, 3:4], in_=sca[:, 2:3], func=AF.Ln, scale=1.0 / 128, bias=sc[:, 1:2]), r=[B_ch], w=[B_ch])
                kb.op("act", lambda e: e.activation(out=sca[:, 4:5], in_=sca[:, 3:4], func=AF.Exp, scale=-0.5), r=[B_ch], w=[B_ch])
                kb.op("dve", lambda e: e.scalar_tensor_tensor(out=yb[:], in0=Po[:, 0:128], scalar=sca[:, 4:5], in1=og[:, i, :],
                                                              op0=ALU.mult, op1=ALU.mult), r=[B_Po, B_ch, B_vo[i]], w=[B_y])
                kb.op("pe", lambda e: e.transpose(out=Pty[:, 2, :], in_=yb[:], identity=ident_bf[:]), r=[B_y, B_const], w=[B_Pty])
                kb.op("act", lambda e: e.copy(out=y_mlT[:, h, tsl], in_=Pty[:, 2, :]), r=[B_Pty], w=[B_ymlT[h * NT + i]])

            LOOKAHEAD = ML_LOOKAHEAD
            if LOOKAHEAD == 0:
                for i in range(NT):
                    pre(i)
                    preB(i)
                    main(i)
            elif LOOKAHEAD == 1:
                pre(0)
                for i in range(NT):
                    preB(i)
                    if i + 1 < NT:
                        pre(i + 1)
                    main(i)
            elif LOOKAHEAD == 3:
                def rec_ops(fns):
                    rec = []
                    kb.op = lambda e, fn, r=(), w=(), same=None: rec.append((e, fn, list(r), list(w), same))
                    try:
                        for f_ in fns:
                            f_()
                    finally:
                        del kb.op
                    return rec
                pre(0)
                for i in range(NT + 1):
                    fa = []
                    if i < NT:
                        fa.append(lambda i=i: preB(i))
                        if i + 1 < NT:
                            fa.append(lambda i=i: pre(i + 1))
                        fa.append(lambda i=i: mainA(i))
                    ra = rec_ops(fa)
                    rb = rec_ops([lambda i=i: tail(i - 1)]) if i >= 1 else []
                    ia = ib = 0
                    while ia < len(ra) or ib < len(rb):
                        for _ in range(2):
                            if ia < len(ra):
                                kb.op(*ra[ia])
                                ia += 1
                        if ib < len(rb):
                            kb.op(*rb[ib])
                            ib += 1
            else:
                pre(0)
                preB(0)
                for i in range(NT):
                    if i + 1 < NT:
                        pre(i + 1)
                        preB(i + 1)
                    main(i)
        if "y_mlT" in dbg_d:
            kb.dma("sp", "dbg", lambda q: q.dma_start(out=dbg_d["y_mlT"], in_=y_mlT[:]), r=B_ymlT)
        kb.barrier()


def _merge_moe(nc, kb, S, hT, B_hT, y_daT, B_ydaT, y_mlT, B_ymlT, es_mix, x_d, out_d, w_bda, w_bml, w_gate, w_out,
               bg_col, g_ffn, wr_d, b_rt, w1, w3, w2, xs_d, ys_d, hn_d, cd, ident_bf, ident_f, ones_f, zero_bf,
               B_const, B_zero, sb, ps, dbg_d, stage, B_xs, wconv):
    w1b_d, w3b_d, w2b_d, B_wconv = wconv
    NT = S // 128
    NB5 = S // 512
    CAP = 2 * S + NE * BS
    NBLK = CAP // BS
    NSUB = BS // 128
    with ExitStack() as s:
        wg = sb(s, "wg", [128, 8, 2048], BF16)
        wda = sb(s, "wda", [128, 4, 1024], BF16)
        wml = sb(s, "wml", [128, 4, 1024], BF16)
        bg = sb(s, "bg", [128, 16], F32)
        B_w = kb.buf()
        for k4 in range(4):
            kb.dma("pool", "mw", lambda q: q.dma_start(out=wg[:, :, k4 * 512:(k4 + 1) * 512],
                   in_=w_gate[:, k4 * 512:(k4 + 1) * 512].rearrange("(c p) f -> p c f", p=128)), w=[B_w])
        kb.dma("pool", "mw", lambda q: q.dma_start(out=wda[:], in_=w_bda.rearrange("(c p) f -> p c f", p=128)), w=[B_w])
        kb.dma("pool", "mw", lambda q: q.dma_start(out=wml[:], in_=w_bml.rearrange("(c p) f -> p c f", p=128)), w=[B_w])
        kb.dma("sp", "const", lambda q: q.dma_start(out=bg[:], in_=bg_col), w=[B_w])
        Pg = [ps(s, f"mPg{i}", [128, 512], F32) for i in range(2)]
        Pab = [ps(s, f"mPab{i}", [128, 512], F32) for i in range(2)]
        B_Pg, B_Pab = kb.bufs(2), kb.bufs(2)
        gs = [sb(s, f"gs{i}", [128, 512], F32) for i in range(2)]
        tt = [sb(s, f"tt{i}", [128, 512], F32) for i in range(2)]
        B_gs, B_tt = kb.bufs(2), kb.bufs(2)
        mixb = sb(s, "mixb", [128, 8, 512], BF16)
        B_mix = kb.bufs(8)
        for tb in range(NB5):
            tsl = slice(tb * 512, (tb + 1) * 512)
            hb = B_hT[tb * 4:tb * 4 + 4]
            for fc in range(8):
                for g in range(2):
                    kb_w = wda if g == 0 else wml
                    yT = y_daT if g == 0 else y_mlT
                    By = (B_ydaT if g == 0 else B_ymlT)
                    byl = [By[hh * NT + tb * 4 + t4] for hh in range(4) for t4 in range(4)]
                    for c in range(8):
                        kb.op("pe", lambda e: e.matmul(Pg[g][:], lhsT=wg[:, c, g * 1024 + fc * 128:g * 1024 + (fc + 1) * 128], rhs=hT[:, c, tsl],
                                                       start=(c == 0), stop=(c == 7)), r=hb + [B_w], w=[B_Pg[g]])
                    kb.op("act", lambda e: e.activation(out=gs[g][:], in_=Pg[g][:], func=AF.Sigmoid, bias=bg[:, g * 8 + fc:g * 8 + fc + 1]),
                          r=[B_Pg[g], B_w], w=[B_gs[g]])
                    for c in range(4):
                        kb.op("pe", lambda e: e.matmul(Pab[g][:], lhsT=kb_w[:, c, fc * 128:(fc + 1) * 128], rhs=yT[:, c, tsl],
                                                       start=(c == 0), stop=(c == 3)), r=byl + [B_w], w=[B_Pab[g]])
                    kb.op("dve", lambda e: e.tensor_tensor(out=tt[g][:], in0=gs[g][:], in1=Pab[g][:], op=ALU.mult),
                          r=[B_gs[g], B_Pab[g]], w=[B_tt[g]])
                kb.op("pool", lambda e: e.tensor_tensor(out=mixb[:, fc, :], in0=tt[0][:], in1=tt[1][:], op=ALU.add),
                      r=[B_tt[0], B_tt[1]], w=[B_mix[fc]])
            for fc in range(8):
                kb.op("pool", lambda e: e.tensor_copy(out=hT[:, fc, tsl], in_=mixb[:, fc, :]), r=[B_mix[fc]], w=hb)
        kb.barrier()
    es_mix.close()

    es2 = ExitStack()
    with es2:
        s = es2
        A1a = sb(s, "A1a", [128, NT, 32], F32)
        A2a = sb(s, "A2a", [128, NT, 32], F32)
        rka = sb(s, "rka", [128, NT, 32], F32)
        gta = sb(s, "gta", [128, NT, 2], F32)
        base = sb(s, "base", [128, 32], F32)
        d1i = sb(s, "d1i", [128, NT], I32)
        d2i = sb(s, "d2i", [128, NT], I32)
        bexp = sb(s, "bexp", [128, NBLK], I32)
        idxw = sb(s, "idxw", [128, NBLK], I32)
        B_rt = kb.buf()
        B_out = kb.bufs(NT)
        B_hn = kb.bufs(NT)
        with ExitStack() as sB:
            wo = sb(sB, "wo", [128, 8, 1024], BF16)
            gff = sb(sB, "gff", [128, 1024], F32)
            wr = sb(sB, "wr", [128, 8, 36], F32)
            brt = sb(sB, "brt", [128, 36], F32)
            ustr = sb(sB, "ustr", [128, 128], F32)
            B_w = kb.buf()
            for k2 in range(2):
                kb.dma("pool", "mw", lambda q: q.dma_start(out=wo[:, :, k2 * 512:(k2 + 1) * 512],
                       in_=w_out[:, k2 * 512:(k2 + 1) * 512].rearrange("(c p) f -> p c f", p=128)), w=[B_w])
            kb.dma("sp", "const", lambda q: q.dma_start(out=gff[:], in_=g_ffn.partition_broadcast(128)), w=[B_w])
            kb.dma("sp", "const", lambda q: q.dma_start(out=wr[:], in_=wr_d.rearrange("(c p) f -> p c f", p=128)), w=[B_w])
            kb.dma("sp", "const", lambda q: q.dma_start(out=brt[:], in_=b_rt.partition_broadcast(128)), w=[B_w])
            kb.dma("sp", "const", lambda q: q.dma_start(out=ustr[:], in_=cd["ustrict"]), w=[B_w])
            kb.op("pool", lambda e: e.memset(base[:], 0.0), w=[B_rt])
            Px = ps(sB, "bPx", [128, 2, 512], F32)
            PT = ps(sB, "bPT", [128, 8, 128], F32)
            Pr = ps(sB, "bPr", [128, 512], F32)
            B_Px, B_PT, B_Pr = kb.bufs(3)
            xt = [sb(sB, f"bxt{i}", [128, 1024], F32) for i in range(2)]
            x2 = [sb(sB, f"bx2{i}", [128, 1024], F32) for i in range(2)]
            hnf = sb(sB, "hnf", [128, 1024], F32)
            hnb = [sb(sB, f"hnb{i}", [128, 1024], BF16) for i in range(2)]
            hnT = sb(sB, "hnT", [128, 8, 128], F32)
            junk = sb(sB, "bjunk", [128, 512], F32)
            st = sb(sB, "bst", [128, 8], F32)
            sd_ = sb(sB, "bsd", [128, 16], F32)
            lg = sb(sB, "lg", [128, 36], F32)
            oh = sb(sB, "oh", [128, 4], F32)
            t48 = sb(sB, "t48", [128, 4, 8], F32)
            es8 = sb(sB, "es8", [128, 8], F32)
            e28 = sb(sB, "e28", [128, 8], F32)
            mk1 = sb(sB, "mk1", [128, 8], F32)
            mk2 = sb(sB, "mk2", [128, 8], F32)
            At = sb(sB, "At", [128, 32], F32)
            B_xt, B_x2, B_hnb = kb.bufs(2), kb.bufs(2), kb.bufs(2)
            B_hnf, B_hnT, B_r = kb.buf(), kb.buf(), kb.buf()
            B_rx = kb.buf()
            B_lg = kb.bufs(4)
            lg2 = [sb(sB, f"lg2{i}", [128, 36], F32) for i in range(4)]
            B_r2 = kb.bufs(2)
            sd2 = [sd_, sb(sB, "bsd_b", [128, 16], F32)]
            st2 = [st, sb(sB, "bst_b", [128, 8], F32)]
            oh2 = [oh, sb(sB, "oh_b", [128, 4], F32)]
            t482 = [t48, sb(sB, "t48_b", [128, 4, 8], F32)]
            es82 = [es8, sb(sB, "es8_b", [128, 8], F32)]
            e282 = [e28, sb(sB, "e28_b", [128, 8], F32)]
            mk12 = [mk1, sb(sB, "mk1_b", [128, 8], F32)]
            mk22 = [mk2, sb(sB, "mk2_b", [128, 8], F32)]
            stX = sb(sB, "bstX", [128, 8], F32)
            sdX = sb(sB, "bsdX", [128, 16], F32)
            junkR2 = [sb(sB, f"bjunkR{i}", [128, 8], F32) for i in range(2)]
            Pr2 = ps(sB, "bPr2", [128, 512], F32)
            B_Pr2 = kb.buf()
            hnf2 = [hnf, sb(sB, "hnf_b", [128, 1024], F32)]
            hnT2 = [hnT, sb(sB, "hnT_b", [128, 8, 128], F32)]
            stX2 = [stX, sb(sB, "bstX_b", [128, 8], F32)]
            junk2 = [junk, sb(sB, "bjunk_b", [128, 512], F32)]
            PT2 = [PT, ps(sB, "bPT_b", [128, 8, 128], F32)]
            Pr_2 = [Pr, Pr2]
            B_hnf2, B_hnT2, B_rx2, B_PT2 = [B_hnf, kb.buf()], [B_hnT, kb.buf()], [B_rx, kb.buf()], [B_PT, kb.buf()]
            B_Px2 = [B_Px, kb.buf()]
            B_Pr_2 = [B_Pr, B_Pr2]
            ingroup = [None]

            def gb():
                ingroup[0] = []

            def ge():
                g_, ingroup[0] = ingroup[0], None
                return g_

            def stageX(i):
                j = i % 2
                lgi = lg2[i % 4]
                hnf, hnT, stX, junk, PT, Pr = hnf2[j], hnT2[j], stX2[j], junk2[j], PT2[j], Pr_2[j]
                B_hnf, B_hnT, B_rx, B_PT, B_Pr, B_Px = B_hnf2[j], B_hnT2[j], B_rx2[j], B_PT2[j], B_Pr_2[j], B_Px2[j]
                rows = slice(i * 128, (i + 1) * 128)
                half = fc = hs = c = None
                kb.dma("sp", f"bxt{j}", lambda q: q.dma_start(out=xt[j][:], in_=x_d[rows, :]), w=[B_xt[j]])
                for half in range(2):
                    hs = slice(half * 512, (half + 1) * 512)
                    gb()
                    for fc in range(8):
                        kb.op("pe", lambda e, half=half, fc=fc, hs=hs, c=c: e.matmul(Px[:, j, :], lhsT=hT[:, fc, rows], rhs=wo[:, fc, half * 512:(half + 1) * 512],
                                                       start=(fc == 0), stop=(fc == 7)), r=[B_hT[i], B_w], w=[B_Px])
                    grp_done(ge())
                    kb.op("dve", lambda e, half=half, fc=fc, hs=hs, c=c: e.tensor_tensor(out=x2[j][:, hs], in0=xt[j][:, hs], in1=Px[:, j, :], op=ALU.add),
                          r=[B_xt[j], B_Px], w=[B_x2[j]])
                kb.dma("sp", "ost", lambda q: q.dma_start(out=out_d[rows, :], in_=x2[j][:]), r=[B_x2[j]], w=[B_out[i]])
                for half in range(2):
                    hs = slice(half * 512, (half + 1) * 512)
                    kb.op("act", lambda e, half=half, fc=fc, hs=hs, c=c: e.activation(out=junk[:], in_=x2[j][:, hs], func=AF.Square, accum_out=stX[:, half:half + 1]),
                          r=[B_x2[j]], w=[B_rx])
                kb.op("act", lambda e, half=half, fc=fc, hs=hs, c=c: e.activation(out=stX[:, 2:3], in_=stX[:, 0:1], func=AF.Identity, bias=stX[:, 1:2]), r=[B_rx], w=[B_rx])
                kb.op("act", lambda e, half=half, fc=fc, hs=hs, c=c: e.activation(out=stX[:, 3:4], in_=stX[:, 2:3], func=AF.Ln, scale=1.0 / D, bias=EPS), r=[B_rx], w=[B_rx])
                kb.op("act", lambda e, half=half, fc=fc, hs=hs, c=c: e.activation(out=stX[:, 6:7], in_=stX[:, 3:4], func=AF.Exp, scale=-0.5), r=[B_rx], w=[B_rx])
                for half in range(2):
                    hs = slice(half * 512, (half + 1) * 512)
                    kb.op("dve", lambda e, half=half, fc=fc, hs=hs, c=c: e.scalar_tensor_tensor(out=hnf[:, hs], in0=x2[j][:, hs], scalar=stX[:, 6:7], in1=gff[:, hs],
                                                                  op0=ALU.mult, op1=ALU.mult), r=[B_x2[j], B_rx, B_w], w=[B_hnf])
                    kb.op("pool", lambda e, half=half, fc=fc, hs=hs, c=c: e.tensor_copy(out=hnb[j][:, hs], in_=hnf[:, hs]), r=[B_hnf], w=[B_hnb[j]])
                kb.dma("sp", "hnst", lambda q: q.dma_start(out=hn_d[rows, :], in_=hnb[j][:]), r=[B_hnb[j]], w=[B_hn[i]])
                for c in range(8):
                    kb.op("pe", lambda e, half=half, fc=fc, hs=hs, c=c: e.transpose(out=PT[:, c, :], in_=hnf[:, c * 128:(c + 1) * 128], identity=ident_f[:]),
                          r=[B_hnf, B_const], w=[B_PT])
                for half in range(2):
                    kb.op("act", lambda e, half=half, fc=fc, hs=hs, c=c: e.copy(out=hnT[:, half * 4:half * 4 + 4, :], in_=PT[:, half * 4:half * 4 + 4, :]), r=[B_PT], w=[B_hnT])
                gb()
                for c in range(8):
                    kb.op("pe", lambda e, half=half, fc=fc, hs=hs, c=c: e.matmul(Pr[:, 0:36], lhsT=hnT[:, c, :], rhs=wr[:, c, :], start=(c == 0), stop=(c == 7)),
                          r=[B_hnT, B_w], w=[B_Pr])
                grp_done(ge())
                kb.op("dve", lambda e, half=half, fc=fc, hs=hs, c=c: e.tensor_tensor(out=lgi[:], in0=Pr[:, 0:36], in1=brt[:], op=ALU.add), r=[B_Pr, B_w], w=[B_lg[i % 4]])

            def stageR(i):
                lgi = lg2[i % 4]
                q = i % 2
                sd_, st, oh, t48, es8, e28, mk1, mk2, junkR = sd2[q], st2[q], oh2[q], t482[q], es82[q], e282[q], mk12[q], mk22[q], junkR2[q]
                R_ = [B_r2[q]]
                RL = [B_r2[q], B_lg[i % 4]]
                kb.op("dve", lambda e: e.tensor_reduce(out=sd_[:, 1:2], in_=lgi[:, 0:4], axis=AX.X, op=ALU.max), r=RL, w=R_)
                kb.op("dve", lambda e: e.tensor_scalar(out=sd_[:, 2:3], in0=sd_[:, 1:2], scalar1=-1.0, scalar2=None, op0=ALU.mult), r=R_, w=R_)
                kb.op("act", lambda e: e.activation(out=junkR[:, 0:4], in_=lgi[:, 0:4], func=AF.Exp, bias=sd_[:, 2:3], accum_out=st[:, 4:5]), r=RL, w=R_)
                kb.op("dve", lambda e: e.reciprocal(out=sd_[:, 3:4], in_=st[:, 4:5]), r=R_, w=R_)
                kb.op("dve", lambda e: e.tensor_scalar(out=oh[:], in0=lgi[:, 0:4], scalar1=sd_[:, 1:2], scalar2=None, op0=ALU.is_equal), r=RL, w=R_)
                kb.op("dve", lambda e: e.tensor_tensor(out=t48[:], in0=lgi[:, 4:36].rearrange("p (g j) -> p g j", g=4),
                                                       in1=oh[:].unsqueeze(2).to_broadcast([128, 4, 8]), op=ALU.mult), r=RL, w=R_)
                kb.op("dve", lambda e: e.tensor_reduce(out=es8[:], in_=t48[:].rearrange("p g j -> p j g"), axis=AX.X, op=ALU.add), r=R_, w=R_)
                kb.op("dve", lambda e: e.tensor_reduce(out=sd_[:, 4:5], in_=es8[:], axis=AX.X, op=ALU.max), r=R_, w=R_)
                kb.op("dve", lambda e: e.tensor_scalar(out=mk1[:], in0=es8[:], scalar1=sd_[:, 4:5], scalar2=None, op0=ALU.is_equal), r=R_, w=R_)
                kb.op("dve", lambda e: e.scalar_tensor_tensor(out=e28[:], in0=mk1[:], scalar=-1e30, in1=es8[:], op0=ALU.mult, op1=ALU.add), r=R_, w=R_)
                kb.op("dve", lambda e: e.tensor_reduce(out=sd_[:, 5:6], in_=e28[:], axis=AX.X, op=ALU.max), r=R_, w=R_)
                kb.op("dve", lambda e: e.tensor_scalar(out=mk2[:], in0=e28[:], scalar1=sd_[:, 5:6], scalar2=None, op0=ALU.is_equal), r=R_, w=R_)
                kb.op("dve", lambda e: e.tensor_tensor(out=sd_[:, 6:7], in0=sd_[:, 5:6], in1=sd_[:, 4:5], op=ALU.subtract), r=R_, w=R_)
                kb.op("act", lambda e: e.activation(out=st[:, 5:6], in_=sd_[:, 6:7], func=AF.Exp), r=R_, w=R_)
                kb.op("dve", lambda e: e.tensor_scalar(out=sd_[:, 7:8], in0=st[:, 5:6], scalar1=1.0, scalar2=None, op0=ALU.add), r=R_, w=R_)
                kb.op("dve", lambda e: e.reciprocal(out=sd_[:, 8:9], in_=sd_[:, 7:8]), r=R_, w=R_)
                kb.op("dve", lambda e: e.tensor_tensor(out=gta[:, i, 0:1], in0=sd_[:, 3:4], in1=sd_[:, 8:9], op=ALU.mult), r=R_, w=R_ + [B_rt])
                kb.op("dve", lambda e: e.tensor_tensor(out=gta[:, i, 1:2], in0=gta[:, i, 0:1], in1=st[:, 5:6], op=ALU.mult), r=R_, w=R_ + [B_rt])
                kb.op("dve", lambda e: e.tensor_tensor(out=A1a[:, i, :].rearrange("p (g j) -> p g j", g=4),
                                                       in0=oh[:].unsqueeze(2).to_broadcast([128, 4, 8]),
                                                       in1=mk1[:].unsqueeze(1).to_broadcast([128, 4, 8]), op=ALU.mult), r=R_, w=R_ + [B_rt])
                kb.op("dve", lambda e: e.tensor_tensor(out=A2a[:, i, :].rearrange("p (g j) -> p g j", g=4),
                                                       in0=oh[:].unsqueeze(2).to_broadcast([128, 4, 8]),
                                                       in1=mk2[:].unsqueeze(1).to_broadcast([128, 4, 8]), op=ALU.mult), r=R_, w=R_ + [B_rt])
            cur_rec = [None]

            def grp_done(g_):
                cur_rec[0].append(("grp", g_))

            def record(fn_stage, i):
                rec = []
                cur_rec[0] = rec

                def rop(e, fn, r=(), w=(), same=None):
                    (ingroup[0] if ingroup[0] is not None else rec).append((e, fn, list(r), list(w), same))
                kb.op = rop
                kb.dma = lambda q, sname, fn, r=(), w=(): rec.append(("dma", q, sname, fn, list(r), list(w)))
                try:
                    fn_stage(i)
                finally:
                    del kb.op
                    del kb.dma
                return rec

            def replay(t):
                if t[0] == "grp":
                    for t2 in t[1]:
                        kb.op(*t2)
                elif t[0] == "dma":
                    kb.dma(*t[1:])
                else:
                    kb.op(*t)

            def emit_x_pair(i2a):
                xa = record(stageX, i2a) if i2a < NT else []
                xb_ = record(stageX, i2a + 1) if i2a + 1 < NT else []
                for k_ in range(max(len(xa), len(xb_))):
                    if k_ < len(xa):
                        replay(xa[k_])
                    if k_ < len(xb_):
                        replay(xb_[k_])

            emit_x_pair(0)
            for i in range(0, NT, 2):
                emit_x_pair(i + 2)
                ra = record(stageR, i)
                rb = record(stageR, i + 1) if i + 1 < NT else []
                for k_ in range(max(len(ra), len(rb))):
                    if k_ < len(ra):
                        replay(ra[k_])
                    if k_ < len(rb):
                        replay(rb[k_])
            Ata = sb(sB, "Ata", [128, NT, 32], F32)
            B_Ata = kb.buf()
            for i0_ in range(0, NT, 16):
                n_ = min(16, NT - i0_)
                kb.op("dve", lambda e: e.tensor_tensor(out=Ata[:, i0_:i0_ + n_, :], in0=A1a[:, i0_:i0_ + n_, :], in1=A2a[:, i0_:i0_ + n_, :], op=ALU.add),
                      r=[B_rt], w=[B_Ata])
            Apre = sb(sB, "Apre", [128, NT, 32], F32)
            kb.op("pool", lambda e: e.memset(Apre[:, 0, :], 0.0), w=[B_Ata])
            for i in range(1, NT):
                kb.op("dve", lambda e: e.tensor_tensor(out=Apre[:, i, :], in0=Apre[:, i - 1, :], in1=Ata[:, i - 1, :], op=ALU.add),
                      r=[B_Ata], w=[B_Ata])
            for i in range(NT):
                bank, col = i // 16, (i % 16) * 32
                kb.op("pe", lambda e: e.matmul(Px[:, bank, col:col + 32], lhsT=ustr[:], rhs=Ata[:, i, :], start=True, stop=False),
                      r=[B_Ata, B_w], w=[B_Px2[bank]])
                kb.op("pe", lambda e: e.matmul(Px[:, bank, col:col + 32], lhsT=ones_f[:], rhs=Apre[:, i, :], start=False, stop=True),
                      r=[B_Ata, B_const], w=[B_Px2[bank]])
            for i in range(NT):
                kb.op("pe", lambda e: e.matmul(Pr2[:, 0:32], lhsT=ones_f[:], rhs=Ata[:, i, :], start=(i == 0), stop=(i == NT - 1)),
                      r=[B_Ata, B_const], w=[B_Pr2])
            for i0_ in range(0, NT, 16):
                n_ = min(16, NT - i0_)
                kb.op("dve", lambda e: e.tensor_copy(out=rka[:, i0_:i0_ + n_, :], in_=Px[:, i0_ // 16, 0:n_ * 32].rearrange("p (i e) -> p i e", e=32)),
                      r=[B_Px2[i0_ // 16]], w=[B_rt])
            kb.op("dve", lambda e: e.tensor_copy(out=base[:], in_=Pr2[:, 0:32]), r=[B_Pr2], w=[B_rt])
            kb.barrier()
        with ExitStack() as sC:
            thr = sb(sC, "thr", [128, NBLK, 32], F32)
            cmp_ = sb(sC, "cmp", [128, 16, 32], F32)
            nbk = sb(sC, "nbk", [128, 32], F32)
            tmp32 = sb(sC, "tmp32", [128, 32], F32)
            pend = sb(sC, "pend", [128, 32], F32)
            pst = sb(sC, "pst", [128, 32], F32)
            zer = sb(sC, "zer", [128, 32], F32)
            bef = sb(sC, "bef", [128, NBLK], F32)
            dst = sb(sC, "dst", [128, NT, 32], F32)
            prod = sb(sC, "prod", [128, 16, 32], F32)
            d1f = sb(sC, "d1f", [128, NT], F32)
            d2f = sb(sC, "d2f", [128, NT], F32)
            Q = [B_rt]
            kb.dma("sp", "const", lambda q: q.dma_start(out=thr[:].rearrange("p b e -> p (b e)"), in_=cd["blkthr"].partition_broadcast(128)), w=Q)
            kb.op("pool", lambda e: e.memset(nbk[:], 0.0), w=Q)
            kb.op("pool", lambda e: e.memset(zer[:], 0.0), w=Q)
            for b0 in range(0, NBLK, 16):
                nb_ = min(16, NBLK - b0)
                kb.op("dve", lambda e: e.tensor_tensor(out=cmp_[:, 0:nb_, :], in0=base[:].unsqueeze(1).to_broadcast([128, nb_, 32]),
                                                       in1=thr[:, b0:b0 + nb_, :], op=ALU.is_gt), r=Q, w=Q)
                kb.op("dve", lambda e: e.tensor_reduce(out=tmp32[:], in_=cmp_[:, 0:nb_, :].rearrange("p b e -> p e b"), axis=AX.X, op=ALU.add), r=Q, w=Q)
                kb.op("dve", lambda e: e.tensor_tensor(out=nbk[:], in0=nbk[:], in1=tmp32[:], op=ALU.add), r=Q, w=Q)
            kb.op("dve", lambda e: e.tensor_scalar(out=nbk[:], in0=nbk[:], scalar1=float(BS), scalar2=None, op0=ALU.mult), r=Q, w=Q)
            kb.op("dve", lambda e: e.tensor_tensor_scan(out=pend[:], data0=nbk[:], data1=zer[:], initial=0.0, op0=ALU.add, op1=ALU.add), r=Q, w=Q)
            kb.op("dve", lambda e: e.tensor_tensor(out=pst[:], in0=pend[:], in1=nbk[:], op=ALU.subtract), r=Q, w=Q)
            for b0 in range(0, NBLK, 16):
                nb_ = min(16, NBLK - b0)
                kb.op("dve", lambda e: e.tensor_tensor(out=cmp_[:, 0:nb_, :], in0=pend[:].unsqueeze(1).to_broadcast([128, nb_, 32]),
                                                       in1=thr[:, b0:b0 + nb_, :], op=ALU.is_le), r=Q, w=Q)
                kb.op("dve", lambda e: e.tensor_reduce(out=bef[:, b0:b0 + nb_], in_=cmp_[:, 0:nb_, :], axis=AX.X, op=ALU.add), r=Q, w=Q)
            kb.op("dve", lambda e: e.tensor_scalar(out=bef[:], in0=bef[:], scalar1=float(NE - 1), scalar2=None, op0=ALU.min), r=Q, w=Q)
            kb.op("dve", lambda e: e.tensor_copy(out=bexp[:], in_=bef[:]), r=Q, w=Q)
            iot = sb(sC, "iot", [128, 8], F32)
            idf = sb(sC, "idf", [128, NBLK], F32)
            kb.dma("sp", "const", lambda q: q.dma_start(out=iot[:], in_=cd["iota_pc"]), w=Q)
            kb.op("dve", lambda e: e.scalar_tensor_tensor(out=idf[:], in0=bef[:], scalar=128.0, in1=iot[:, 0:1].to_broadcast([128, NBLK]),
                                                          op0=ALU.mult, op1=ALU.add), r=Q, w=Q)
            kb.op("dve", lambda e: e.tensor_copy(out=idxw[:], in_=idf[:]), r=Q, w=Q)
            for i0_ in range(0, NT, 16):
                n_ = min(16, NT - i0_)
                kb.op("dve", lambda e: e.tensor_tensor(out=dst[:, i0_:i0_ + n_, :], in0=rka[:, i0_:i0_ + n_, :],
                                                       in1=pst[:].unsqueeze(1).to_broadcast([128, n_, 32]), op=ALU.add), r=Q, w=Q)
                for Aa, df in ((A1a, d1f), (A2a, d2f)):
                    kb.op("dve", lambda e: e.tensor_tensor(out=prod[:, 0:n_, :], in0=dst[:, i0_:i0_ + n_, :], in1=Aa[:, i0_:i0_ + n_, :], op=ALU.mult), r=Q, w=Q)
                    kb.op("dve", lambda e: e.tensor_reduce(out=df[:, i0_:i0_ + n_], in_=prod[:, 0:n_, :], axis=AX.X, op=ALU.add), r=Q, w=Q)
            kb.op("dve", lambda e: e.tensor_copy(out=d1i[:], in_=d1f[:]), r=Q, w=Q)
            kb.op("dve", lambda e: e.tensor_copy(out=d2i[:], in_=d2f[:]), r=Q, w=Q)
            kb.barrier()
        if "d1i" in dbg_d:
            kb.dma("sp", "dbg", lambda q: q.dma_start(out=dbg_d["d1i"], in_=d1i[:]), r=[B_rt])
            kb.dma("sp", "dbg", lambda q: q.dma_start(out=dbg_d["d2i"], in_=d2i[:]), r=[B_rt])
            kb.dma("sp", "dbg", lambda q: q.dma_start(out=dbg_d["bexp"], in_=bexp[:]), r=[B_rt])
            kb.dma("sp", "dbg", lambda q: q.dma_start(out=dbg_d["gta"], in_=gta[:]), r=[B_rt])
        if stage < 5:
            return
        with ExitStack() as sD:
            hb_ = [sb(sD, f"dhb{i}", [128, 1024], BF16) for i in range(4)]
            B_hb = kb.bufs(4)
            for i in range(NT):
                j = i % 4
                kb.dma("sp", f"dhb{j}", lambda q: q.dma_start(out=hb_[j][:], in_=hn_d[i * 128:(i + 1) * 128, :]), r=[B_hn[i]], w=[B_hb[j]])
                for di in (d1i, d2i):
                    kb.dma("pool", "disp", lambda q: q.indirect_dma_start(out=xs_d[:, :], out_offset=bass.IndirectOffsetOnAxis(ap=di[:, i:i + 1], axis=0),
                                                                           in_=hb_[j][:], in_offset=None), r=[B_hb[j], B_rt], w=[B_xs])
            kb.barrier()
        B_ys = kb.buf()
        with ExitStack() as sE:
            NWB = 3
            W1 = [sb(sE, f"eW1{i}", [128, 8, 512], BF16) for i in range(NWB)]
            W3 = [sb(sE, f"eW3{i}", [128, 8, 512], BF16) for i in range(NWB)]
            W2 = [sb(sE, f"eW2{i}", [128, 4, 1024], BF16) for i in range(NWB)]
            B_W = kb.bufs(NWB)
            xb = [sb(sE, f"exb{i}", [128, 1024], BF16) for i in range(2)]
            B_xb = kb.bufs(2)
            xT = [sb(sE, f"exT{i}", [128, 8, 128], BF16) for i in range(2)]
            sl = [sb(sE, f"esl{i}", [128, 512], F32) for i in range(2)]
            hid = [sb(sE, f"ehid{i}", [128, 512], BF16) for i in range(2)]
            hidT = [sb(sE, f"ehidT{i}", [128, 4, 128], BF16) for i in range(2)]
            ysb = [sb(sE, f"eys{i}", [128, 1024], F32) for i in range(2)]
            B_ysb = kb.bufs(2)
            B_xT, B_sl, B_hid, B_hidT = kb.bufs(2), kb.bufs(2), kb.bufs(2), kb.bufs(2)
            PTb = [ps(sE, f"ePT{i}", [128, 8, 128], BF16) for i in range(2)]
            Ph1 = [ps(sE, f"ePh1{i}", [128, 512], F32) for i in range(2)]
            Ph3 = [ps(sE, f"ePh3{i}", [128, 512], F32) for i in range(2)]
            Py = ps(sE, "ePy", [128, 2, 512], F32)
            B_PTb, B_Ph1, B_Ph3 = kb.bufs(2), kb.bufs(2), kb.bufs(2)
            B_Py = kb.buf()
            w1r, w3r, w2r = w1b_d, w3b_d, w2b_d
            B_xT2 = [kb.bufs(2), kb.bufs(2)]
            B_ysb2 = [kb.bufs(2), kb.bufs(2)]
            NB128 = NBLK * NSUB

            def load_w(sbi):
                k = sbi % NWB
                for Wt_, wr_ in ((W1, w1r), (W3, w3r), (W2, w2r)):
                    kb.dma("pool", f"ew{k}", lambda q: q.indirect_dma_start(out=Wt_[k][:].rearrange("p c f -> p (c f)"), out_offset=None, in_=wr_[:, :],
                           in_offset=bass.IndirectOffsetOnAxis(ap=idxw[:, sbi:sbi + 1], axis=0)), r=[B_rt, B_wconv], w=[B_W[k]])

            def load_x(b):
                j = b % 2
                kb.dma("sp", f"exb{j}", lambda q: q.dma_start(out=xb[j][:], in_=xs_d[b * 128:(b + 1) * 128, :]), r=[B_xs], w=[B_xb[j]])

            def stageA(b):
                j = b % 2
                k = (b // NSUB) % NWB
                for c in range(8):
                    kb.op("pe", lambda e: e.transpose(out=PTb[j][:, c, :], in_=xb[j][:].rearrange("s (p c) -> s c p", c=8)[:, c, :], identity=ident_bf[:]),
                          r=[B_xb[j], B_const], w=[B_PTb[j]])
                kb.op("dve", lambda e: e.tensor_copy(out=xT[j][:, 0:4, :], in_=PTb[j][:, 0:4, :]), r=[B_PTb[j]], w=[B_xT2[j][0]])
                kb.op("dve", lambda e: e.tensor_copy(out=xT[j][:, 4:8, :], in_=PTb[j][:, 4:8, :]), r=[B_PTb[j]], w=[B_xT2[j][1]])
                for c in range(8):
                    kb.op("pe", lambda e: e.matmul(Ph1[j][:], lhsT=xT[j][:, c, :], rhs=W1[k][:, c, :], start=(c == 0), stop=(c == 7)),
                          r=[B_xT2[j][c // 4], B_W[k]], w=[B_Ph1[j]])
                for c in range(8):
                    kb.op("pe", lambda e: e.matmul(Ph3[j][:], lhsT=xT[j][:, c, :], rhs=W3[k][:, c, :], start=(c == 0), stop=(c == 7)),
                          r=[B_xT2[j][c // 4], B_W[k]], w=[B_Ph3[j]])
                kb.op("act", lambda e: e.activation(out=sl[j][:], in_=Ph1[j][:], func=AF.Silu), r=[B_Ph1[j]], w=[B_sl[j]])
                kb.op("dve", lambda e: e.tensor_tensor(out=hid[j][:], in0=sl[j][:], in1=Ph3[j][:], op=ALU.mult), r=[B_sl[j], B_Ph3[j]], w=[B_hid[j]])

            def stageB(b):
                j = b % 2
                k = (b // NSUB) % NWB
                for c in range(4):
                    kb.op("pe", lambda e: e.transpose(out=PTb[j][:, c, :], in_=hid[j][:].rearrange("s (p c) -> s c p", c=4)[:, c, :], identity=ident_bf[:]),
                          r=[B_hid[j], B_const], w=[B_PTb[j]])
                kb.op("act", lambda e: e.copy(out=hidT[j][:], in_=PTb[j][:, 0:4, :]), r=[B_PTb[j]], w=[B_hidT[j]])
                for half in range(2):
                    for c in range(4):
                        kb.op("pe", lambda e: e.matmul(Py[:, half, :], lhsT=hidT[j][:, c, :], rhs=W2[k][:, c, half * 512:(half + 1) * 512],
                                                       start=(c == 0), stop=(c == 3)), r=[B_hidT[j], B_W[k]], w=[B_Py])
                kb.op("act", lambda e: e.copy(out=ysb[j][:, 0:512], in_=Py[:, 0, :]), r=[B_Py], w=[B_ysb2[j][0]])
                kb.op("dve", lambda e: e.tensor_copy(out=ysb[j][:, 512:1024], in_=Py[:, 1, :]), r=[B_Py], w=[B_ysb2[j][1]])
                kb.dma("sp", "yst", lambda q: q.dma_start(out=ys_d[b * 128:(b + 1) * 128, :], in_=ysb[j][:]), r=B_ysb2[j], w=[B_ys])

            for s0 in range(min(NWB, NBLK)):
                load_w(s0)
            load_x(0)
            for b in range(NB128 + 1):
                if b + 1 < NB128:
                    load_x(b + 1)
                if b < NB128:
                    stageA(b)
                if b >= 1:
                    stageB(b - 1)
                    if (b - 1) % NSUB == NSUB - 1:
                        nxt = (b - 1) // NSUB + NWB
                        if nxt < NBLK:
                            load_w(nxt)
            kb.barrier()
        with ExitStack() as sF:
            NCB = 4
            y1 = [sb(sF, f"cy1{i}", [128, 1024], F32) for i in range(NCB)]
            y2 = [sb(sF, f"cy2{i}", [128, 1024], F32) for i in range(NCB)]
            xr = [sb(sF, f"cxr{i}", [128, 1024], F32) for i in range(NCB)]
            B_y1, B_y2, B_xr = kb.bufs(NCB), kb.bufs(NCB), kb.bufs(NCB)

            def cload(i):
                j = i % NCB
                rows = slice(i * 128, (i + 1) * 128)
                kb.dma("pool", f"cg1{j}", lambda q: q.indirect_dma_start(out=y1[j][:], out_offset=None, in_=ys_d[:, :],
                       in_offset=bass.IndirectOffsetOnAxis(ap=d1i[:, i:i + 1], axis=0)), r=[B_ys, B_rt], w=[B_y1[j]])
                kb.dma("pool", f"cg2{j}", lambda q: q.indirect_dma_start(out=y2[j][:], out_offset=None, in_=ys_d[:, :],
                       in_offset=bass.IndirectOffsetOnAxis(ap=d2i[:, i:i + 1], axis=0)), r=[B_ys, B_rt], w=[B_y2[j]])
                kb.dma("act", f"cxr{j}", lambda q: q.dma_start(out=xr[j][:], in_=out_d[rows, :]), r=[B_out[i]], w=[B_xr[j]])

            for i in range(min(NCB - 1, NT)):
                cload(i)
            for i in range(NT):
                j = i % NCB
                rows = slice(i * 128, (i + 1) * 128)
                if i + NCB - 1 < NT:
                    cload(i + NCB - 1)
                for half in range(2):
                    hs = slice(half * 512, (half + 1) * 512)
                    kb.op("dve", lambda e: e.scalar_tensor_tensor(out=xr[j][:, hs], in0=y1[j][:, hs], scalar=gta[:, i, 0:1], in1=xr[j][:, hs],
                                                                  op0=ALU.mult, op1=ALU.add), r=[B_y1[j], B_rt], w=[B_xr[j]])
                    kb.op("dve", lambda e: e.scalar_tensor_tensor(out=xr[j][:, hs], in0=y2[j][:, hs], scalar=gta[:, i, 1:2], in1=xr[j][:, hs],
                                                                  op0=ALU.mult, op1=ALU.add), r=[B_y2[j], B_rt], w=[B_xr[j]])
                kb.dma("sp", "ost2", lambda q: q.dma_start(out=out_d[rows, :], in_=xr[j][:]), r=[B_xr[j]], w=[B_out[i]])
            kb.barrier()


def _shared_maps(inp, consts):
    f = np.float32
    g = lambda k: np.ascontiguousarray(np.asarray(inp[k], dtype=f)[0])
    m = {}
    m["w_in"] = g("w_in")
    m["w_branch_da"] = g("w_branch_da")
    m["w_branch_ml"] = g("w_branch_ml")
    m["w_gate"] = g("w_gate")
    m["w_out"] = g("w_out")
    m["w1"] = g("w1")
    m["w3"] = g("w3")
    m["w2"] = g("w2")
    m["w_rt"] = np.ascontiguousarray(np.concatenate([g("w_group"), g("w_router")], axis=1))
    m["attn_norm_g"] = g("attn_norm_g")[None, :]
    m["ffn_norm_g"] = g("ffn_norm_g")[None, :]
    m["da_out_norm_g"] = g("da_out_norm_g")[None, :]
    m["ml_out_norm_g"] = g("ml_out_norm_g")[None, :]
    m["b_rt"] = np.concatenate([g("b_group"), g("b_router")])[None, :]
    m["b_if"] = np.concatenate([g("ml_i_bias"), g("ml_f_bias")])[None, :]
    m["lamv"] = np.concatenate([g("da_lambda_q1"), g("da_lambda_k1"), g("da_lambda_q2"), g("da_lambda_k2")])[None, :]
    m["gqk_col"] = np.ascontiguousarray(np.stack([np.tile(g("da_q_norm_g"), 2), np.tile(g("da_k_norm_g"), 2)], axis=1))
    cw = g("ml_conv_w")
    m["cw_col"] = np.ascontiguousarray(cw.reshape(4, 8, 128).transpose(2, 1, 0).reshape(128, 32))
    m["cb_col"] = np.ascontiguousarray(g("ml_conv_b").reshape(8, 128).T)
    m["bg_col"] = np.ascontiguousarray(g("b_gate").reshape(16, 128).T)
    for k, v in consts.items():
        m["c_" + k] = v
    return m


_CACHE = {}


def kernel(**inputs):
    x = np.asarray(inputs["x"], dtype=np.float32)
    B, S, _ = x.shape
    key = (S,)
    if key not in _CACHE:
        _CACHE[key] = build_nc(S)
    nc, consts = _CACHE[key]
    shared = _shared_maps(inputs, consts)
    in_maps = []
    for b in range(B):
        m = dict(shared)
        m["x"] = np.ascontiguousarray(x[b])
        in_maps.append(m)
    res = run_bass_kernel_spmd(nc, in_maps, core_ids=list(range(B)))
    return np.stack([np.asarray(r["out"], dtype=np.float32) for r in res.results], axis=0)
```

```python
import math
from contextlib import ExitStack
import numpy as np
import ml_dtypes
import concourse.bass as bass
import concourse.mybir as mybir
from concourse.bass_utils import run_bass_kernel_spmd

F32 = mybir.dt.float32
BF16 = mybir.dt.bfloat16
I32 = mybir.dt.int32
AF = mybir.ActivationFunctionType
ALU = mybir.AluOpType
AX = mybir.AxisListType

D = 1024
NH = 4
IN_W = 3592
OFF_DA_Q, OFF_DA_K, OFF_DA_V = 0, 512, 1024
OFF_ML_QK, OFF_ML_V, OFF_ML_O, OFF_ML_I = 1536, 2560, 3072, 3584
NE = 32
EPS = 1e-6
LAM_INIT = 0.8 - 0.6 * math.exp(-0.3 * 0)
SLOPES = [2.0 ** (-8.0 * (i + 1) / NH) for i in range(NH)]
SKIP_T = 60.0
NOFF = 36
OFF0 = 31
ML_LOOKAHEAD = 3
BS = 256


class Buf:
    __slots__ = ("w", "r", "name")

    def __init__(self, name=""):
        self.w = None
        self.r = {}
        self.name = name


class KB:
    def __init__(self, nc, es):
        self.nc = nc
        self.es = es
        self.eng = {"pe": nc.tensor, "act": nc.scalar, "dve": nc.vector, "pool": nc.gpsimd, "sp": nc.sync}
        self.sem = {k: es.enter_context(nc.semaphore("sem_" + k)) for k in self.eng}
        self.cnt = {k: 0 for k in self.eng}
        self.seen = {k: {} for k in self.eng}
        self.dsem = {}
        self.dcnt = {}
        self.nbuf = 0

    def buf(self, name=""):
        self.nbuf += 1
        return Buf(name)

    def bufs(self, n, name=""):
        return [self.buf(name + str(i)) for i in range(n)]

    def dma_sem(self, name):
        if name not in self.dsem:
            self.dsem[name] = self.es.enter_context(self.nc.semaphore("dsem_" + name))
            self.dcnt[name] = 0
        return name

    def _semh(self, key):
        return self.sem[key] if key in self.sem else self.dsem[key]

    def _wait(self, e, reads, writes, same=True, skip=None):
        deps = {}
        for b in reads:
            if b.w is not None:
                k, v = b.w
                deps[k] = max(deps.get(k, 0), v)
        for b in writes:
            if b.w is not None:
                k, v = b.w
                deps[k] = max(deps.get(k, 0), v)
            for k, v in b.r.items():
                deps[k] = max(deps.get(k, 0), v)
        for k, v in deps.items():
            if k == e and not same:
                continue
            if k == skip:
                continue
            if k in self.dcnt:
                v = self.dcnt[k]
            if self.seen[e].get(k, 0) >= v:
                continue
            self.eng[e].wait_ge(self._semh(k), v)
            self.seen[e][k] = v

    def op(self, e, fn, r=(), w=(), same=None):
        if same is None:
            same = (e != "pe")
        self._wait(e, r, w, same)
        inst = fn(self.eng[e])
        self.cnt[e] += 1
        inst.then_inc(self.sem[e], 1)
        ev = (e, self.cnt[e])
        for b in w:
            b.w = ev
            b.r = {}
        for b in r:
            if b not in w:
                b.r[e] = self.cnt[e]
        return inst

    def dma(self, q, sname, fn, r=(), w=()):
        self.dma_sem(sname)
        self._wait(q, r, w, True, skip=sname)
        inst = fn(self.eng[q])
        self.dcnt[sname] += 16
        inst.then_inc(self.dsem[sname], 16)
        ev = (sname, self.dcnt[sname])
        for b in w:
            b.w = ev
            b.r = {}
        for b in r:
            if b not in w:
                b.r[sname] = self.dcnt[sname]
        return inst

    def barrier(self):
        evs = [(k, self.cnt[k]) for k in self.sem if self.cnt[k] > 0] + [(k, v) for k, v in self.dcnt.items() if v > 0]
        for e in self.eng:
            for k, v in evs:
                if k == e:
                    continue
                if self.seen[e].get(k, 0) >= v:
                    continue
                self.eng[e].wait_ge(self._semh(k), v)
                self.seen[e][k] = v

    def wait_all(self, e, bufs):
        self._wait(e, bufs, bufs, True)


def _mk_consts(S):
    c = {}
    bf = ml_dtypes.bfloat16
    c["ident_bf"] = np.eye(128, dtype=np.float32).astype(bf)
    c["ident_f"] = np.eye(128, dtype=np.float32)
    blk = np.zeros((128, 128), np.float32)
    blk[:64, :64] = 1.0 / 64
    blk[64:, 64:] = 1.0 / 64
    c["blk64"] = blk.astype(bf)
    k = np.arange(128)[:, None]
    q = np.arange(128)[None, :]
    c["cmask"] = (k <= q).astype(np.float32).astype(bf)
    c["negm"] = np.where(k <= q, 0.0, -30000.0).astype(np.float32)
    c["ones_f"] = np.ones((128, 128), np.float32)
    c["ustrict"] = (k < q).astype(np.float32)
    tab = np.zeros((128, NH * NOFF), np.float32)
    for h in range(NH):
        for j in range(NOFF):
            tab[:, h * NOFF + j] = SLOPES[h] * (np.arange(128) + 128.0 * (j - OFF0))
    c["atab"] = tab
    c["iota_pc"] = (np.arange(8)[None, :] * 128.0 + np.arange(128)[:, None]).astype(np.float32)
    nb = (2 * S) // BS + NE
    c["blkthr"] = np.tile((np.arange(nb, dtype=np.float32) * float(BS))[None, :, None], (1, 1, NE)).reshape(1, nb * NE)
    return c


CONST_DT = {"ident_bf": BF16, "ident_f": F32, "blk64": BF16, "cmask": BF16, "negm": F32, "ones_f": F32,
            "ustrict": F32, "atab": F32, "blkthr": F32, "iota_pc": F32}


def build_nc(S=4096, stage=99, dbg=None):
    NT = S // 128
    NB5 = S // 512
    CAP = 2 * S + NE * BS
    nc = bass.Bass("TRN2", target_bir_lowering=False)
    consts = _mk_consts(S)

    def din(name, shape, dt=F32):
        return nc.dram_tensor(name, list(shape), dt, kind="ExternalInput").ap()

    x_d = din("x", [S, D])
    w_in = din("w_in", [D, IN_W])
    w_bda = din("w_branch_da", [512, D])
    w_bml = din("w_branch_ml", [512, D])
    w_gate = din("w_gate", [D, 2 * D])
    w_out = din("w_out", [D, D])
    w1 = din("w1", [NE, D, 512])
    w3 = din("w3", [NE, D, 512])
    w2 = din("w2", [NE, 512, D])
    wr_d = din("w_rt", [D, 36])
    g_attn = din("attn_norm_g", [1, D])
    g_ffn = din("ffn_norm_g", [1, D])
    g_dao = din("da_out_norm_g", [1, 128])
    g_mlo = din("ml_out_norm_g", [1, 512])
    b_rt = din("b_rt", [1, 36])
    b_if = din("b_if", [1, 8])
    lamv = din("lamv", [1, 256])
    gqk_col = din("gqk_col", [128, 2])
    cw_col = din("cw_col", [128, 8 * 4])
    cb_col = din("cb_col", [128, 8])
    bg_col = din("bg_col", [128, 16])
    cd = {k: din("c_" + k, list(v.shape), CONST_DT[k]) for k, v in consts.items()}
    out_d = nc.dram_tensor("out", [S, D], F32, kind="ExternalOutput").ap()
    dbg_d = {}
    if dbg:
        for k, (shape, dt) in dbg.items():
            dbg_d[k] = nc.dram_tensor("dbg_" + k, list(shape), dt, kind="ExternalOutput").ap()
    xs_d = nc.dram_tensor("xs_scr", [CAP, D], BF16).ap()
    ys_d = nc.dram_tensor("ys_scr", [CAP, D], F32).ap()
    hn_d = nc.dram_tensor("hn_scr", [S, D], BF16).ap()
    w1b_d = nc.dram_tensor("w1b_scr", [NE * 128, 4096], BF16).ap()
    w3b_d = nc.dram_tensor("w3b_scr", [NE * 128, 4096], BF16).ap()
    w2b_d = nc.dram_tensor("w2b_scr", [NE * 128, 4096], BF16).ap()

    es = ExitStack()
    with es:
        kb = KB(nc, es)

        def sb(stack, name, shape, dt):
            return stack.enter_context(nc.sbuf_tensor(name, list(shape), dt))

        def ps(stack, name, shape, dt=F32):
            return stack.enter_context(nc.psum_tensor(name, list(shape), dt))

        ident_bf = sb(es, "ident_bf", [128, 128], BF16)
        ident_f = sb(es, "ident_f", [128, 128], F32)
        ones_f = sb(es, "ones_f", [128, 128], F32)
        B_const = kb.buf("const")
        for t, k in ((ident_bf, "ident_bf"), (ident_f, "ident_f"), (ones_f, "ones_f")):
            kb.dma("sp", "const", lambda q, t=t, k=k: q.dma_start(out=t[:], in_=cd[k]), w=[B_const])
        zero_bf = sb(es, "zero_bf", [128, 2048], BF16)
        B_zero = kb.buf("zero")
        for zi in range(4):
            kb.op("pool", lambda e: e.memset(zero_bf[:, zi * 512:(zi + 1) * 512], 0.0), w=[B_zero])

        hT = sb(es, "hT", [128, 8, S], BF16)
        B_hT = kb.bufs(NT, "hT")
        es_mix = ExitStack()
        y_daT = sb(es_mix, "y_daT", [128, 4, S], BF16)
        B_ydaT = kb.bufs(NT * NH, "ydaT")
        B_ymlT = kb.bufs(NT * NH, "ymlT")

        B_xs = kb.buf()
        zf_rows = list(range(0, CAP, 256)) if stage >= 4 else []
        zf_per = (len(zf_rows) + NT - 1) // NT
        with ExitStack() as s1:
            gat = sb(s1, "gat", [128, D], F32)
            B_gat = kb.buf()
            kb.dma("sp", "const", lambda q: q.dma_start(out=gat[:], in_=g_attn.partition_broadcast(128)), w=[B_gat])
            xt = [sb(s1, f"xt{i}", [128, D], F32) for i in range(2)]
            xn = [sb(s1, f"xn{i}", [128, D], BF16) for i in range(2)]
            junk = sb(s1, "junk1", [128, D], F32)
            ssq = sb(s1, "ssq", [128, NT], F32)
            rst = sb(s1, "rst", [128, NT], F32)
            rsd = sb(s1, "rsd", [128, NT], F32)
            ssq2 = sb(s1, "ssq2", [128, 2 * NT], F32)
            pT = [ps(s1, f"pT{i}", [128, 8, 128], BF16) for i in range(2)]
            B_xt, B_xn, B_pT = kb.bufs(2), kb.bufs(2), kb.bufs(2)
            B_junk, B_ss = kb.buf(), kb.bufs(NT)
            for i in range(NT):
                j = i % 2
                kb.dma("sp", f"xt{j}", lambda q: q.dma_start(out=xt[j][:], in_=x_d[i * 128:(i + 1) * 128, :]), w=[B_xt[j]])
                for r0 in zf_rows[i * zf_per:(i + 1) * zf_per]:
                    kb.dma("sp", "xsz", lambda q: q.dma_start(out=xs_d[r0:r0 + 256, :].rearrange("(p a) d -> p (a d)", a=2), in_=zero_bf[:, 0:2048]),
                           r=[B_zero], w=[B_xs])
                for hf in range(2):
                    kb.op("act", lambda e: e.activation(out=junk[:, hf * 512:(hf + 1) * 512], in_=xt[j][:, hf * 512:(hf + 1) * 512],
                                                        func=AF.Square, accum_out=ssq2[:, 2 * i + hf:2 * i + hf + 1]),
                          r=[B_xt[j]], w=[B_junk, B_ss[i]])
                kb.op("act", lambda e: e.activation(out=rst[:, i:i + 1], in_=ssq2[:, 2 * i:2 * i + 1], func=AF.Identity,
                                                    bias=ssq2[:, 2 * i + 1:2 * i + 2]), r=[B_ss[i]], w=[B_ss[i]])
                kb.op("act", lambda e: e.activation(out=rst[:, i:i + 1], in_=rst[:, i:i + 1], func=AF.Sqrt, scale=1.0 / D, bias=EPS),
                      r=[B_ss[i]], w=[B_ss[i]])
                kb.op("dve", lambda e: e.reciprocal(out=rsd[:, i:i + 1], in_=rst[:, i:i + 1]), r=[B_ss[i]], w=[B_ss[i]])
                for hf in range(2):
                    kb.op("dve", lambda e: e.scalar_tensor_tensor(out=xn[j][:, hf * 512:(hf + 1) * 512], in0=xt[j][:, hf * 512:(hf + 1) * 512],
                                                                  scalar=rsd[:, i:i + 1], in1=gat[:, hf * 512:(hf + 1) * 512],
                                                                  op0=ALU.mult, op1=ALU.mult),
                          r=[B_xt[j], B_ss[i], B_gat], w=[B_xn[j]])
                for c in range(8):
                    kb.op("pe", lambda e: e.transpose(out=pT[j][:, c, :], in_=xn[j][:, c * 128:(c + 1) * 128], identity=ident_bf[:]),
                          r=[B_xn[j], B_const], w=[B_pT[j]])
                for hf in range(2):
                    kb.op("act", lambda e: e.copy(out=hT[:, hf * 4:hf * 4 + 4, i * 128:(i + 1) * 128], in_=pT[j][:, hf * 4:hf * 4 + 4, :]),
                          r=[B_pT[j]], w=[B_hT[i]])

        kb.barrier()
        if "hT" in dbg_d:
            kb.dma("sp", "dbg", lambda q: q.dma_start(out=dbg_d["hT"], in_=hT[:]), r=B_hT)

        B_wconv = kb.buf()
        conv_state = [0]

        def conv_next(n=1):
            if stage < 5:
                return
            for _ in range(n):
                e_ = conv_state[0]
                if e_ >= NE:
                    return
                conv_state[0] += 1
                for src_, dst_, c_ in ((w1, w1b_d, 8), (w3, w3b_d, 8), (w2, w2b_d, 4)):
                    kb.dma("pool", "wconv", lambda q: q.dma_start(out=dst_[e_ * 128:(e_ + 1) * 128, :],
                           in_=src_[e_].rearrange("(p c) f -> p (c f)", c=c_)), w=[B_wconv])
        if stage >= 2:
            _da_phase(nc, kb, S, hT, B_hT, y_daT, B_ydaT, w_in, cd, gqk_col, g_dao, lamv, ident_bf, zero_bf, B_const, B_zero, sb, ps, dbg_d, conv_next)
        conv_next(NE)
        y_mlT = sb(es_mix, "y_mlT", [128, 4, S], BF16)
        if stage >= 3:
            _ml_phase(nc, kb, S, hT, B_hT, y_mlT, B_ymlT, w_in, cd, cw_col, cb_col, g_mlo, b_if, ident_bf, ident_f, ones_f,
                      B_const, sb, ps, dbg_d)
        if stage >= 4:
            _merge_moe(nc, kb, S, hT, B_hT, y_daT, B_ydaT, y_mlT, B_ymlT, es_mix, x_d, out_d, w_bda, w_bml, w_gate, w_out,
                       bg_col, g_ffn, wr_d, b_rt, w1, w3, w2, xs_d, ys_d, hn_d, cd, ident_bf, ident_f, ones_f, zero_bf,
                       B_const, B_zero, sb, ps, dbg_d, stage, B_xs, (w1b_d, w3b_d, w2b_d, B_wconv))
        else:
            es_mix.close()

        allb = []
        for k in list(kb.dsem.keys()):
            b = Buf()
            b.w = (k, kb.dcnt[k])
            allb.append(b)
        for k in kb.sem:
            if kb.cnt[k] > 0:
                b = Buf()
                b.w = (k, kb.cnt[k])
                allb.append(b)
        kb._wait("sp", allb, [], True)
    return nc, consts


def _da_phase(nc, kb, S, hT, B_hT, y_daT, B_ydaT, w_in, cd, gqk_col, g_dao, lamv, ident_bf, zero_bf, B_const, B_zero, sb, ps, dbg_d, conv_next):
    NT = S // 128
    NB5 = S // 512
    with ExitStack() as s:
        blk64 = sb(s, "blk64", [128, 128], BF16)
        cmask = sb(s, "cmask", [128, 128], BF16)
        atab = sb(s, "atab", [128, NH * NOFF], F32)
        gqk = sb(s, "gqk", [128, 2], F32)
        gdo = sb(s, "gdo", [128, 128], F32)
        lam_t = sb(s, "lam_t", [128, 256], F32)
        B_c = kb.buf()
        for t, src in ((blk64, cd["blk64"]), (cmask, cd["cmask"]), (atab, cd["atab"]), (gqk, gqk_col),
                       (gdo, g_dao.partition_broadcast(128)), (lam_t, lamv.partition_broadcast(128))):
            kb.dma("sp", "const", lambda q, t=t, src=src: q.dma_start(out=t[:], in_=src), w=[B_c])
        lj = sb(s, "lj", [128, 128], F32)
        ls = sb(s, "ls", [128, 4], F32)
        neglam = sb(s, "neglam", [128, 1], F32)
        B_l = kb.buf()
        lv = lam_t[:].rearrange("p (a b d) -> p a b d", a=2, b=2)
        kb.op("dve", lambda e: e.tensor_tensor(out=lj[:].rearrange("p (a d) -> p a d", a=2), in0=lv[:, :, 0, :], in1=lv[:, :, 1, :],
                                               op=ALU.mult), r=[B_c], w=[B_l])
        kb.op("dve", lambda e: e.tensor_reduce(out=ls[:, 0:2], in_=lj[:].rearrange("p (a d) -> p a d", a=2), axis=AX.X, op=ALU.add),
              r=[B_l], w=[B_l])
        kb.op("act", lambda e: e.activation(out=ls[:, 2:4], in_=ls[:, 0:2], func=AF.Exp), r=[B_l], w=[B_l])
        kb.op("dve", lambda e: e.tensor_tensor(out=neglam[:], in0=ls[:, 3:4], in1=ls[:, 2:3], op=ALU.subtract), r=[B_l], w=[B_l])
        kb.op("dve", lambda e: e.tensor_scalar(out=neglam[:], in0=neglam[:], scalar1=-LAM_INIT, scalar2=None, op0=ALU.add),
              r=[B_l], w=[B_l])
        kb.op("dve", lambda e: e.tensor_scalar(out=gdo[:], in0=gdo[:], scalar1=1.0 - LAM_INIT, scalar2=None, op0=ALU.mult),
              r=[B_c], w=[B_c])

        P3 = [ps(s, f"daP{i}", [128, 512], F32) for i in range(3)]
        B_P3 = kb.bufs(3)
        acc4 = ps(s, "daAcc", [128, 4, 512], F32)
        B_acc = kb.buf()
        ptr = ps(s, "daPtr", [128, 8, 128], BF16)
        B_ptr = kb.buf()
        pcnt = [0]

        def nextP():
            i = pcnt[0] % 3
            pcnt[0] += 1
            return P3[i], B_P3[i]

        Vda = sb(s, "Vda", [128, NT, 4, 130], BF16)
        B_V = kb.bufs(NT)
        B_Vones = kb.buf()
        kb.op("pool", lambda e: e.memset(Vda[:, :, :, 128:130], 1.0), w=[B_Vones])
        with ExitStack() as sv:
            wv = sb(sv, "wv", [128, 8, 512], BF16)
            B_wv = kb.buf()
            kb.dma("pool", "wv", lambda q: q.dma_start(out=wv[:], in_=w_in[:, OFF_DA_V:OFF_DA_V + 512].rearrange("(c p) f -> p c f", p=128)),
                   w=[B_wv])
            for i in range(NT):
                P, BP = nextP()
                for c in range(8):
                    kb.op("pe", lambda e: e.matmul(P[:], lhsT=hT[:, c, i * 128:(i + 1) * 128], rhs=wv[:, c, :], start=(c == 0), stop=(c == 7)),
                          r=[B_hT[i], B_wv], w=[BP])
                kb.op("act", lambda e: e.copy(out=Vda[:, i, :, 0:128], in_=P[:].rearrange("p (h d) -> p h d", h=4)),
                      r=[BP, B_Vones], w=[B_V[i]])
        kb.barrier()

        wqk = [sb(s, f"wqk{i}", [128, 8, 256], BF16) for i in range(2)]
        B_wqk = kb.bufs(2)
        qkT = [sb(s, f"qkT{i}", [128, 2, S], BF16) for i in range(2)]
        B_qk = [kb.bufs(NB5 * 2) for _ in range(2)]
        sq_sb = [sb(s, f"sq_sb{i}", [128, 512], BF16) for i in range(2)]
        sd_sb = [sb(s, f"sd_sb{i}", [128, 512], F32) for i in range(2)]
        B_sq, B_sd = kb.bufs(2), kb.bufs(2)
        Et = [sb(s, f"Et{i}", [128, 512], BF16) for i in range(4)]
        B_Et = kb.bufs(4)
        ecnt = 0
        o_sb = sb(s, "o_sb", [128, 4, 128], F32)
        t_sb = sb(s, "t_sb", [128, 4, 128], F32)
        y_sb = sb(s, "y_sb", [128, 4, 128], BF16)
        rr = sb(s, "rr", [128, 16], F32)
        rra = sb(s, "rra", [128, 8], F32)
        B_o = kb.buf()

        def load_w(h):
            hb = h % 2
            kb.dma("pool", f"wqk{hb}", lambda q: q.dma_start(out=wqk[hb][:, :, 0:128],
                   in_=w_in[:, OFF_DA_Q + h * 128:OFF_DA_Q + (h + 1) * 128].rearrange("(c p) f -> p c f", p=128)), w=[B_wqk[hb]])
            kb.dma("pool", f"wqk{hb}", lambda q: q.dma_start(out=wqk[hb][:, :, 128:256],
                   in_=w_in[:, OFF_DA_K + h * 128:OFF_DA_K + (h + 1) * 128].rearrange("(c p) f -> p c f", p=128)), w=[B_wqk[hb]])

        load_w(0)
        for h in range(NH):
            hb = h % 2
            if h + 1 < NH:
                load_w(h + 1)
            slope = SLOPES[h]
            k2 = 0
            for tb in range(NB5):
                for which in range(2):
                    P, BP = nextP()
                    for c in range(8):
                        kb.op("pe", lambda e: e.matmul(P[:], lhsT=wqk[hb][:, c, which * 128:(which + 1) * 128],
                                                       rhs=hT[:, c, tb * 512:(tb + 1) * 512], start=(c == 0), stop=(c == 7)),
                              r=B_hT[tb * 4:tb * 4 + 4] + [B_wqk[hb]], w=[BP])
                    kk = k2 % 2
                    k2 += 1
                    kb.op("act", lambda e: e.activation(out=sq_sb[kk][:], in_=P[:], func=AF.Square), r=[BP], w=[B_sq[kk]])
                    P2, BP2 = nextP()
                    kb.op("pe", lambda e: e.matmul(P2[:], lhsT=blk64[:], rhs=sq_sb[kk][:], start=True, stop=True),
                          r=[B_sq[kk], B_c], w=[BP2])
                    kb.op("act", lambda e: e.activation(out=sd_sb[kk][:], in_=P2[:], func=AF.Ln, bias=EPS), r=[BP2], w=[B_sd[kk]])
                    kb.op("act", lambda e: e.activation(out=sd_sb[kk][:], in_=sd_sb[kk][:], func=AF.Exp, scale=-0.5), r=[B_sd[kk]], w=[B_sd[kk]])
                    kb.op("dve", lambda e: e.scalar_tensor_tensor(out=qkT[hb][:, which, tb * 512:(tb + 1) * 512], in0=P[:],
                                                                  scalar=gqk[:, which:which + 1], in1=sd_sb[kk][:],
                                                                  op0=ALU.mult, op1=ALU.mult),
                          r=[BP, B_sd[kk], B_c], w=[B_qk[hb][tb * 2 + which]])
            if h == 0 and "wqk" in dbg_d:
                kb.dma("sp", "dbg", lambda q: q.dma_start(out=dbg_d["wqk"], in_=wqk[0][:]), r=[B_wqk[0]])
            if h == 0 and "sd" in dbg_d:
                kb.dma("sp", "dbg", lambda q: q.dma_start(out=dbg_d["sd"], in_=sd_sb[0][:]), r=[B_sd[0]])
                kb.dma("sp", "dbg", lambda q: q.dma_start(out=dbg_d["sq"], in_=sq_sb[0][:]), r=[B_sq[0]])
            if h == 0 and "qkT" in dbg_d:
                kb.dma("sp", "dbg", lambda q: q.dma_start(out=dbg_d["qkT"], in_=qkT[0][:]), r=B_qk[0])
            sub = 128 if slope * 511 > 32.0 else 512
            units = []
            for qb in range(NB5):
                kt_max_ = 4 * qb + 3
                kt_min_ = max(0, int(math.ceil((qb * 512 - SKIP_T / slope - 127) / 128.0)))
                for kt in range(kt_min_, kt_max_ + 1):
                    for m in range(2):
                        units.append((qb, kt, m, kt == kt_min_ and m == 0, kt == kt_max_ and m == 1))
            ustate = {}
            pending = []

            def emit_qk(u):
                nonlocal ecnt
                qb, kt, m, _, _ = u
                q0 = qb * 512
                jj = kt - 4 * qb
                j_lo = max(0, jj)
                P, BP = nextP()
                kb.op("pe", lambda e: e.matmul(P[:, j_lo * 128:512], lhsT=qkT[hb][m * 64:(m + 1) * 64, 1, kt * 128:(kt + 1) * 128],
                                               rhs=qkT[hb][m * 64:(m + 1) * 64, 0, q0 + j_lo * 128:q0 + 512], start=True, stop=True),
                      r=[B_qk[hb][(kt // 4) * 2 + 1], B_qk[hb][qb * 2]], w=[BP])
                ei = ecnt % 4
                ecnt += 1
                E, BE = Et[ei], B_Et[ei]
                if sub == 512:
                    col = h * NOFF + (kt - 4 * qb) + OFF0
                    kb.op("act", lambda e: e.activation(out=E[:, j_lo * 128:512], in_=P[:, j_lo * 128:512], func=AF.Exp,
                                                        scale=0.125, bias=atab[:, col:col + 1]), r=[BP, B_c], w=[BE])
                else:
                    for j in range(j_lo, 4):
                        col = h * NOFF + (kt - 4 * qb - j) + OFF0
                        kb.op("act", lambda e: e.activation(out=E[:, j * 128:(j + 1) * 128], in_=P[:, j * 128:(j + 1) * 128],
                                                            func=AF.Exp, scale=0.125, bias=atab[:, col:col + 1]),
                              r=[BP, B_c], w=[BE])
                if jj >= 0:
                    kb.op("pool", lambda e: e.tensor_tensor(out=E[:, jj * 128:(jj + 1) * 128], in0=E[:, jj * 128:(jj + 1) * 128],
                                                            in1=cmask[:], op=ALU.mult), r=[B_c], w=[BE])
                ustate[u] = (E, BE, j_lo)

            def emit_av(u):
                qb, kt, m, first, last = u
                q0 = qb * 512
                E, BE, j_lo = ustate.pop(u)
                if first:
                    for j in range(4):
                        kb.op("pe", lambda e: e.matmul(acc4[:, j, :], lhsT=zero_bf[0:1, 0:128], rhs=zero_bf[0:1, 0:512],
                                                       start=True, stop=True, skip_group_check=True), r=[B_zero], w=[B_acc])
                for j in range(j_lo, 4):
                    kb.op("pe", lambda e: e.matmul(acc4[:, j, m * 129:(m + 1) * 129], lhsT=E[:, j * 128:(j + 1) * 128],
                                                   rhs=Vda[:, kt, h, 0:129], start=False, stop=last, skip_group_check=True),
                          r=[BE, B_V[kt]], w=[B_acc])
                if last:
                    while pending:
                        kb.op(*pending.pop(0))
                    evac(qb, q0)
                    conv_next(1)

            def evac(qb, q0):
                kb.op("dve", lambda e: e.reciprocal(out=rr[:, 0:4], in_=acc4[:, :, 128:129]), r=[B_acc], w=[B_o])
                kb.op("dve", lambda e: e.reciprocal(out=rr[:, 4:8], in_=acc4[:, :, 257:258]), r=[B_acc], w=[B_o])
                kb.op("dve", lambda e: e.tensor_tensor(out=o_sb[:], in0=acc4[:, :, 0:128], in1=rr[:, 0:4].unsqueeze(2).to_broadcast([128, 4, 128]),
                                                       op=ALU.mult), r=[B_acc], w=[B_o])
                kb.op("dve", lambda e: e.tensor_tensor(out=t_sb[:], in0=acc4[:, :, 129:257], in1=rr[:, 4:8].unsqueeze(2).to_broadcast([128, 4, 128]),
                                                       op=ALU.mult), r=[B_acc], w=[B_o])
                rec_ = []
                kb.op = lambda e, fn, r=(), w=(), same=None: rec_.append((e, fn, list(r), list(w), same))
                try:
                    evac_tail(qb, q0)
                finally:
                    del kb.op
                pending.extend(rec_)

            def evac_tail(qb, q0):
                kb.op("dve", lambda e: e.scalar_tensor_tensor(out=o_sb[:], in0=t_sb[:], scalar=neglam[:, 0:1], in1=o_sb[:],
                                                              op0=ALU.mult, op1=ALU.add), r=[B_o, B_l], w=[B_o])
                kb.op("dve", lambda e: e.tensor_tensor(out=t_sb[:], in0=o_sb[:], in1=o_sb[:], op=ALU.mult), r=[B_o], w=[B_o])
                kb.op("dve", lambda e: e.tensor_reduce(out=rr[:, 8:12], in_=t_sb[:], axis=AX.X, op=ALU.add), r=[B_o], w=[B_o])
                kb.op("act", lambda e: e.activation(out=rra[:, 0:4], in_=rr[:, 8:12], func=AF.Ln, scale=1.0 / 128, bias=EPS), r=[B_o], w=[B_o])
                kb.op("act", lambda e: e.activation(out=rra[:, 4:8], in_=rra[:, 0:4], func=AF.Exp, scale=-0.5), r=[B_o], w=[B_o])
                kb.op("dve", lambda e: e.tensor_tensor(out=t_sb[:], in0=o_sb[:], in1=rra[:, 4:8].unsqueeze(2).to_broadcast([128, 4, 128]),
                                                       op=ALU.mult), r=[B_o], w=[B_o])
                kb.op("dve", lambda e: e.tensor_tensor(out=y_sb[:], in0=t_sb[:], in1=gdo[:].unsqueeze(1).to_broadcast([128, 4, 128]),
                                                       op=ALU.mult), r=[B_o, B_c], w=[B_o])
                for j in range(4):
                    kb.op("pe", lambda e, j=j: e.transpose(out=ptr[:, j, :], in_=y_sb[:, j, :], identity=ident_bf[:]), r=[B_o, B_const], w=[B_ptr])
                kb.op("act", lambda e, h=h: e.copy(out=y_daT[:, h, q0:q0 + 512].rearrange("p (j t) -> p j t", j=4), in_=ptr[:, 0:4, :]),
                      r=[B_ptr], w=B_ydaT[h * NT + qb * 4:h * NT + qb * 4 + 4])

            emit_qk(units[0])
            if len(units) > 1:
                emit_qk(units[1])
            for ui in range(len(units)):
                if ui + 2 < len(units):
                    emit_qk(units[ui + 2])
                emit_av(units[ui])
                if pending and not units[ui][4]:
                    kb.op(*pending.pop(0))
            while pending:
                kb.op(*pending.pop(0))
        if "y_daT" in dbg_d:
            kb.dma("sp", "dbg", lambda q: q.dma_start(out=dbg_d["y_daT"], in_=y_daT[:]), r=B_ydaT)
        kb.barrier()


def _ml_phase(nc, kb, S, hT, B_hT, y_mlT, B_ymlT, w_in, cd, cw_col, cb_col, g_mlo, b_if, ident_bf, ident_f, ones_f,
              B_const, sb, ps, dbg_d):
    NT = S // 128
    NB5 = S // 512
    NHC = NT * 4
    QS = 128.0 ** -0.5
    with ExitStack() as s:
        negm = sb(s, "negm", [128, 128], F32)
        cw = sb(s, "cw", [128, 32], F32)
        cb = sb(s, "cb", [128, 8], F32)
        gml = sb(s, "gml", [128, 512], F32)
        bif = sb(s, "bif", [128, 8], F32)
        wif = sb(s, "wif", [128, 8, 8], BF16)
        B_c = kb.buf()
        for t, src in ((negm, cd["negm"]), (cw, cw_col), (cb, cb_col), (gml, g_mlo.partition_broadcast(128)),
                       (bif, b_if.partition_broadcast(128))):
            kb.dma("sp", "const", lambda q, t=t, src=src: q.dma_start(out=t[:], in_=src), w=[B_c])
        kb.dma("pool", "wif", lambda q: q.dma_start(out=wif[:], in_=w_in[:, OFF_ML_I:OFF_ML_I + 8].rearrange("(c p) f -> p c f", p=128)), w=[B_c])

        PA = [ps(s, f"mlPA{i}", [128, 512], F32) for i in range(2)]
        B_PA = kb.bufs(2)
        pacnt = [0]

        def nextPA():
            i = pacnt[0] % 2
            pacnt[0] += 1
            return PA[i], B_PA[i]
        Pew2 = [ps(s, f"mlPew{i}", [128, 512], F32) for i in range(2)]
        B_Pew2 = kb.bufs(2)
        Pew, B_Pew = Pew2[0], B_Pew2[0]
        Po2 = [ps(s, f"mlPo{i}", [128, 512], F32) for i in range(2)]
        B_Po2 = kb.bufs(2)
        Po, B_Po = Po2[0], B_Po2[0]
        Pc = ps(s, "mlPc", [128, 512], F32)
        Ptk = ps(s, "mlPtk", [128, 8, 128], BF16)
        Pty = Ptk
        Pg = Po
        B_Pc, B_Ptk = kb.bufs(2)
        B_Pty = B_Ptk
        B_Pg = B_Po

        for i in range(NT):
            for c in range(8):
                kb.op("pe", lambda e: e.matmul(Pg[:, i * 8:(i + 1) * 8], lhsT=hT[:, c, i * 128:(i + 1) * 128], rhs=wif[:, c, :],
                                               start=(c == 0), stop=(c == 7)), r=[B_hT[i], B_c], w=[B_Pg])
        gsb = sb(s, "gsb", [128, NT, 8], F32)
        XC = sb(s, "XC", [128, 2, NT, 4], F32)
        tmpg = sb(s, "tmpg", [128, NT, 4], F32)
        B_g = kb.buf()
        kb.op("dve", lambda e: e.tensor_tensor(out=gsb[:], in0=Pg[:, 0:NT * 8].rearrange("p (i g) -> p i g", g=8),
                                               in1=bif[:].unsqueeze(1).to_broadcast([128, NT, 8]), op=ALU.add), r=[B_Pg, B_c], w=[B_g])
        kb.op("act", lambda e: e.activation(out=tmpg[:], in_=gsb[:, :, 4:8], func=AF.Exp, scale=-1.0), r=[B_g], w=[B_g])
        kb.op("act", lambda e: e.activation(out=tmpg[:], in_=tmpg[:], func=AF.Ln, bias=1.0), r=[B_g], w=[B_g])
        kb.op("dve", lambda e: e.tensor_scalar(out=XC[:, 1], in0=tmpg[:], scalar1=-1.0, scalar2=None, op0=ALU.mult), r=[B_g], w=[B_g])
        kb.op("dve", lambda e: e.tensor_copy(out=XC[:, 0], in_=gsb[:, :, 0:4]), r=[B_g], w=[B_g])
        RN = ["iR", "lfR", "bR", "betaR", "pmR", "mxR", "alphaR", "mrowR", "winterR", "emrR", "winR", "zR"]
        Rt = {n: sb(s, n, [128, 128], F32) for n in RN}
        B_R = kb.buf()
        for a, n in ((0, "iR"), (1, "lfR")):
            kb.op("pe", lambda e: e.transpose(out=Pew[0:NHC, a * 128:(a + 1) * 128], in_=XC[:, a].rearrange("p i h -> p (i h)"),
                                              identity=ident_f[:]), r=[B_g, B_const], w=[B_Pew])
            kb.op("dve", lambda e: e.tensor_copy(out=Rt[n][0:NHC, :], in_=Pew[0:NHC, a * 128:(a + 1) * 128]), r=[B_Pew], w=[B_R])
        R = {n: Rt[n][0:NHC, :] for n in RN}
        kb.op("pool", lambda e: e.memset(Rt["zR"][:], 0.0), w=[B_R])
        kb.op("dve", lambda e: e.tensor_tensor_scan(out=R["bR"], data0=R["lfR"], data1=R["zR"], initial=0.0, op0=ALU.add, op1=ALU.add),
              r=[B_R], w=[B_R])
        kb.op("dve", lambda e: e.tensor_tensor(out=R["betaR"], in0=R["iR"], in1=R["bR"], op=ALU.subtract), r=[B_R], w=[B_R])
        kb.op("dve", lambda e: e.tensor_tensor_scan(out=R["pmR"], data0=R["betaR"], data1=R["betaR"], initial=-1e30, op0=ALU.max, op1=ALU.max),
              r=[B_R], w=[B_R])
        c2 = sb(s, "c2", [128, 8], F32)
        rows = sb(s, "rows", [1, 4, 128], F32)
        drow = sb(s, "drow", [1, 128], F32)
        kb.op("dve", lambda e: e.tensor_copy(out=c2[0:NHC, 0:1], in_=R["bR"][:, 127:128]), r=[B_R], w=[B_R])
        kb.op("dve", lambda e: e.tensor_tensor(out=c2[0:NHC, 1:2], in0=R["bR"][:, 127:128], in1=R["pmR"][:, 127:128], op=ALU.add), r=[B_R], w=[B_R])
        for a in range(2):
            kb.op("pe", lambda e: e.transpose(out=Pew[0:1, a * 128:a * 128 + NHC], in_=c2[0:NHC, a:a + 1], identity=ident_f[0:NHC, 0:NHC]),
                  r=[B_R, B_const], w=[B_Pew])
        kb.op("dve", lambda e: e.tensor_copy(out=rows[:, 0:2, 0:NHC], in_=Pew[0:1, 0:256].rearrange("p (a n) -> p a n", a=2)[:, :, 0:NHC]),
              r=[B_Pew], w=[B_R])
        for h in range(4):
            v = lambda a: rows[:, a, 0:NHC].rearrange("p (i h) -> p h i", h=4)[:, h, :]
            kb.op("dve", lambda e: e.tensor_tensor_scan(out=v(2), data0=v(0), data1=v(1), initial=0.0, op0=ALU.add, op1=ALU.max),
                  r=[B_R], w=[B_R])
        kb.op("pool", lambda e: e.memset(rows[:, 3, 0:4], 0.0), r=[B_R], w=[B_R])
        if NHC > 4:
            kb.op("dve", lambda e: e.tensor_copy(out=rows[:, 3, 4:NHC], in_=rows[:, 2, 0:NHC - 4]), r=[B_R], w=[B_R])
        kb.op("pe", lambda e: e.matmul(Pew[0:NHC, 0:1], lhsT=rows[:, 3, 0:NHC], rhs=ones_f[0:1, 0:1], start=True, stop=True),
              r=[B_R, B_const], w=[B_Pew])
        kb.op("dve", lambda e: e.tensor_copy(out=c2[0:NHC, 2:3], in_=Pew[0:NHC, 0:1]), r=[B_Pew], w=[B_R])
        ms = c2[0:NHC, 2:3]
        kb.op("dve", lambda e: e.tensor_scalar(out=R["mxR"], in0=R["pmR"], scalar1=ms, scalar2=None, op0=ALU.max), r=[B_R], w=[B_R])
        kb.op("dve", lambda e: e.tensor_scalar(out=R["alphaR"], in0=R["mxR"], scalar1=-1.0, scalar2=None, op0=ALU.mult), r=[B_R], w=[B_R])
        kb.op("dve", lambda e: e.tensor_tensor(out=R["mrowR"], in0=R["bR"], in1=R["mxR"], op=ALU.add), r=[B_R], w=[B_R])
        kb.op("act", lambda e: e.activation(out=R["winterR"], in_=R["alphaR"], func=AF.Exp, bias=ms), r=[B_R], w=[B_R])
        kb.op("act", lambda e: e.activation(out=R["emrR"], in_=R["mrowR"], func=AF.Exp, scale=-1.0), r=[B_R], w=[B_R])
        kb.op("dve", lambda e: e.tensor_tensor(out=c2[0:NHC, 6:7], in0=ms, in1=R["pmR"][:, 127:128], op=ALU.max), r=[B_R], w=[B_R])
        kb.op("dve", lambda e: e.tensor_tensor(out=c2[0:NHC, 3:4], in0=c2[0:NHC, 6:7], in1=c2[0:NHC, 0:1], op=ALU.add), r=[B_R], w=[B_R])
        kb.op("dve", lambda e: e.tensor_tensor(out=c2[0:NHC, 5:6], in0=c2[0:NHC, 0:1], in1=c2[0:NHC, 3:4], op=ALU.subtract), r=[B_R], w=[B_R])
        kb.op("act", lambda e: e.activation(out=c2[0:NHC, 4:5], in_=ms, func=AF.Exp, bias=c2[0:NHC, 5:6]), r=[B_R], w=[B_R])
        kb.op("act", lambda e: e.activation(out=R["winR"], in_=R["betaR"], func=AF.Exp, bias=c2[0:NHC, 5:6]), r=[B_R], w=[B_R])
        CN = ["betaR", "alphaR", "winterR", "emrR", "winR"]
        Ct = {n: sb(s, "C_" + n, [128, 128], F32) for n in CN}
        dbc = sb(s, "dbc", [128, 128], F32)
        B_C = kb.buf()
        for n in CN:
            kb.op("pe", lambda e: e.transpose(out=Pew[:, 0:NHC], in_=R[n], identity=ident_f[0:NHC, 0:NHC]), r=[B_R, B_const], w=[B_Pew])
            kb.op("dve", lambda e: e.tensor_copy(out=Ct[n][:, 0:NHC], in_=Pew[:, 0:NHC]), r=[B_Pew], w=[B_C])
        kb.op("pe", lambda e: e.transpose(out=Pew[0:1, 0:NHC], in_=c2[0:NHC, 4:5], identity=ident_f[0:NHC, 0:NHC]), r=[B_R, B_const], w=[B_Pew])
        kb.op("dve", lambda e: e.tensor_copy(out=drow[:, 0:NHC], in_=Pew[0:1, 0:NHC]), r=[B_Pew], w=[B_R])
        kb.op("pe", lambda e: e.matmul(Pew[:, 0:NHC], lhsT=ones_f[0:1, :], rhs=drow[:, 0:NHC], start=True, stop=True), r=[B_R, B_const], w=[B_Pew])
        kb.op("dve", lambda e: e.tensor_copy(out=dbc[:, 0:NHC], in_=Pew[:, 0:NHC]), r=[B_Pew], w=[B_C])

        wq4 = sb(s, "wq4", [128, 8, 512], BF16)
        B_w = kb.buf()
        qkT = sb(s, "mlqkT", [128, 2, S], BF16)
        B_qk = [kb.bufs(NB5), kb.bufs(NB5)]
        Vml = sb(s, "Vml", [128, NT, 130], BF16)
        og = sb(s, "og", [128, NT, 128], BF16)
        B_vo = kb.bufs(NT)
        B_vones = kb.buf()
        kb.op("pool", lambda e: e.memset(Vml[:, :, 128:130], 1.0), w=[B_vones])
        U2 = [sb(s, f"U2{i}", [128, 515], F32) for i in range(2)]
        B_U = kb.bufs(2)
        acc = [sb(s, f"cacc{i}", [128, 512], F32) for i in range(2)]
        B_a = kb.bufs(2)
        Cf = sb(s, "Cf", [128, 130], F32)
        Cbf = [sb(s, f"Cbf{i}", [128, 130], BF16) for i in range(2)]
        B_Cf = kb.buf()
        B_Cbf = kb.bufs(2)
        dA = [sb(s, f"dA{i}", [128, 128], F32) for i in range(2)]
        dW = [sb(s, f"dW{i}", [128, 128], F32) for i in range(2)]
        Wt = [sb(s, f"Wt{i}", [128, 128], F32) for i in range(2)]
        Pt = [sb(s, f"Pt{i}", [128, 128], BF16) for i in range(2)]
        qs = [sb(s, f"qs{i}", [128, 128], BF16) for i in range(2)]
        kw = [sb(s, f"kw{i}", [128, 128], BF16) for i in range(2)]
        t1_2 = [sb(s, f"t1{i}", [128, 128], F32) for i in range(2)]
        yb_2 = [sb(s, f"yb{i}", [128, 128], BF16) for i in range(2)]
        jk_2 = [sb(s, f"jk{i}", [128, 128], F32) for i in range(2)]
        sc_2 = [sb(s, f"sc{i}", [128, 8], F32) for i in range(2)]
        sca_2 = [sb(s, f"sca{i}", [128, 8], F32) for i in range(2)]
        B_ch2 = kb.bufs(2)
        B_y2 = kb.bufs(2)
        B_dA, B_dW, B_Wt, B_Pt, B_qs, B_kw = [kb.bufs(2) for _ in range(6)]
        offs = [OFF_ML_QK, OFF_ML_QK + 512, OFF_ML_V, OFF_ML_O]
        for h in range(NH):
            for a in range(4):
                kb.dma("pool", "wq4", lambda q: q.dma_start(out=wq4[:, :, a * 128:(a + 1) * 128],
                       in_=w_in[:, offs[a] + h * 128:offs[a] + (h + 1) * 128].rearrange("(c p) f -> p c f", p=128)), w=[B_w])
            for i in range(NT):
                P, BP = nextPA()
                for c in range(8):
                    kb.op("pe", lambda e: e.matmul(P[:, 0:256], lhsT=hT[:, c, i * 128:(i + 1) * 128], rhs=wq4[:, c, 256:512],
                                                   start=(c == 0), stop=(c == 7)), r=[B_hT[i], B_w], w=[BP])
                kb.op("act", lambda e: e.copy(out=Vml[:, i, 0:128], in_=P[:, 0:128]), r=[BP, B_vones], w=[B_vo[i]])
                kb.op("act", lambda e: e.activation(out=og[:, i, :], in_=P[:, 128:256], func=AF.Sigmoid), r=[BP], w=[B_vo[i]])
                kb.op("pool", lambda e: e.tensor_tensor(out=og[:, i, :], in0=og[:, i, :], in1=gml[:, h * 128:(h + 1) * 128], op=ALU.mult),
                      r=[B_c], w=[B_vo[i]])
            for which in range(2):
                cc = which * 4 + h
                for tb in range(NB5):
                    ub = tb % 2
                    P, BP = nextPA()
                    for c in range(8):
                        kb.op("pe", lambda e: e.matmul(P[:], lhsT=wq4[:, c, which * 128:(which + 1) * 128], rhs=hT[:, c, tb * 512:(tb + 1) * 512],
                                                       start=(c == 0), stop=(c == 7)), r=B_hT[tb * 4:tb * 4 + 4] + [B_w], w=[BP])
                    kb.op("act", lambda e: e.copy(out=U2[ub][:, 3:515], in_=P[:]), r=[BP], w=[B_U[ub]])
                    if tb == 0:
                        kb.op("pool", lambda e: e.memset(U2[ub][:, 0:3], 0.0), w=[B_U[ub]])
                    else:
                        kb.op("pool", lambda e: e.tensor_copy(out=U2[ub][:, 0:3], in_=U2[1 - ub][:, 512:515]), r=[B_U[1 - ub]], w=[B_U[ub]])
                    A_, BA = acc[ub], B_a[ub]
                    kb.op("dve", lambda e: e.tensor_scalar(out=A_[:], in0=U2[ub][:, 3:515], scalar1=cw[:, cc * 4 + 3:cc * 4 + 4],
                                                           scalar2=cb[:, cc:cc + 1], op0=ALU.mult, op1=ALU.add), r=[B_U[ub], B_c], w=[BA])
                    for j in range(3):
                        kb.op("dve", lambda e: e.scalar_tensor_tensor(out=A_[:], in0=U2[ub][:, j:j + 512], scalar=cw[:, cc * 4 + j:cc * 4 + j + 1],
                                                                      in1=A_[:], op0=ALU.mult, op1=ALU.add), r=[B_U[ub], B_c], w=[BA])
                    if which == 1:
                        kb.op("act", lambda e: e.activation(out=qkT[:, 1, tb * 512:(tb + 1) * 512], in_=A_[:], func=AF.Silu),
                              r=[BA], w=[B_qk[1][tb]])
                    else:
                        kb.op("act", lambda e: e.activation(out=A_[:], in_=A_[:], func=AF.Silu), r=[BA], w=[BA])
                        kb.op("pool", lambda e: e.tensor_scalar(out=qkT[:, 0, tb * 512:(tb + 1) * 512], in0=A_[:], scalar1=QS, scalar2=None,
                                                                op0=ALU.mult), r=[BA], w=[B_qk[0][tb]])
            kb.op("pool", lambda e: e.memset(Cf[:], 0.0), w=[B_Cf])
            kb.op("pool", lambda e: e.memset(Cbf[1][:], 0.0), w=[B_Cbf[1]])

            def pre(i):
                jj = i % 2
                hc = i * 4 + h
                tsl = slice(i * 128, (i + 1) * 128)
                tb = i // 4
                Ps_, BPs = PA[jj], B_PA[jj]
                Pw_, BPw = Pew2[jj], B_Pew2[jj]
                kb.op("pe", lambda e: e.matmul(Ps_[:, 0:128], lhsT=qkT[:, 1, tsl], rhs=qkT[:, 0, tsl], start=True, stop=True),
                      r=[B_qk[0][tb], B_qk[1][tb]], w=[BPs])
                kb.op("dve", lambda e: e.tensor_scalar(out=dA[jj][:], in0=ident_f[:], scalar1=Ct["alphaR"][:, hc:hc + 1], scalar2=None, op0=ALU.mult),
                      r=[B_C, B_const], w=[B_dA[jj]])
                kb.op("dve", lambda e: e.tensor_scalar(out=dW[jj][:], in0=ident_f[:], scalar1=Ct["winterR"][:, hc:hc + 1], scalar2=None, op0=ALU.mult),
                      r=[B_C, B_const], w=[B_dW[jj]])
                kb.op("pe", lambda e: e.matmul(Pw_[:, 0:128], lhsT=ones_f[:], rhs=dA[jj][:], start=True, stop=False), r=[B_dA[jj], B_const], w=[BPw])
                kb.op("pe", lambda e: e.matmul(Pw_[:, 0:128], lhsT=ident_f[:], rhs=negm[:], start=False, stop=True), r=[B_c, B_const], w=[BPw])
                kb.op("pe", lambda e: e.matmul(Pw_[:, 128:256], lhsT=ones_f[:], rhs=dW[jj][:], start=True, stop=True), r=[B_dW[jj], B_const], w=[BPw])
                kb.op("pe", lambda e: e.transpose(out=Ptk[:, jj, :], in_=qkT[:, 1, tsl], identity=ident_bf[:]), r=[B_qk[1][tb], B_const], w=[B_Ptk])

            def preB(i):
                jj = i % 2
                hc = i * 4 + h
                tsl = slice(i * 128, (i + 1) * 128)
                tb = i // 4
                Ps_, BPs = PA[jj], B_PA[jj]
                Pw_, BPw = Pew2[jj], B_Pew2[jj]
                kb.op("act", lambda e: e.activation(out=Wt[jj][:], in_=Pw_[:, 0:128], func=AF.Exp, bias=Ct["betaR"][:, hc:hc + 1]),
                      r=[BPw, B_C], w=[B_Wt[jj]])
                kb.op("dve", lambda e: e.tensor_tensor(out=qs[jj][:], in0=qkT[:, 0, tsl], in1=Pw_[:, 128:256], op=ALU.mult),
                      r=[BPw, B_qk[0][tb], B_Wt[jj]], w=[B_qs[jj]])
                kb.op("dve", lambda e: e.tensor_scalar(out=kw[jj][:], in0=Ptk[:, jj, :], scalar1=Ct["winR"][:, hc:hc + 1], scalar2=None, op0=ALU.mult),
                      r=[B_Ptk, B_C], w=[B_kw[jj]])
                kb.op("dve", lambda e: e.tensor_tensor(out=Pt[jj][:], in0=Ps_[:, 0:128], in1=Wt[jj][:], op=ALU.mult), r=[BPs, B_Wt[jj]], w=[B_Pt[jj]])

            def main(i):
                mainA(i)
                tail(i)

            def mainA(i):
                jj = i % 2
                hc = i * 4 + h
                tsl = slice(i * 128, (i + 1) * 128)
                Po, B_Po = Po2[jj], B_Po2[jj]
                kb.op("pe", lambda e: e.matmul(Pc[:, 0:129], lhsT=kw[jj][:], rhs=Vml[:, i, 0:129], start=True, stop=True), r=[B_kw[jj], B_vo[i]], w=[B_Pc])
                kb.op("pe", lambda e: e.matmul(Po[:, 0:129], lhsT=Pt[jj][:], rhs=Vml[:, i, 0:129], start=True, stop=False), r=[B_Pt[jj], B_vo[i]], w=[B_Po])
                kb.op("pe", lambda e: e.matmul(Po[:, 0:129], lhsT=qs[jj][:], rhs=Cbf[(i + 1) % 2][:, 0:129], start=False, stop=True),
                      r=[B_qs[jj], B_Cbf[(i + 1) % 2]], w=[B_Po])
                kb.op("dve", lambda e: e.scalar_tensor_tensor(out=Cf[:, 0:129], in0=Cf[:, 0:129], scalar=dbc[:, hc:hc + 1], in1=Pc[:, 0:129],
                                                              op0=ALU.mult, op1=ALU.add), r=[B_Pc, B_C], w=[B_Cf])
                kb.op("act", lambda e: e.copy(out=Cbf[jj][:, 0:129], in_=Cf[:, 0:129]), r=[B_Cf], w=[B_Cbf[jj]])

            def tail(i):
                jj = i % 2
                hc = i * 4 + h
                tsl = slice(i * 128, (i + 1) * 128)
                Po, B_Po = Po2[jj], B_Po2[jj]
                t1, yb, jk, sc, sca, B_ch, B_y = t1_2[jj], yb_2[jj], jk_2[jj], sc_2[jj], sca_2[jj], B_ch2[jj], B_y2[jj]
                kb.op("act", lambda e: e.activation(out=sca[:, 0:1], in_=Po[:, 128:129], func=AF.Abs), r=[B_Po], w=[B_ch])
                kb.op("dve", lambda e: e.tensor_tensor(out=sc[:, 0:1], in0=sca[:, 0:1], in1=Ct["emrR"][:, hc:hc + 1], op=ALU.max), r=[B_ch, B_C], w=[B_ch])
                kb.op("dve", lambda e: e.tensor_scalar(out=sc[:, 1:2], in0=sc[:, 0:1], scalar1=sc[:, 0:1], scalar2=EPS, op0=ALU.mult, op1=ALU.mult),
                      r=[B_ch], w=[B_ch])
                kb.op("act", lambda e: e.activation(out=jk[:], in_=Po[:, 0:128], func=AF.Square, accum_out=sca[:, 2:3]), r=[B_Po], w=[B_ch])
                kb.op("act", lambda e: e.activation(out=sca[:, 3:4], in_=sca[:, 2:3], func=AF.Ln, scale=1.0 / 128, bias=sc[:, 1:2]), r=[B_ch], w=[B_ch])
                kb.op("act", lambda e: e.activation(out=sca[:, 4:5], in_=sca[:, 3:4], func=AF.Exp, scale=-0.5), r=[B_ch], w=[B_ch])
                kb.op("dve", lambda e: e.scalar_tensor_tensor(out=yb[:], in0=Po[:, 0:128], scalar=sca[:, 4:5], in1=og[:, i, :],
                                                              op0=ALU.mult, op1=ALU.mult), r=[B_Po, B_ch, B_vo[i]], w=[B_y])
                kb.op("pe", lambda e: e.transpose(out=Pty[:, 2, :], in_=yb[:], identity=ident_bf[:]), r=[B_y, B_const], w=[B_Pty])
                kb.op("act", lambda e: e.copy(out=y_mlT[:, h, tsl], in_=Pty[:, 2, :]), r=[B_Pty], w=[B_ymlT[h * NT + i]])

            LOOKAHEAD = ML_LOOKAHEAD
            if LOOKAHEAD == 0:
                for i in range(NT):
                    pre(i)
                    preB(i)
                    main(i)
            elif LOOKAHEAD == 1:
                pre(0)
                for i in range(NT):
                    preB(i)
                    if i + 1 < NT:
                        pre(i + 1)
                    main(i)
            elif LOOKAHEAD == 3:
                def rec_ops(fns):
                    rec = []
                    kb.op = lambda e, fn, r=(), w=(), same=None: rec.append((e, fn, list(r), list(w), same))
                    try:
                        for f_ in fns:
                            f_()
                    finally:
                        del kb.op
                    return rec
                pre(0)
                for i in range(NT + 1):
                    fa = []
                    if i < NT:
                        fa.append(lambda i=i: preB(i))
                        if i + 1 < NT:
                            fa.append(lambda i=i: pre(i + 1))
                        fa.append(lambda i=i: mainA(i))
                    ra = rec_ops(fa)
                    rb = rec_ops([lambda i=i: tail(i - 1)]) if i >= 1 else []
                    ia = ib = 0
                    while ia < len(ra) or ib < len(rb):
                        for _ in range(2):
                            if ia < len(ra):
                                kb.op(*ra[ia])
                                ia += 1
                        if ib < len(rb):
                            kb.op(*rb[ib])
                            ib += 1
            else:
                pre(0)
                preB(0)
                for i in range(NT):
                    if i + 1 < NT:
                        pre(i + 1)
                        preB(i + 1)
                    main(i)
        if "y_mlT" in dbg_d:
            kb.dma("sp", "dbg", lambda q: q.dma_start(out=dbg_d["y_mlT"], in_=y_mlT[:]), r=B_ymlT)
        kb.barrier()


def _merge_moe(nc, kb, S, hT, B_hT, y_daT, B_ydaT, y_mlT, B_ymlT, es_mix, x_d, out_d, w_bda, w_bml, w_gate, w_out,
               bg_col, g_ffn, wr_d, b_rt, w1, w3, w2, xs_d, ys_d, hn_d, cd, ident_bf, ident_f, ones_f, zero_bf,
               B_const, B_zero, sb, ps, dbg_d, stage, B_xs, wconv):
    w1b_d, w3b_d, w2b_d, B_wconv = wconv
    NT = S // 128
    NB5 = S // 512
    CAP = 2 * S + NE * BS
    NBLK = CAP // BS
    NSUB = BS // 128
    with ExitStack() as s:
        wg = sb(s, "wg", [128, 8, 2048], BF16)
        wda = sb(s, "wda", [128, 4, 1024], BF16)
        wml = sb(s, "wml", [128, 4, 1024], BF16)
        bg = sb(s, "bg", [128, 16], F32)
        B_w = kb.buf()
        for k4 in range(4):
            kb.dma("pool", "mw", lambda q: q.dma_start(out=wg[:, :, k4 * 512:(k4 + 1) * 512],
                   in_=w_gate[:, k4 * 512:(k4 + 1) * 512].rearrange("(c p) f -> p c f", p=128)), w=[B_w])
        kb.dma("pool", "mw", lambda q: q.dma_start(out=wda[:], in_=w_bda.rearrange("(c p) f -> p c f", p=128)), w=[B_w])
        kb.dma("pool", "mw", lambda q: q.dma_start(out=wml[:], in_=w_bml.rearrange("(c p) f -> p c f", p=128)), w=[B_w])
        kb.dma("sp", "const", lambda q: q.dma_start(out=bg[:], in_=bg_col), w=[B_w])
        Pg = [ps(s, f"mPg{i}", [128, 512], F32) for i in range(2)]
        Pab = [ps(s, f"mPab{i}", [128, 512], F32) for i in range(2)]
        B_Pg, B_Pab = kb.bufs(2), kb.bufs(2)
        gs = [sb(s, f"gs{i}", [128, 512], F32) for i in range(2)]
        tt = [sb(s, f"tt{i}", [128, 512], F32) for i in range(2)]
        B_gs, B_tt = kb.bufs(2), kb.bufs(2)
        mixb = sb(s, "mixb", [128, 8, 512], BF16)
        B_mix = kb.bufs(8)
        for tb in range(NB5):
            tsl = slice(tb * 512, (tb + 1) * 512)
            hb = B_hT[tb * 4:tb * 4 + 4]
            for fc in range(8):
                for g in range(2):
                    kb_w = wda if g == 0 else wml
                    yT = y_daT if g == 0 else y_mlT
                    By = (B_ydaT if g == 0 else B_ymlT)
                    byl = [By[hh * NT + tb * 4 + t4] for hh in range(4) for t4 in range(4)]
                    for c in range(8):
                        kb.op("pe", lambda e: e.matmul(Pg[g][:], lhsT=wg[:, c, g * 1024 + fc * 128:g * 1024 + (fc + 1) * 128], rhs=hT[:, c, tsl],
                                                       start=(c == 0), stop=(c == 7)), r=hb + [B_w], w=[B_Pg[g]])
                    kb.op("act", lambda e: e.activation(out=gs[g][:], in_=Pg[g][:], func=AF.Sigmoid, bias=bg[:, g * 8 + fc:g * 8 + fc + 1]),
                          r=[B_Pg[g], B_w], w=[B_gs[g]])
                    for c in range(4):
                        kb.op("pe", lambda e: e.matmul(Pab[g][:], lhsT=kb_w[:, c, fc * 128:(fc + 1) * 128], rhs=yT[:, c, tsl],
                                                       start=(c == 0), stop=(c == 3)), r=byl + [B_w], w=[B_Pab[g]])
                    kb.op("dve", lambda e: e.tensor_tensor(out=tt[g][:], in0=gs[g][:], in1=Pab[g][:], op=ALU.mult),
                          r=[B_gs[g], B_Pab[g]], w=[B_tt[g]])
                kb.op("pool", lambda e: e.tensor_tensor(out=mixb[:, fc, :], in0=tt[0][:], in1=tt[1][:], op=ALU.add),
                      r=[B_tt[0], B_tt[1]], w=[B_mix[fc]])
            for fc in range(8):
                kb.op("pool", lambda e: e.tensor_copy(out=hT[:, fc, tsl], in_=mixb[:, fc, :]), r=[B_mix[fc]], w=hb)
        kb.barrier()
    es_mix.close()

    es2 = ExitStack()
    with es2:
        s = es2
        A1a = sb(s, "A1a", [128, NT, 32], F32)
        A2a = sb(s, "A2a", [128, NT, 32], F32)
        rka = sb(s, "rka", [128, NT, 32], F32)
        gta = sb(s, "gta", [128, NT, 2], F32)
        base = sb(s, "base", [128, 32], F32)
        d1i = sb(s, "d1i", [128, NT], I32)
        d2i = sb(s, "d2i", [128, NT], I32)
        bexp = sb(s, "bexp", [128, NBLK], I32)
        idxw = sb(s, "idxw", [128, NBLK], I32)
        B_rt = kb.buf()
        B_out = kb.bufs(NT)
        B_hn = kb.bufs(NT)
        with ExitStack() as sB:
            wo = sb(sB, "wo", [128, 8, 1024], BF16)
            gff = sb(sB, "gff", [128, 1024], F32)
            wr = sb(sB, "wr", [128, 8, 36], F32)
            brt = sb(sB, "brt", [128, 36], F32)
            ustr = sb(sB, "ustr", [128, 128], F32)
            B_w = kb.buf()
            for k2 in range(2):
                kb.dma("pool", "mw", lambda q: q.dma_start(out=wo[:, :, k2 * 512:(k2 + 1) * 512],
                       in_=w_out[:, k2 * 512:(k2 + 1) * 512].rearrange("(c p) f -> p c f", p=128)), w=[B_w])
            kb.dma("sp", "const", lambda q: q.dma_start(out=gff[:], in_=g_ffn.partition_broadcast(128)), w=[B_w])
            kb.dma("sp", "const", lambda q: q.dma_start(out=wr[:], in_=wr_d.rearrange("(c p) f -> p c f", p=128)), w=[B_w])
            kb.dma("sp", "const", lambda q: q.dma_start(out=brt[:], in_=b_rt.partition_broadcast(128)), w=[B_w])
            kb.dma("sp", "const", lambda q: q.dma_start(out=ustr[:], in_=cd["ustrict"]), w=[B_w])
            kb.op("pool", lambda e: e.memset(base[:], 0.0), w=[B_rt])
            Px = ps(sB, "bPx", [128, 2, 512], F32)
            PT = ps(sB, "bPT", [128, 8, 128], F32)
            Pr = ps(sB, "bPr", [128, 512], F32)
            B_Px, B_PT, B_Pr = kb.bufs(3)
            xt = [sb(sB, f"bxt{i}", [128, 1024], F32) for i in range(2)]
            x2 = [sb(sB, f"bx2{i}", [128, 1024], F32) for i in range(2)]
            hnf = sb(sB, "hnf", [128, 1024], F32)
            hnb = [sb(sB, f"hnb{i}", [128, 1024], BF16) for i in range(2)]
            hnT = sb(sB, "hnT", [128, 8, 128], F32)
            junk = sb(sB, "bjunk", [128, 512], F32)
            st = sb(sB, "bst", [128, 8], F32)
            sd_ = sb(sB, "bsd", [128, 16], F32)
            lg = sb(sB, "lg", [128, 36], F32)
            oh = sb(sB, "oh", [128, 4], F32)
            t48 = sb(sB, "t48", [128, 4, 8], F32)
            es8 = sb(sB, "es8", [128, 8], F32)
            e28 = sb(sB, "e28", [128, 8], F32)
            mk1 = sb(sB, "mk1", [128, 8], F32)
            mk2 = sb(sB, "mk2", [128, 8], F32)
            At = sb(sB, "At", [128, 32], F32)
            B_xt, B_x2, B_hnb = kb.bufs(2), kb.bufs(2), kb.bufs(2)
            B_hnf, B_hnT, B_r = kb.buf(), kb.buf(), kb.buf()
            B_rx = kb.buf()
            B_lg = kb.bufs(4)
            lg2 = [sb(sB, f"lg2{i}", [128, 36], F32) for i in range(4)]
            B_r2 = kb.bufs(2)
            sd2 = [sd_, sb(sB, "bsd_b", [128, 16], F32)]
            st2 = [st, sb(sB, "bst_b", [128, 8], F32)]
            oh2 = [oh, sb(sB, "oh_b", [128, 4], F32)]
            t482 = [t48, sb(sB, "t48_b", [128, 4, 8], F32)]
            es82 = [es8, sb(sB, "es8_b", [128, 8], F32)]
            e282 = [e28, sb(sB, "e28_b", [128, 8], F32)]
            mk12 = [mk1, sb(sB, "mk1_b", [128, 8], F32)]
            mk22 = [mk2, sb(sB, "mk2_b", [128, 8], F32)]
            stX = sb(sB, "bstX", [128, 8], F32)
            sdX = sb(sB, "bsdX", [128, 16], F32)
            junkR2 = [sb(sB, f"bjunkR{i}", [128, 8], F32) for i in range(2)]
            Pr2 = ps(sB, "bPr2", [128, 512], F32)
            B_Pr2 = kb.buf()
            hnf2 = [hnf, sb(sB, "hnf_b", [128, 1024], F32)]
            hnT2 = [hnT, sb(sB, "hnT_b", [128, 8, 128], F32)]
            stX2 = [stX, sb(sB, "bstX_b", [128, 8], F32)]
            junk2 = [junk, sb(sB, "bjunk_b", [128, 512], F32)]
            PT2 = [PT, ps(sB, "bPT_b", [128, 8, 128], F32)]
            Pr_2 = [Pr, Pr2]
            B_hnf2, B_hnT2, B_rx2, B_PT2 = [B_hnf, kb.buf()], [B_hnT, kb.buf()], [B_rx, kb.buf()], [B_PT, kb.buf()]
            B_Px2 = [B_Px, kb.buf()]
            B_Pr_2 = [B_Pr, B_Pr2]
            ingroup = [None]

            def gb():
                ingroup[0] = []

            def ge():
                g_, ingroup[0] = ingroup[0], None
                return g_

            def stageX(i):
                j = i % 2
                lgi = lg2[i % 4]
                hnf, hnT, stX, junk, PT, Pr = hnf2[j], hnT2[j], stX2[j], junk2[j], PT2[j], Pr_2[j]
                B_hnf, B_hnT, B_rx, B_PT, B_Pr, B_Px = B_hnf2[j], B_hnT2[j], B_rx2[j], B_PT2[j], B_Pr_2[j], B_Px2[j]
                rows = slice(i * 128, (i + 1) * 128)
                half = fc = hs = c = None
                kb.dma("sp", f"bxt{j}", lambda q: q.dma_start(out=xt[j][:], in_=x_d[rows, :]), w=[B_xt[j]])
                for half in range(2):
                    hs = slice(half * 512, (half + 1) * 512)
                    gb()
                    for fc in range(8):
                        kb.op("pe", lambda e, half=half, fc=fc, hs=hs, c=c: e.matmul(Px[:, j, :], lhsT=hT[:, fc, rows], rhs=wo[:, fc, half * 512:(half + 1) * 512],
                                                       start=(fc == 0), stop=(fc == 7)), r=[B_hT[i], B_w], w=[B_Px])
                    grp_done(ge())
                    kb.op("dve", lambda e, half=half, fc=fc, hs=hs, c=c: e.tensor_tensor(out=x2[j][:, hs], in0=xt[j][:, hs], in1=Px[:, j, :], op=ALU.add),
                          r=[B_xt[j], B_Px], w=[B_x2[j]])
                kb.dma("sp", "ost", lambda q: q.dma_start(out=out_d[rows, :], in_=x2[j][:]), r=[B_x2[j]], w=[B_out[i]])
                for half in range(2):
                    hs = slice(half * 512, (half + 1) * 512)
                    kb.op("act", lambda e, half=half, fc=fc, hs=hs, c=c: e.activation(out=junk[:], in_=x2[j][:, hs], func=AF.Square, accum_out=stX[:, half:half + 1]),
                          r=[B_x2[j]], w=[B_rx])
                kb.op("act", lambda e, half=half, fc=fc, hs=hs, c=c: e.activation(out=stX[:, 2:3], in_=stX[:, 0:1], func=AF.Identity, bias=stX[:, 1:2]), r=[B_rx], w=[B_rx])
                kb.op("act", lambda e, half=half, fc=fc, hs=hs, c=c: e.activation(out=stX[:, 3:4], in_=stX[:, 2:3], func=AF.Ln, scale=1.0 / D, bias=EPS), r=[B_rx], w=[B_rx])
                kb.op("act", lambda e, half=half, fc=fc, hs=hs, c=c: e.activation(out=stX[:, 6:7], in_=stX[:, 3:4], func=AF.Exp, scale=-0.5), r=[B_rx], w=[B_rx])
                for half in range(2):
                    hs = slice(half * 512, (half + 1) * 512)
                    kb.op("dve", lambda e, half=half, fc=fc, hs=hs, c=c: e.scalar_tensor_tensor(out=hnf[:, hs], in0=x2[j][:, hs], scalar=stX[:, 6:7], in1=gff[:, hs],
                                                                  op0=ALU.mult, op1=ALU.mult), r=[B_x2[j], B_rx, B_w], w=[B_hnf])
                    kb.op("pool", lambda e, half=half, fc=fc, hs=hs, c=c: e.tensor_copy(out=hnb[j][:, hs], in_=hnf[:, hs]), r=[B_hnf], w=[B_hnb[j]])
                kb.dma("sp", "hnst", lambda q: q.dma_start(out=hn_d[rows, :], in_=hnb[j][:]), r=[B_hnb[j]], w=[B_hn[i]])
                for c in range(8):
                    kb.op("pe", lambda e, half=half, fc=fc, hs=hs, c=c: e.transpose(out=PT[:, c, :], in_=hnf[:, c * 128:(c + 1) * 128], identity=ident_f[:]),
                          r=[B_hnf, B_const], w=[B_PT])
                for half in range(2):
                    kb.op("act", lambda e, half=half, fc=fc, hs=hs, c=c: e.copy(out=hnT[:, half * 4:half * 4 + 4, :], in_=PT[:, half * 4:half * 4 + 4, :]), r=[B_PT], w=[B_hnT])
                gb()
                for c in range(8):
                    kb.op("pe", lambda e, half=half, fc=fc, hs=hs, c=c: e.matmul(Pr[:, 0:36], lhsT=hnT[:, c, :], rhs=wr[:, c, :], start=(c == 0), stop=(c == 7)),
                          r=[B_hnT, B_w], w=[B_Pr])
                grp_done(ge())
                kb.op("dve", lambda e, half=half, fc=fc, hs=hs, c=c: e.tensor_tensor(out=lgi[:], in0=Pr[:, 0:36], in1=brt[:], op=ALU.add), r=[B_Pr, B_w], w=[B_lg[i % 4]])

            def stageR(i):
                lgi = lg2[i % 4]
                q = i % 2
                sd_, st, oh, t48, es8, e28, mk1, mk2, junkR = sd2[q], st2[q], oh2[q], t482[q], es82[q], e282[q], mk12[q], mk22[q], junkR2[q]
                R_ = [B_r2[q]]
                RL = [B_r2[q], B_lg[i % 4]]
                kb.op("dve", lambda e: e.tensor_reduce(out=sd_[:, 1:2], in_=lgi[:, 0:4], axis=AX.X, op=ALU.max), r=RL, w=R_)
                kb.op("dve", lambda e: e.tensor_scalar(out=sd_[:, 2:3], in0=sd_[:, 1:2], scalar1=-1.0, scalar2=None, op0=ALU.mult), r=R_, w=R_)
                kb.op("act", lambda e: e.activation(out=junkR[:, 0:4], in_=lgi[:, 0:4], func=AF.Exp, bias=sd_[:, 2:3], accum_out=st[:, 4:5]), r=RL, w=R_)
                kb.op("dve", lambda e: e.reciprocal(out=sd_[:, 3:4], in_=st[:, 4:5]), r=R_, w=R_)
                kb.op("dve", lambda e: e.tensor_scalar(out=oh[:], in0=lgi[:, 0:4], scalar1=sd_[:, 1:2], scalar2=None, op0=ALU.is_equal), r=RL, w=R_)
                kb.op("dve", lambda e: e.tensor_tensor(out=t48[:], in0=lgi[:, 4:36].rearrange("p (g j) -> p g j", g=4),
                                                       in1=oh[:].unsqueeze(2).to_broadcast([128, 4, 8]), op=ALU.mult), r=RL, w=R_)
                kb.op("dve", lambda e: e.tensor_reduce(out=es8[:], in_=t48[:].rearrange("p g j -> p j g"), axis=AX.X, op=ALU.add), r=R_, w=R_)
                kb.op("dve", lambda e: e.tensor_reduce(out=sd_[:, 4:5], in_=es8[:], axis=AX.X, op=ALU.max), r=R_, w=R_)
                kb.op("dve", lambda e: e.tensor_scalar(out=mk1[:], in0=es8[:], scalar1=sd_[:, 4:5], scalar2=None, op0=ALU.is_equal), r=R_, w=R_)
                kb.op("dve", lambda e: e.scalar_tensor_tensor(out=e28[:], in0=mk1[:], scalar=-1e30, in1=es8[:], op0=ALU.mult, op1=ALU.add), r=R_, w=R_)
                kb.op("dve", lambda e: e.tensor_reduce(out=sd_[:, 5:6], in_=e28[:], axis=AX.X, op=ALU.max), r=R_, w=R_)
                kb.op("dve", lambda e: e.tensor_scalar(out=mk2[:], in0=e28[:], scalar1=sd_[:, 5:6], scalar2=None, op0=ALU.is_equal), r=R_, w=R_)
                kb.op("dve", lambda e: e.tensor_tensor(out=sd_[:, 6:7], in0=sd_[:, 5:6], in1=sd_[:, 4:5], op=ALU.subtract), r=R_, w=R_)
                kb.op("act", lambda e: e.activation(out=st[:, 5:6], in_=sd_[:, 6:7], func=AF.Exp), r=R_, w=R_)
                kb.op("dve", lambda e: e.tensor_scalar(out=sd_[:, 7:8], in0=st[:, 5:6], scalar1=1.0, scalar2=None, op0=ALU.add), r=R_, w=R_)
                kb.op("dve", lambda e: e.reciprocal(out=sd_[:, 8:9], in_=sd_[:, 7:8]), r=R_, w=R_)
                kb.op("dve", lambda e: e.tensor_tensor(out=gta[:, i, 0:1], in0=sd_[:, 3:4], in1=sd_[:, 8:9], op=ALU.mult), r=R_, w=R_ + [B_rt])
                kb.op("dve", lambda e: e.tensor_tensor(out=gta[:, i, 1:2], in0=gta[:, i, 0:1], in1=st[:, 5:6], op=ALU.mult), r=R_, w=R_ + [B_rt])
                kb.op("dve", lambda e: e.tensor_tensor(out=A1a[:, i, :].rearrange("p (g j) -> p g j", g=4),
                                                       in0=oh[:].unsqueeze(2).to_broadcast([128, 4, 8]),
                                                       in1=mk1[:].unsqueeze(1).to_broadcast([128, 4, 8]), op=ALU.mult), r=R_, w=R_ + [B_rt])
                kb.op("dve", lambda e: e.tensor_tensor(out=A2a[:, i, :].rearrange("p (g j) -> p g j", g=4),
                                                       in0=oh[:].unsqueeze(2).to_broadcast([128, 4, 8]),
                                                       in1=mk2[:].unsqueeze(1).to_broadcast([128, 4, 8]), op=ALU.mult), r=R_, w=R_ + [B_rt])
            cur_rec = [None]

            def grp_done(g_):
                cur_rec[0].append(("grp", g_))

            def record(fn_stage, i):
                rec = []
                cur_rec[0] = rec

                def rop(e, fn, r=(), w=(), same=None):
                    (ingroup[0] if ingroup[0] is not None else rec).append((e, fn, list(r), list(w), same))
                kb.op = rop
                kb.dma = lambda q, sname, fn, r=(), w=(): rec.append(("dma", q, sname, fn, list(r), list(w)))
                try:
                    fn_stage(i)
                finally:
                    del kb.op
                    del kb.dma
                return rec

            def replay(t):
                if t[0] == "grp":
                    for t2 in t[1]:
                        kb.op(*t2)
                elif t[0] == "dma":
                    kb.dma(*t[1:])
                else:
                    kb.op(*t)

            def emit_x_pair(i2a):
                xa = record(stageX, i2a) if i2a < NT else []
                xb_ = record(stageX, i2a + 1) if i2a + 1 < NT else []
                for k_ in range(max(len(xa), len(xb_))):
                    if k_ < len(xa):
                        replay(xa[k_])
                    if k_ < len(xb_):
                        replay(xb_[k_])

            emit_x_pair(0)
            for i in range(0, NT, 2):
                emit_x_pair(i + 2)
                ra = record(stageR, i)
                rb = record(stageR, i + 1) if i + 1 < NT else []
                for k_ in range(max(len(ra), len(rb))):
                    if k_ < len(ra):
                        replay(ra[k_])
                    if k_ < len(rb):
                        replay(rb[k_])
            Ata = sb(sB, "Ata", [128, NT, 32], F32)
            B_Ata = kb.buf()
            for i0_ in range(0, NT, 16):
                n_ = min(16, NT - i0_)
                kb.op("dve", lambda e: e.tensor_tensor(out=Ata[:, i0_:i0_ + n_, :], in0=A1a[:, i0_:i0_ + n_, :], in1=A2a[:, i0_:i0_ + n_, :], op=ALU.add),
                      r=[B_rt], w=[B_Ata])
            Apre = sb(sB, "Apre", [128, NT, 32], F32)
            kb.op("pool", lambda e: e.memset(Apre[:, 0, :], 0.0), w=[B_Ata])
            for i in range(1, NT):
                kb.op("dve", lambda e: e.tensor_tensor(out=Apre[:, i, :], in0=Apre[:, i - 1, :], in1=Ata[:, i - 1, :], op=ALU.add),
                      r=[B_Ata], w=[B_Ata])
            for i in range(NT):
                bank, col = i // 16, (i % 16) * 32
                kb.op("pe", lambda e: e.matmul(Px[:, bank, col:col + 32], lhsT=ustr[:], rhs=Ata[:, i, :], start=True, stop=False),
                      r=[B_Ata, B_w], w=[B_Px2[bank]])
                kb.op("pe", lambda e: e.matmul(Px[:, bank, col:col + 32], lhsT=ones_f[:], rhs=Apre[:, i, :], start=False, stop=True),
                      r=[B_Ata, B_const], w=[B_Px2[bank]])
            for i in range(NT):
                kb.op("pe", lambda e: e.matmul(Pr2[:, 0:32], lhsT=ones_f[:], rhs=Ata[:, i, :], start=(i == 0), stop=(i == NT - 1)),
                      r=[B_Ata, B_const], w=[B_Pr2])
            for i0_ in range(0, NT, 16):
                n_ = min(16, NT - i0_)
                kb.op("dve", lambda e: e.tensor_copy(out=rka[:, i0_:i0_ + n_, :], in_=Px[:, i0_ // 16, 0:n_ * 32].rearrange("p (i e) -> p i e", e=32)),
                      r=[B_Px2[i0_ // 16]], w=[B_rt])
            kb.op("dve", lambda e: e.tensor_copy(out=base[:], in_=Pr2[:, 0:32]), r=[B_Pr2], w=[B_rt])
            kb.barrier()
        with ExitStack() as sC:
            thr = sb(sC, "thr", [128, NBLK, 32], F32)
            cmp_ = sb(sC, "cmp", [128, 16, 32], F32)
            nbk = sb(sC, "nbk", [128, 32], F32)
            tmp32 = sb(sC, "tmp32", [128, 32], F32)
            pend = sb(sC, "pend", [128, 32], F32)
            pst = sb(sC, "pst", [128, 32], F32)
            zer = sb(sC, "zer", [128, 32], F32)
            bef = sb(sC, "bef", [128, NBLK], F32)
            dst = sb(sC, "dst", [128, NT, 32], F32)
            prod = sb(sC, "prod", [128, 16, 32], F32)
            d1f = sb(sC, "d1f", [128, NT], F32)
            d2f = sb(sC, "d2f", [128, NT], F32)
            Q = [B_rt]
            kb.dma("sp", "const", lambda q: q.dma_start(out=thr[:].rearrange("p b e -> p (b e)"), in_=cd["blkthr"].partition_broadcast(128)), w=Q)
            kb.op("pool", lambda e: e.memset(nbk[:], 0.0), w=Q)
            kb.op("pool", lambda e: e.memset(zer[:], 0.0), w=Q)
            for b0 in range(0, NBLK, 16):
                nb_ = min(16, NBLK - b0)
                kb.op("dve", lambda e: e.tensor_tensor(out=cmp_[:, 0:nb_, :], in0=base[:].unsqueeze(1).to_broadcast([128, nb_, 32]),
                                                       in1=thr[:, b0:b0 + nb_, :], op=ALU.is_gt), r=Q, w=Q)
                kb.op("dve", lambda e: e.tensor_reduce(out=tmp32[:], in_=cmp_[:, 0:nb_, :].rearrange("p b e -> p e b"), axis=AX.X, op=ALU.add), r=Q, w=Q)
                kb.op("dve", lambda e: e.tensor_tensor(out=nbk[:], in0=nbk[:], in1=tmp32[:], op=ALU.add), r=Q, w=Q)
            kb.op("dve", lambda e: e.tensor_scalar(out=nbk[:], in0=nbk[:], scalar1=float(BS), scalar2=None, op0=ALU.mult), r=Q, w=Q)
            kb.op("dve", lambda e: e.tensor_tensor_scan(out=pend[:], data0=nbk[:], data1=zer[:], initial=0.0, op0=ALU.add, op1=ALU.add), r=Q, w=Q)
            kb.op("dve", lambda e: e.tensor_tensor(out=pst[:], in0=pend[:], in1=nbk[:], op=ALU.subtract), r=Q, w=Q)
            for b0 in range(0, NBLK, 16):
                nb_ = min(16, NBLK - b0)
                kb.op("dve", lambda e: e.tensor_tensor(out=cmp_[:, 0:nb_, :], in0=pend[:].unsqueeze(1).to_broadcast([128, nb_, 32]),
                                                       in1=thr[:, b0:b0 + nb_, :], op=ALU.is_le), r=Q, w=Q)
                kb.op("dve", lambda e: e.tensor_reduce(out=bef[:, b0:b0 + nb_], in_=cmp_[:, 0:nb_, :], axis=AX.X, op=ALU.add), r=Q, w=Q)
            kb.op("dve", lambda e: e.tensor_scalar(out=bef[:], in0=bef[:], scalar1=float(NE - 1), scalar2=None, op0=ALU.min), r=Q, w=Q)
            kb.op("dve", lambda e: e.tensor_copy(out=bexp[:], in_=bef[:]), r=Q, w=Q)
            iot = sb(sC, "iot", [128, 8], F32)
            idf = sb(sC, "idf", [128, NBLK], F32)
            kb.dma("sp", "const", lambda q: q.dma_start(out=iot[:], in_=cd["iota_pc"]), w=Q)
            kb.op("dve", lambda e: e.scalar_tensor_tensor(out=idf[:], in0=bef[:], scalar=128.0, in1=iot[:, 0:1].to_broadcast([128, NBLK]),
                                                          op0=ALU.mult, op1=ALU.add), r=Q, w=Q)
            kb.op("dve", lambda e: e.tensor_copy(out=idxw[:], in_=idf[:]), r=Q, w=Q)
            for i0_ in range(0, NT, 16):
                n_ = min(16, NT - i0_)
                kb.op("dve", lambda e: e.tensor_tensor(out=dst[:, i0_:i0_ + n_, :], in0=rka[:, i0_:i0_ + n_, :],
                                                       in1=pst[:].unsqueeze(1).to_broadcast([128, n_, 32]), op=ALU.add), r=Q, w=Q)
                for Aa, df in ((A1a, d1f), (A2a, d2f)):
                    kb.op("dve", lambda e: e.tensor_tensor(out=prod[:, 0:n_, :], in0=dst[:, i0_:i0_ + n_, :], in1=Aa[:, i0_:i0_ + n_, :], op=ALU.mult), r=Q, w=Q)
                    kb.op("dve", lambda e: e.tensor_reduce(out=df[:, i0_:i0_ + n_], in_=prod[:, 0:n_, :], axis=AX.X, op=ALU.add), r=Q, w=Q)
            kb.op("dve", lambda e: e.tensor_copy(out=d1i[:], in_=d1f[:]), r=Q, w=Q)
            kb.op("dve", lambda e: e.tensor_copy(out=d2i[:], in_=d2f[:]), r=Q, w=Q)
            kb.barrier()
        if "d1i" in dbg_d:
            kb.dma("sp", "dbg", lambda q: q.dma_start(out=dbg_d["d1i"], in_=d1i[:]), r=[B_rt])
            kb.dma("sp", "dbg", lambda q: q.dma_start(out=dbg_d["d2i"], in_=d2i[:]), r=[B_rt])
            kb.dma("sp", "dbg", lambda q: q.dma_start(out=dbg_d["bexp"], in_=bexp[:]), r=[B_rt])
            kb.dma("sp", "dbg", lambda q: q.dma_start(out=dbg_d["gta"], in_=gta[:]), r=[B_rt])
        if stage < 5:
            return
        with ExitStack() as sD:
            hb_ = [sb(sD, f"dhb{i}", [128, 1024], BF16) for i in range(6)]
            B_hb = kb.bufs(6)
            for i in range(NT):
                j = i % 6
                kb.dma("sp", f"dhb{j}", lambda q: q.dma_start(out=hb_[j][:], in_=hn_d[i * 128:(i + 1) * 128, :]), r=[B_hn[i]], w=[B_hb[j]])
                for di in (d1i, d2i):
                    kb.dma("pool", "disp", lambda q: q.indirect_dma_start(out=xs_d[:, :], out_offset=bass.IndirectOffsetOnAxis(ap=di[:, i:i + 1], axis=0),
                                                                           in_=hb_[j][:], in_offset=None), r=[B_hb[j], B_rt], w=[B_xs])
            kb.barrier()
        B_ys = kb.buf()
        with ExitStack() as sE:
            NWB = 3
            W1 = [sb(sE, f"eW1{i}", [128, 8, 512], BF16) for i in range(NWB)]
            W3 = [sb(sE, f"eW3{i}", [128, 8, 512], BF16) for i in range(NWB)]
            W2 = [sb(sE, f"eW2{i}", [128, 4, 1024], BF16) for i in range(NWB)]
            B_W = kb.bufs(NWB)
            xb = [sb(sE, f"exb{i}", [128, 1024], BF16) for i in range(2)]
            B_xb = kb.bufs(2)
            xT = [sb(sE, f"exT{i}", [128, 8, 128], BF16) for i in range(2)]
            sl = [sb(sE, f"esl{i}", [128, 512], F32) for i in range(2)]
            hid = [sb(sE, f"ehid{i}", [128, 512], BF16) for i in range(2)]
            hidT = [sb(sE, f"ehidT{i}", [128, 4, 128], BF16) for i in range(2)]
            ysb = [sb(sE, f"eys{i}", [128, 1024], F32) for i in range(2)]
            B_ysb = kb.bufs(2)
            B_xT, B_sl, B_hid, B_hidT = kb.bufs(2), kb.bufs(2), kb.bufs(2), kb.bufs(2)
            PTb = [ps(sE, f"ePT{i}", [128, 8, 128], BF16) for i in range(2)]
            Ph1 = [ps(sE, f"ePh1{i}", [128, 512], F32) for i in range(2)]
            Ph3 = [ps(sE, f"ePh3{i}", [128, 512], F32) for i in range(2)]
            Py = ps(sE, "ePy", [128, 2, 512], F32)
            B_PTb, B_Ph1, B_Ph3 = kb.bufs(2), kb.bufs(2), kb.bufs(2)
            B_Py = kb.buf()
            w1r, w3r, w2r = w1b_d, w3b_d, w2b_d
            B_xT2 = [kb.bufs(2), kb.bufs(2)]
            B_ysb2 = [kb.bufs(2), kb.bufs(2)]
            NB128 = NBLK * NSUB

            def load_w(sbi):
                k = sbi % NWB
                for Wt_, wr_ in ((W1, w1r), (W3, w3r), (W2, w2r)):
                    kb.dma("pool", f"ew{k}", lambda q: q.indirect_dma_start(out=Wt_[k][:].rearrange("p c f -> p (c f)"), out_offset=None, in_=wr_[:, :],
                           in_offset=bass.IndirectOffsetOnAxis(ap=idxw[:, sbi:sbi + 1], axis=0)), r=[B_rt, B_wconv], w=[B_W[k]])

            def load_x(b):
                j = b % 2
                kb.dma("sp", f"exb{j}", lambda q: q.dma_start(out=xb[j][:], in_=xs_d[b * 128:(b + 1) * 128, :]), r=[B_xs], w=[B_xb[j]])

            def stageA(b):
                j = b % 2
                k = (b // NSUB) % NWB
                for c in range(8):
                    kb.op("pe", lambda e: e.transpose(out=PTb[j][:, c, :], in_=xb[j][:].rearrange("s (p c) -> s c p", c=8)[:, c, :], identity=ident_bf[:]),
                          r=[B_xb[j], B_const], w=[B_PTb[j]])
                kb.op("dve", lambda e: e.tensor_copy(out=xT[j][:, 0:4, :], in_=PTb[j][:, 0:4, :]), r=[B_PTb[j]], w=[B_xT2[j][0]])
                kb.op("dve", lambda e: e.tensor_copy(out=xT[j][:, 4:8, :], in_=PTb[j][:, 4:8, :]), r=[B_PTb[j]], w=[B_xT2[j][1]])
                for c in range(8):
                    kb.op("pe", lambda e: e.matmul(Ph1[j][:], lhsT=xT[j][:, c, :], rhs=W1[k][:, c, :], start=(c == 0), stop=(c == 7)),
                          r=[B_xT2[j][c // 4], B_W[k]], w=[B_Ph1[j]])
                for c in range(8):
                    kb.op("pe", lambda e: e.matmul(Ph3[j][:], lhsT=xT[j][:, c, :], rhs=W3[k][:, c, :], start=(c == 0), stop=(c == 7)),
                          r=[B_xT2[j][c // 4], B_W[k]], w=[B_Ph3[j]])
                kb.op("act", lambda e: e.activation(out=sl[j][:], in_=Ph1[j][:], func=AF.Silu), r=[B_Ph1[j]], w=[B_sl[j]])
                kb.op("dve", lambda e: e.tensor_tensor(out=hid[j][:], in0=sl[j][:], in1=Ph3[j][:], op=ALU.mult), r=[B_sl[j], B_Ph3[j]], w=[B_hid[j]])

            def stageB(b):
                j = b % 2
                k = (b // NSUB) % NWB
                for c in range(4):
                    kb.op("pe", lambda e: e.transpose(out=PTb[j][:, c, :], in_=hid[j][:].rearrange("s (p c) -> s c p", c=4)[:, c, :], identity=ident_bf[:]),
                          r=[B_hid[j], B_const], w=[B_PTb[j]])
                kb.op("act", lambda e: e.copy(out=hidT[j][:], in_=PTb[j][:, 0:4, :]), r=[B_PTb[j]], w=[B_hidT[j]])
                for half in range(2):
                    for c in range(4):
                        kb.op("pe", lambda e: e.matmul(Py[:, half, :], lhsT=hidT[j][:, c, :], rhs=W2[k][:, c, half * 512:(half + 1) * 512],
                                                       start=(c == 0), stop=(c == 3)), r=[B_hidT[j], B_W[k]], w=[B_Py])
                kb.op("act", lambda e: e.copy(out=ysb[j][:, 0:512], in_=Py[:, 0, :]), r=[B_Py], w=[B_ysb2[j][0]])
                kb.op("dve", lambda e: e.tensor_copy(out=ysb[j][:, 512:1024], in_=Py[:, 1, :]), r=[B_Py], w=[B_ysb2[j][1]])
                kb.dma("sp", "yst", lambda q: q.dma_start(out=ys_d[b * 128:(b + 1) * 128, :], in_=ysb[j][:]), r=B_ysb2[j], w=[B_ys])

            for s0 in range(min(NWB, NBLK)):
                load_w(s0)
            load_x(0)
            for b in range(NB128 + 1):
                if b + 1 < NB128:
                    load_x(b + 1)
                if b < NB128:
                    stageA(b)
                if b >= 1:
                    stageB(b - 1)
                    if (b - 1) % NSUB == NSUB - 1:
                        nxt = (b - 1) // NSUB + NWB
                        if nxt < NBLK:
                            load_w(nxt)
            kb.barrier()
        with ExitStack() as sF:
            NCB = 6
            y1 = [sb(sF, f"cy1{i}", [128, 1024], F32) for i in range(NCB)]
            y2 = [sb(sF, f"cy2{i}", [128, 1024], F32) for i in range(NCB)]
            xr = [sb(sF, f"cxr{i}", [128, 1024], F32) for i in range(NCB)]
            B_y1, B_y2, B_xr = kb.bufs(NCB), kb.bufs(NCB), kb.bufs(NCB)

            def cload(i):
                j = i % NCB
                rows = slice(i * 128, (i + 1) * 128)
                kb.dma("pool", f"cg1{j}", lambda q: q.indirect_dma_start(out=y1[j][:], out_offset=None, in_=ys_d[:, :],
                       in_offset=bass.IndirectOffsetOnAxis(ap=d1i[:, i:i + 1], axis=0)), r=[B_ys, B_rt], w=[B_y1[j]])
                kb.dma("pool", f"cg2{j}", lambda q: q.indirect_dma_start(out=y2[j][:], out_offset=None, in_=ys_d[:, :],
                       in_offset=bass.IndirectOffsetOnAxis(ap=d2i[:, i:i + 1], axis=0)), r=[B_ys, B_rt], w=[B_y2[j]])
                kb.dma("act", f"cxr{j}", lambda q: q.dma_start(out=xr[j][:], in_=out_d[rows, :]), r=[B_out[i]], w=[B_xr[j]])

            for i in range(min(NCB - 1, NT)):
                cload(i)
            for i in range(NT):
                j = i % NCB
                rows = slice(i * 128, (i + 1) * 128)
                if i + NCB - 1 < NT:
                    cload(i + NCB - 1)
                for half in range(2):
                    hs = slice(half * 512, (half + 1) * 512)
                    kb.op("dve", lambda e: e.scalar_tensor_tensor(out=xr[j][:, hs], in0=y1[j][:, hs], scalar=gta[:, i, 0:1], in1=xr[j][:, hs],
                                                                  op0=ALU.mult, op1=ALU.add), r=[B_y1[j], B_rt], w=[B_xr[j]])
                    kb.op("dve", lambda e: e.scalar_tensor_tensor(out=xr[j][:, hs], in0=y2[j][:, hs], scalar=gta[:, i, 1:2], in1=xr[j][:, hs],
                                                                  op0=ALU.mult, op1=ALU.add), r=[B_y2[j], B_rt], w=[B_xr[j]])
                kb.dma("sp", "ost2", lambda q: q.dma_start(out=out_d[rows, :], in_=xr[j][:]), r=[B_xr[j]], w=[B_out[i]])
            kb.barrier()


def _shared_maps(inp, consts):
    f = np.float32
    g = lambda k: np.ascontiguousarray(np.asarray(inp[k], dtype=f)[0])
    m = {}
    m["w_in"] = g("w_in")
    m["w_branch_da"] = g("w_branch_da")
    m["w_branch_ml"] = g("w_branch_ml")
    m["w_gate"] = g("w_gate")
    m["w_out"] = g("w_out")
    m["w1"] = g("w1")
    m["w3"] = g("w3")
    m["w2"] = g("w2")
    m["w_rt"] = np.ascontiguousarray(np.concatenate([g("w_group"), g("w_router")], axis=1))
    m["attn_norm_g"] = g("attn_norm_g")[None, :]
    m["ffn_norm_g"] = g("ffn_norm_g")[None, :]
    m["da_out_norm_g"] = g("da_out_norm_g")[None, :]
    m["ml_out_norm_g"] = g("ml_out_norm_g")[None, :]
    m["b_rt"] = np.concatenate([g("b_group"), g("b_router")])[None, :]
    m["b_if"] = np.concatenate([g("ml_i_bias"), g("ml_f_bias")])[None, :]
    m["lamv"] = np.concatenate([g("da_lambda_q1"), g("da_lambda_k1"), g("da_lambda_q2"), g("da_lambda_k2")])[None, :]
    m["gqk_col"] = np.ascontiguousarray(np.stack([np.tile(g("da_q_norm_g"), 2), np.tile(g("da_k_norm_g"), 2)], axis=1))
    cw = g("ml_conv_w")
    m["cw_col"] = np.ascontiguousarray(cw.reshape(4, 8, 128).transpose(2, 1, 0).reshape(128, 32))
    m["cb_col"] = np.ascontiguousarray(g("ml_conv_b").reshape(8, 128).T)
    m["bg_col"] = np.ascontiguousarray(g("b_gate").reshape(16, 128).T)
    for k, v in consts.items():
        m["c_" + k] = v
    return m


_CACHE = {}


def kernel(**inputs):
    x = np.asarray(inputs["x"], dtype=np.float32)
    B, S, _ = x.shape
    key = (S,)
    if key not in _CACHE:
        _CACHE[key] = build_nc(S)
    nc, consts = _CACHE[key]
    shared = _shared_maps(inputs, consts)
    in_maps = []
    for b in range(B):
        m = dict(shared)
        m["x"] = np.ascontiguousarray(x[b])
        in_maps.append(m)
    res = run_bass_kernel_spmd(nc, in_maps, core_ids=list(range(B)))
    return np.stack([np.asarray(r["out"], dtype=np.float32) for r in res.results], axis=0)
```
